# Optimizing a Trainium2 kernel written in Bass

```python
import math
import jax
import jax.numpy as jnp
from jax import lax
import numpy as np

D_MODEL = 1024
BATCH = 8
SEQ = 4096
DEPTH = 4

GRID_W = 64
CTX_LEN = 256
D_MIX = D_MODEL
HY_W = D_MIX // 4
ATT_W = D_MIX // 2
S5_W = D_MIX - HY_W - ATT_W
HEAD_DIM = 64
N_HEADS = ATT_W // HEAD_DIM
N_KV_HEADS = 2
Q_PER_KV = N_HEADS // N_KV_HEADS
KV_W = N_KV_HEADS * HEAD_DIM
WINDOW = 128
QBLK = 128
KBLK = QBLK + 2 * WINDOW
ROPE_BASE = 10000.0
NEG_INF = -1e30
HY_ORDER = 2
HY_SHORT = 3
HY_BANDS = 16
HY_EMB = 1 + 2 * HY_BANDS
HY_FILTER_HIDDEN = 64
HY_DECAY_MIN = math.log(1e-2) / 1.5
HY_DECAY_MAX = math.log(1e-2) / 0.3
S5_CPG = 16
S5_GROUPS = S5_W // S5_CPG
S5_STATE = 64
S5_DT_MIN = 1e-3
S5_DT_MAX = 1e-1
N_EXPERTS = 16
N_EXPERT_GROUPS = 4
EXPERTS_PER_GROUP = N_EXPERTS // N_EXPERT_GROUPS
TOP_K = 2
D_EXPERT = 1024
MOE_BLK = 128
HY_END = 3 * HY_W
Q_END = HY_END + ATT_W
K_END = Q_END + KV_W
V_END = K_END + KV_W
IN_W = V_END + S5_W
EPS = 1e-6
F32 = jnp.float32

kernel_name = 'hybrid_hyena_swa_s5_moe_dit'


def rms_norm(x, g):
    xf = x.astype(F32)
    y = xf * lax.rsqrt(jnp.mean(xf * xf, axis=-1, keepdims=True) + EPS)
    return (y * g.astype(F32)).astype(x.dtype)


def group_rms_norm(parts, g):
    normed = []
    for p in parts:
        pf = p.astype(F32)
        normed.append(pf * lax.rsqrt(jnp.mean(pf * pf, axis=-1, keepdims=True) + EPS))
    return (jnp.concatenate(normed, axis=-1) * g.astype(F32)).astype(parts[0].dtype)


def modulate(h, shift, scale):
    return h * (1.0 + scale) + shift


def axial_rope(x, rows, cols):
    half = HEAD_DIM // 2
    quarter = half // 2
    inv_freq = ROPE_BASE ** (-jnp.arange(quarter, dtype=F32) / quarter)
    extra = (1,) * (x.ndim - 3)

    def rotate(xa, pos):
        ang = pos.astype(F32)[:, None] * inv_freq[None, :]
        cos = jnp.cos(ang).reshape((1, pos.shape[0]) + extra + (quarter,))
        sin = jnp.sin(ang).reshape((1, pos.shape[0]) + extra + (quarter,))
        x1 = xa[..., :quarter].astype(F32)
        x2 = xa[..., quarter:].astype(F32)
        return jnp.concatenate([x1 * cos - x2 * sin, x1 * sin + x2 * cos], axis=-1)

    out = jnp.concatenate([rotate(x[..., :half], rows), rotate(x[..., half:], cols)], axis=-1)
    return out.astype(x.dtype)


def short_conv(z, w, b):
    L = z.shape[1]
    pad = HY_SHORT // 2
    zp = jnp.pad(z, ((0, 0), (pad, pad), (0, 0)))
    out = b
    for j in range(HY_SHORT):
        out = out + zp[:, j:j + L] * w[j]
    return out


def hyena_filter_spectrum(L, w1, b1, freq1, w2, b2, freq2, w3):
    pos = jnp.arange(L, dtype=F32)
    t = pos / float(max(L - 1, 1))
    bands = jnp.linspace(1e-4, HY_BANDS - 1, HY_BANDS, dtype=F32)
    ang = (2.0 * math.pi / L) * pos[:, None] * bands[None, :]
    feats = jnp.concatenate([t[:, None], jnp.cos(ang), -jnp.sin(ang)], axis=-1)
    hid = jnp.sin(freq1.astype(F32) * (feats @ w1.astype(F32) + b1.astype(F32)))
    hid = jnp.sin(freq2.astype(F32) * (hid @ w2.astype(F32) + b2.astype(F32)))
    taps = (hid @ w3.astype(F32)).reshape(L, HY_ORDER, 2, HY_W)
    decay = jnp.abs(jnp.linspace(HY_DECAY_MIN, HY_DECAY_MAX, HY_W, dtype=F32))
    taps = taps * jnp.exp(-t[:, None, None, None] * decay)
    fwd = taps[:, :, 0]
    bwd = taps[:, :, 1]
    kern = jnp.concatenate([fwd, jnp.zeros((1, HY_ORDER, HY_W), F32), bwd[:0:-1]], axis=0)
    kern = kern / jnp.sum(jnp.abs(kern), axis=0, keepdims=True)
    return jnp.fft.rfft(kern, axis=0)


def fft_long_conv(u, spec, bias):
    L = u.shape[1]
    uf = jnp.fft.rfft(u.astype(F32), n=2 * L, axis=1)
    y = jnp.fft.irfft(uf * spec[None], n=2 * L, axis=1)[:, :L]
    return (y + u.astype(F32) * bias.astype(F32)).astype(u.dtype)


def hyena_mix(z, conv_w, conv_b, w1, b1, freq1, w2, b2, freq2, w3, bias):
    L = z.shape[1]
    z = short_conv(z, conv_w, conv_b)
    v, x1, x2 = jnp.split(z, 3, axis=-1)
    spec = hyena_filter_spectrum(L, w1, b1, freq1, w2, b2, freq2, w3)
    z1 = x1 * fft_long_conv(v, spec[:, 0], bias[0])
    return x2 * fft_long_conv(z1, spec[:, 1], bias[1])


def banded_attention(q, k, v, k_c, v_c, sink):
    B, N = q.shape[0], q.shape[1]
    C = k_c.shape[1]
    scale = HEAD_DIM ** -0.5
    kp = jnp.pad(k, ((0, 0), (WINDOW, WINDOW), (0, 0), (0, 0)))
    vp = jnp.pad(v, ((0, 0), (WINDOW, WINDOW), (0, 0), (0, 0)))
    sink_cols = jnp.broadcast_to(sink[None, :, :, None, None], (B, N_KV_HEADS, Q_PER_KV, QBLK, 1))

    def block(n):
        start = n * QBLK
        qb = lax.dynamic_slice_in_dim(q, start, QBLK, axis=1)
        kb = lax.dynamic_slice_in_dim(kp, start, KBLK, axis=1)
        vb = lax.dynamic_slice_in_dim(vp, start, KBLK, axis=1)
        s_loc = jnp.einsum('bqkgd,bskd->bkgqs', qb, kb).astype(F32) * scale
        s_ctx = jnp.einsum('bqkgd,bckd->bkgqc', qb, k_c).astype(F32) * scale
        q_pos = start + jnp.arange(QBLK)
        k_pos = start - WINDOW + jnp.arange(KBLK)
        valid = (jnp.abs(k_pos[None, :] - q_pos[:, None]) <= WINDOW) & (k_pos >= 0)[None, :] & (k_pos < N)[None, :]
        s_loc = jnp.where(valid, s_loc, NEG_INF)
        p = jax.nn.softmax(jnp.concatenate([s_loc, s_ctx, sink_cols], axis=-1), axis=-1)
        p_loc = p[..., :KBLK].astype(v.dtype)
        p_ctx = p[..., KBLK:KBLK + C].astype(v.dtype)
        return (jnp.einsum('bkgqs,bskd->bqkgd', p_loc, vb)
                + jnp.einsum('bkgqc,bckd->bqkgd', p_ctx, v_c))

    out = lax.map(block, jnp.arange(N // QBLK))
    return jnp.moveaxis(out, 0, 1).reshape(B, N, ATT_W)


def context_attention(q_c, k_c, v_c, sink):
    B, C = q_c.shape[0], q_c.shape[1]
    s = jnp.einsum('bqkgd,bckd->bkgqc', q_c, k_c).astype(F32) * (HEAD_DIM ** -0.5)
    sink_cols = jnp.broadcast_to(sink[None, :, :, None, None], s.shape[:-1] + (1,))
    p = jax.nn.softmax(jnp.concatenate([s, sink_cols], axis=-1), axis=-1)[..., :C].astype(v_c.dtype)
    return jnp.einsum('bkgqc,bckd->bqkgd', p, v_c).reshape(B, C, ATT_W)


def s5_discretize(lam_re, lam_im, log_dt, b_re, b_im):
    lam_re = lam_re.astype(F32)
    lam_im = lam_im.astype(F32)
    b_re = b_re.astype(F32)
    b_im = b_im.astype(F32)
    dt = jnp.exp(log_dt.astype(F32))[:, None]
    mag = jnp.exp(lam_re * dt)
    ab_re = mag * jnp.cos(lam_im * dt)
    ab_im = mag * jnp.sin(lam_im * dt)
    num_re = ab_re - 1.0
    num_im = ab_im
    den = lam_re * lam_re + lam_im * lam_im
    co_re = ((num_re * lam_re + num_im * lam_im) / den)[..., None]
    co_im = ((num_im * lam_re - num_re * lam_im) / den)[..., None]
    bb_re = co_re * b_re - co_im * b_im
    bb_im = co_re * b_im + co_im * b_re
    return ab_re, ab_im, bb_re, bb_im


def s5_combine(e1, e2):
    a1r, a1i, b1r, b1i = e1
    a2r, a2i, b2r, b2i = e2
    return (a1r * a2r - a1i * a2i,
            a1r * a2i + a1i * a2r,
            a2r * b1r - a2i * b1i + b2r,
            a2r * b1i + a2i * b1r + b2i)


def s5_scan(u, ab_re, ab_im, bb_re, bb_im, h0_re, h0_im):
    L = u.shape[0]
    bu_re = jnp.einsum('lbgc,gpc->lbgp', u, bb_re)
    bu_im = jnp.einsum('lbgc,gpc->lbgp', u, bb_im)
    bu_re = bu_re.at[0].add(ab_re * h0_re - ab_im * h0_im)
    bu_im = bu_im.at[0].add(ab_re * h0_im + ab_im * h0_re)
    a_re = jnp.broadcast_to(ab_re[None, None], (L, 1, S5_GROUPS, S5_STATE))
    a_im = jnp.broadcast_to(ab_im[None, None], (L, 1, S5_GROUPS, S5_STATE))
    _, _, s_re, s_im = lax.associative_scan(s5_combine, (a_re, a_im, bu_re, bu_im), axis=0)
    return s_re, s_im


def s5_readout(s_re, s_im, c_re, c_im):
    return (jnp.einsum('lbgp,gcp->lbgc', s_re, c_re.astype(F32))
            - jnp.einsum('lbgp,gcp->lbgc', s_im, c_im.astype(F32)))


def s5_output(y, u, d_skip, glu_w, glu_b):
    Lq, B = y.shape[0], y.shape[1]
    y = y + u * d_skip.astype(F32).reshape(S5_GROUPS, S5_CPG)
    y = jnp.swapaxes(y, 0, 1).reshape(B, Lq, S5_W)
    g = jax.nn.gelu(y)
    return g * jax.nn.sigmoid(g @ glu_w.astype(F32) + glu_b.astype(F32))


def flip_time(a, rev):
    return a[::-1] if rev else a


def s5_mix(u_ctx, u_lat, lam_re, lam_im, log_dt, b_re, b_im, c_re, c_im, d_skip, glu_w, glu_b, with_ctx):
    B = u_lat.shape[0]

    def time_major(u):
        return jnp.swapaxes(u.astype(F32).reshape(B, u.shape[1], S5_GROUPS, S5_CPG), 0, 1)

    uc = time_major(u_ctx)
    ul = time_major(u_lat)
    zero = jnp.zeros((B, S5_GROUPS, S5_STATE), F32)
    y_lat = 0.0
    y_ctx = 0.0
    for d in range(2):
        rev = d == 1
        ab_re, ab_im, bb_re, bb_im = s5_discretize(lam_re[d], lam_im[d], log_dt[d], b_re[d], b_im[d])
        sc_re, sc_im = s5_scan(flip_time(uc, rev), ab_re, ab_im, bb_re, bb_im, zero, zero)
        sl_re, sl_im = s5_scan(flip_time(ul, rev), ab_re, ab_im, bb_re, bb_im, sc_re[-1], sc_im[-1])
        y_lat = y_lat + flip_time(s5_readout(sl_re, sl_im, c_re[d], c_im[d]), rev)
        if with_ctx:
            y_ctx = y_ctx + flip_time(s5_readout(sc_re, sc_im, c_re[d], c_im[d]), rev)
    lat = s5_output(y_lat, ul, d_skip, glu_w, glu_b).astype(u_lat.dtype)
    ctx_out = s5_output(y_ctx, uc, d_skip, glu_w, glu_b).astype(u_ctx.dtype) if with_ctx else None
    return lat, ctx_out


def hybrid_mixer(h, hc, w_in_l, conv_w, conv_b, f_w1, f_b1, f_freq1, f_w2, f_b2, f_freq2, f_w3, hy_bias_l,
                 sink, lam_re, lam_im, log_dt, b_re, b_im, c_re, c_im, d_skip, glu_w, glu_b, out_g,
                 rows, cols, with_ctx):
    def heads_q(a):
        return a.reshape(a.shape[0], a.shape[1], N_KV_HEADS, Q_PER_KV, HEAD_DIM)

    def heads_kv(a):
        return a.reshape(a.shape[0], a.shape[1], N_KV_HEADS, HEAD_DIM)

    p = h @ w_in_l
    if with_ctx:
        pc = hc @ w_in_l
        kvs_c = pc[..., Q_END:]
    else:
        kvs_c = hc @ w_in_l[:, Q_END:]
    k_c = heads_kv(kvs_c[..., :KV_W])
    v_c = heads_kv(kvs_c[..., KV_W:2 * KV_W])
    u_c = kvs_c[..., 2 * KV_W:]
    sink_kg = sink.astype(F32).reshape(N_KV_HEADS, Q_PER_KV)

    hy = hyena_mix(p[..., :HY_END], conv_w, conv_b, f_w1, f_b1, f_freq1, f_w2, f_b2, f_freq2, f_w3, hy_bias_l)
    q = axial_rope(heads_q(p[..., HY_END:Q_END]), rows, cols)
    k = axial_rope(heads_kv(p[..., Q_END:K_END]), rows, cols)
    v = heads_kv(p[..., K_END:V_END])
    att = banded_attention(q, k, v, k_c, v_c, sink_kg)
    s5_lat, s5_ctx = s5_mix(u_c, p[..., V_END:], lam_re, lam_im, log_dt, b_re, b_im, c_re, c_im,
                            d_skip, glu_w, glu_b, with_ctx)
    lat = group_rms_norm([hy, att, s5_lat], out_g)
    if not with_ctx:
        return lat, None
    hy_c = hyena_mix(pc[..., :HY_END], conv_w, conv_b, f_w1, f_b1, f_freq1, f_w2, f_b2, f_freq2, f_w3, hy_bias_l)
    att_c = context_attention(heads_q(pc[..., HY_END:Q_END]), k_c, v_c, sink_kg)
    ctx_out = group_rms_norm([hy_c, att_c, s5_ctx], out_g)
    return lat, ctx_out


def moe_ffn(h, router_w, router_b, w_gate, w_up, w_down):
    T, D = h.shape
    probs = jax.nn.softmax(jnp.dot(h.astype(F32), router_w.astype(F32)), axis=-1)
    sel = (probs + router_b.astype(F32)).reshape(T, N_EXPERT_GROUPS, EXPERTS_PER_GROUP)
    group_score = jnp.sum(lax.top_k(sel, 2)[0], axis=-1)
    g_idx = jnp.argmax(group_score, axis=-1)
    in_group = sel[jnp.arange(T), g_idx]
    _, local = lax.top_k(in_group, TOP_K)
    e_idx = g_idx[:, None] * EXPERTS_PER_GROUP + local
    gates = jnp.take_along_axis(probs, e_idx, axis=1)
    gates = gates / jnp.sum(gates, axis=-1, keepdims=True)
    M = T * TOP_K
    flat_e = e_idx.reshape(-1)
    flat_t = jnp.repeat(jnp.arange(T), TOP_K)
    flat_w = gates.reshape(-1)
    order = jnp.argsort(flat_e)
    se, st, sw = flat_e[order], flat_t[order], flat_w[order]
    counts = jnp.bincount(flat_e, length=N_EXPERTS)
    starts = jnp.cumsum(counts) - counts
    padded = (counts + MOE_BLK - 1) // MOE_BLK * MOE_BLK
    ends = jnp.cumsum(padded)
    pstarts = ends - padded
    dest = pstarts[se] + (jnp.arange(M) - starts[se])
    n_blocks = -(-M // MOE_BLK) + N_EXPERTS
    row_tok = jnp.zeros((n_blocks * MOE_BLK,), jnp.int32).at[dest].set(st)
    row_w = jnp.zeros((n_blocks * MOE_BLK,), F32).at[dest].set(sw)
    block_e = jnp.clip(jnp.searchsorted(ends, jnp.arange(n_blocks) * MOE_BLK, side='right'), 0, N_EXPERTS - 1)
    xb = h[row_tok].reshape(n_blocks, MOE_BLK, D)

    def expert_block(args):
        xblk, e = args
        a = xblk @ w_gate[e]
        u = xblk @ w_up[e]
        return (jax.nn.silu(a) * u) @ w_down[e]

    yb = lax.map(expert_block, (xb, block_e)).reshape(n_blocks * MOE_BLK, D)
    yb = yb * row_w[:, None].astype(yb.dtype)
    return jnp.zeros_like(h).at[row_tok].add(yb)


def setup_inputs(seed: int = 0) -> dict:
    key = jax.random.key(seed)
    ks = iter(jax.random.split(key, 40))

    def nrm(shape, s):
        return jax.random.normal(next(ks), shape, F32) * s

    L = DEPTH
    lam_im_base = jnp.broadcast_to(math.pi * jnp.arange(S5_STATE, dtype=F32), (L, 2, S5_GROUPS, S5_STATE))
    return {
        'x': nrm((BATCH, SEQ, D_MODEL), 1.0),
        'c': nrm((BATCH, D_MODEL), 1.0),
        'ctx': nrm((BATCH, CTX_LEN, D_MODEL), 1.0),
        'c_ctx': nrm((D_MODEL,), 1.0),
        'norm1_g': 1.0 + nrm((L, D_MODEL), 0.02),
        'norm2_g': 1.0 + nrm((L, D_MODEL), 0.02),
        'ada_w': nrm((L, D_MODEL, 6 * D_MODEL), 0.01),
        'ada_b': nrm((L, 6 * D_MODEL), 0.02),
        'w_in': nrm((L, D_MODEL, IN_W), D_MODEL ** -0.5),
        'w_out': nrm((L, D_MIX, D_MODEL), D_MIX ** -0.5),
        'mix_norm_g': 1.0 + nrm((L, D_MIX), 0.02),
        'hy_conv_w': nrm((L, HY_SHORT, 3 * HY_W), HY_SHORT ** -0.5),
        'hy_conv_b': nrm((L, 3 * HY_W), 0.02),
        'hy_f_w1': nrm((L, HY_EMB, HY_FILTER_HIDDEN), HY_EMB ** -0.5),
        'hy_f_b1': nrm((L, HY_FILTER_HIDDEN), 0.1),
        'hy_f_freq1': 1.0 + nrm((L, HY_FILTER_HIDDEN), 0.02),
        'hy_f_w2': nrm((L, HY_FILTER_HIDDEN, HY_FILTER_HIDDEN), HY_FILTER_HIDDEN ** -0.5),
        'hy_f_b2': nrm((L, HY_FILTER_HIDDEN), 0.1),
        'hy_f_freq2': 1.0 + nrm((L, HY_FILTER_HIDDEN), 0.02),
        'hy_f_w3': nrm((L, HY_FILTER_HIDDEN, HY_ORDER * 2 * HY_W), HY_FILTER_HIDDEN ** -0.5),
        'hy_bias': nrm((L, HY_ORDER, HY_W), 1.0),
        'attn_sink': nrm((L, N_HEADS), 0.5),
        's5_lam_re': -0.5 + nrm((L, 2, S5_GROUPS, S5_STATE), 0.01),
        's5_lam_im': lam_im_base + nrm((L, 2, S5_GROUPS, S5_STATE), 0.01),
        's5_log_dt': jax.random.uniform(next(ks), (L, 2, S5_GROUPS), F32,
                                        minval=math.log(S5_DT_MIN), maxval=math.log(S5_DT_MAX)),
        's5_b_re': nrm((L, 2, S5_GROUPS, S5_STATE, S5_CPG), (2 * S5_CPG) ** -0.5),
        's5_b_im': nrm((L, 2, S5_GROUPS, S5_STATE, S5_CPG), (2 * S5_CPG) ** -0.5),
        's5_c_re': nrm((L, 2, S5_GROUPS, S5_CPG, S5_STATE), (2 * S5_STATE) ** -0.5),
        's5_c_im': nrm((L, 2, S5_GROUPS, S5_CPG, S5_STATE), (2 * S5_STATE) ** -0.5),
        's5_d': nrm((L, S5_W), 1.0),
        's5_glu_w': nrm((L, S5_W, S5_W), S5_W ** -0.5),
        's5_glu_b': nrm((L, S5_W), 0.02),
        'router_w': nrm((D_MODEL, N_EXPERTS), D_MODEL ** -0.5),
        'router_b': nrm((N_EXPERTS,), 0.01),
        'moe_w_gate': nrm((L, N_EXPERTS, D_MODEL, D_EXPERT), D_MODEL ** -0.5),
        'moe_w_up': nrm((L, N_EXPERTS, D_MODEL, D_EXPERT), D_MODEL ** -0.5),
        'moe_w_down': nrm((L, N_EXPERTS, D_EXPERT, D_MODEL), D_EXPERT ** -0.5),
        'final_g': 1.0 + nrm((D_MODEL,), 0.02),
    }


def reference(x, c, ctx, c_ctx, norm1_g, norm2_g, ada_w, ada_b, w_in, w_out, mix_norm_g,
              hy_conv_w, hy_conv_b, hy_f_w1, hy_f_b1, hy_f_freq1, hy_f_w2, hy_f_b2, hy_f_freq2, hy_f_w3, hy_bias,
              attn_sink, s5_lam_re, s5_lam_im, s5_log_dt, s5_b_re, s5_b_im, s5_c_re, s5_c_im, s5_d,
              s5_glu_w, s5_glu_b, router_w, router_b, moe_w_gate, moe_w_up, moe_w_down, final_g):
    B, N, D = x.shape
    C = ctx.shape[1]
    ROWS = N // GRID_W
    rows = jnp.repeat(jnp.arange(ROWS), GRID_W)
    cols = jnp.tile(jnp.arange(GRID_W), ROWS)
    xc = ctx
    for l in range(DEPTH):
        with_ctx = l < DEPTH - 1
        mod = jax.nn.silu(c) @ ada_w[l] + ada_b[l]
        mod_c = jax.nn.silu(c_ctx) @ ada_w[l] + ada_b[l]
        sh1, sc1, g1, sh2, sc2, g2 = jnp.split(mod[:, None, :], 6, axis=-1)
        csh1, csc1, cg1, csh2, csc2, cg2 = jnp.split(mod_c, 6)
        h = modulate(rms_norm(x, norm1_g[l]), sh1, sc1)
        hc = modulate(rms_norm(xc, norm1_g[l]), csh1, csc1)
        mix, mix_c = hybrid_mixer(h, hc, w_in[l], hy_conv_w[l], hy_conv_b[l], hy_f_w1[l], hy_f_b1[l],
                                  hy_f_freq1[l], hy_f_w2[l], hy_f_b2[l], hy_f_freq2[l], hy_f_w3[l], hy_bias[l],
                                  attn_sink[l], s5_lam_re[l], s5_lam_im[l], s5_log_dt[l], s5_b_re[l], s5_b_im[l],
                                  s5_c_re[l], s5_c_im[l], s5_d[l], s5_glu_w[l], s5_glu_b[l], mix_norm_g[l],
                                  rows, cols, with_ctx)
        x = x + g1 * (mix @ w_out[l])
        h2 = modulate(rms_norm(x, norm2_g[l]), sh2, sc2)
        if with_ctx:
            xc = xc + cg1 * (mix_c @ w_out[l])
            hc2 = modulate(rms_norm(xc, norm2_g[l]), csh2, csc2)
            tokens = jnp.concatenate([hc2.reshape(B * C, D), h2.reshape(B * N, D)], axis=0)
            y = moe_ffn(tokens, router_w, router_b, moe_w_gate[l], moe_w_up[l], moe_w_down[l])
            xc = xc + cg2 * y[:B * C].reshape(B, C, D)
            x = x + g2 * y[B * C:].reshape(B, N, D)
        else:
            y = moe_ffn(h2.reshape(B * N, D), router_w, router_b, moe_w_gate[l], moe_w_up[l], moe_w_down[l])
            x = x + g2 * y.reshape(B, N, D)
    return rms_norm(x, final_g)
```

```python
import math
import numpy as np
import concourse.bass as bass
import concourse.mybir as mybir
from concourse.bass_utils import run_bass_kernel_spmd

F32 = mybir.dt.float32
AF = mybir.ActivationFunctionType
ALU = mybir.AluOpType
AX = mybir.AxisListType

D = 1024
NLAT = 4096
NCTX = 256
T = NLAT + NCTX
NT = T // 128
DEPTH = 4
HY_W = 256
ATT_W = 512
S5_W = 256
HY_END = 768
Q_END = HY_END + ATT_W
K_END = Q_END + 128
V_END = K_END + 128
IN_W = V_END + S5_W
EPS = 1e-6
MAGIC = 12582912.0
TWO_PI = 6.283185
NE = 16


class Tok:
    def __init__(self, name):
        self.name = name

    def __repr__(self):
        return self.name


class Prog:
    NDS = 24

    def __init__(self):
        nc = bass.Bass("TRN2", target_bir_lowering=False)
        self.nc = nc
        self.eng = {'pe': nc.tensor, 'dve': nc.vector, 'act': nc.scalar, 'pool': nc.gpsimd, 'sp': nc.sync}
        self.esem = {k: nc.alloc_semaphore("es_" + k) for k in self.eng}
        self.ecnt = {k: 0 for k in self.eng}
        self.dsem = [nc.alloc_semaphore("ds%d" % i) for i in range(self.NDS)]
        self.dval = [0] * self.NDS
        self.dnext = 0
        self.know = {k: {} for k in self.eng}
        self.last_w = {}
        self.readers = {}
        self.n_inst = 0
        self.psum = [nc.alloc_psum_tensor("psb%d" % i, [128, 512], F32) for i in range(8)]
        self.ps_tok = [Tok("ps%d" % i) for i in range(8)]
        self.ps_next = 0
        self.ps_held = set()
        self.dram = {}

    def _sem_of(self, key):
        return self.esem[key] if isinstance(key, str) else self.dsem[key]

    def _deps(self, reads, writes):
        deps = {}

        def add(kv):
            k, v = kv
            if deps.get(k, 0) < v:
                deps[k] = v
        for t in reads:
            if t in self.last_w:
                add(self.last_w[t])
        for t in writes:
            if t in self.last_w:
                add(self.last_w[t])
            for kv in self.readers.get(t, {}).items():
                add(kv)
        return deps

    def _wait(self, e, deps):
        kn = self.know[e]
        for k, v in deps.items():
            if k == e and e == 'pe':
                continue
            if kn.get(k, 0) >= v:
                continue
            self.eng[e].wait_ge(self._sem_of(k), v)
            kn[k] = v
            self.n_inst += 1

    def _commit(self, me, reads, writes):
        for t in writes:
            self.last_w[t] = me
            self.readers[t] = {}
        for t in reads:
            r = self.readers.setdefault(t, {})
            if r.get(me[0], 0) < me[1]:
                r[me[0]] = me[1]

    def op(self, e, fn, reads=(), writes=()):
        self._wait(e, self._deps(reads, writes))
        inst = fn(self.eng[e])
        inst.then_inc(self.esem[e], 1)
        self.ecnt[e] += 1
        self.n_inst += 1
        self._commit((e, self.ecnt[e]), reads, writes)

    def dma(self, out, in_, reads=(), writes=(), q='sp'):
        self._wait(q, self._deps(reads, writes))
        s = self.dnext
        self.dnext = (self.dnext + 1) % self.NDS
        if self.know[q].get(s, 0) < self.dval[s]:
            self.eng[q].wait_ge(self.dsem[s], self.dval[s])
            self.know[q][s] = self.dval[s]
        self.eng[q].dma_start(out=out, in_=in_, allow_slow_non_contiguous=True).then_inc(self.dsem[s], 16)
        self.dval[s] += 16
        self.n_inst += 1
        self._commit((s, self.dval[s]), reads, writes)

    def barrier(self):
        for e in self.eng:
            deps = {k: self.ecnt[k] for k in self.eng if self.ecnt[k] > 0}
            for s in range(self.NDS):
                if self.dval[s] > 0:
                    deps[s] = self.dval[s]
            self._wait(e, deps)
        self.last_w = {}
        self.readers = {}

    def finish(self):
        deps = {s: self.dval[s] for s in range(self.NDS) if self.dval[s] > 0}
        for k in self.eng:
            if self.ecnt[k] > 0:
                deps[k] = self.ecnt[k]
        self._wait('sp', deps)

    def ps(self):
        while True:
            i = self.ps_next
            self.ps_next = (i + 1) % 8
            if i not in self.ps_held:
                return self.psum[i], self.ps_tok[i]

    def ps_hold(self, n):
        got = []
        for i in range(8):
            if i not in self.ps_held and len(got) < n:
                self.ps_held.add(i)
                got.append(i)
        return [(self.psum[i], self.ps_tok[i]) for i in got], got

    def ps_release(self, got):
        for i in got:
            self.ps_held.discard(i)

    def mm(self, out, lhsT, rhs, start, stop, reads, writes):
        self.op('pe', lambda e: e.matmul(out, lhsT, rhs, start=start, stop=stop), reads, writes)

    def tr(self, out, in_, ident, reads, writes):
        self.op('pe', lambda e: e.transpose(out=out, in_=in_, identity=ident), reads, writes)

    def act(self, out, in_, func, reads, writes, **kw):
        self.op('act', lambda e: e.activation(out=out, in_=in_, func=func, **kw), reads, writes)

    def tt(self, out, in0, in1, op, reads, writes, e='dve'):
        self.op(e, lambda g: g.tensor_tensor(out=out, in0=in0, in1=in1, op=op), reads, writes)

    def ts(self, out, in0, s1, s2, op0, op1, reads, writes, e='dve'):
        if op1 is None:
            self.op(e, lambda g: g.tensor_scalar(out=out, in0=in0, scalar1=s1, scalar2=None, op0=op0), reads, writes)
        else:
            self.op(e, lambda g: g.tensor_scalar(out=out, in0=in0, scalar1=s1, scalar2=s2, op0=op0, op1=op1), reads, writes)

    def stt(self, out, in0, scalar, in1, op0, op1, reads, writes):
        self.op('dve', lambda g: g.scalar_tensor_tensor(out=out, in0=in0, scalar=scalar, in1=in1, op0=op0, op1=op1), reads, writes)

    def copy(self, out, in_, reads, writes, e='dve'):
        if e == 'act':
            self.op('act', lambda g: g.copy(out=out, in_=in_), reads, writes)
        else:
            self.op(e, lambda g: g.tensor_copy(out=out, in_=in_), reads, writes)

    def memset(self, ap, val, writes, e='dve'):
        self.op(e, lambda g: g.memset(ap, val), (), writes)

    def recip(self, out, in_, reads, writes):
        self.op('dve', lambda g: g.reciprocal(out=out, in_=in_), reads, writes)

    def dram_t(self, name, shape, kind="Internal"):
        t = self.nc.dram_tensor(name, list(shape), F32, kind=kind).ap()
        self.dram[name] = t
        return t


class Ctx:
    def dump(self, name, tile_ap, shape, reads):
        if 'dump' not in self.dbg:
            return
        o = self.P.nc.dram_tensor("dmp_" + name, list(shape), F32, kind="ExternalOutput").ap()
        self.P.dma(o, tile_ap, reads, (), q='sp')


_UID = [0]


def SB(nc, name, shape, dt):
    _UID[0] += 1
    return nc.sbuf_tensor("%s_%d" % (name, _UID[0]), shape, dt)


def rope_tables():
    cosT = np.ones((128, T), np.float64)
    sinT = np.zeros((128, T), np.float64)
    n = np.arange(NLAT)
    row = n // 64
    col = n % 64
    for r in range(128):
        d = r % 64
        dd = d if d < 32 else d - 32
        pos = row if d < 32 else col
        j = dd % 16
        first = dd < 16
        inv = 10000.0 ** (-(j / 16.0))
        ang = (pos.astype(np.float32) * np.float32(inv)).astype(np.float64)
        cosT[r, NCTX:] = np.cos(ang)
        sinT[r, NCTX:] = (-np.sin(ang)) if first else np.sin(ang)
    return cosT.astype(np.float32), sinT.astype(np.float32)


def perm_cols(width):
    src = np.zeros(width, np.int64)
    for c in range(width):
        d = c % 64
        dd = d % 32
        src[c] = c + 16 if dd < 16 else c - 16
    return src


def dft_blocks(nhalf, nchunk):
    idx = np.arange(nchunk * 128)
    valid = (idx <= nhalf)
    prod = np.outer(idx, idx).astype(np.float64)
    ang = 2.0 * np.pi * (prod % (2 * nhalf)) / (2 * nhalf)
    m = np.outer(valid, valid)
    cc = (np.cos(ang) * m).astype(np.float32)
    ss = (np.sin(ang) * m).astype(np.float32)

    def blk(mat):
        return np.ascontiguousarray(mat.reshape(nchunk, 128, nchunk, 128).transpose(2, 1, 0, 3))
    return blk(cc), blk(ss)


def hyena_consts(L):
    pos = np.arange(L, dtype=np.float32)
    t = pos / np.float32(max(L - 1, 1))
    bands = np.linspace(1e-4, 15, 16, dtype=np.float32)
    ang = (np.float32(2.0 * math.pi / L) * pos[:, None] * bands[None, :]).astype(np.float32)
    feats = np.concatenate([t[:, None], np.cos(ang), -np.sin(ang)], axis=-1).astype(np.float32)
    dmin = math.log(1e-2) / 1.5
    dmax = math.log(1e-2) / 0.3
    decay = np.abs(np.linspace(dmin, dmax, HY_W, dtype=np.float32))
    dec = np.exp(-t[:, None] * decay[None, :]).astype(np.float32)
    nk = L // 128 + 1
    wk = np.zeros(nk * 128, np.float32)
    wk[:L + 1] = 2.0 / (2 * L)
    wk[0] = 1.0 / (2 * L)
    wk[L] = 1.0 / (2 * L)
    return np.ascontiguousarray(feats.T), dec, wk


def build(n_layers=DEPTH, dbg=()):
    P = Prog()
    nc = P.nc
    K = Ctx()
    K.P = P
    K.dbg = dbg
    K.dbg_out = {}

    def inp(name, shape):
        return nc.dram_tensor(name, list(shape), F32, kind="ExternalInput").ap()

    I = {}
    I['x'] = inp('x', [NLAT, D])
    I['ctx'] = inp('ctx', [NCTX, D])
    I['c'] = inp('c', [1, D])
    I['c_ctx'] = inp('c_ctx', [1, D])
    shapes = dict(
        norm1_g=[4, D], norm2_g=[4, D], ada_w=[4, D, 6 * D], ada_b=[4, 6 * D], w_in=[4, D, IN_W], w_out=[4, D, D],
        mix_norm_g=[4, D], hy_conv_w=[4, 3, 768], hy_conv_b=[4, 768], hy_f_w1=[4, 33, 64], hy_f_b1=[4, 64],
        hy_f_freq1=[4, 64], hy_f_w2=[4, 64, 64], hy_f_b2=[4, 64], hy_f_freq2=[4, 64], hy_f_w3=[4, 64, 1024],
        hy_bias=[4, 2, 256], attn_sink=[4, 8], s5_lam_re=[4, 2, 16, 64], s5_lam_im=[4, 2, 16, 64], s5_log_dt=[4, 2, 16],
        s5_b_re=[4, 2, 16, 64, 16], s5_b_im=[4, 2, 16, 64, 16], s5_c_re=[4, 2, 16, 16, 64], s5_c_im=[4, 2, 16, 16, 64],
        s5_d=[4, 256], s5_glu_w=[4, 256, 256], s5_glu_b=[4, 256], router_w=[D, 16], router_b=[1, 16],
        moe_w_gate=[4, 16, D, D], moe_w_up=[4, 16, D, D], moe_w_down=[4, 16, D, D], final_g=[1, D],
        k_ident=[128, 128], k_ropec=[128, T], k_ropes=[128, T], k_maskl=[128, 128], k_maskr=[128, 128],
        k_ccL=[33, 128, 33, 128], k_ssL=[33, 128, 33, 128], k_ccC=[3, 128, 3, 128], k_ssC=[3, 128, 3, 128],
        k_featL=[33, NLAT], k_decL=[NLAT, 256], k_wkL=[33 * 128, 1], k_featC=[33, NCTX], k_decC=[NCTX, 256], k_wkC=[3 * 128, 1],
        k_iota=[1, 512],
    )
    for k, s in shapes.items():
        I[k] = inp(k, s)
    K.I = I
    out = nc.dram_tensor("out", [NLAT, D], F32, kind="ExternalOutput").ap()
    K.out = out

    K.X = P.dram_t("sX", [T, D])
    K.ZT = P.dram_t("sZT", [T, 768])
    K.QF = P.dram_t("sQF", [512, T])
    K.KF = P.dram_t("sKF", [128, T])
    K.VT = P.dram_t("sVT", [T, 128])
    K.UF = P.dram_t("sUF", [256, T])
    K.MIX = P.dram_t("sMIX", [T, D])
    K.H2T = P.dram_t("sH2T", [D, T])
    K.GTT = P.dram_t("sGTT", [16, T])

    K.ident = nc.alloc_sbuf_tensor("ident", [128, 128], F32)
    K.ones = nc.alloc_sbuf_tensor("ones", [128, 128], F32)
    K.MOD = P.dram_t("sMOD", [12, D])
    K.ZC = P.dram_t("sZC", [T, 768])
    K.KRE = P.dram_t("sKRE", [33 * 128, 512])
    K.KIM = P.dram_t("sKIM", [33 * 128, 512])
    K.t_ident = Tok("ident")
    K.t_ones = Tok("ones")
    K.t_mod = Tok("modbc")
    P.dma(K.ident[:], I['k_ident'][:, :], (), [K.t_ident])
    P.memset(K.ones[:], 1.0, [K.t_ones])

    tX = [("X", i) for i in range(NT)]
    K.tX = tX
    P.dma(K.X[0:NCTX, :], I['ctx'][:, :], (), tX[0:2])
    for i in range(4):
        P.dma(K.X[NCTX + i * 1024: NCTX + (i + 1) * 1024, :], I['x'][i * 1024:(i + 1) * 1024, :], (), tX[2 + 8 * i: 2 + 8 * (i + 1)])
    P.barrier()

    for l in range(n_layers):
        stage_mod(K, l)
        P.barrier()
        stage_inproj(K, l)
        P.barrier()
        if 'inproj' in dbg and l == 0:
            break
        stage_hyena(K, l, NLAT, NCTX, 'L')
        P.barrier()
        stage_hyena(K, l, NCTX, 0, 'C')
        P.barrier()
        if 'hyena' in dbg and l == 0:
            break
        stage_attn(K, l)
        P.barrier()
        if 'attn' in dbg and l == 0:
            break
        stage_s5(K, l)
        P.barrier()
        if 's5' in dbg and l == 0:
            break
        stage_outproj(K, l)
        P.barrier()
        if 'outproj' in dbg and l == 0:
            break
        stage_moe(K, l)
        P.barrier()
    if not dbg:
        stage_final(K)
    for name in dbg:
        if name in P.dram and name not in ('inproj', 'hyena', 'attn', 's5', 'outproj'):
            src = P.dram[name]
            o = nc.dram_tensor("dbg_" + name, list(src.shape), F32, kind="ExternalOutput").ap()
            P.barrier()
            P.dma(o, src, (), ())
    P.finish()
    return P


def stage_mod(K, l):
    P, nc, I = K.P, K.P.nc, K.I
    with SB(nc, "m_cs", [128, 2, 8], F32) as cs, SB(nc, "m_w", [128, 6 * D], F32) as wk, \
            SB(nc, "m_ws", [128, 6 * D], F32) as ws, SB(nc, "m_b", [1, 6 * D], F32) as bt, \
            SB(nc, "m_raw", [128, 6 * D], F32) as raw, SB(nc, "m_g", [128, 2, D], F32) as gn, \
            SB(nc, "m_o", [128, 12, D], F32) as mo:
        t_cs, t_w, t_ws, t_b, t_raw, t_g = Tok("cs"), Tok("w"), Tok("ws"), Tok("b"), Tok("raw"), Tok("g")
        P.dma(cs[:, 0, :], I['c'][0, :].rearrange("(c p) -> p c", p=128), (), [t_cs])
        P.dma(cs[:, 1, :], I['c_ctx'][0, :].rearrange("(c p) -> p c", p=128), (), [t_cs])
        P.act(cs[:], cs[:], AF.Silu, [t_cs], [t_cs])
        P.dma(bt[:], I['ada_b'][l:l + 1, :], (), [t_b])
        P.dma(gn[:, 0, :], I['norm1_g'][l, :].partition_broadcast(128), (), [t_g])
        P.dma(gn[:, 1, :], I['norm2_g'][l, :].partition_broadcast(128), (), [t_g])
        for who in range(2):
            for j in range(12):
                ps, tp = P.ps()
                for k in range(8):
                    P.dma(wk[:, 0:512], I['ada_w'][l, k * 128:(k + 1) * 128, j * 512:(j + 1) * 512], (), [t_w])
                    P.ts(ws[:, 0:512], wk[:, 0:512], cs[:, who, k:k + 1], None, ALU.mult, None, [t_w, t_cs], [t_ws])
                    P.mm(ps[:, :], K.ones[:, :], ws[:, 0:512], k == 0, False, [t_ws, K.t_ones], [tp])
                P.mm(ps[:, :], K.ones[0:1, :], bt[0:1, j * 512:(j + 1) * 512], False, True, [t_b, K.t_ones], [tp])
                P.copy(raw[:, j * 512:(j + 1) * 512], ps[:, :], [tp], [t_raw])
            base = who * 6
            m = mo
            for half in range(2):
                sh = raw[:, (3 * half) * D:(3 * half + 1) * D]
                sc = raw[:, (3 * half + 1) * D:(3 * half + 2) * D]
                gg = raw[:, (3 * half + 2) * D:(3 * half + 3) * D]
                P.stt(m[:, base + 3 * half + 0, :], sc, 1.0, gn[:, half, :], ALU.add, ALU.mult, [t_raw, t_g], [K.t_mod])
                P.copy(m[:, base + 3 * half + 1, :], sh, [t_raw], [K.t_mod])
                P.copy(m[:, base + 3 * half + 2, :], gg, [t_raw], [K.t_mod])
        P.dma(K.MOD.rearrange("(o j) d -> o j d", o=1), mo[0:1, :, :], [K.t_mod], [("MOD", 0)], q='pool')
        P.barrier()


def rms_rstd(K, xt, t_x, width, junk, ss, t_s):
    P = K.P
    P.act(junk, xt, AF.Square, [t_x], [t_s], accum_out=ss[:, 0:1])
    P.ts(ss[:, 0:1], ss[:, 0:1], 1.0 / width, EPS, ALU.mult, ALU.add, [t_s], [t_s])
    P.act(ss[:, 0:1], ss[:, 0:1], AF.Sqrt, [t_s], [t_s])
    P.recip(ss[:, 0:1], ss[:, 0:1], [t_s], [t_s])


def stage_inproj(K, l):
    P, nc, I = K.P, K.P.nc, K.I
    src = perm_cols(640)
    with SB(nc, "i_w", [128, 8, IN_W], F32) as W, SB(nc, "i_wp", [128, 8, 640], F32) as WP, \
            SB(nc, "i_x", [128, 2, D], F32) as xt, SB(nc, "i_h", [128, 2, D], F32) as ht, \
            SB(nc, "i_hT", [128, 8, 512], F32) as hT, SB(nc, "i_ss", [128, 2], F32) as ss, \
            SB(nc, "i_junk", [128, D], F32) as junk, SB(nc, "i_o", [128, 2, 768], F32) as ot, \
            SB(nc, "i_rc", [128, 512], F32) as rc, SB(nc, "i_rs", [128, 512], F32) as rs, \
            SB(nc, "i_t1", [128, 512], F32) as t1, SB(nc, "i_t2", [128, 512], F32) as t2, \
            SB(nc, "i_mod", [128, 4, D], F32) as modbc:
        t_W, t_WP = Tok("W"), Tok("WP")
        for jj, r in enumerate((0, 1, 6, 7)):
            P.dma(modbc[:, jj, :], K.MOD[r, :].partition_broadcast(128), (), [K.t_mod])
        t_x = [Tok("x0"), Tok("x1")]
        t_h = [Tok("h0"), Tok("h1")]
        t_s = [Tok("s0"), Tok("s1")]
        t_hT, t_o, t_rc, t_rs, t_t1, t_t2 = Tok("hT"), [Tok("o0"), Tok("o1")], Tok("rc"), Tok("rs"), Tok("t1"), Tok("t2")
        for k in range(8):
            P.dma(W[:, k, :], I['w_in'][l, k * 128:(k + 1) * 128, :], (), [t_W])
        wv = I['w_in'][l, :, HY_END:HY_END + 640].rearrange("(k p) (b two s) -> k p b two s", p=128, two=2, s=16)
        for k in range(8):
            dst = WP[:, k, :].rearrange("p (b two s) -> p b two s", two=2, s=16)
            P.dma(dst[:, :, 0, :], wv[k, :, :, 1, :], (), [t_WP])
            P.dma(dst[:, :, 1, :], wv[k, :, :, 0, :], (), [t_WP])
        ngroups = (T + 511) // 512
        for g in range(ngroups):
            t0 = g * 512
            ntok = min(512, T - t0)
            ntile = ntok // 128
            for i in range(ntile):
                ti = g * 4 + i
                b = i % 2
                isctx = ti < 2
                mb = 2 if isctx else 0
                P.dma(xt[:, b, :], K.X[ti * 128:(ti + 1) * 128, :], [K.tX[ti]], [t_x[b]])
                rms_rstd(K, xt[:, b, :], t_x[b], D, junk[:], ss[:, b:b + 1], t_s[b])
                P.stt(ht[:, b, :], xt[:, b, :], ss[:, b:b + 1], modbc[:, mb + 0, :], ALU.mult, ALU.mult,
                      [t_x[b], t_s[b], K.t_mod], [t_h[b]])
                P.tt(ht[:, b, :], ht[:, b, :], modbc[:, mb + 1, :], ALU.add, [t_h[b], K.t_mod], [t_h[b]])
                for half in range(2):
                    ps, tp = P.ps()
                    for j in range(4):
                        kk = half * 4 + j
                        P.tr(ps[:, j * 128:(j + 1) * 128], ht[:, b, kk * 128:(kk + 1) * 128], K.ident[:], [t_h[b], K.t_ident], [tp])
                    P.copy(hT[:, half * 4:(half + 1) * 4, i * 128:(i + 1) * 128],
                           ps[:, :].rearrange("p (j t) -> p j t", j=4), [tp], [t_hT], e='act' if half else 'dve')
            for i in range(ntile):
                ti = g * 4 + i
                b = i % 2
                for (c0, cw, dst, dcol) in ((0, 512, K.ZT, 0), (512, 256, K.ZT, 512), (K_END, 128, K.VT, 0)):
                    ps, tp = P.ps()
                    for k in range(8):
                        P.mm(ps[:, 0:cw], hT[:, k, i * 128:(i + 1) * 128], W[:, k, c0:c0 + cw], k == 0, k == 7, [t_hT, t_W], [tp])
                    P.copy(ot[:, b, 0:cw], ps[:, 0:cw], [tp], [t_o[b]], e='act')
                    P.dma(dst[ti * 128:(ti + 1) * 128, dcol:dcol + cw], ot[:, b, 0:cw], [t_o[b]], [(dst.tensor.name, ti)], q='pool')
            P.dma(rc[:, 0:ntok], I['k_ropec'][:, t0:t0 + ntok], (), [t_rc])
            P.dma(rs[:, 0:ntok], I['k_ropes'][:, t0:t0 + ntok], (), [t_rs])
            for cidx in range(5):
                c0 = HY_END + cidx * 128
                ps, tp = P.ps()
                ps2, tp2 = P.ps()
                for k in range(8):
                    P.mm(ps[:, 0:ntok], W[:, k, c0:c0 + 128], hT[:, k, 0:ntok], k == 0, k == 7, [t_hT, t_W], [tp])
                for k in range(8):
                    P.mm(ps2[:, 0:ntok], WP[:, k, cidx * 128:(cidx + 1) * 128], hT[:, k, 0:ntok], k == 0, k == 7, [t_hT, t_WP], [tp2])
                P.tt(t1[:, 0:ntok], ps[:, 0:ntok], rc[:, 0:ntok], ALU.mult, [tp, t_rc], [t_t1])
                P.tt(t2[:, 0:ntok], ps2[:, 0:ntok], rs[:, 0:ntok], ALU.mult, [tp2, t_rs], [t_t2])
                P.tt(t1[:, 0:ntok], t1[:, 0:ntok], t2[:, 0:ntok], ALU.add, [t_t1, t_t2], [t_t1])
                if cidx < 4:
                    P.dma(K.QF[cidx * 128:(cidx + 1) * 128, t0:t0 + ntok], t1[:, 0:ntok], [t_t1], [("QF", g)], q='pool')
                else:
                    P.dma(K.KF[:, t0:t0 + ntok], t1[:, 0:ntok], [t_t1], [("KF", g)], q='pool')
            for cidx in range(2):
                c0 = V_END + cidx * 128
                ps, tp = P.ps()
                for k in range(8):
                    P.mm(ps[:, 0:ntok], W[:, k, c0:c0 + 128], hT[:, k, 0:ntok], k == 0, k == 7, [t_hT, t_W], [tp])
                P.copy(t2[:, 0:ntok], ps[:, 0:ntok], [tp], [t_t2], e='act')
                P.dma(K.UF[cidx * 128:(cidx + 1) * 128, t0:t0 + ntok], t2[:, 0:ntok], [t_t2], [("UF", g)], q='pool')


def sin_chain(K, ps_ap, bcol, fcol, v, r, hid, n, t_ps, t_consts, t_v, t_r, t_hid):
    P = K.P
    P.act(v[:, 0:n], ps_ap, AF.Identity, [t_ps] + t_consts, [t_v], bias=bcol, scale=fcol)
    P.ts(r[:, 0:n], v[:, 0:n], MAGIC, MAGIC, ALU.add, ALU.subtract, [t_v], [t_r])
    P.tt(v[:, 0:n], v[:, 0:n], r[:, 0:n], ALU.subtract, [t_v, t_r], [t_v])
    P.act(hid[:, 0:n], v[:, 0:n], AF.Sin, [t_v], [t_hid], scale=TWO_PI)


def stage_hyena(K, l, L, row0, tag):
    P, nc, I = K.P, K.P.nc, K.I
    NCH = L // 128
    NK = NCH + 1
    CC, SS = I['k_cc' + tag], I['k_ss' + tag]
    featT, dec, wkc = I['k_feat' + tag], I['k_dec' + tag], I['k_wk' + tag]
    with SB(nc, "ha_w", [128, 4, 768], F32) as wb, SB(nc, "ha_z", [128, 3, 768], F32) as z, \
            SB(nc, "ha_t", [128, 2, 768], F32) as tt_:
        t_wb, t_z, t_t = Tok("wb"), [Tok("zm"), Tok("z0"), Tok("zp")], [Tok("ta"), Tok("tb")]
        for j in range(3):
            P.dma(wb[:, j, :], I['hy_conv_w'][l, j, :].partition_broadcast(128), (), [t_wb])
        P.dma(wb[:, 3, :], I['hy_conv_b'][l, :].partition_broadcast(128), (), [t_wb])
        for i in range(NCH):
            r0 = row0 + i * 128
            if i == 0:
                P.memset(z[0:1, 0, :], 0.0, [t_z[0]])
                P.dma(z[1:128, 0, :], K.ZT[r0:r0 + 127, 0:768], (), [t_z[0]])
            else:
                P.dma(z[:, 0, :], K.ZT[r0 - 1:r0 + 127, 0:768], (), [t_z[0]])
            P.dma(z[:, 1, :], K.ZT[r0:r0 + 128, 0:768], (), [t_z[1]])
            if i == NCH - 1:
                P.memset(z[:, 2, :], 0.0, [t_z[2]])
                P.dma(z[0:127, 2, :], K.ZT[r0 + 1:r0 + 128, 0:768], (), [t_z[2]])
            else:
                P.dma(z[:, 2, :], K.ZT[r0 + 1:r0 + 129, 0:768], (), [t_z[2]])
            P.tt(tt_[:, 0, :], z[:, 0, :], wb[:, 0, :], ALU.mult, [t_z[0], t_wb], [t_t[0]])
            P.tt(tt_[:, 1, :], z[:, 1, :], wb[:, 1, :], ALU.mult, [t_z[1], t_wb], [t_t[1]], e='pool')
            P.tt(tt_[:, 0, :], tt_[:, 0, :], tt_[:, 1, :], ALU.add, [t_t[0], t_t[1]], [t_t[0]])
            P.tt(tt_[:, 1, :], z[:, 2, :], wb[:, 2, :], ALU.mult, [t_z[2], t_wb], [t_t[1]], e='pool')
            P.tt(tt_[:, 0, :], tt_[:, 0, :], tt_[:, 1, :], ALU.add, [t_t[0], t_t[1]], [t_t[0]])
            P.tt(tt_[:, 0, :], tt_[:, 0, :], wb[:, 3, :], ALU.add, [t_t[0], t_wb], [t_t[0]])
            P.dma(K.ZC[r0:r0 + 128, :], tt_[:, 0, :], [t_t[0]], [("ZC", i)], q='pool')
    P.barrier()
    with SB(nc, "hb_taps", [128, NCH, 1024], F32) as taps, SB(nc, "hb_cc", [128, NK, 128], F32) as cct, \
            SB(nc, "hb_ss", [128, NK, 128], F32) as sst, SB(nc, "hb_f", [33, 512], F32) as ft, \
            SB(nc, "hb_w1", [33, 64], F32) as w1, SB(nc, "hb_w2", [64, 64], F32) as w2, \
            SB(nc, "hb_w3", [64, 1024], F32) as w3, SB(nc, "hb_c", [64, 8], F32) as cst, \
            SB(nc, "hb_v", [64, 512], F32) as v, SB(nc, "hb_r", [64, 512], F32) as r, \
            SB(nc, "hb_h1", [64, 512], F32) as h1, SB(nc, "hb_h2", [64, 512], F32) as h2, \
            SB(nc, "hb_dec", [128, 256], F32) as dct, SB(nc, "hb_abs", [128, 1024], F32) as ab, \
            SB(nc, "hb_rn", [128, 512], F32) as rn, SB(nc, "hb_tmp", [128, 512], F32) as tmp, \
            SB(nc, "hb_wk", [128, NK], F32) as wkt, SB(nc, "hb_o", [128, 2, 512], F32) as ko:
        t_taps = [Tok("taps%d" % c) for c in range(NCH)]
        t_cc, t_ss, t_f, t_w, t_c = Tok("cc"), Tok("ss"), Tok("f"), Tok("w"), Tok("c")
        t_v, t_r, t_h1, t_h2, t_dec, t_ab, t_rn, t_tmp, t_wk = (Tok(n) for n in ("v", "r", "h1", "h2", "dec", "ab", "rn", "tmp", "wk"))
        t_ko = [Tok("ko0"), Tok("ko1")]
        P.dma(w1[:], I['hy_f_w1'][l, :, :], (), [t_w])
        P.dma(w2[:], I['hy_f_w2'][l, :, :], (), [t_w])
        P.dma(w3[:], I['hy_f_w3'][l, :, :], (), [t_w])
        for j, nm in enumerate(('hy_f_b1', 'hy_f_freq1', 'hy_f_b2', 'hy_f_freq2')):
            P.dma(cst[:, j:j + 1], I[nm][l, :].rearrange("(p o) -> p o", o=1), (), [t_c])
        P.ts(cst[:, 4:5], cst[:, 1:2], 1.0 / (2.0 * math.pi), None, ALU.mult, None, [t_c], [t_c])
        P.ts(cst[:, 5:6], cst[:, 3:4], 1.0 / (2.0 * math.pi), None, ALU.mult, None, [t_c], [t_c])
        P.tt(cst[:, 6:7], cst[:, 0:1], cst[:, 4:5], ALU.mult, [t_c], [t_c])
        P.tt(cst[:, 7:8], cst[:, 2:3], cst[:, 5:6], ALU.mult, [t_c], [t_c])
        P.dma(wkt[:], wkc[:, 0].rearrange("(c p) -> p c", p=128), (), [t_wk])
        ng = (L + 511) // 512
        for g in range(ng):
            n0 = g * 512
            n = min(512, L - n0)
            P.dma(ft[:, 0:n], featT[:, n0:n0 + n], (), [t_f])
            ps, tp = P.ps()
            P.mm(ps[0:64, 0:n], w1[0:33, :], ft[0:33, 0:n], True, True, [t_w, t_f], [tp])
            sin_chain(K, ps[0:64, 0:n], cst[:, 6:7], cst[:, 4:5], v, r, h1, n, tp, [t_c], t_v, t_r, t_h1)
            ps, tp = P.ps()
            P.mm(ps[0:64, 0:n], w2[:, :], h1[:, 0:n], True, True, [t_w, t_h1], [tp])
            sin_chain(K, ps[0:64, 0:n], cst[:, 7:8], cst[:, 5:6], v, r, h2, n, tp, [t_c], t_v, t_r, t_h2)
            for sub in range(n // 128):
                c = (n0 // 128) + sub
                P.dma(dct[:], dec[c * 128:(c + 1) * 128, :], (), [t_dec])
                for half in range(2):
                    ps, tp = P.ps()
                    P.mm(ps[:, :], h2[:, sub * 128:(sub + 1) * 128], w3[:, half * 512:(half + 1) * 512], True, True, [t_h2, t_w], [tp])
                    P.tt(taps[:, c, half * 512:(half + 1) * 512].rearrange("p (d c) -> p d c", d=2),
                         ps[:, :].rearrange("p (d c) -> p d c", d=2),
                         dct[:, :].unsqueeze(1).broadcast_to([128, 2, 256]), ALU.mult, [tp, t_dec], [t_taps[c]])
        tv0 = taps[0:1, 0, :].rearrange("p (o d c) -> p o d c", o=2, d=2)
        P.memset(tv0[:, :, 1, :], 0.0, [t_taps[0]])
        held, hid_ = P.ps_hold(2)
        for c in range(NCH):
            P.act(ab[:], taps[:, c, :], AF.Abs, [t_taps[c]], [t_ab])
            for half in range(2):
                P.mm(held[half][0][:, :], K.ones[:, :], ab[:, half * 512:(half + 1) * 512], c == 0, c == NCH - 1, [t_ab, K.t_ones], [held[half][1]])
        for o in range(2):
            P.copy(ab[:, o * 512:o * 512 + 256], held[o][0][:, 0:256], [held[o][1]], [t_ab])
            P.tt(rn[:, o * 256:(o + 1) * 256], ab[:, o * 512:o * 512 + 256], held[o][0][:, 256:512], ALU.add, [held[o][1], t_ab], [t_rn])
        P.ps_release(hid_)
        P.recip(rn[:], rn[:], [t_rn], [t_rn])
        for c in range(NCH):
            tv = taps[:, c, :].rearrange("p (o d c) -> p o d c", o=2, d=2)
            tm = tmp[:, :].rearrange("p (o c) -> p o c", o=2)
            P.tt(tm, tv[:, :, 0, :], tv[:, :, 1, :], ALU.add, [t_taps[c]], [t_tmp])
            P.tt(tv[:, :, 1, :], tv[:, :, 1, :], tv[:, :, 0, :], ALU.subtract, [t_taps[c]], [t_taps[c]], e='pool')
            P.copy(tv[:, :, 0, :], tm, [t_tmp], [t_taps[c]])
        rn3 = rn[:, :].rearrange("p (o c) -> p o c", o=2)
        for kc in range(NK):
            P.dma(cct[:], CC[kc, :, :, :], (), [t_cc])
            P.dma(sst[:], SS[kc, :, :, :], (), [t_ss])
            for part, tab, ttab, d in ((0, cct, t_cc, 0), (1, sst, t_ss, 1)):
                ps, tp = P.ps()
                for c in range(NCH):
                    tv = taps[:, c, :].rearrange("p (o d c) -> p o d c", o=2, d=2)
                    P.mm(ps[:, :].rearrange("p (o c) -> p o c", o=2), tab[:, c, :], tv[:, :, d, :], c == 0, c == NCH - 1, [ttab, t_taps[c]], [tp])
                P.stt(ko[:, part, :].rearrange("p (o c) -> p o c", o=2), ps[:, :].rearrange("p (o c) -> p o c", o=2),
                      wkt[:, kc:kc + 1], rn3, ALU.mult, ALU.mult, [tp, t_wk, t_rn], [t_ko[part]])
                dst = K.KRE if part == 0 else K.KIM
                P.dma(dst[kc * 128:(kc + 1) * 128, :], ko[:, part, :], [t_ko[part]], [(dst.tensor.name, kc)], q='pool')
    P.barrier()
    with SB(nc, "hc_a", [128, NCH, 256], F32) as a, SB(nc, "hc_p1", [128, NK, 256], F32) as p1, \
            SB(nc, "hc_p2", [128, NK, 256], F32) as p2, SB(nc, "hc_cc", [128, NK, 128], F32) as cct, \
            SB(nc, "hc_ss", [128, NK, 128], F32) as sst, SB(nc, "hc_k", [128, 2, 256], F32) as kk, \
            SB(nc, "hc_t", [128, 2, 256], F32) as tq, SB(nc, "hc_b", [128, 2, 256], F32) as bb, \
            SB(nc, "hc_x", [128, 256], F32) as xg, SB(nc, "hc_y", [128, 256], F32) as yy:
        t_a = [Tok("a%d" % c) for c in range(NCH)]
        t_p1, t_p2, t_cc, t_ss, t_k, t_b, t_x, t_y = (Tok(n) for n in ("p1", "p2", "cc", "ss", "k", "b", "x", "y"))
        t_q = [Tok("q0"), Tok("q1")]
        for o in range(2):
            P.dma(bb[:, o, :], I['hy_bias'][l, o, :].partition_broadcast(128), (), [t_b])
        for c in range(NCH):
            P.dma(a[:, c, :], K.ZC[row0 + c * 128: row0 + (c + 1) * 128, 0:256], (), [t_a[c]])
        for o in range(2):
            for kc in range(NK):
                P.dma(cct[:], CC[kc, :, :, :], (), [t_cc])
                P.dma(sst[:], SS[kc, :, :, :], (), [t_ss])
                P.dma(kk[:, 0, :], K.KRE[kc * 128:(kc + 1) * 128, o * 256:(o + 1) * 256], (), [t_k])
                P.dma(kk[:, 1, :], K.KIM[kc * 128:(kc + 1) * 128, o * 256:(o + 1) * 256], (), [t_k])
                psr, tpr = P.ps()
                psi, tpi = P.ps()
                for c in range(NCH):
                    P.mm(psr[:, 0:256], cct[:, c, :], a[:, c, :], c == 0, c == NCH - 1, [t_cc, t_a[c]], [tpr])
                for c in range(NCH):
                    P.mm(psi[:, 0:256], sst[:, c, :], a[:, c, :], c == 0, c == NCH - 1, [t_ss, t_a[c]], [tpi])
                P.tt(tq[:, 0, :], psr[:, 0:256], kk[:, 0, :], ALU.mult, [tpr, t_k], [t_q[0]])
                P.tt(tq[:, 1, :], psi[:, 0:256], kk[:, 1, :], ALU.mult, [tpi, t_k], [t_q[1]])
                P.tt(p1[:, kc, :], tq[:, 0, :], tq[:, 1, :], ALU.add, [t_q[0], t_q[1]], [t_p1])
                P.tt(tq[:, 0, :], psi[:, 0:256], kk[:, 0, :], ALU.mult, [tpi, t_k], [t_q[0]])
                P.tt(tq[:, 1, :], psr[:, 0:256], kk[:, 1, :], ALU.mult, [tpr, t_k], [t_q[1]])
                P.tt(p2[:, kc, :], tq[:, 0, :], tq[:, 1, :], ALU.subtract, [t_q[0], t_q[1]], [t_p2])
            for tc in range(NCH):
                r0 = row0 + tc * 128
                P.dma(cct[:], CC[tc, :, :, :], (), [t_cc])
                P.dma(sst[:], SS[tc, :, :, :], (), [t_ss])
                P.dma(xg[:], K.ZC[r0:r0 + 128, 256 * (o + 1):256 * (o + 2)], (), [t_x])
                ps, tp = P.ps()
                for kc in range(NK):
                    P.mm(ps[:, 0:256], cct[:, kc, :], p1[:, kc, :], kc == 0, False, [t_cc, t_p1], [tp])
                for kc in range(NK):
                    P.mm(ps[:, 0:256], sst[:, kc, :], p2[:, kc, :], False, kc == NK - 1, [t_ss, t_p2], [tp])
                P.tt(yy[:], a[:, tc, :], bb[:, o, :], ALU.mult, [t_a[tc], t_b], [t_y])
                P.tt(yy[:], yy[:], ps[:, 0:256], ALU.add, [t_y, tp], [t_y])
                if o == 0:
                    P.tt(a[:, tc, :], yy[:], xg[:], ALU.mult, [t_y, t_x], [t_a[tc]])
                else:
                    P.tt(yy[:], yy[:], xg[:], ALU.mult, [t_y, t_x], [t_y])
                    P.dma(K.MIX[r0:r0 + 128, 0:256], yy[:], [t_y], [("MIX", r0)], q='pool')


def stage_attn(K, l):
    P, nc, I = K.P, K.P.nc, K.I
    with SB(nc, "at_k", [64, 2, T], F32) as kf, SB(nc, "at_v", [128, NT, 2, 65], F32) as v1, \
            SB(nc, "at_es", [128, 8], F32) as es, SB(nc, "at_ml", [128, 128], F32) as ml, \
            SB(nc, "at_mr", [128, 128], F32) as mr, SB(nc, "at_q", [64, 2, 4, 128], F32) as q4, \
            SB(nc, "at_pt", [128, 5, 512], F32) as pt, SB(nc, "at_o", [128, 2, 512], F32) as ot, \
            SB(nc, "at_d", [128, 2, 4], F32) as den:
        t_k, t_v, t_es, t_m = Tok("k"), Tok("v"), Tok("es"), Tok("m")
        t_q = [Tok("q0"), Tok("q1")]
        t_pt = [Tok("pt%d" % i) for i in range(5)]
        t_o = [Tok("o0"), Tok("o1")]
        t_d = Tok("d")
        for h in range(2):
            P.dma(kf[:, h, :], K.KF[h * 64:(h + 1) * 64, :], (), [t_k])
        P.memset(v1[:, :, :, 64:65], 1.0, [t_v])
        vtv = K.VT.rearrange("(t p) (h d) -> p t h d", p=128, h=2)
        for h in range(2):
            P.dma(v1[:, :, h, 0:64], vtv[:, :, h, :], (), [t_v])
        P.dma(es[:], I['attn_sink'][l, :].partition_broadcast(128), (), [t_es])
        P.act(es[:], es[:], AF.Exp, [t_es], [t_es])
        P.dma(ml[:], I['k_maskl'][:, :], (), [t_m])
        P.dma(mr[:], I['k_maskr'][:, :], (), [t_m])
        for qb in range(NT):
            kts = [(0, None), (1, None)]
            if qb >= 2:
                if qb - 1 >= 2:
                    kts.append((qb - 1, ml))
                kts.append((qb, None))
                if qb + 1 < NT:
                    kts.append((qb + 1, mr))
            ob = qb % 2
            for kvh in range(2):
                qi = kvh
                P.dma(q4[:, qi, :, :], K.QF[kvh * 256:(kvh + 1) * 256, qb * 128:(qb + 1) * 128].rearrange("(h d) t -> d h t", d=64),
                      (), [t_q[qi]])
                for idx, (kt, mask) in enumerate(kts):
                    ps, tp = P.ps()
                    P.mm(ps[:, :], kf[:, kvh, kt * 128:(kt + 1) * 128], q4[:, qi, :, :].rearrange("d h t -> d (h t)"), True, True,
                         [t_k, t_q[qi]], [tp])
                    P.act(pt[:, idx, :], ps[:, :], AF.Exp, [tp], [t_pt[idx]], scale=0.125)
                    if mask is not None:
                        pv = pt[:, idx, :].rearrange("p (h t) -> p h t", h=4)
                        P.tt(pv, pv, mask[:, :].unsqueeze(1).broadcast_to([128, 4, 128]), ALU.mult, [t_pt[idx], t_m], [t_pt[idx]])
                pso, tpo = P.ps()
                for hh in range(4):
                    for idx, (kt, mask) in enumerate(kts):
                        P.mm(pso[:, hh * 65:(hh + 1) * 65], pt[:, idx, hh * 128:(hh + 1) * 128], v1[:, kt, kvh, :],
                             idx == 0, idx == len(kts) - 1, [t_pt[idx], t_v], [tpo])
                pv = pso[:, 0:260].rearrange("p (h c) -> p h c", h=4)
                P.tt(den[:, kvh, :], pv[:, :, 64], es[:, kvh * 4:(kvh + 1) * 4], ALU.add, [tpo, t_es], [t_d])
                P.recip(den[:, kvh, :], den[:, kvh, :], [t_d], [t_d])
                P.tt(ot[:, ob, kvh * 256:(kvh + 1) * 256].rearrange("p (h d) -> p h d", h=4), pv[:, :, 0:64],
                     den[:, kvh, :].unsqueeze(2).broadcast_to([128, 4, 64]), ALU.mult, [tpo, t_d], [t_o[ob]])
            P.dma(K.MIX[qb * 128:(qb + 1) * 128, 256:768], ot[:, ob, :], [t_o[ob]], [("MIXa", qb)], q='pool')


def round_frac(K, v, r, t_v, t_r, e='dve'):
    P = K.P
    P.ts(r, v, MAGIC, MAGIC, ALU.add, ALU.subtract, [t_v], [t_r], e=e)
    P.tt(v, v, r, ALU.subtract, [t_v, t_r], [t_v], e=e)


def stage_s5(K, l):
    P, nc, I = K.P, K.P.nc, K.I
    K.GS = K.P.dram.get("sGS")
    if K.GS is None:
        K.GS = P.dram_t("sGS", [256, T])
    with SB(nc, "s_bt", [32, 16, 2, 128], F32) as BT, SB(nc, "s_cb", [128, 16, 2, 32], F32) as CB, \
            SB(nc, "s_par", [128, 12, 16], F32) as par, SB(nc, "s_dsk", [32, 8], F32) as dsk, \
            SB(nc, "s_iota", [128, 512], F32) as iot:
        t_BT, t_CB, t_par, t_dsk, t_iota = Tok("BT"), Tok("CB"), Tok("par"), Tok("dsk"), Tok("iota")
        MAG, PHI = 4, 6
        with SB(nc, "s_b", [128, 2, 16, 16], F32) as bri, SB(nc, "s_bb", [128, 2, 16, 16], F32) as bb, \
                SB(nc, "s_bd", [128, 16, 2, 32], F32) as BD, SB(nc, "s_cnd", [32, 16, 2, 128], F32) as CND, \
                SB(nc, "s_t1", [128, 16, 16], F32) as t1, SB(nc, "s_t2", [128, 16, 16], F32) as t2:
            t_b, t_bb, t_BD, t_CND, t_t1, t_t2 = Tok("b"), Tok("bb"), Tok("BD"), Tok("CND"), Tok("t1"), Tok("t2")
            for d in range(2):
                P.dma(par[:, 0, d * 8:(d + 1) * 8], I['s5_lam_re'][l, d].rearrange("g p -> (g p)").rearrange("(gh q) -> q gh", q=128), (), [t_par])
                P.dma(par[:, 1, d * 8:(d + 1) * 8], I['s5_lam_im'][l, d].rearrange("g p -> (g p)").rearrange("(gh q) -> q gh", q=128), (), [t_par])
                ldv = I['s5_log_dt'][l, d].rearrange("(gh gl) -> gl gh", gl=2)
                for gl in range(2):
                    P.dma(par[gl * 64:(gl + 1) * 64, 2, d * 8:(d + 1) * 8], ldv[gl].partition_broadcast(64), (), [t_par])
                for ri, nm in enumerate(('s5_b_re', 's5_b_im')):
                    P.dma(bri[:, ri, d * 8:(d + 1) * 8, :],
                          I[nm][l, d].rearrange("g p c -> (g p c)").rearrange("(gh q c) -> q gh c", q=128, c=16), (), [t_b])
            P.dma(dsk[:], I['s5_d'][l, :].rearrange("(j r) -> r j", r=32), (), [t_dsk])
            P.dma(iot[:], I['k_iota'][0, :].partition_broadcast(128), (), [t_iota])
            pp = lambda i: par[:, i, :]
            tp_ = [t_par]
            P.act(pp(2), pp(2), AF.Exp, tp_, tp_)
            P.tt(pp(3), pp(0), pp(2), ALU.mult, tp_, tp_)
            P.act(pp(MAG), pp(3), AF.Exp, tp_, tp_)
            P.tt(pp(3), pp(1), pp(2), ALU.mult, tp_, tp_)
            P.ts(pp(PHI), pp(3), 1.0 / (2.0 * math.pi), None, ALU.mult, None, tp_, tp_)
            P.copy(pp(5), pp(PHI), tp_, tp_)
            round_frac(K, pp(5), pp(11), t_par, t_par)
            P.act(pp(8), pp(5), AF.Sin, tp_, tp_, scale=TWO_PI)
            P.ts(pp(5), pp(PHI), 0.25, None, ALU.add, None, tp_, tp_)
            round_frac(K, pp(5), pp(11), t_par, t_par)
            P.act(pp(7), pp(5), AF.Sin, tp_, tp_, scale=TWO_PI)
            P.tt(pp(7), pp(7), pp(MAG), ALU.mult, tp_, tp_)
            P.tt(pp(8), pp(8), pp(MAG), ALU.mult, tp_, tp_)
            P.ts(pp(7), pp(7), -1.0, None, ALU.add, None, tp_, tp_)
            P.tt(pp(3), pp(0), pp(0), ALU.mult, tp_, tp_)
            P.tt(pp(5), pp(1), pp(1), ALU.mult, tp_, tp_)
            P.tt(pp(3), pp(3), pp(5), ALU.add, tp_, tp_)
            P.recip(pp(3), pp(3), tp_, tp_)
            P.tt(pp(9), pp(7), pp(0), ALU.mult, tp_, tp_)
            P.tt(pp(5), pp(8), pp(1), ALU.mult, tp_, tp_)
            P.tt(pp(9), pp(9), pp(5), ALU.add, tp_, tp_)
            P.tt(pp(9), pp(9), pp(3), ALU.mult, tp_, tp_)
            P.tt(pp(10), pp(8), pp(0), ALU.mult, tp_, tp_)
            P.tt(pp(5), pp(7), pp(1), ALU.mult, tp_, tp_)
            P.tt(pp(10), pp(10), pp(5), ALU.subtract, tp_, tp_)
            P.tt(pp(10), pp(10), pp(3), ALU.mult, tp_, tp_)
            cre = par[:, 9, :].unsqueeze(2).broadcast_to([128, 16, 16])
            cim = par[:, 10, :].unsqueeze(2).broadcast_to([128, 16, 16])
            P.tt(t1[:], bri[:, 0, :, :], cre, ALU.mult, [t_b, t_par], [t_t1])
            P.tt(t2[:], bri[:, 1, :, :], cim, ALU.mult, [t_b, t_par], [t_t2])
            P.tt(bb[:, 0, :, :], t1[:], t2[:], ALU.subtract, [t_t1, t_t2], [t_bb])
            P.tt(t1[:], bri[:, 1, :, :], cre, ALU.mult, [t_b, t_par], [t_t1])
            P.tt(t2[:], bri[:, 0, :, :], cim, ALU.mult, [t_b, t_par], [t_t2])
            P.tt(bb[:, 1, :, :], t1[:], t2[:], ALU.add, [t_t1, t_t2], [t_bb])
            P.memset(BD[:], 0.0, [t_BD])
            for ri in range(2):
                P.copy(BD[0:64, :, ri, 0:16], bb[0:64, ri, :, :], [t_bb], [t_BD])
                P.copy(BD[64:128, :, ri, 16:32], bb[64:128, ri, :, :], [t_bb], [t_BD])
            for dg in range(16):
                ps, tp = P.ps()
                for ri in range(2):
                    P.tr(ps[0:32, ri * 128:(ri + 1) * 128], BD[:, dg, ri, :], K.ident[:], [t_BD, K.t_ident], [tp])
                P.copy(BT[:, dg, :, :], ps[0:32, 0:256].rearrange("p (r m) -> p r m", r=2), [tp], [t_BT])
            P.memset(CND[:], 0.0, [t_CND])
            for d in range(2):
                for ri, nm in enumerate(('s5_c_re', 's5_c_im')):
                    cv = I[nm][l, d].rearrange("(gh gl) c p -> gl c gh p", gl=2)
                    for gl in range(2):
                        P.dma(CND[gl * 16:(gl + 1) * 16, d * 8:(d + 1) * 8, ri, gl * 64:(gl + 1) * 64], cv[gl], (), [t_CND])
            for dg in range(16):
                ps, tp = P.ps()
                for ri in range(2):
                    P.tr(ps[:, ri * 32:(ri + 1) * 32], CND[:, dg, ri, :], K.ident[0:32, 0:32], [t_CND, K.t_ident], [tp])
                P.copy(CB[:, dg, 0, :], ps[:, 0:32], [tp], [t_CB])
                P.ts(CB[:, dg, 1, :], ps[:, 32:64], -1.0, None, ALU.mult, None, [tp], [t_CB])
        P.barrier()
        with SB(nc, "s_us", [32, T], F32) as us, SB(nc, "s_y", [32, T], F32) as ysb, \
                SB(nc, "s_tab", [128, 4, 512], F32) as tab, SB(nc, "s_m", [128, 2, 512], F32) as mm_, \
                SB(nc, "s_w", [128, 2, 512], F32) as ww, SB(nc, "s_s", [128, 2, 512], F32) as ss_, \
                SB(nc, "s_q", [128, 2, 512], F32) as qq, SB(nc, "s_c0", [128, 4], F32) as c0, \
                SB(nc, "s_car", [128, 2], F32) as car, SB(nc, "s_mag", [128, 512], F32) as magt, SB(nc, "s_g", [32, 2, T], F32) as gg:
            t_us, t_y, t_m, t_w, t_s, t_q, t_c0, t_car, t_g = (Tok(n) for n in ("us", "y", "m", "w", "s", "q", "c0", "car", "g"))
            t_tab = [Tok("tabs"), Tok("tabc"), Tok("vr"), Tok("vr2")]
            t_mag = Tok("mag")
            for j in range(8):
                P.dma(us[:], K.UF[32 * j:32 * (j + 1), :], (), [t_us])
                for d in range(2):
                    dg = d * 8 + j
                    if d == 0:
                        chunks = [(a, min(a + 512, T), False, a) for a in range(0, T, 512)]
                    else:
                        chunks = [(0, NCTX, True, 0)]
                        b = T
                        while b > NCTX:
                            a = max(b - 512, NCTX)
                            chunks.append((a, b, True, NCTX + (T - b)))
                            b = a
                    first = True
                    phi = par[:, PHI, dg:dg + 1]
                    P.copy(magt[:, :], par[:, MAG, dg:dg + 1].broadcast_to([128, 512]), [t_par], [t_mag])
                    for (a, b, rev, i0) in chunks:
                        n = b - a
                        rv = (lambda ap: ap[:, ::-1]) if rev else (lambda ap: ap)
                        ps1, tp1 = P.ps()
                        ps2, tp2 = P.ps()
                        P.mm(ps1[:, 0:n], BT[:, dg, 0, :], us[:, a:b], True, True, [t_BT, t_us], [tp1])
                        P.mm(ps2[:, 0:n], BT[:, dg, 1, :], us[:, a:b], True, True, [t_BT, t_us], [tp2])
                        P.ts(c0[:, 0:1], phi, float(i0), None, ALU.mult, None, [t_par], [t_c0])
                        round_frac(K, c0[:, 0:1], c0[:, 2:3], t_c0, t_c0)
                        P.ts(c0[:, 1:2], c0[:, 0:1], 0.25, None, ALU.add, None, [t_c0], [t_c0])
                        for which in range(2):
                            P.act(tab[:, 2 + which, 0:n], iot[:, 0:n], AF.Identity, [t_iota, t_c0, t_par], [t_tab[2 + which]],
                                  bias=c0[:, which:which + 1], scale=phi)
                            P.ts(qq[:, which, 0:n], tab[:, 2 + which, 0:n], MAGIC, MAGIC, ALU.add, ALU.subtract, [t_tab[2 + which]], [t_q])
                            P.tt(tab[:, 2 + which, 0:n], tab[:, 2 + which, 0:n], qq[:, which, 0:n], ALU.subtract, [t_tab[2 + which], t_q],
                                 [t_tab[2 + which]])
                            P.act(tab[:, which, 0:n], tab[:, 2 + which, 0:n], AF.Sin, [t_tab[2 + which]], [t_tab[which]], scale=TWO_PI)
                        sn = rv(tab[:, 0, 0:n])
                        cs = rv(tab[:, 1, 0:n])
                        tS, tC = t_tab[0], t_tab[1]
                        P.tt(mm_[:, 0, 0:n], ps1[:, 0:n], cs, ALU.mult, [tp1, tC], [t_m])
                        P.tt(qq[:, 0, 0:n], ps2[:, 0:n], sn, ALU.mult, [tp2, tS], [t_q])
                        P.tt(mm_[:, 0, 0:n], mm_[:, 0, 0:n], qq[:, 0, 0:n], ALU.add, [t_m, t_q], [t_m])
                        P.tt(mm_[:, 1, 0:n], ps2[:, 0:n], cs, ALU.mult, [tp2, tC], [t_m])
                        P.tt(qq[:, 1, 0:n], ps1[:, 0:n], sn, ALU.mult, [tp1, tS], [t_q])
                        P.tt(mm_[:, 1, 0:n], mm_[:, 1, 0:n], qq[:, 1, 0:n], ALU.subtract, [t_m, t_q], [t_m])
                        magb = magt[:, 0:n]
                        for ri in range(2):
                            init = 0.0 if first else car[:, ri:ri + 1]
                            P.op('dve', lambda g, ri=ri, init=init: g.tensor_tensor_scan(
                                out=rv(ww[:, ri, 0:n]), data0=magb, data1=rv(mm_[:, ri, 0:n]), initial=init,
                                op0=ALU.mult, op1=ALU.add), [t_m, t_mag, t_car], [t_w])
                        last = a if rev else b - 1
                        P.copy(car[:, :], ww[:, :, last - a], [t_w], [t_car])
                        first = False
                        P.tt(ss_[:, 0, 0:n], ww[:, 0, 0:n], cs, ALU.mult, [t_w, tC], [t_s], e='pool')
                        P.tt(qq[:, 0, 0:n], ww[:, 1, 0:n], sn, ALU.mult, [t_w, tS], [t_q], e='pool')
                        P.tt(ss_[:, 0, 0:n], ss_[:, 0, 0:n], qq[:, 0, 0:n], ALU.subtract, [t_s, t_q], [t_s], e='pool')
                        P.tt(ss_[:, 1, 0:n], ww[:, 0, 0:n], sn, ALU.mult, [t_w, tS], [t_s], e='pool')
                        P.tt(qq[:, 1, 0:n], ww[:, 1, 0:n], cs, ALU.mult, [t_w, tC], [t_q], e='pool')
                        P.tt(ss_[:, 1, 0:n], ss_[:, 1, 0:n], qq[:, 1, 0:n], ALU.add, [t_s, t_q], [t_s], e='pool')
                        psy, tpy = P.ps()
                        P.mm(psy[0:32, 0:n], CB[:, dg, 0, :], ss_[:, 0, 0:n], True, False, [t_CB, t_s], [tpy])
                        P.mm(psy[0:32, 0:n], CB[:, dg, 1, :], ss_[:, 1, 0:n], False, True, [t_CB, t_s], [tpy])
                        if d == 0:
                            P.copy(ysb[:, a:b], psy[0:32, 0:n], [tpy], [t_y], e='act')
                        else:
                            P.tt(ysb[:, a:b], ysb[:, a:b], psy[0:32, 0:n], ALU.add, [t_y, tpy], [t_y])
                P.stt(ysb[:], us[:], dsk[:, j:j + 1], ysb[:], ALU.mult, ALU.add, [t_us, t_dsk, t_y], [t_y])
                P.tt(gg[:, 0, :], ysb[:], ysb[:], ALU.mult, [t_y], [t_g])
                P.ts(gg[:, 0, :], gg[:, 0, :], 0.044715, 1.0, ALU.mult, ALU.add, [t_g], [t_g])
                P.tt(gg[:, 0, :], gg[:, 0, :], ysb[:], ALU.mult, [t_g, t_y], [t_g])
                P.act(gg[:, 1, :], gg[:, 0, :], AF.Sigmoid, [t_g], [t_g], scale=1.5957691216)
                P.tt(gg[:, 1, :], gg[:, 1, :], ysb[:], ALU.mult, [t_g, t_y], [t_g])
                P.dma(K.GS[32 * j:32 * (j + 1), :], gg[:, 1, :], [t_g], [("GS", j)], q='pool')
        P.barrier()
        with SB(nc, "s_gw", [128, 2, 256], F32) as gw, SB(nc, "s_gb", [128, 2], F32) as gb, \
                SB(nc, "s_gt", [128, 2, 512], F32) as gt, SB(nc, "s_sg", [128, 2, 512], F32) as sg, \
                SB(nc, "s_o", [128, 4, 256], F32) as so:
            t_gw, t_gt, t_sg, t_so = Tok("gw"), Tok("gt"), Tok("sg"), Tok("so")
            P.dma(gw[:], I['s5_glu_w'][l].rearrange("(c p) n -> p c n", p=128), (), [t_gw])
            P.dma(gb[:], I['s5_glu_b'][l, :].rearrange("(c p) -> p c", p=128), (), [t_gw])
            for g in range((T + 511) // 512):
                t0 = g * 512
                n = min(512, T - t0)
                P.dma(gt[:, :, 0:n], K.GS[:, t0:t0 + n].rearrange("(c p) t -> p c t", p=128), (), [t_gt])
                for oc in range(2):
                    ps, tp = P.ps()
                    for kc in range(2):
                        P.mm(ps[:, 0:n], gw[:, kc, oc * 128:(oc + 1) * 128], gt[:, kc, 0:n], kc == 0, kc == 1, [t_gw, t_gt], [tp])
                    P.act(sg[:, oc, 0:n], ps[:, 0:n], AF.Sigmoid, [tp, t_gw], [t_sg], bias=gb[:, oc:oc + 1], scale=1.0)
                    P.tt(sg[:, oc, 0:n], sg[:, oc, 0:n], gt[:, oc, 0:n], ALU.mult, [t_sg, t_gt], [t_sg])
                for i in range(n // 128):
                    ps, tp = P.ps()
                    for oc in range(2):
                        P.tr(ps[:, oc * 128:(oc + 1) * 128], sg[:, oc, i * 128:(i + 1) * 128], K.ident[:], [t_sg, K.t_ident], [tp])
                    P.copy(so[:, i, :], ps[:, 0:256], [tp], [t_so], e='act')
                P.dma(K.MIX[t0:t0 + n, 768:1024].rearrange("(i p) c -> p i c", p=128), so[:, 0:n // 128, :], [t_so], [("MIXs", g)], q='pool')


def stage_outproj(K, l):
    P, nc, I = K.P, K.P.nc, K.I
    with SB(nc, "o_w", [128, 8, D], F32) as W, SB(nc, "o_rw", [128, 8, 16], F32) as RW, \
            SB(nc, "o_gain", [128, D], F32) as gain, SB(nc, "o_mod", [128, 6, D], F32) as mod, \
            SB(nc, "o_rb", [128, 16], F32) as rb, SB(nc, "o_mix", [128, D], F32) as mix, \
            SB(nc, "o_x", [128, D], F32) as xt, SB(nc, "o_junk", [128, D], F32) as junk, \
            SB(nc, "o_mT", [128, 8, 128], F32) as mT, SB(nc, "o_h2", [128, D], F32) as h2, \
            SB(nc, "o_hT", [128, 8, 128], F32) as hT, SB(nc, "o_tmp", [128, D], F32) as tmp, \
            SB(nc, "o_st", [128, 8], F32) as st, SB(nc, "o_r", [128, 12, 16], F32) as rr, \
            SB(nc, "o_gT", [16, 128], F32) as gT:
        t_W, t_c, t_mix, t_x, t_mT, t_h2, t_hT, t_tmp, t_st, t_rr, t_gT = (Tok(n) for n in (
            "W", "c", "mix", "x", "mT", "h2", "hT", "tmp", "st", "rr", "gT"))
        P.dma(W[:], I['w_out'][l].rearrange("(k p) n -> p k n", p=128), (), [t_W])
        P.dma(RW[:], I['router_w'].rearrange("(k p) n -> p k n", p=128), (), [t_c])
        P.dma(gain[:], I['mix_norm_g'][l, :].partition_broadcast(128), (), [t_c])
        P.dma(rb[:], I['router_b'][0, :].partition_broadcast(128), (), [t_c])
        for jj, r in enumerate((2, 3, 4, 8, 9, 10)):
            P.dma(mod[:, jj, :], K.MOD[r, :].partition_broadcast(128), (), [t_c])
        groups = ((0, 256), (256, 768), (768, 1024))
        for ti in range(NT):
            mb = 3 if ti < 2 else 0
            rows = slice(ti * 128, (ti + 1) * 128)
            P.dma(mix[:], K.MIX[rows, :], (), [t_mix])
            P.dma(xt[:], K.X[rows, :], (), [t_x])
            for gi, (c0, c1) in enumerate(groups):
                rms_rstd(K, mix[:, c0:c1], t_mix, c1 - c0, junk[:, c0:c1], st[:, gi:gi + 1], t_st)
            for gi, (c0, c1) in enumerate(groups):
                P.stt(mix[:, c0:c1], mix[:, c0:c1], st[:, gi:gi + 1], gain[:, c0:c1], ALU.mult, ALU.mult, [t_mix, t_st, t_c], [t_mix])
            for half in range(2):
                ps, tp = P.ps()
                for j in range(4):
                    kk = half * 4 + j
                    P.tr(ps[:, j * 128:(j + 1) * 128], mix[:, kk * 128:(kk + 1) * 128], K.ident[:], [t_mix, K.t_ident], [tp])
                P.copy(mT[:, half * 4:(half + 1) * 4, :], ps[:, :].rearrange("p (j t) -> p j t", j=4), [tp], [t_mT], e='act' if half else 'dve')
            for half in range(2):
                ps, tp = P.ps()
                for k in range(8):
                    P.mm(ps[:, :], mT[:, k, :], W[:, k, half * 512:(half + 1) * 512], k == 0, k == 7, [t_mT, t_W], [tp])
                hs = slice(half * 512, (half + 1) * 512)
                P.tt(tmp[:, hs], ps[:, :], mod[:, mb + 0, hs], ALU.mult, [tp, t_c], [t_tmp])
                P.tt(xt[:, hs], xt[:, hs], tmp[:, hs], ALU.add, [t_x, t_tmp], [t_x])
            P.dma(K.X[rows, :], xt[:], [t_x], [("X", ti)], q='pool')
            rms_rstd(K, xt[:], t_x, D, junk[:], st[:, 3:4], t_st)
            P.stt(h2[:], xt[:], st[:, 3:4], mod[:, mb + 1, :], ALU.mult, ALU.mult, [t_x, t_st, t_c], [t_h2])
            P.tt(h2[:], h2[:], mod[:, mb + 2, :], ALU.add, [t_h2, t_c], [t_h2])
            for half in range(2):
                ps, tp = P.ps()
                for j in range(4):
                    kk = half * 4 + j
                    P.tr(ps[:, j * 128:(j + 1) * 128], h2[:, kk * 128:(kk + 1) * 128], K.ident[:], [t_h2, K.t_ident], [tp])
                P.copy(hT[:, half * 4:(half + 1) * 4, :], ps[:, :].rearrange("p (j t) -> p j t", j=4), [tp], [t_hT], e='act' if half else 'dve')
            P.dma(K.H2T[:, ti * 128:(ti + 1) * 128].rearrange("(k p) t -> p k t", p=128), hT[:], [t_hT], [("H2T", ti)], q='pool')
            ps, tp = P.ps()
            for k in range(8):
                P.mm(ps[:, 0:16], hT[:, k, :], RW[:, k, :], k == 0, k == 7, [t_hT, t_c], [tp])
            R = lambda i: rr[:, i, :]
            R4 = lambda i: rr[:, i, :].rearrange("p (g e) -> p g e", g=4)
            trr = [t_rr]
            P.op('dve', lambda g: g.reduce_max(out=st[:, 4:5], in_=ps[:, 0:16], axis=AX.X), [tp], [t_st])
            P.ts(st[:, 4:5], st[:, 4:5], -1.0, None, ALU.mult, None, [t_st], [t_st])
            P.act(R(0), ps[:, 0:16], AF.Exp, [tp, t_st], trr, bias=st[:, 4:5], scale=1.0, accum_out=st[:, 5:6])
            P.recip(st[:, 5:6], st[:, 5:6], [t_st, t_rr], [t_st])
            P.ts(R(0), R(0), st[:, 5:6], None, ALU.mult, None, trr + [t_st], trr)
            P.tt(R(1), R(0), rb[:], ALU.add, trr + [t_c], trr)
            P.op('dve', lambda g: g.reduce_max(out=rr[:, 2, 0:4], in_=R4(1), axis=AX.X), trr, trr)
            P.tt(R4(3), R4(1), rr[:, 2, 0:4].unsqueeze(2).broadcast_to([128, 4, 4]), ALU.is_equal, trr, trr)
            P.stt(R(4), R(3), -1e9, R(1), ALU.mult, ALU.add, trr, trr)
            P.op('dve', lambda g: g.reduce_max(out=rr[:, 5, 0:4], in_=R4(4), axis=AX.X), trr, trr)
            P.tt(rr[:, 6, 0:4], rr[:, 2, 0:4], rr[:, 5, 0:4], ALU.add, trr, trr)
            P.op('dve', lambda g: g.reduce_max(out=st[:, 6:7], in_=rr[:, 6, 0:4], axis=AX.X), trr, [t_st])
            P.ts(rr[:, 7, 0:4], rr[:, 6, 0:4], st[:, 6:7], None, ALU.is_equal, None, trr + [t_st], trr)
            P.tt(R4(8), R4(1), rr[:, 5, 0:4].unsqueeze(2).broadcast_to([128, 4, 4]), ALU.is_ge, trr, trr)
            P.tt(R4(8), R4(8), rr[:, 7, 0:4].unsqueeze(2).broadcast_to([128, 4, 4]), ALU.mult, trr, trr)
            P.tt(R(9), R(8), R(0), ALU.mult, trr, trr)
            P.op('dve', lambda g: g.reduce_sum(out=st[:, 7:8], in_=R(9), axis=AX.X), trr, [t_st])
            P.recip(st[:, 7:8], st[:, 7:8], [t_st], [t_st])
            P.ts(R(9), R(9), st[:, 7:8], None, ALU.mult, None, trr + [t_st], trr)
            ps2, tp2 = P.ps()
            P.tr(ps2[0:16, 0:128], R(9), K.ident[:], trr + [K.t_ident], [tp2])
            P.copy(gT[:], ps2[0:16, 0:128], [tp2], [t_gT], e='act')
            P.dma(K.GTT[:, ti * 128:(ti + 1) * 128], gT[:], [t_gT], [("GTT", ti)], q='pool')


def stage_moe(K, l):
    P, nc, I = K.P, K.P.nc, K.I
    GN = 1024
    with SB(nc, "e_h", [128, 8, GN], F32) as hT, SB(nc, "e_g", [128, 8, GN], F32) as GT, \
            SB(nc, "e_y", [128, GN // 128, D], F32) as Y, SB(nc, "e_wd", [128, 8, D], F32) as wd, \
            SB(nc, "e_wg", [128, 2, 8, 128], F32) as wg, SB(nc, "e_wu", [128, 2, 8, 128], F32) as wu, \
            SB(nc, "e_gt", [16, GN], F32) as gt, SB(nc, "e_gb", [128, GN], F32) as gB, \
            SB(nc, "e_sel", [16, 16, 128], F32) as sel, SB(nc, "e_sa", [128, 2, 512], F32) as sA, \
            SB(nc, "e_mod", [128, 2, D], F32) as mod, SB(nc, "e_x", [128, D], F32) as xt:
        t_h, t_G, t_Y, t_wd, t_gt, t_gB, t_sel, t_mod, t_x = (Tok(n) for n in ("h", "G", "Y", "wd", "gt", "gB", "sel", "mod", "x"))
        t_wg = [Tok("wg0"), Tok("wg1")]
        t_wu = [Tok("wu0"), Tok("wu1")]
        t_sa = [Tok("sa0"), Tok("sa1")]
        for e in range(16):
            P.copy(sel[:, e, :], K.ident[0:16, e:e + 1].broadcast_to([16, 128]), [K.t_ident], [t_sel])
        P.dma(mod[:, 0, :], K.MOD[5, :].partition_broadcast(128), (), [t_mod])
        P.dma(mod[:, 1, :], K.MOD[11, :].partition_broadcast(128), (), [t_mod])
        for g in range((T + GN - 1) // GN):
            t0 = g * GN
            n = min(GN, T - t0)
            nt = n // 128
            P.dma(hT[:, :, 0:n], K.H2T[:, t0:t0 + n].rearrange("(k p) t -> p k t", p=128), (), [t_h])
            P.dma(gt[:, 0:n], K.GTT[:, t0:t0 + n], (), [t_gt])
            P.memset(Y[:, 0:nt, :], 0.0, [t_Y], e='pool')
            for e in range(16):
                P.dma(wd[:], I['moe_w_down'][l, e].rearrange("(c p) n -> p c n", p=128), (), [t_wd])
                for s0 in range(0, n, 512):
                    sn = min(512, n - s0)
                    ps, tp = P.ps()
                    P.mm(ps[:, 0:sn], sel[:, e, :], gt[:, s0:s0 + sn], True, True, [t_sel, t_gt], [tp])
                    P.copy(gB[:, s0:s0 + sn], ps[:, 0:sn], [tp], [t_gB], e='act')
                wgv = I['moe_w_gate'][l, e].rearrange("(k p) n -> p k n", p=128)
                wuv = I['moe_w_up'][l, e].rearrange("(k p) n -> p k n", p=128)
                for c in range(8):
                    b = c % 2
                    P.dma(wg[:, b, :, :], wgv[:, :, c * 128:(c + 1) * 128], (), [t_wg[b]])
                    P.dma(wu[:, b, :, :], wuv[:, :, c * 128:(c + 1) * 128], (), [t_wu[b]])
                    for si, s0 in enumerate(range(0, n, 512)):
                        sn = min(512, n - s0)
                        psa, tpa = P.ps()
                        psu, tpu = P.ps()
                        for k in range(8):
                            P.mm(psa[:, 0:sn], wg[:, b, k, :], hT[:, k, s0:s0 + sn], k == 0, k == 7, [t_wg[b], t_h], [tpa])
                        for k in range(8):
                            P.mm(psu[:, 0:sn], wu[:, b, k, :], hT[:, k, s0:s0 + sn], k == 0, k == 7, [t_wu[b], t_h], [tpu])
                        sb_ = si % 2
                        P.act(sA[:, sb_, 0:sn], psa[:, 0:sn], AF.Silu, [tpa], [t_sa[sb_]])
                        P.tt(sA[:, sb_, 0:sn], sA[:, sb_, 0:sn], psu[:, 0:sn], ALU.mult, [t_sa[sb_], tpu], [t_sa[sb_]])
                        P.tt(GT[:, c, s0:s0 + sn], sA[:, sb_, 0:sn], gB[:, s0:s0 + sn], ALU.mult, [t_sa[sb_], t_gB], [t_G], e='pool')
                for i in range(nt):
                    for half in range(2):
                        ps, tp = P.ps()
                        for c in range(8):
                            P.mm(ps[:, :], GT[:, c, i * 128:(i + 1) * 128], wd[:, c, half * 512:(half + 1) * 512], c == 0, c == 7, [t_G, t_wd], [tp])
                        hs = slice(half * 512, (half + 1) * 512)
                        P.tt(Y[:, i, hs], Y[:, i, hs], ps[:, :], ALU.add, [t_Y, tp], [t_Y])
            for i in range(nt):
                ti = g * (GN // 128) + i
                mb = 1 if ti < 2 else 0
                rows = slice(ti * 128, (ti + 1) * 128)
                P.dma(xt[:], K.X[rows, :], (), [t_x])
                P.tt(Y[:, i, :], Y[:, i, :], mod[:, mb, :], ALU.mult, [t_Y, t_mod], [t_Y], e='pool')
                P.tt(xt[:], xt[:], Y[:, i, :], ALU.add, [t_x, t_Y], [t_x])
                P.dma(K.X[rows, :], xt[:], [t_x], [("X", ti)], q='pool')


def stage_final(K):
    P, nc, I = K.P, K.P.nc, K.I
    with SB(nc, "f_g", [128, D], F32) as gbc, SB(nc, "f_x", [128, 2, D], F32) as xt, \
            SB(nc, "f_j", [128, D], F32) as junk, SB(nc, "f_s", [128, 2], F32) as st:
        t_g = Tok("g")
        t_x = [Tok("x0"), Tok("x1")]
        t_s = [Tok("s0"), Tok("s1")]
        P.dma(gbc[:], I['final_g'][0, :].partition_broadcast(128), (), [t_g])
        for i in range(NLAT // 128):
            b = i % 2
            P.dma(xt[:, b, :], K.X[NCTX + i * 128:NCTX + (i + 1) * 128, :], (), [t_x[b]])
            rms_rstd(K, xt[:, b, :], t_x[b], D, junk[:], st[:, b:b + 1], t_s[b])
            P.stt(xt[:, b, :], xt[:, b, :], st[:, b:b + 1], gbc[:], ALU.mult, ALU.mult, [t_x[b], t_s[b], t_g], [t_x[b]])
            P.dma(K.out[i * 128:(i + 1) * 128, :], xt[:, b, :], [t_x[b]], [("out", i)], q='pool')


_CONSTS = None


def consts():
    global _CONSTS
    if _CONSTS is not None:
        return _CONSTS
    c = {}
    c['k_ident'] = np.eye(128, dtype=np.float32)
    rc, rs = rope_tables()
    c['k_ropec'], c['k_ropes'] = rc, rs
    j = np.arange(128)[:, None]
    i = np.arange(128)[None, :]
    c['k_maskl'] = (j >= i).astype(np.float32)
    c['k_maskr'] = (j <= i).astype(np.float32)
    c['k_ccL'], c['k_ssL'] = dft_blocks(NLAT, 33)
    c['k_ccC'], c['k_ssC'] = dft_blocks(NCTX, 3)
    f, d, w = hyena_consts(NLAT)
    c['k_featL'], c['k_decL'], c['k_wkL'] = f, d, w.reshape(-1, 1)
    f, d, w = hyena_consts(NCTX)
    c['k_featC'], c['k_decC'], c['k_wkC'] = f, d, w.reshape(-1, 1)
    c['k_iota'] = np.arange(512, dtype=np.float32).reshape(1, 512)
    _CONSTS = c
    return c


def make_in_maps(inputs, cores):
    cs = consts()
    shared = {}
    for k, v in inputs.items():
        if k in ('x', 'c', 'ctx', 'c_ctx'):
            continue
        a = np.ascontiguousarray(np.asarray(v, dtype=np.float32))
        if k in ('router_b', 'final_g'):
            a = a.reshape(1, -1)
        shared[k] = a
    shared.update(cs)
    maps = []
    for b in cores:
        m = dict(shared)
        m['x'] = np.ascontiguousarray(inputs['x'][b], dtype=np.float32)
        m['ctx'] = np.ascontiguousarray(inputs['ctx'][b], dtype=np.float32)
        m['c'] = np.ascontiguousarray(inputs['c'][b], dtype=np.float32).reshape(1, D)
        m['c_ctx'] = np.ascontiguousarray(inputs['c_ctx'], dtype=np.float32).reshape(1, D)
        maps.append(m)
    return maps


def kernel(**inputs):
    P = build()
    maps = make_in_maps(inputs, list(range(8)))
    res = run_bass_kernel_spmd(P.nc, maps, core_ids=list(range(8)))
    return np.stack([r["out"] for r in res.results], axis=0).astype(np.float32)
```

```python
import math
import numpy as np
import concourse.bass as bass
import concourse.mybir as mybir
from concourse.bass_utils import run_bass_kernel_spmd

F32 = mybir.dt.float32
F32R = mybir.dt.float32r
FAST = True
AF = mybir.ActivationFunctionType
ALU = mybir.AluOpType
AX = mybir.AxisListType

D = 1024
NLAT = 4096
NCTX = 256
T = NLAT + NCTX
NT = T // 128
DEPTH = 4
HY_W = 256
ATT_W = 512
S5_W = 256
HY_END = 768
Q_END = HY_END + ATT_W
K_END = Q_END + 128
V_END = K_END + 128
IN_W = V_END + S5_W
EPS = 1e-6
MAGIC = 12582912.0
TWO_PI = 6.283185
NE = 16


class Tok:
    def __init__(self, name):
        self.name = name

    def __repr__(self):
        return self.name


class Prog:
    NDS = 24

    def __init__(self):
        nc = bass.Bass("TRN2", target_bir_lowering=False)
        self.nc = nc
        self.eng = {'pe': nc.tensor, 'dve': nc.vector, 'act': nc.scalar, 'pool': nc.gpsimd, 'sp': nc.sync}
        self.esem = {k: nc.alloc_semaphore("es_" + k) for k in self.eng}
        self.ecnt = {k: 0 for k in self.eng}
        self.dsem = [nc.alloc_semaphore("ds%d" % i) for i in range(self.NDS)]
        self.dval = [0] * self.NDS
        self.dnext = 0
        self.know = {k: {} for k in self.eng}
        self.last_w = {}
        self.readers = {}
        self.n_inst = 0
        self.psum = [nc.alloc_psum_tensor("psb%d" % i, [128, 512], F32) for i in range(8)]
        self.ps_tok = [Tok("ps%d" % i) for i in range(8)]
        self.ps_next = 0
        self.ps_held = set()
        self.dram = {}

    def _sem_of(self, key):
        return self.esem[key] if isinstance(key, str) else self.dsem[key]

    def _deps(self, reads, writes):
        deps = {}

        def add(kv):
            k, v = kv
            if deps.get(k, 0) < v:
                deps[k] = v
        for t in reads:
            if t in self.last_w:
                add(self.last_w[t])
        for t in writes:
            if t in self.last_w:
                add(self.last_w[t])
            for kv in self.readers.get(t, {}).items():
                add(kv)
        return deps

    def _wait(self, e, deps):
        kn = self.know[e]
        for k, v in deps.items():
            if k == e and e == 'pe':
                continue
            if kn.get(k, 0) >= v:
                continue
            self.eng[e].wait_ge(self._sem_of(k), v)
            kn[k] = v
            self.n_inst += 1

    def _commit(self, me, reads, writes):
        for t in writes:
            self.last_w[t] = me
            self.readers[t] = {}
        for t in reads:
            r = self.readers.setdefault(t, {})
            if r.get(me[0], 0) < me[1]:
                r[me[0]] = me[1]

    def op(self, e, fn, reads=(), writes=()):
        self._wait(e, self._deps(reads, writes))
        inst = fn(self.eng[e])
        inst.then_inc(self.esem[e], 1)
        self.ecnt[e] += 1
        self.n_inst += 1
        self._commit((e, self.ecnt[e]), reads, writes)

    def dma(self, out, in_, reads=(), writes=(), q='sp'):
        self._wait(q, self._deps(reads, writes))
        s = self.dnext
        self.dnext = (self.dnext + 1) % self.NDS
        if self.know[q].get(s, 0) < self.dval[s]:
            self.eng[q].wait_ge(self.dsem[s], self.dval[s])
            self.know[q][s] = self.dval[s]
        self.eng[q].dma_start(out=out, in_=in_, allow_slow_non_contiguous=True).then_inc(self.dsem[s], 16)
        self.dval[s] += 16
        self.n_inst += 1
        self._commit((s, self.dval[s]), reads, writes)

    def barrier(self):
        for e in self.eng:
            deps = {k: self.ecnt[k] for k in self.eng if self.ecnt[k] > 0}
            for s in range(self.NDS):
                if self.dval[s] > 0:
                    deps[s] = self.dval[s]
            self._wait(e, deps)
        self.last_w = {}
        self.readers = {}

    def finish(self):
        deps = {s: self.dval[s] for s in range(self.NDS) if self.dval[s] > 0}
        for k in self.eng:
            if self.ecnt[k] > 0:
                deps[k] = self.ecnt[k]
        self._wait('sp', deps)

    def ps(self):
        while True:
            i = self.ps_next
            self.ps_next = (i + 1) % 8
            if i not in self.ps_held:
                return self.psum[i], self.ps_tok[i]

    def ps_hold(self, n):
        got = []
        for i in range(8):
            if i not in self.ps_held and len(got) < n:
                self.ps_held.add(i)
                got.append(i)
        return [(self.psum[i], self.ps_tok[i]) for i in got], got

    def ps_release(self, got):
        for i in got:
            self.ps_held.discard(i)

    def mm(self, out, lhsT, rhs, start, stop, reads, writes, r=False):
        if r:
            lhsT = lhsT.bitcast(F32R)
            rhs = rhs.bitcast(F32R)
        self.op('pe', lambda e: e.matmul(out, lhsT, rhs, start=start, stop=stop), reads, writes)

    def tr(self, out, in_, ident, reads, writes):
        self.op('pe', lambda e: e.transpose(out=out, in_=in_, identity=ident), reads, writes)

    def act(self, out, in_, func, reads, writes, **kw):
        self.op('act', lambda e: e.activation(out=out, in_=in_, func=func, **kw), reads, writes)

    def tt(self, out, in0, in1, op, reads, writes, e='dve'):
        self.op(e, lambda g: g.tensor_tensor(out=out, in0=in0, in1=in1, op=op), reads, writes)

    def ts(self, out, in0, s1, s2, op0, op1, reads, writes, e='dve'):
        if op1 is None:
            self.op(e, lambda g: g.tensor_scalar(out=out, in0=in0, scalar1=s1, scalar2=None, op0=op0), reads, writes)
        else:
            self.op(e, lambda g: g.tensor_scalar(out=out, in0=in0, scalar1=s1, scalar2=s2, op0=op0, op1=op1), reads, writes)

    def stt(self, out, in0, scalar, in1, op0, op1, reads, writes):
        self.op('dve', lambda g: g.scalar_tensor_tensor(out=out, in0=in0, scalar=scalar, in1=in1, op0=op0, op1=op1), reads, writes)

    def copy(self, out, in_, reads, writes, e='dve'):
        if e == 'act':
            self.op('act', lambda g: g.copy(out=out, in_=in_), reads, writes)
        else:
            self.op(e, lambda g: g.tensor_copy(out=out, in_=in_), reads, writes)

    def load_r(self, dst, src, stage, t_dst, t_stage, e='dve'):
        if not FAST:
            self.dma(dst, src, (), [t_dst])
            return
        self.dma(stage, src, (), [t_stage])
        self.copy(dst.bitcast(F32R), stage, [t_stage], [t_dst], e=e)

    def rnd(self, ap, tok, e='dve'):
        if FAST:
            self.copy(ap.bitcast(F32R), ap, [tok], [tok], e=e)

    def memset(self, ap, val, writes, e='dve'):
        self.op(e, lambda g: g.memset(ap, val), (), writes)

    def recip(self, out, in_, reads, writes):
        self.op('dve', lambda g: g.reciprocal(out=out, in_=in_), reads, writes)

    def dram_t(self, name, shape, kind="Internal"):
        t = self.nc.dram_tensor(name, list(shape), F32, kind=kind).ap()
        self.dram[name] = t
        return t


def RR(ap):
    return ap.bitcast(F32R) if FAST else ap


class Ctx:
    def dump(self, name, tile_ap, shape, reads):
        if 'dump' not in self.dbg:
            return
        o = self.P.nc.dram_tensor("dmp_" + name, list(shape), F32, kind="ExternalOutput").ap()
        self.P.dma(o, tile_ap, reads, (), q='sp')


_UID = [0]


def SB(nc, name, shape, dt):
    _UID[0] += 1
    return nc.sbuf_tensor("%s_%d" % (name, _UID[0]), shape, dt)


def rope_tables():
    cosT = np.ones((128, T), np.float64)
    sinT = np.zeros((128, T), np.float64)
    n = np.arange(NLAT)
    row = n // 64
    col = n % 64
    for r in range(128):
        d = r % 64
        dd = d if d < 32 else d - 32
        pos = row if d < 32 else col
        j = dd % 16
        first = dd < 16
        inv = 10000.0 ** (-(j / 16.0))
        ang = (pos.astype(np.float32) * np.float32(inv)).astype(np.float64)
        cosT[r, NCTX:] = np.cos(ang)
        sinT[r, NCTX:] = (-np.sin(ang)) if first else np.sin(ang)
    return cosT.astype(np.float32), sinT.astype(np.float32)


def perm_cols(width):
    src = np.zeros(width, np.int64)
    for c in range(width):
        d = c % 64
        dd = d % 32
        src[c] = c + 16 if dd < 16 else c - 16
    return src


def dft_blocks(nhalf, nchunk):
    idx = np.arange(nchunk * 128)
    valid = (idx <= nhalf)
    prod = np.outer(idx, idx).astype(np.float64)
    ang = 2.0 * np.pi * (prod % (2 * nhalf)) / (2 * nhalf)
    m = np.outer(valid, valid)
    cc = (np.cos(ang) * m).astype(np.float32)
    ss = (np.sin(ang) * m).astype(np.float32)

    def blk(mat):
        return np.ascontiguousarray(mat.reshape(nchunk, 128, nchunk, 128).transpose(2, 1, 0, 3))
    return blk(cc), blk(ss)


def hyena_consts(L):
    pos = np.arange(L, dtype=np.float32)
    t = pos / np.float32(max(L - 1, 1))
    bands = np.linspace(1e-4, 15, 16, dtype=np.float32)
    ang = (np.float32(2.0 * math.pi / L) * pos[:, None] * bands[None, :]).astype(np.float32)
    feats = np.concatenate([t[:, None], np.cos(ang), -np.sin(ang)], axis=-1).astype(np.float32)
    dmin = math.log(1e-2) / 1.5
    dmax = math.log(1e-2) / 0.3
    decay = np.abs(np.linspace(dmin, dmax, HY_W, dtype=np.float32))
    dec = np.exp(-t[:, None] * decay[None, :]).astype(np.float32)
    nk = L // 128 + 1
    wk = np.zeros(nk * 128, np.float32)
    wk[:L + 1] = 2.0 / (2 * L)
    wk[0] = 1.0 / (2 * L)
    wk[L] = 1.0 / (2 * L)
    return np.ascontiguousarray(feats.T), dec, wk


def build(n_layers=DEPTH, dbg=()):
    P = Prog()
    nc = P.nc
    K = Ctx()
    K.P = P
    K.dbg = dbg
    K.dbg_out = {}

    def inp(name, shape):
        return nc.dram_tensor(name, list(shape), F32, kind="ExternalInput").ap()

    I = {}
    I['x'] = inp('x', [NLAT, D])
    I['ctx'] = inp('ctx', [NCTX, D])
    I['c'] = inp('c', [1, D])
    I['c_ctx'] = inp('c_ctx', [1, D])
    shapes = dict(
        norm1_g=[4, D], norm2_g=[4, D], ada_w=[4, D, 6 * D], ada_b=[4, 6 * D], w_in=[4, D, IN_W], w_out=[4, D, D],
        mix_norm_g=[4, D], hy_conv_w=[4, 3, 768], hy_conv_b=[4, 768], hy_f_w1=[4, 33, 64], hy_f_b1=[4, 64],
        hy_f_freq1=[4, 64], hy_f_w2=[4, 64, 64], hy_f_b2=[4, 64], hy_f_freq2=[4, 64], hy_f_w3=[4, 64, 1024],
        hy_bias=[4, 2, 256], attn_sink=[4, 8], s5_lam_re=[4, 2, 16, 64], s5_lam_im=[4, 2, 16, 64], s5_log_dt=[4, 2, 16],
        s5_b_re=[4, 2, 16, 64, 16], s5_b_im=[4, 2, 16, 64, 16], s5_c_re=[4, 2, 16, 16, 64], s5_c_im=[4, 2, 16, 16, 64],
        s5_d=[4, 256], s5_glu_w=[4, 256, 256], s5_glu_b=[4, 256], router_w=[D, 16], router_b=[1, 16],
        moe_w_gate=[4, 16, D, D], moe_w_up=[4, 16, D, D], moe_w_down=[4, 16, D, D], final_g=[1, D],
        k_ident=[128, 128], k_ropec=[128, T], k_ropes=[128, T], k_maskl=[128, 128], k_maskr=[128, 128],
        k_ccL=[33, 128, 33, 128], k_ssL=[33, 128, 33, 128], k_ccC=[3, 128, 3, 128], k_ssC=[3, 128, 3, 128],
        k_featL=[33, NLAT], k_decL=[NLAT, 256], k_wkL=[33 * 128, 1], k_featC=[33, NCTX], k_decC=[NCTX, 256], k_wkC=[3 * 128, 1],
        k_iota=[1, 512],
    )
    for k, s in shapes.items():
        I[k] = inp(k, s)
    K.I = I
    out = nc.dram_tensor("out", [NLAT, D], F32, kind="ExternalOutput").ap()
    K.out = out

    K.X = P.dram_t("sX", [T, D])
    K.ZT = P.dram_t("sZT", [T, 768])
    K.QF = P.dram_t("sQF", [512, T])
    K.KF = P.dram_t("sKF", [128, T])
    K.VT = P.dram_t("sVT", [T, 128])
    K.UF = P.dram_t("sUF", [256, T])
    K.MIX = P.dram_t("sMIX", [T, D])
    K.H2T = P.dram_t("sH2T", [D, T])
    K.GTT = P.dram_t("sGTT", [16, T])

    K.ident = nc.alloc_sbuf_tensor("ident", [128, 128], F32)
    K.ones = nc.alloc_sbuf_tensor("ones", [128, 128], F32)
    K.MOD = P.dram_t("sMOD", [12, D])
    K.ZC = P.dram_t("sZC", [T, 768])
    K.KRE = P.dram_t("sKRE", [33 * 128, 512])
    K.KIM = P.dram_t("sKIM", [33 * 128, 512])
    K.t_ident = Tok("ident")
    K.t_ones = Tok("ones")
    K.t_mod = Tok("modbc")
    P.dma(K.ident[:], I['k_ident'][:, :], (), [K.t_ident])
    P.memset(K.ones[:], 1.0, [K.t_ones])

    tX = [("X", i) for i in range(NT)]
    K.tX = tX
    P.dma(K.X[0:NCTX, :], I['ctx'][:, :], (), tX[0:2])
    for i in range(4):
        P.dma(K.X[NCTX + i * 1024: NCTX + (i + 1) * 1024, :], I['x'][i * 1024:(i + 1) * 1024, :], (), tX[2 + 8 * i: 2 + 8 * (i + 1)])
    P.barrier()

    for l in range(n_layers):
        stage_mod(K, l)
        P.barrier()
        stage_inproj(K, l)
        P.barrier()
        if 'inproj' in dbg and l == 0:
            break
        stage_hyena(K, l, NLAT, NCTX, 'L')
        P.barrier()
        stage_hyena(K, l, NCTX, 0, 'C')
        P.barrier()
        if 'hyena' in dbg and l == 0:
            break
        stage_attn(K, l)
        P.barrier()
        if 'attn' in dbg and l == 0:
            break
        stage_s5(K, l)
        P.barrier()
        if 's5' in dbg and l == 0:
            break
        stage_outproj(K, l)
        P.barrier()
        if 'outproj' in dbg and l == 0:
            break
        stage_moe(K, l)
        P.barrier()
    if not dbg:
        stage_final(K)
    for name in dbg:
        if name in P.dram and name not in ('inproj', 'hyena', 'attn', 's5', 'outproj'):
            src = P.dram[name]
            o = nc.dram_tensor("dbg_" + name, list(src.shape), F32, kind="ExternalOutput").ap()
            P.barrier()
            P.dma(o, src, (), ())
    P.finish()
    return P


def stage_mod(K, l):
    P, nc, I = K.P, K.P.nc, K.I
    with SB(nc, "m_cs", [128, 2, 8], F32) as cs, SB(nc, "m_w", [128, 6 * D], F32) as wk, \
            SB(nc, "m_ws", [128, 6 * D], F32) as ws, SB(nc, "m_b", [1, 6 * D], F32) as bt, \
            SB(nc, "m_raw", [128, 6 * D], F32) as raw, SB(nc, "m_g", [128, 2, D], F32) as gn, \
            SB(nc, "m_o", [128, 12, D], F32) as mo:
        t_cs, t_w, t_ws, t_b, t_raw, t_g = Tok("cs"), Tok("w"), Tok("ws"), Tok("b"), Tok("raw"), Tok("g")
        P.dma(cs[:, 0, :], I['c'][0, :].rearrange("(c p) -> p c", p=128), (), [t_cs])
        P.dma(cs[:, 1, :], I['c_ctx'][0, :].rearrange("(c p) -> p c", p=128), (), [t_cs])
        P.act(cs[:], cs[:], AF.Silu, [t_cs], [t_cs])
        P.dma(bt[:], I['ada_b'][l:l + 1, :], (), [t_b])
        P.dma(gn[:, 0, :], I['norm1_g'][l, :].partition_broadcast(128), (), [t_g])
        P.dma(gn[:, 1, :], I['norm2_g'][l, :].partition_broadcast(128), (), [t_g])
        for who in range(2):
            for j in range(12):
                ps, tp = P.ps()
                for k in range(8):
                    P.dma(wk[:, 0:512], I['ada_w'][l, k * 128:(k + 1) * 128, j * 512:(j + 1) * 512], (), [t_w])
                    P.ts(ws[:, 0:512], wk[:, 0:512], cs[:, who, k:k + 1], None, ALU.mult, None, [t_w, t_cs], [t_ws])
                    P.mm(ps[:, :], K.ones[:, :], ws[:, 0:512], k == 0, False, [t_ws, K.t_ones], [tp])
                P.mm(ps[:, :], K.ones[0:1, :], bt[0:1, j * 512:(j + 1) * 512], False, True, [t_b, K.t_ones], [tp])
                P.copy(raw[:, j * 512:(j + 1) * 512], ps[:, :], [tp], [t_raw])
            base = who * 6
            m = mo
            for half in range(2):
                sh = raw[:, (3 * half) * D:(3 * half + 1) * D]
                sc = raw[:, (3 * half + 1) * D:(3 * half + 2) * D]
                gg = raw[:, (3 * half + 2) * D:(3 * half + 3) * D]
                P.stt(m[:, base + 3 * half + 0, :], sc, 1.0, gn[:, half, :], ALU.add, ALU.mult, [t_raw, t_g], [K.t_mod])
                P.copy(m[:, base + 3 * half + 1, :], sh, [t_raw], [K.t_mod])
                P.copy(m[:, base + 3 * half + 2, :], gg, [t_raw], [K.t_mod])
        P.dma(K.MOD.rearrange("(o j) d -> o j d", o=1), mo[0:1, :, :], [K.t_mod], [("MOD", 0)], q='pool')
        P.barrier()


def rms_rstd(K, xt, t_x, width, junk, ss, t_s):
    P = K.P
    P.act(junk, xt, AF.Square, [t_x], [t_s], accum_out=ss[:, 0:1])
    P.ts(ss[:, 0:1], ss[:, 0:1], 1.0 / width, EPS, ALU.mult, ALU.add, [t_s], [t_s])
    P.act(ss[:, 0:1], ss[:, 0:1], AF.Sqrt, [t_s], [t_s])
    P.recip(ss[:, 0:1], ss[:, 0:1], [t_s], [t_s])


def stage_inproj(K, l):
    P, nc, I = K.P, K.P.nc, K.I
    src = perm_cols(640)
    with SB(nc, "i_w", [128, 8, IN_W], F32) as W, SB(nc, "i_wp", [128, 8, 640], F32) as WP, \
            SB(nc, "i_x", [128, 2, D], F32) as xt, SB(nc, "i_h", [128, 2, D], F32) as ht, \
            SB(nc, "i_hT", [128, 8, 512], F32) as hT, SB(nc, "i_ss", [128, 2], F32) as ss, \
            SB(nc, "i_junk", [128, D], F32) as junk, SB(nc, "i_o", [128, 2, 768], F32) as ot, \
            SB(nc, "i_rc", [128, 512], F32) as rc, SB(nc, "i_rs", [128, 512], F32) as rs, \
            SB(nc, "i_t1", [128, 512], F32) as t1, SB(nc, "i_t2", [128, 512], F32) as t2, \
            SB(nc, "i_mod", [128, 4, D], F32) as modbc, SB(nc, "i_wst", [128, 2, IN_W], F32) as wst:
        t_W, t_WP = Tok("W"), Tok("WP")
        t_wst = [Tok("wst0"), Tok("wst1")]
        for jj, r in enumerate((0, 1, 6, 7)):
            P.dma(modbc[:, jj, :], K.MOD[r, :].partition_broadcast(128), (), [K.t_mod])
        t_x = [Tok("x0"), Tok("x1")]
        t_h = [Tok("h0"), Tok("h1")]
        t_s = [Tok("s0"), Tok("s1")]
        t_hT, t_o, t_rc, t_rs, t_t1, t_t2 = Tok("hT"), [Tok("o0"), Tok("o1")], Tok("rc"), Tok("rs"), Tok("t1"), Tok("t2")
        for k in range(8):
            P.load_r(W[:, k, :], I['w_in'][l, k * 128:(k + 1) * 128, :], wst[:, k % 2, :], t_W, t_wst[k % 2], e='act' if k % 2 else 'dve')
        wv = I['w_in'][l, :, HY_END:HY_END + 640].rearrange("(k p) (b two s) -> k p b two s", p=128, two=2, s=16)
        for k in range(8):
            dst = wst[:, k % 2, 0:640].rearrange("p (b two s) -> p b two s", two=2, s=16)
            P.dma(dst[:, :, 0, :], wv[k, :, :, 1, :], (), [t_wst[k % 2]])
            P.dma(dst[:, :, 1, :], wv[k, :, :, 0, :], (), [t_wst[k % 2]])
            P.copy(RR(WP[:, k, :]), wst[:, k % 2, 0:640], [t_wst[k % 2]], [t_WP], e='act' if k % 2 else 'dve')
        ngroups = (T + 511) // 512
        for g in range(ngroups):
            t0 = g * 512
            ntok = min(512, T - t0)
            ntile = ntok // 128
            for i in range(ntile):
                ti = g * 4 + i
                b = i % 2
                isctx = ti < 2
                mb = 2 if isctx else 0
                P.dma(xt[:, b, :], K.X[ti * 128:(ti + 1) * 128, :], [K.tX[ti]], [t_x[b]])
                rms_rstd(K, xt[:, b, :], t_x[b], D, junk[:], ss[:, b:b + 1], t_s[b])
                P.stt(ht[:, b, :], xt[:, b, :], ss[:, b:b + 1], modbc[:, mb + 0, :], ALU.mult, ALU.mult,
                      [t_x[b], t_s[b], K.t_mod], [t_h[b]])
                P.tt(ht[:, b, :], ht[:, b, :], modbc[:, mb + 1, :], ALU.add, [t_h[b], K.t_mod], [t_h[b]])
                for half in range(2):
                    ps, tp = P.ps()
                    for j in range(4):
                        kk = half * 4 + j
                        P.tr(ps[:, j * 128:(j + 1) * 128], ht[:, b, kk * 128:(kk + 1) * 128], K.ident[:], [t_h[b], K.t_ident], [tp])
                    P.copy(RR(hT[:, half * 4:(half + 1) * 4, i * 128:(i + 1) * 128]),
                           ps[:, :].rearrange("p (j t) -> p j t", j=4), [tp], [t_hT], e='act' if half else 'dve')
            for i in range(ntile):
                ti = g * 4 + i
                b = i % 2
                for (c0, cw, dst, dcol) in ((0, 512, K.ZT, 0), (512, 256, K.ZT, 512), (K_END, 128, K.VT, 0)):
                    ps, tp = P.ps()
                    for k in range(8):
                        P.mm(ps[:, 0:cw], hT[:, k, i * 128:(i + 1) * 128], W[:, k, c0:c0 + cw], k == 0, k == 7, [t_hT, t_W], [tp], r=FAST)
                    P.copy(ot[:, b, 0:cw], ps[:, 0:cw], [tp], [t_o[b]], e='act')
                    P.dma(dst[ti * 128:(ti + 1) * 128, dcol:dcol + cw], ot[:, b, 0:cw], [t_o[b]], [(dst.tensor.name, ti)], q='pool')
            P.dma(rc[:, 0:ntok], I['k_ropec'][:, t0:t0 + ntok], (), [t_rc])
            P.dma(rs[:, 0:ntok], I['k_ropes'][:, t0:t0 + ntok], (), [t_rs])
            for cidx in range(5):
                c0 = HY_END + cidx * 128
                ps, tp = P.ps()
                ps2, tp2 = P.ps()
                for k in range(8):
                    P.mm(ps[:, 0:ntok], W[:, k, c0:c0 + 128], hT[:, k, 0:ntok], k == 0, k == 7, [t_hT, t_W], [tp], r=FAST)
                for k in range(8):
                    P.mm(ps2[:, 0:ntok], WP[:, k, cidx * 128:(cidx + 1) * 128], hT[:, k, 0:ntok], k == 0, k == 7, [t_hT, t_WP], [tp2], r=FAST)
                P.tt(t1[:, 0:ntok], ps[:, 0:ntok], rc[:, 0:ntok], ALU.mult, [tp, t_rc], [t_t1])
                P.tt(t2[:, 0:ntok], ps2[:, 0:ntok], rs[:, 0:ntok], ALU.mult, [tp2, t_rs], [t_t2])
                P.tt(t1[:, 0:ntok], t1[:, 0:ntok], t2[:, 0:ntok], ALU.add, [t_t1, t_t2], [t_t1])
                if cidx < 4:
                    P.dma(K.QF[cidx * 128:(cidx + 1) * 128, t0:t0 + ntok], t1[:, 0:ntok], [t_t1], [("QF", g)], q='pool')
                else:
                    P.dma(K.KF[:, t0:t0 + ntok], t1[:, 0:ntok], [t_t1], [("KF", g)], q='pool')
            for cidx in range(2):
                c0 = V_END + cidx * 128
                ps, tp = P.ps()
                for k in range(8):
                    P.mm(ps[:, 0:ntok], W[:, k, c0:c0 + 128], hT[:, k, 0:ntok], k == 0, k == 7, [t_hT, t_W], [tp], r=FAST)
                P.copy(t2[:, 0:ntok], ps[:, 0:ntok], [tp], [t_t2], e='act')
                P.dma(K.UF[cidx * 128:(cidx + 1) * 128, t0:t0 + ntok], t2[:, 0:ntok], [t_t2], [("UF", g)], q='pool')


def sin_chain(K, ps_ap, bcol, fcol, v, r, hid, n, t_ps, t_consts, t_v, t_r, t_hid):
    P = K.P
    P.act(v[:, 0:n], ps_ap, AF.Identity, [t_ps] + t_consts, [t_v], bias=bcol, scale=fcol)
    P.ts(r[:, 0:n], v[:, 0:n], MAGIC, MAGIC, ALU.add, ALU.subtract, [t_v], [t_r])
    P.tt(v[:, 0:n], v[:, 0:n], r[:, 0:n], ALU.subtract, [t_v, t_r], [t_v])
    P.act(hid[:, 0:n], v[:, 0:n], AF.Sin, [t_v], [t_hid], scale=TWO_PI)


def stage_hyena(K, l, L, row0, tag):
    P, nc, I = K.P, K.P.nc, K.I
    NCH = L // 128
    NK = NCH + 1
    CC, SS = I['k_cc' + tag], I['k_ss' + tag]
    featT, dec, wkc = I['k_feat' + tag], I['k_dec' + tag], I['k_wk' + tag]
    with SB(nc, "ha_w", [128, 4, 768], F32) as wb, SB(nc, "ha_z", [128, 3, 768], F32) as z, \
            SB(nc, "ha_t", [128, 2, 768], F32) as tt_:
        t_wb, t_z, t_t = Tok("wb"), [Tok("zm"), Tok("z0"), Tok("zp")], [Tok("ta"), Tok("tb")]
        for j in range(3):
            P.dma(wb[:, j, :], I['hy_conv_w'][l, j, :].partition_broadcast(128), (), [t_wb])
        P.dma(wb[:, 3, :], I['hy_conv_b'][l, :].partition_broadcast(128), (), [t_wb])
        for i in range(NCH):
            r0 = row0 + i * 128
            if i == 0:
                P.memset(z[0:1, 0, :], 0.0, [t_z[0]])
                P.dma(z[1:128, 0, :], K.ZT[r0:r0 + 127, 0:768], (), [t_z[0]])
            else:
                P.dma(z[:, 0, :], K.ZT[r0 - 1:r0 + 127, 0:768], (), [t_z[0]])
            P.dma(z[:, 1, :], K.ZT[r0:r0 + 128, 0:768], (), [t_z[1]])
            if i == NCH - 1:
                P.memset(z[:, 2, :], 0.0, [t_z[2]])
                P.dma(z[0:127, 2, :], K.ZT[r0 + 1:r0 + 128, 0:768], (), [t_z[2]])
            else:
                P.dma(z[:, 2, :], K.ZT[r0 + 1:r0 + 129, 0:768], (), [t_z[2]])
            P.tt(tt_[:, 0, :], z[:, 0, :], wb[:, 0, :], ALU.mult, [t_z[0], t_wb], [t_t[0]])
            P.tt(tt_[:, 1, :], z[:, 1, :], wb[:, 1, :], ALU.mult, [t_z[1], t_wb], [t_t[1]], e='pool')
            P.tt(tt_[:, 0, :], tt_[:, 0, :], tt_[:, 1, :], ALU.add, [t_t[0], t_t[1]], [t_t[0]])
            P.tt(tt_[:, 1, :], z[:, 2, :], wb[:, 2, :], ALU.mult, [t_z[2], t_wb], [t_t[1]], e='pool')
            P.tt(tt_[:, 0, :], tt_[:, 0, :], tt_[:, 1, :], ALU.add, [t_t[0], t_t[1]], [t_t[0]])
            P.tt(tt_[:, 0, :], tt_[:, 0, :], wb[:, 3, :], ALU.add, [t_t[0], t_wb], [t_t[0]])
            P.dma(K.ZC[r0:r0 + 128, :], tt_[:, 0, :], [t_t[0]], [("ZC", i)], q='pool')
    P.barrier()
    with SB(nc, "hb_taps", [128, NCH, 1024], F32) as taps, SB(nc, "hb_cc", [128, NK, 128], F32) as cct, \
            SB(nc, "hb_ss", [128, NK, 128], F32) as sst, SB(nc, "hb_f", [33, 512], F32) as ft, \
            SB(nc, "hb_w1", [33, 64], F32) as w1, SB(nc, "hb_w2", [64, 64], F32) as w2, \
            SB(nc, "hb_w3", [64, 1024], F32) as w3, SB(nc, "hb_c", [64, 8], F32) as cst, \
            SB(nc, "hb_v", [64, 512], F32) as v, SB(nc, "hb_r", [64, 512], F32) as r, \
            SB(nc, "hb_h1", [64, 512], F32) as h1, SB(nc, "hb_h2", [64, 512], F32) as h2, \
            SB(nc, "hb_dec", [128, 256], F32) as dct, SB(nc, "hb_abs", [128, 1024], F32) as ab, \
            SB(nc, "hb_rn", [128, 512], F32) as rn, SB(nc, "hb_tmp", [128, 512], F32) as tmp, \
            SB(nc, "hb_wk", [128, NK], F32) as wkt, SB(nc, "hb_o", [128, 2, 512], F32) as ko:
        t_taps = [Tok("taps%d" % c) for c in range(NCH)]
        t_cc, t_ss, t_f, t_w, t_c = Tok("cc"), Tok("ss"), Tok("f"), Tok("w"), Tok("c")
        t_v, t_r, t_h1, t_h2, t_dec, t_ab, t_rn, t_tmp, t_wk = (Tok(n) for n in ("v", "r", "h1", "h2", "dec", "ab", "rn", "tmp", "wk"))
        t_ko = [Tok("ko0"), Tok("ko1")]
        P.dma(w1[:], I['hy_f_w1'][l, :, :], (), [t_w])
        P.dma(w2[:], I['hy_f_w2'][l, :, :], (), [t_w])
        P.dma(w3[:], I['hy_f_w3'][l, :, :], (), [t_w])
        for j, nm in enumerate(('hy_f_b1', 'hy_f_freq1', 'hy_f_b2', 'hy_f_freq2')):
            P.dma(cst[:, j:j + 1], I[nm][l, :].rearrange("(p o) -> p o", o=1), (), [t_c])
        P.ts(cst[:, 4:5], cst[:, 1:2], 1.0 / (2.0 * math.pi), None, ALU.mult, None, [t_c], [t_c])
        P.ts(cst[:, 5:6], cst[:, 3:4], 1.0 / (2.0 * math.pi), None, ALU.mult, None, [t_c], [t_c])
        P.tt(cst[:, 6:7], cst[:, 0:1], cst[:, 4:5], ALU.mult, [t_c], [t_c])
        P.tt(cst[:, 7:8], cst[:, 2:3], cst[:, 5:6], ALU.mult, [t_c], [t_c])
        P.dma(wkt[:], wkc[:, 0].rearrange("(c p) -> p c", p=128), (), [t_wk])
        ng = (L + 511) // 512
        for g in range(ng):
            n0 = g * 512
            n = min(512, L - n0)
            P.dma(ft[:, 0:n], featT[:, n0:n0 + n], (), [t_f])
            ps, tp = P.ps()
            P.mm(ps[0:64, 0:n], w1[0:33, :], ft[0:33, 0:n], True, True, [t_w, t_f], [tp])
            sin_chain(K, ps[0:64, 0:n], cst[:, 6:7], cst[:, 4:5], v, r, h1, n, tp, [t_c], t_v, t_r, t_h1)
            ps, tp = P.ps()
            P.mm(ps[0:64, 0:n], w2[:, :], h1[:, 0:n], True, True, [t_w, t_h1], [tp])
            sin_chain(K, ps[0:64, 0:n], cst[:, 7:8], cst[:, 5:6], v, r, h2, n, tp, [t_c], t_v, t_r, t_h2)
            for sub in range(n // 128):
                c = (n0 // 128) + sub
                P.dma(dct[:], dec[c * 128:(c + 1) * 128, :], (), [t_dec])
                for half in range(2):
                    ps, tp = P.ps()
                    P.mm(ps[:, :], h2[:, sub * 128:(sub + 1) * 128], w3[:, half * 512:(half + 1) * 512], True, True, [t_h2, t_w], [tp])
                    P.tt(taps[:, c, half * 512:(half + 1) * 512].rearrange("p (d c) -> p d c", d=2),
                         ps[:, :].rearrange("p (d c) -> p d c", d=2),
                         dct[:, :].unsqueeze(1).broadcast_to([128, 2, 256]), ALU.mult, [tp, t_dec], [t_taps[c]])
        tv0 = taps[0:1, 0, :].rearrange("p (o d c) -> p o d c", o=2, d=2)
        P.memset(tv0[:, :, 1, :], 0.0, [t_taps[0]])
        held, hid_ = P.ps_hold(2)
        for c in range(NCH):
            P.act(ab[:], taps[:, c, :], AF.Abs, [t_taps[c]], [t_ab])
            for half in range(2):
                P.mm(held[half][0][:, :], K.ones[:, :], ab[:, half * 512:(half + 1) * 512], c == 0, c == NCH - 1, [t_ab, K.t_ones], [held[half][1]])
        for o in range(2):
            P.copy(ab[:, o * 512:o * 512 + 256], held[o][0][:, 0:256], [held[o][1]], [t_ab])
            P.tt(rn[:, o * 256:(o + 1) * 256], ab[:, o * 512:o * 512 + 256], held[o][0][:, 256:512], ALU.add, [held[o][1], t_ab], [t_rn])
        P.ps_release(hid_)
        P.recip(rn[:], rn[:], [t_rn], [t_rn])
        for c in range(NCH):
            tv = taps[:, c, :].rearrange("p (o d c) -> p o d c", o=2, d=2)
            tm = tmp[:, :].rearrange("p (o c) -> p o c", o=2)
            P.tt(tm, tv[:, :, 0, :], tv[:, :, 1, :], ALU.add, [t_taps[c]], [t_tmp])
            P.tt(tv[:, :, 1, :], tv[:, :, 1, :], tv[:, :, 0, :], ALU.subtract, [t_taps[c]], [t_taps[c]], e='pool')
            P.copy(tv[:, :, 0, :], tm, [t_tmp], [t_taps[c]])
        rn3 = rn[:, :].rearrange("p (o c) -> p o c", o=2)
        for kc in range(NK):
            P.dma(cct[:], CC[kc, :, :, :], (), [t_cc])
            P.dma(sst[:], SS[kc, :, :, :], (), [t_ss])
            for part, tab, ttab, d in ((0, cct, t_cc, 0), (1, sst, t_ss, 1)):
                ps, tp = P.ps()
                for c in range(NCH):
                    tv = taps[:, c, :].rearrange("p (o d c) -> p o d c", o=2, d=2)
                    P.mm(ps[:, :].rearrange("p (o c) -> p o c", o=2), tab[:, c, :], tv[:, :, d, :], c == 0, c == NCH - 1, [ttab, t_taps[c]], [tp])
                P.stt(ko[:, part, :].rearrange("p (o c) -> p o c", o=2), ps[:, :].rearrange("p (o c) -> p o c", o=2),
                      wkt[:, kc:kc + 1], rn3, ALU.mult, ALU.mult, [tp, t_wk, t_rn], [t_ko[part]])
                dst = K.KRE if part == 0 else K.KIM
                P.dma(dst[kc * 128:(kc + 1) * 128, :], ko[:, part, :], [t_ko[part]], [(dst.tensor.name, kc)], q='pool')
    P.barrier()
    with SB(nc, "hc_a", [128, NCH, 256], F32) as a, SB(nc, "hc_p1", [128, NK, 256], F32) as p1, \
            SB(nc, "hc_p2", [128, NK, 256], F32) as p2, SB(nc, "hc_cc", [128, NK, 128], F32) as cct, \
            SB(nc, "hc_ss", [128, NK, 128], F32) as sst, SB(nc, "hc_k", [128, 2, 256], F32) as kk, \
            SB(nc, "hc_t", [128, 2, 256], F32) as tq, SB(nc, "hc_b", [128, 2, 256], F32) as bb, \
            SB(nc, "hc_x", [128, 256], F32) as xg, SB(nc, "hc_y", [128, 256], F32) as yy, \
            SB(nc, "hc_stg", [128, 2, NK, 128], F32) as stg:
        t_stg = [Tok("stg0"), Tok("stg1")]
        t_a = [Tok("a%d" % c) for c in range(NCH)]
        t_p1, t_p2, t_cc, t_ss, t_k, t_b, t_x, t_y = (Tok(n) for n in ("p1", "p2", "cc", "ss", "k", "b", "x", "y"))
        t_q = [Tok("q0"), Tok("q1")]
        for o in range(2):
            P.dma(bb[:, o, :], I['hy_bias'][l, o, :].partition_broadcast(128), (), [t_b])
        for c in range(NCH):
            P.load_r(a[:, c, :], K.ZC[row0 + c * 128: row0 + (c + 1) * 128, 0:256], xg[:], t_a[c], t_x, e='act' if c % 2 else 'dve')
        for o in range(2):
            for kc in range(NK):
                P.load_r(cct[:], CC[kc, :, :, :], stg[:, 0, :, :], t_cc, t_stg[0], e='act')
                P.load_r(sst[:], SS[kc, :, :, :], stg[:, 1, :, :], t_ss, t_stg[1], e='dve')
                P.dma(kk[:, 0, :], K.KRE[kc * 128:(kc + 1) * 128, o * 256:(o + 1) * 256], (), [t_k])
                P.dma(kk[:, 1, :], K.KIM[kc * 128:(kc + 1) * 128, o * 256:(o + 1) * 256], (), [t_k])
                psr, tpr = P.ps()
                psi, tpi = P.ps()
                for c in range(NCH):
                    P.mm(psr[:, 0:256], cct[:, c, :], a[:, c, :], c == 0, c == NCH - 1, [t_cc, t_a[c]], [tpr], r=FAST)
                for c in range(NCH):
                    P.mm(psi[:, 0:256], sst[:, c, :], a[:, c, :], c == 0, c == NCH - 1, [t_ss, t_a[c]], [tpi], r=FAST)
                P.tt(tq[:, 0, :], psr[:, 0:256], kk[:, 0, :], ALU.mult, [tpr, t_k], [t_q[0]])
                P.tt(tq[:, 1, :], psi[:, 0:256], kk[:, 1, :], ALU.mult, [tpi, t_k], [t_q[1]])
                P.tt(RR(p1[:, kc, :]), tq[:, 0, :], tq[:, 1, :], ALU.add, [t_q[0], t_q[1]], [t_p1])
                P.tt(tq[:, 0, :], psi[:, 0:256], kk[:, 0, :], ALU.mult, [tpi, t_k], [t_q[0]])
                P.tt(tq[:, 1, :], psr[:, 0:256], kk[:, 1, :], ALU.mult, [tpr, t_k], [t_q[1]])
                P.tt(RR(p2[:, kc, :]), tq[:, 0, :], tq[:, 1, :], ALU.subtract, [t_q[0], t_q[1]], [t_p2])
            for tc in range(NCH):
                r0 = row0 + tc * 128
                P.load_r(cct[:], CC[tc, :, :, :], stg[:, 0, :, :], t_cc, t_stg[0], e='act')
                P.load_r(sst[:], SS[tc, :, :, :], stg[:, 1, :, :], t_ss, t_stg[1], e='dve')
                P.dma(xg[:], K.ZC[r0:r0 + 128, 256 * (o + 1):256 * (o + 2)], (), [t_x])
                ps, tp = P.ps()
                for kc in range(NK):
                    P.mm(ps[:, 0:256], cct[:, kc, :], p1[:, kc, :], kc == 0, False, [t_cc, t_p1], [tp], r=FAST)
                for kc in range(NK):
                    P.mm(ps[:, 0:256], sst[:, kc, :], p2[:, kc, :], False, kc == NK - 1, [t_ss, t_p2], [tp], r=FAST)
                P.tt(yy[:], a[:, tc, :], bb[:, o, :], ALU.mult, [t_a[tc], t_b], [t_y])
                P.tt(yy[:], yy[:], ps[:, 0:256], ALU.add, [t_y, tp], [t_y])
                if o == 0:
                    P.tt(RR(a[:, tc, :]), yy[:], xg[:], ALU.mult, [t_y, t_x], [t_a[tc]])
                else:
                    P.tt(yy[:], yy[:], xg[:], ALU.mult, [t_y, t_x], [t_y])
                    P.dma(K.MIX[r0:r0 + 128, 0:256], yy[:], [t_y], [("MIX", r0)], q='pool')


def stage_attn(K, l):
    P, nc, I = K.P, K.P.nc, K.I
    with SB(nc, "at_k", [64, 2, T], F32) as kf, SB(nc, "at_v", [128, NT, 2, 65], F32) as v1, \
            SB(nc, "at_es", [128, 8], F32) as es, SB(nc, "at_ml", [128, 128], F32) as ml, \
            SB(nc, "at_mr", [128, 128], F32) as mr, SB(nc, "at_q", [64, 2, 4, 128], F32) as q4, \
            SB(nc, "at_pt", [128, 5, 512], F32) as pt, SB(nc, "at_o", [128, 2, 512], F32) as ot, \
            SB(nc, "at_d", [128, 2, 4], F32) as den:
        t_k, t_v, t_es, t_m = Tok("k"), Tok("v"), Tok("es"), Tok("m")
        t_q = [Tok("q0"), Tok("q1")]
        t_pt = [Tok("pt%d" % i) for i in range(5)]
        t_o = [Tok("o0"), Tok("o1")]
        t_d = Tok("d")
        for h in range(2):
            P.dma(kf[:, h, :], K.KF[h * 64:(h + 1) * 64, :], (), [t_k])
        P.memset(v1[:, :, :, 64:65], 1.0, [t_v])
        vtv = K.VT.rearrange("(t p) (h d) -> p t h d", p=128, h=2)
        for h in range(2):
            P.dma(v1[:, :, h, 0:64], vtv[:, :, h, :], (), [t_v])
        P.dma(es[:], I['attn_sink'][l, :].partition_broadcast(128), (), [t_es])
        P.act(es[:], es[:], AF.Exp, [t_es], [t_es])
        P.dma(ml[:], I['k_maskl'][:, :], (), [t_m])
        P.dma(mr[:], I['k_maskr'][:, :], (), [t_m])
        for qb in range(NT):
            kts = [(0, None), (1, None)]
            if qb >= 2:
                if qb - 1 >= 2:
                    kts.append((qb - 1, ml))
                kts.append((qb, None))
                if qb + 1 < NT:
                    kts.append((qb + 1, mr))
            ob = qb % 2
            for kvh in range(2):
                qi = kvh
                P.dma(q4[:, qi, :, :], K.QF[kvh * 256:(kvh + 1) * 256, qb * 128:(qb + 1) * 128].rearrange("(h d) t -> d h t", d=64),
                      (), [t_q[qi]])
                for idx, (kt, mask) in enumerate(kts):
                    ps, tp = P.ps()
                    P.mm(ps[:, :], kf[:, kvh, kt * 128:(kt + 1) * 128], q4[:, qi, :, :].rearrange("d h t -> d (h t)"), True, True,
                         [t_k, t_q[qi]], [tp])
                    P.act(pt[:, idx, :], ps[:, :], AF.Exp, [tp], [t_pt[idx]], scale=0.125)
                    if mask is not None:
                        pv = pt[:, idx, :].rearrange("p (h t) -> p h t", h=4)
                        P.tt(pv, pv, mask[:, :].unsqueeze(1).broadcast_to([128, 4, 128]), ALU.mult, [t_pt[idx], t_m], [t_pt[idx]])
                pso, tpo = P.ps()
                for hh in range(4):
                    for idx, (kt, mask) in enumerate(kts):
                        P.mm(pso[:, hh * 65:(hh + 1) * 65], pt[:, idx, hh * 128:(hh + 1) * 128], v1[:, kt, kvh, :],
                             idx == 0, idx == len(kts) - 1, [t_pt[idx], t_v], [tpo])
                pv = pso[:, 0:260].rearrange("p (h c) -> p h c", h=4)
                P.tt(den[:, kvh, :], pv[:, :, 64], es[:, kvh * 4:(kvh + 1) * 4], ALU.add, [tpo, t_es], [t_d])
                P.recip(den[:, kvh, :], den[:, kvh, :], [t_d], [t_d])
                P.tt(ot[:, ob, kvh * 256:(kvh + 1) * 256].rearrange("p (h d) -> p h d", h=4), pv[:, :, 0:64],
                     den[:, kvh, :].unsqueeze(2).broadcast_to([128, 4, 64]), ALU.mult, [tpo, t_d], [t_o[ob]])
            P.dma(K.MIX[qb * 128:(qb + 1) * 128, 256:768], ot[:, ob, :], [t_o[ob]], [("MIXa", qb)], q='pool')


def round_frac(K, v, r, t_v, t_r, e='dve'):
    P = K.P
    P.ts(r, v, MAGIC, MAGIC, ALU.add, ALU.subtract, [t_v], [t_r], e=e)
    P.tt(v, v, r, ALU.subtract, [t_v, t_r], [t_v], e=e)


def stage_s5(K, l):
    P, nc, I = K.P, K.P.nc, K.I
    K.GS = K.P.dram.get("sGS")
    if K.GS is None:
        K.GS = P.dram_t("sGS", [256, T])
    with SB(nc, "s_bt", [32, 16, 2, 128], F32) as BT, SB(nc, "s_cb", [128, 16, 2, 32], F32) as CB, \
            SB(nc, "s_par", [128, 12, 16], F32) as par, SB(nc, "s_dsk", [32, 8], F32) as dsk, \
            SB(nc, "s_iota", [128, 512], F32) as iot:
        t_BT, t_CB, t_par, t_dsk, t_iota = Tok("BT"), Tok("CB"), Tok("par"), Tok("dsk"), Tok("iota")
        MAG, PHI = 4, 6
        with SB(nc, "s_b", [128, 2, 16, 16], F32) as bri, SB(nc, "s_bb", [128, 2, 16, 16], F32) as bb, \
                SB(nc, "s_bd", [128, 16, 2, 32], F32) as BD, SB(nc, "s_cnd", [32, 16, 2, 128], F32) as CND, \
                SB(nc, "s_t1", [128, 16, 16], F32) as t1, SB(nc, "s_t2", [128, 16, 16], F32) as t2:
            t_b, t_bb, t_BD, t_CND, t_t1, t_t2 = Tok("b"), Tok("bb"), Tok("BD"), Tok("CND"), Tok("t1"), Tok("t2")
            for d in range(2):
                P.dma(par[:, 0, d * 8:(d + 1) * 8], I['s5_lam_re'][l, d].rearrange("g p -> (g p)").rearrange("(gh q) -> q gh", q=128), (), [t_par])
                P.dma(par[:, 1, d * 8:(d + 1) * 8], I['s5_lam_im'][l, d].rearrange("g p -> (g p)").rearrange("(gh q) -> q gh", q=128), (), [t_par])
                ldv = I['s5_log_dt'][l, d].rearrange("(gh gl) -> gl gh", gl=2)
                for gl in range(2):
                    P.dma(par[gl * 64:(gl + 1) * 64, 2, d * 8:(d + 1) * 8], ldv[gl].partition_broadcast(64), (), [t_par])
                for ri, nm in enumerate(('s5_b_re', 's5_b_im')):
                    P.dma(bri[:, ri, d * 8:(d + 1) * 8, :],
                          I[nm][l, d].rearrange("g p c -> (g p c)").rearrange("(gh q c) -> q gh c", q=128, c=16), (), [t_b])
            P.dma(dsk[:], I['s5_d'][l, :].rearrange("(j r) -> r j", r=32), (), [t_dsk])
            P.dma(iot[:], I['k_iota'][0, :].partition_broadcast(128), (), [t_iota])
            pp = lambda i: par[:, i, :]
            tp_ = [t_par]
            P.act(pp(2), pp(2), AF.Exp, tp_, tp_)
            P.tt(pp(3), pp(0), pp(2), ALU.mult, tp_, tp_)
            P.act(pp(MAG), pp(3), AF.Exp, tp_, tp_)
            P.tt(pp(3), pp(1), pp(2), ALU.mult, tp_, tp_)
            P.ts(pp(PHI), pp(3), 1.0 / (2.0 * math.pi), None, ALU.mult, None, tp_, tp_)
            P.copy(pp(5), pp(PHI), tp_, tp_)
            round_frac(K, pp(5), pp(11), t_par, t_par)
            P.act(pp(8), pp(5), AF.Sin, tp_, tp_, scale=TWO_PI)
            P.ts(pp(5), pp(PHI), 0.25, None, ALU.add, None, tp_, tp_)
            round_frac(K, pp(5), pp(11), t_par, t_par)
            P.act(pp(7), pp(5), AF.Sin, tp_, tp_, scale=TWO_PI)
            P.tt(pp(7), pp(7), pp(MAG), ALU.mult, tp_, tp_)
            P.tt(pp(8), pp(8), pp(MAG), ALU.mult, tp_, tp_)
            P.ts(pp(7), pp(7), -1.0, None, ALU.add, None, tp_, tp_)
            P.tt(pp(3), pp(0), pp(0), ALU.mult, tp_, tp_)
            P.tt(pp(5), pp(1), pp(1), ALU.mult, tp_, tp_)
            P.tt(pp(3), pp(3), pp(5), ALU.add, tp_, tp_)
            P.recip(pp(3), pp(3), tp_, tp_)
            P.tt(pp(9), pp(7), pp(0), ALU.mult, tp_, tp_)
            P.tt(pp(5), pp(8), pp(1), ALU.mult, tp_, tp_)
            P.tt(pp(9), pp(9), pp(5), ALU.add, tp_, tp_)
            P.tt(pp(9), pp(9), pp(3), ALU.mult, tp_, tp_)
            P.tt(pp(10), pp(8), pp(0), ALU.mult, tp_, tp_)
            P.tt(pp(5), pp(7), pp(1), ALU.mult, tp_, tp_)
            P.tt(pp(10), pp(10), pp(5), ALU.subtract, tp_, tp_)
            P.tt(pp(10), pp(10), pp(3), ALU.mult, tp_, tp_)
            cre = par[:, 9, :].unsqueeze(2).broadcast_to([128, 16, 16])
            cim = par[:, 10, :].unsqueeze(2).broadcast_to([128, 16, 16])
            P.tt(t1[:], bri[:, 0, :, :], cre, ALU.mult, [t_b, t_par], [t_t1])
            P.tt(t2[:], bri[:, 1, :, :], cim, ALU.mult, [t_b, t_par], [t_t2])
            P.tt(bb[:, 0, :, :], t1[:], t2[:], ALU.subtract, [t_t1, t_t2], [t_bb])
            P.tt(t1[:], bri[:, 1, :, :], cre, ALU.mult, [t_b, t_par], [t_t1])
            P.tt(t2[:], bri[:, 0, :, :], cim, ALU.mult, [t_b, t_par], [t_t2])
            P.tt(bb[:, 1, :, :], t1[:], t2[:], ALU.add, [t_t1, t_t2], [t_bb])
            P.memset(BD[:], 0.0, [t_BD])
            for ri in range(2):
                P.copy(BD[0:64, :, ri, 0:16], bb[0:64, ri, :, :], [t_bb], [t_BD])
                P.copy(BD[64:128, :, ri, 16:32], bb[64:128, ri, :, :], [t_bb], [t_BD])
            for dg in range(16):
                ps, tp = P.ps()
                for ri in range(2):
                    P.tr(ps[0:32, ri * 128:(ri + 1) * 128], BD[:, dg, ri, :], K.ident[:], [t_BD, K.t_ident], [tp])
                P.copy(BT[:, dg, :, :], ps[0:32, 0:256].rearrange("p (r m) -> p r m", r=2), [tp], [t_BT])
            P.memset(CND[:], 0.0, [t_CND])
            for d in range(2):
                for ri, nm in enumerate(('s5_c_re', 's5_c_im')):
                    cv = I[nm][l, d].rearrange("(gh gl) c p -> gl c gh p", gl=2)
                    for gl in range(2):
                        P.dma(CND[gl * 16:(gl + 1) * 16, d * 8:(d + 1) * 8, ri, gl * 64:(gl + 1) * 64], cv[gl], (), [t_CND])
            for dg in range(16):
                ps, tp = P.ps()
                for ri in range(2):
                    P.tr(ps[:, ri * 32:(ri + 1) * 32], CND[:, dg, ri, :], K.ident[0:32, 0:32], [t_CND, K.t_ident], [tp])
                P.copy(CB[:, dg, 0, :], ps[:, 0:32], [tp], [t_CB])
                P.ts(CB[:, dg, 1, :], ps[:, 32:64], -1.0, None, ALU.mult, None, [tp], [t_CB])
        P.barrier()
        with SB(nc, "s_us", [32, T], F32) as us, SB(nc, "s_y", [32, T], F32) as ysb, \
                SB(nc, "s_tab", [128, 4, 512], F32) as tab, SB(nc, "s_m", [128, 2, 512], F32) as mm_, \
                SB(nc, "s_w", [128, 2, 512], F32) as ww, SB(nc, "s_s", [128, 2, 512], F32) as ss_, \
                SB(nc, "s_q", [128, 2, 512], F32) as qq, SB(nc, "s_c0", [128, 4], F32) as c0, \
                SB(nc, "s_car", [128, 2], F32) as car, SB(nc, "s_mag", [128, 512], F32) as magt, SB(nc, "s_g", [32, 2, T], F32) as gg:
            t_us, t_y, t_m, t_w, t_s, t_q, t_c0, t_car, t_g = (Tok(n) for n in ("us", "y", "m", "w", "s", "q", "c0", "car", "g"))
            t_tab = [Tok("tabs"), Tok("tabc"), Tok("vr"), Tok("vr2")]
            t_mag = Tok("mag")
            for j in range(8):
                P.dma(us[:], K.UF[32 * j:32 * (j + 1), :], (), [t_us])
                for d in range(2):
                    dg = d * 8 + j
                    if d == 0:
                        chunks = [(a, min(a + 512, T), False, a) for a in range(0, T, 512)]
                    else:
                        chunks = [(0, NCTX, True, 0)]
                        b = T
                        while b > NCTX:
                            a = max(b - 512, NCTX)
                            chunks.append((a, b, True, NCTX + (T - b)))
                            b = a
                    first = True
                    phi = par[:, PHI, dg:dg + 1]
                    P.copy(magt[:, :], par[:, MAG, dg:dg + 1].broadcast_to([128, 512]), [t_par], [t_mag])
                    for (a, b, rev, i0) in chunks:
                        n = b - a
                        rv = (lambda ap: ap[:, ::-1]) if rev else (lambda ap: ap)
                        ps1, tp1 = P.ps()
                        ps2, tp2 = P.ps()
                        P.mm(ps1[:, 0:n], BT[:, dg, 0, :], us[:, a:b], True, True, [t_BT, t_us], [tp1])
                        P.mm(ps2[:, 0:n], BT[:, dg, 1, :], us[:, a:b], True, True, [t_BT, t_us], [tp2])
                        P.ts(c0[:, 0:1], phi, float(i0), None, ALU.mult, None, [t_par], [t_c0])
                        round_frac(K, c0[:, 0:1], c0[:, 2:3], t_c0, t_c0)
                        P.ts(c0[:, 1:2], c0[:, 0:1], 0.25, None, ALU.add, None, [t_c0], [t_c0])
                        for which in range(2):
                            P.act(tab[:, 2 + which, 0:n], iot[:, 0:n], AF.Identity, [t_iota, t_c0, t_par], [t_tab[2 + which]],
                                  bias=c0[:, which:which + 1], scale=phi)
                            P.ts(qq[:, which, 0:n], tab[:, 2 + which, 0:n], MAGIC, MAGIC, ALU.add, ALU.subtract, [t_tab[2 + which]], [t_q])
                            P.tt(tab[:, 2 + which, 0:n], tab[:, 2 + which, 0:n], qq[:, which, 0:n], ALU.subtract, [t_tab[2 + which], t_q],
                                 [t_tab[2 + which]])
                            P.act(tab[:, which, 0:n], tab[:, 2 + which, 0:n], AF.Sin, [t_tab[2 + which]], [t_tab[which]], scale=TWO_PI)
                        sn = rv(tab[:, 0, 0:n])
                        cs = rv(tab[:, 1, 0:n])
                        tS, tC = t_tab[0], t_tab[1]
                        P.tt(mm_[:, 0, 0:n], ps1[:, 0:n], cs, ALU.mult, [tp1, tC], [t_m])
                        P.tt(qq[:, 0, 0:n], ps2[:, 0:n], sn, ALU.mult, [tp2, tS], [t_q])
                        P.tt(mm_[:, 0, 0:n], mm_[:, 0, 0:n], qq[:, 0, 0:n], ALU.add, [t_m, t_q], [t_m])
                        P.tt(mm_[:, 1, 0:n], ps2[:, 0:n], cs, ALU.mult, [tp2, tC], [t_m])
                        P.tt(qq[:, 1, 0:n], ps1[:, 0:n], sn, ALU.mult, [tp1, tS], [t_q])
                        P.tt(mm_[:, 1, 0:n], mm_[:, 1, 0:n], qq[:, 1, 0:n], ALU.subtract, [t_m, t_q], [t_m])
                        magb = magt[:, 0:n]
                        for ri in range(2):
                            init = 0.0 if first else car[:, ri:ri + 1]
                            P.op('dve', lambda g, ri=ri, init=init: g.tensor_tensor_scan(
                                out=rv(ww[:, ri, 0:n]), data0=magb, data1=rv(mm_[:, ri, 0:n]), initial=init,
                                op0=ALU.mult, op1=ALU.add), [t_m, t_mag, t_car], [t_w])
                        last = a if rev else b - 1
                        P.copy(car[:, :], ww[:, :, last - a], [t_w], [t_car])
                        first = False
                        P.tt(ss_[:, 0, 0:n], ww[:, 0, 0:n], cs, ALU.mult, [t_w, tC], [t_s], e='pool')
                        P.tt(qq[:, 0, 0:n], ww[:, 1, 0:n], sn, ALU.mult, [t_w, tS], [t_q], e='pool')
                        P.tt(ss_[:, 0, 0:n], ss_[:, 0, 0:n], qq[:, 0, 0:n], ALU.subtract, [t_s, t_q], [t_s], e='pool')
                        P.tt(ss_[:, 1, 0:n], ww[:, 0, 0:n], sn, ALU.mult, [t_w, tS], [t_s], e='pool')
                        P.tt(qq[:, 1, 0:n], ww[:, 1, 0:n], cs, ALU.mult, [t_w, tC], [t_q], e='pool')
                        P.tt(ss_[:, 1, 0:n], ss_[:, 1, 0:n], qq[:, 1, 0:n], ALU.add, [t_s, t_q], [t_s], e='pool')
                        psy, tpy = P.ps()
                        P.mm(psy[0:32, 0:n], CB[:, dg, 0, :], ss_[:, 0, 0:n], True, False, [t_CB, t_s], [tpy])
                        P.mm(psy[0:32, 0:n], CB[:, dg, 1, :], ss_[:, 1, 0:n], False, True, [t_CB, t_s], [tpy])
                        if d == 0:
                            P.copy(ysb[:, a:b], psy[0:32, 0:n], [tpy], [t_y], e='act')
                        else:
                            P.tt(ysb[:, a:b], ysb[:, a:b], psy[0:32, 0:n], ALU.add, [t_y, tpy], [t_y])
                P.stt(ysb[:], us[:], dsk[:, j:j + 1], ysb[:], ALU.mult, ALU.add, [t_us, t_dsk, t_y], [t_y])
                P.tt(gg[:, 0, :], ysb[:], ysb[:], ALU.mult, [t_y], [t_g])
                P.ts(gg[:, 0, :], gg[:, 0, :], 0.044715, 1.0, ALU.mult, ALU.add, [t_g], [t_g])
                P.tt(gg[:, 0, :], gg[:, 0, :], ysb[:], ALU.mult, [t_g, t_y], [t_g])
                P.act(gg[:, 1, :], gg[:, 0, :], AF.Sigmoid, [t_g], [t_g], scale=1.5957691216)
                P.tt(gg[:, 1, :], gg[:, 1, :], ysb[:], ALU.mult, [t_g, t_y], [t_g])
                P.dma(K.GS[32 * j:32 * (j + 1), :], gg[:, 1, :], [t_g], [("GS", j)], q='pool')
        P.barrier()
        with SB(nc, "s_gw", [128, 2, 256], F32) as gw, SB(nc, "s_gb", [128, 2], F32) as gb, \
                SB(nc, "s_gt", [128, 2, 512], F32) as gt, SB(nc, "s_sg", [128, 2, 512], F32) as sg, \
                SB(nc, "s_o", [128, 4, 256], F32) as so:
            t_gw, t_gt, t_sg, t_so = Tok("gw"), Tok("gt"), Tok("sg"), Tok("so")
            P.dma(gw[:], I['s5_glu_w'][l].rearrange("(c p) n -> p c n", p=128), (), [t_gw])
            P.dma(gb[:], I['s5_glu_b'][l, :].rearrange("(c p) -> p c", p=128), (), [t_gw])
            for g in range((T + 511) // 512):
                t0 = g * 512
                n = min(512, T - t0)
                P.dma(gt[:, :, 0:n], K.GS[:, t0:t0 + n].rearrange("(c p) t -> p c t", p=128), (), [t_gt])
                for oc in range(2):
                    ps, tp = P.ps()
                    for kc in range(2):
                        P.mm(ps[:, 0:n], gw[:, kc, oc * 128:(oc + 1) * 128], gt[:, kc, 0:n], kc == 0, kc == 1, [t_gw, t_gt], [tp])
                    P.act(sg[:, oc, 0:n], ps[:, 0:n], AF.Sigmoid, [tp, t_gw], [t_sg], bias=gb[:, oc:oc + 1], scale=1.0)
                    P.tt(sg[:, oc, 0:n], sg[:, oc, 0:n], gt[:, oc, 0:n], ALU.mult, [t_sg, t_gt], [t_sg])
                for i in range(n // 128):
                    ps, tp = P.ps()
                    for oc in range(2):
                        P.tr(ps[:, oc * 128:(oc + 1) * 128], sg[:, oc, i * 128:(i + 1) * 128], K.ident[:], [t_sg, K.t_ident], [tp])
                    P.copy(so[:, i, :], ps[:, 0:256], [tp], [t_so], e='act')
                P.dma(K.MIX[t0:t0 + n, 768:1024].rearrange("(i p) c -> p i c", p=128), so[:, 0:n // 128, :], [t_so], [("MIXs", g)], q='pool')


def stage_outproj(K, l):
    P, nc, I = K.P, K.P.nc, K.I
    with SB(nc, "o_w", [128, 8, D], F32) as W, SB(nc, "o_rw", [128, 8, 16], F32) as RW, \
            SB(nc, "o_gain", [128, D], F32) as gain, SB(nc, "o_mod", [128, 6, D], F32) as mod, \
            SB(nc, "o_rb", [128, 16], F32) as rb, SB(nc, "o_mix", [128, D], F32) as mix, \
            SB(nc, "o_x", [128, D], F32) as xt, SB(nc, "o_junk", [128, D], F32) as junk, \
            SB(nc, "o_mT", [128, 8, 128], F32) as mT, SB(nc, "o_h2", [128, D], F32) as h2, \
            SB(nc, "o_hT", [128, 8, 128], F32) as hT, SB(nc, "o_tmp", [128, D], F32) as tmp, \
            SB(nc, "o_st", [128, 8], F32) as st, SB(nc, "o_r", [128, 12, 16], F32) as rr, \
            SB(nc, "o_gT", [16, 128], F32) as gT:
        t_W, t_c, t_mix, t_x, t_mT, t_h2, t_hT, t_tmp, t_st, t_rr, t_gT = (Tok(n) for n in (
            "W", "c", "mix", "x", "mT", "h2", "hT", "tmp", "st", "rr", "gT"))
        for k in range(8):
            P.load_r(W[:, k, :], I['w_out'][l, k * 128:(k + 1) * 128, :], junk[:], t_W, t_tmp, e='act' if k % 2 else 'dve')
        P.dma(RW[:], I['router_w'].rearrange("(k p) n -> p k n", p=128), (), [t_c])
        P.dma(gain[:], I['mix_norm_g'][l, :].partition_broadcast(128), (), [t_c])
        P.dma(rb[:], I['router_b'][0, :].partition_broadcast(128), (), [t_c])
        for jj, r in enumerate((2, 3, 4, 8, 9, 10)):
            P.dma(mod[:, jj, :], K.MOD[r, :].partition_broadcast(128), (), [t_c])
        groups = ((0, 256), (256, 768), (768, 1024))
        for ti in range(NT):
            mb = 3 if ti < 2 else 0
            rows = slice(ti * 128, (ti + 1) * 128)
            P.dma(mix[:], K.MIX[rows, :], (), [t_mix])
            P.dma(xt[:], K.X[rows, :], (), [t_x])
            for gi, (c0, c1) in enumerate(groups):
                rms_rstd(K, mix[:, c0:c1], t_mix, c1 - c0, junk[:, c0:c1], st[:, gi:gi + 1], t_st)
            for gi, (c0, c1) in enumerate(groups):
                P.stt(mix[:, c0:c1], mix[:, c0:c1], st[:, gi:gi + 1], gain[:, c0:c1], ALU.mult, ALU.mult, [t_mix, t_st, t_c], [t_mix])
            for half in range(2):
                ps, tp = P.ps()
                for j in range(4):
                    kk = half * 4 + j
                    P.tr(ps[:, j * 128:(j + 1) * 128], mix[:, kk * 128:(kk + 1) * 128], K.ident[:], [t_mix, K.t_ident], [tp])
                P.copy(RR(mT[:, half * 4:(half + 1) * 4, :]), ps[:, :].rearrange("p (j t) -> p j t", j=4), [tp], [t_mT], e='act' if half else 'dve')
            for half in range(2):
                ps, tp = P.ps()
                for k in range(8):
                    P.mm(ps[:, :], mT[:, k, :], W[:, k, half * 512:(half + 1) * 512], k == 0, k == 7, [t_mT, t_W], [tp], r=FAST)
                hs = slice(half * 512, (half + 1) * 512)
                P.tt(tmp[:, hs], ps[:, :], mod[:, mb + 0, hs], ALU.mult, [tp, t_c], [t_tmp])
                P.tt(xt[:, hs], xt[:, hs], tmp[:, hs], ALU.add, [t_x, t_tmp], [t_x])
            P.dma(K.X[rows, :], xt[:], [t_x], [("X", ti)], q='pool')
            rms_rstd(K, xt[:], t_x, D, junk[:], st[:, 3:4], t_st)
            P.stt(h2[:], xt[:], st[:, 3:4], mod[:, mb + 1, :], ALU.mult, ALU.mult, [t_x, t_st, t_c], [t_h2])
            P.tt(h2[:], h2[:], mod[:, mb + 2, :], ALU.add, [t_h2, t_c], [t_h2])
            for half in range(2):
                ps, tp = P.ps()
                for j in range(4):
                    kk = half * 4 + j
                    P.tr(ps[:, j * 128:(j + 1) * 128], h2[:, kk * 128:(kk + 1) * 128], K.ident[:], [t_h2, K.t_ident], [tp])
                P.copy(hT[:, half * 4:(half + 1) * 4, :], ps[:, :].rearrange("p (j t) -> p j t", j=4), [tp], [t_hT], e='act' if half else 'dve')
            P.dma(K.H2T[:, ti * 128:(ti + 1) * 128].rearrange("(k p) t -> p k t", p=128), hT[:], [t_hT], [("H2T", ti)], q='pool')
            ps, tp = P.ps()
            for k in range(8):
                P.mm(ps[:, 0:16], hT[:, k, :], RW[:, k, :], k == 0, k == 7, [t_hT, t_c], [tp])
            R = lambda i: rr[:, i, :]
            R4 = lambda i: rr[:, i, :].rearrange("p (g e) -> p g e", g=4)
            trr = [t_rr]
            P.op('dve', lambda g: g.reduce_max(out=st[:, 4:5], in_=ps[:, 0:16], axis=AX.X), [tp], [t_st])
            P.ts(st[:, 4:5], st[:, 4:5], -1.0, None, ALU.mult, None, [t_st], [t_st])
            P.act(R(0), ps[:, 0:16], AF.Exp, [tp, t_st], trr, bias=st[:, 4:5], scale=1.0, accum_out=st[:, 5:6])
            P.recip(st[:, 5:6], st[:, 5:6], [t_st, t_rr], [t_st])
            P.ts(R(0), R(0), st[:, 5:6], None, ALU.mult, None, trr + [t_st], trr)
            P.tt(R(1), R(0), rb[:], ALU.add, trr + [t_c], trr)
            P.op('dve', lambda g: g.reduce_max(out=rr[:, 2, 0:4], in_=R4(1), axis=AX.X), trr, trr)
            P.tt(R4(3), R4(1), rr[:, 2, 0:4].unsqueeze(2).broadcast_to([128, 4, 4]), ALU.is_equal, trr, trr)
            P.stt(R(4), R(3), -1e9, R(1), ALU.mult, ALU.add, trr, trr)
            P.op('dve', lambda g: g.reduce_max(out=rr[:, 5, 0:4], in_=R4(4), axis=AX.X), trr, trr)
            P.tt(rr[:, 6, 0:4], rr[:, 2, 0:4], rr[:, 5, 0:4], ALU.add, trr, trr)
            P.op('dve', lambda g: g.reduce_max(out=st[:, 6:7], in_=rr[:, 6, 0:4], axis=AX.X), trr, [t_st])
            P.ts(rr[:, 7, 0:4], rr[:, 6, 0:4], st[:, 6:7], None, ALU.is_equal, None, trr + [t_st], trr)
            P.tt(R4(8), R4(1), rr[:, 5, 0:4].unsqueeze(2).broadcast_to([128, 4, 4]), ALU.is_ge, trr, trr)
            P.tt(R4(8), R4(8), rr[:, 7, 0:4].unsqueeze(2).broadcast_to([128, 4, 4]), ALU.mult, trr, trr)
            P.tt(R(9), R(8), R(0), ALU.mult, trr, trr)
            P.op('dve', lambda g: g.reduce_sum(out=st[:, 7:8], in_=R(9), axis=AX.X), trr, [t_st])
            P.recip(st[:, 7:8], st[:, 7:8], [t_st], [t_st])
            P.ts(R(9), R(9), st[:, 7:8], None, ALU.mult, None, trr + [t_st], trr)
            ps2, tp2 = P.ps()
            P.tr(ps2[0:16, 0:128], R(9), K.ident[:], trr + [K.t_ident], [tp2])
            P.copy(gT[:], ps2[0:16, 0:128], [tp2], [t_gT], e='act')
            P.dma(K.GTT[:, ti * 128:(ti + 1) * 128], gT[:], [t_gT], [("GTT", ti)], q='pool')


def stage_moe(K, l):
    P, nc, I = K.P, K.P.nc, K.I
    GN = 1024
    with SB(nc, "e_h", [128, 8, GN], F32) as hT, SB(nc, "e_g", [128, 8, GN], F32) as GT, \
            SB(nc, "e_y", [128, GN // 128, D], F32) as Y, SB(nc, "e_wd", [128, 8, D], F32) as wd, \
            SB(nc, "e_wg", [128, 2, 8, 128], F32) as wg, SB(nc, "e_wu", [128, 2, 8, 128], F32) as wu, \
            SB(nc, "e_gt", [16, GN], F32) as gt, SB(nc, "e_gb", [128, GN], F32) as gB, \
            SB(nc, "e_sel", [16, 16, 128], F32) as sel, SB(nc, "e_sa", [128, 2, 512], F32) as sA, \
            SB(nc, "e_mod", [128, 2, D], F32) as mod, SB(nc, "e_x", [128, D], F32) as xt, \
            SB(nc, "e_wst", [128, 6, D], F32) as wst:
        t_wst = [Tok("wst%d" % i) for i in range(6)]
        t_h, t_G, t_Y, t_wd, t_gt, t_gB, t_sel, t_mod, t_x = (Tok(n) for n in ("h", "G", "Y", "wd", "gt", "gB", "sel", "mod", "x"))
        t_wg = [Tok("wg0"), Tok("wg1")]
        t_wu = [Tok("wu0"), Tok("wu1")]
        t_sa = [Tok("sa0"), Tok("sa1")]
        for e in range(16):
            P.copy(sel[:, e, :], K.ident[0:16, e:e + 1].broadcast_to([16, 128]), [K.t_ident], [t_sel])
        P.dma(mod[:, 0, :], K.MOD[5, :].partition_broadcast(128), (), [t_mod])
        P.dma(mod[:, 1, :], K.MOD[11, :].partition_broadcast(128), (), [t_mod])
        for g in range((T + GN - 1) // GN):
            t0 = g * GN
            n = min(GN, T - t0)
            nt = n // 128
            for i in range(nt):
                P.load_r(hT[:, :, i * 128:(i + 1) * 128], K.H2T[:, t0 + i * 128:t0 + (i + 1) * 128].rearrange("(k p) t -> p k t", p=128),
                         wst[:, i % 2, :].rearrange("p (k t) -> p k t", k=8), t_h, t_wst[i % 2], e='act' if i % 2 else 'dve')
            P.dma(gt[:, 0:n], K.GTT[:, t0:t0 + n], (), [t_gt])
            P.memset(Y[:, 0:nt, :], 0.0, [t_Y], e='pool')
            for e in range(16):
                for c in range(8):
                    P.load_r(wd[:, c, :], I['moe_w_down'][l, e, c * 128:(c + 1) * 128, :], wst[:, 2 + c % 2, :], t_wd, t_wst[2 + c % 2], e='dve')
                for s0 in range(0, n, 512):
                    sn = min(512, n - s0)
                    ps, tp = P.ps()
                    P.mm(ps[:, 0:sn], sel[:, e, :], gt[:, s0:s0 + sn], True, True, [t_sel, t_gt], [tp])
                    P.copy(gB[:, s0:s0 + sn], ps[:, 0:sn], [tp], [t_gB], e='act')
                wgv = I['moe_w_gate'][l, e].rearrange("(k p) n -> p k n", p=128)
                wuv = I['moe_w_up'][l, e].rearrange("(k p) n -> p k n", p=128)
                for c in range(8):
                    b = c % 2
                    P.load_r(wg[:, b, :, :], wgv[:, :, c * 128:(c + 1) * 128], wst[:, 4, :].rearrange("p (k t) -> p k t", k=8), t_wg[b], t_wst[4], e='act')
                    P.load_r(wu[:, b, :, :], wuv[:, :, c * 128:(c + 1) * 128], wst[:, 5, :].rearrange("p (k t) -> p k t", k=8), t_wu[b], t_wst[5], e='act')
                    for si, s0 in enumerate(range(0, n, 512)):
                        sn = min(512, n - s0)
                        psa, tpa = P.ps()
                        psu, tpu = P.ps()
                        for k in range(8):
                            P.mm(psa[:, 0:sn], wg[:, b, k, :], hT[:, k, s0:s0 + sn], k == 0, k == 7, [t_wg[b], t_h], [tpa], r=FAST)
                        for k in range(8):
                            P.mm(psu[:, 0:sn], wu[:, b, k, :], hT[:, k, s0:s0 + sn], k == 0, k == 7, [t_wu[b], t_h], [tpu], r=FAST)
                        sb_ = si % 2
                        P.act(sA[:, sb_, 0:sn], psa[:, 0:sn], AF.Silu, [tpa], [t_sa[sb_]])
                        P.tt(sA[:, sb_, 0:sn], sA[:, sb_, 0:sn], psu[:, 0:sn], ALU.mult, [t_sa[sb_], tpu], [t_sa[sb_]])
                        P.tt(RR(GT[:, c, s0:s0 + sn]), sA[:, sb_, 0:sn], gB[:, s0:s0 + sn], ALU.mult, [t_sa[sb_], t_gB], [t_G])
                for i in range(nt):
                    for half in range(2):
                        ps, tp = P.ps()
                        for c in range(8):
                            P.mm(ps[:, :], GT[:, c, i * 128:(i + 1) * 128], wd[:, c, half * 512:(half + 1) * 512], c == 0, c == 7, [t_G, t_wd], [tp], r=FAST)
                        hs = slice(half * 512, (half + 1) * 512)
                        P.tt(Y[:, i, hs], Y[:, i, hs], ps[:, :], ALU.add, [t_Y, tp], [t_Y])
            for i in range(nt):
                ti = g * (GN // 128) + i
                mb = 1 if ti < 2 else 0
                rows = slice(ti * 128, (ti + 1) * 128)
                P.dma(xt[:], K.X[rows, :], (), [t_x])
                P.tt(Y[:, i, :], Y[:, i, :], mod[:, mb, :], ALU.mult, [t_Y, t_mod], [t_Y], e='pool')
                P.tt(xt[:], xt[:], Y[:, i, :], ALU.add, [t_x, t_Y], [t_x])
                P.dma(K.X[rows, :], xt[:], [t_x], [("X", ti)], q='pool')


def stage_final(K):
    P, nc, I = K.P, K.P.nc, K.I
    with SB(nc, "f_g", [128, D], F32) as gbc, SB(nc, "f_x", [128, 2, D], F32) as xt, \
            SB(nc, "f_j", [128, D], F32) as junk, SB(nc, "f_s", [128, 2], F32) as st:
        t_g = Tok("g")
        t_x = [Tok("x0"), Tok("x1")]
        t_s = [Tok("s0"), Tok("s1")]
        P.dma(gbc[:], I['final_g'][0, :].partition_broadcast(128), (), [t_g])
        for i in range(NLAT // 128):
            b = i % 2
            P.dma(xt[:, b, :], K.X[NCTX + i * 128:NCTX + (i + 1) * 128, :], (), [t_x[b]])
            rms_rstd(K, xt[:, b, :], t_x[b], D, junk[:], st[:, b:b + 1], t_s[b])
            P.stt(xt[:, b, :], xt[:, b, :], st[:, b:b + 1], gbc[:], ALU.mult, ALU.mult, [t_x[b], t_s[b], t_g], [t_x[b]])
            P.dma(K.out[i * 128:(i + 1) * 128, :], xt[:, b, :], [t_x[b]], [("out", i)], q='pool')


_CONSTS = None


def consts():
    global _CONSTS
    if _CONSTS is not None:
        return _CONSTS
    c = {}
    c['k_ident'] = np.eye(128, dtype=np.float32)
    rc, rs = rope_tables()
    c['k_ropec'], c['k_ropes'] = rc, rs
    j = np.arange(128)[:, None]
    i = np.arange(128)[None, :]
    c['k_maskl'] = (j >= i).astype(np.float32)
    c['k_maskr'] = (j <= i).astype(np.float32)
    c['k_ccL'], c['k_ssL'] = dft_blocks(NLAT, 33)
    c['k_ccC'], c['k_ssC'] = dft_blocks(NCTX, 3)
    f, d, w = hyena_consts(NLAT)
    c['k_featL'], c['k_decL'], c['k_wkL'] = f, d, w.reshape(-1, 1)
    f, d, w = hyena_consts(NCTX)
    c['k_featC'], c['k_decC'], c['k_wkC'] = f, d, w.reshape(-1, 1)
    c['k_iota'] = np.arange(512, dtype=np.float32).reshape(1, 512)
    _CONSTS = c
    return c


def make_in_maps(inputs, cores):
    cs = consts()
    shared = {}
    for k, v in inputs.items():
        if k in ('x', 'c', 'ctx', 'c_ctx'):
            continue
        a = np.ascontiguousarray(np.asarray(v, dtype=np.float32))
        if k in ('router_b', 'final_g'):
            a = a.reshape(1, -1)
        shared[k] = a
    shared.update(cs)
    maps = []
    for b in cores:
        m = dict(shared)
        m['x'] = np.ascontiguousarray(inputs['x'][b], dtype=np.float32)
        m['ctx'] = np.ascontiguousarray(inputs['ctx'][b], dtype=np.float32)
        m['c'] = np.ascontiguousarray(inputs['c'][b], dtype=np.float32).reshape(1, D)
        m['c_ctx'] = np.ascontiguousarray(inputs['c_ctx'], dtype=np.float32).reshape(1, D)
        maps.append(m)
    return maps


def kernel(**inputs):
    P = build()
    maps = make_in_maps(inputs, list(range(8)))
    res = run_bass_kernel_spmd(P.nc, maps, core_ids=list(range(8)))
    return np.stack([r["out"] for r in res.results], axis=0).astype(np.float32)
```

```python
import math
from contextlib import ExitStack
import numpy as np
import concourse.bass as bass
import concourse.mybir as mybir
from concourse.bass_utils import run_bass_kernel_spmd

F32 = mybir.dt.float32
F32R = mybir.dt.float32r
FAST = True
AF = mybir.ActivationFunctionType
ALU = mybir.AluOpType
AX = mybir.AxisListType

D = 1024
NLAT = 4096
NCTX = 256
T = NLAT + NCTX
NT = T // 128
DEPTH = 4
HY_W = 256
ATT_W = 512
S5_W = 256
HY_END = 768
Q_END = HY_END + ATT_W
K_END = Q_END + 128
V_END = K_END + 128
IN_W = V_END + S5_W
EPS = 1e-6
MAGIC = 12582912.0
TWO_PI = 6.283185
NE = 16


class Tok:
    def __init__(self, name):
        self.name = name

    def __repr__(self):
        return self.name


class Prog:
    NDS = 24

    def __init__(self):
        nc = bass.Bass("TRN2", target_bir_lowering=False)
        self.nc = nc
        self.eng = {'pe': nc.tensor, 'dve': nc.vector, 'act': nc.scalar, 'pool': nc.gpsimd, 'sp': nc.sync}
        self.esem = {k: nc.alloc_semaphore("es_" + k) for k in self.eng}
        self.ecnt = {k: 0 for k in self.eng}
        self.dsem = [nc.alloc_semaphore("ds%d" % i) for i in range(self.NDS)]
        self.dval = [0] * self.NDS
        self.dnext = 0
        self.know = {k: {} for k in self.eng}
        self.last_w = {}
        self.readers = {}
        self.n_inst = 0
        self.psum = [nc.alloc_psum_tensor("psb%d" % i, [128, 512], F32) for i in range(8)]
        self.ps_tok = [Tok("ps%d" % i) for i in range(8)]
        self.ps_next = 0
        self.ps_held = set()
        self.dram = {}

    def _sem_of(self, key):
        return self.esem[key] if isinstance(key, str) else self.dsem[key]

    def _deps(self, reads, writes):
        deps = {}

        def add(kv):
            k, v = kv
            if deps.get(k, 0) < v:
                deps[k] = v
        for t in reads:
            if t in self.last_w:
                add(self.last_w[t])
        for t in writes:
            if t in self.last_w:
                add(self.last_w[t])
            for kv in self.readers.get(t, {}).items():
                add(kv)
        return deps

    def _wait(self, e, deps):
        kn = self.know[e]
        for k, v in deps.items():
            if k == e and e == 'pe':
                continue
            if kn.get(k, 0) >= v:
                continue
            self.eng[e].wait_ge(self._sem_of(k), v)
            kn[k] = v
            self.n_inst += 1

    def _commit(self, me, reads, writes):
        for t in writes:
            self.last_w[t] = me
            self.readers[t] = {}
        for t in reads:
            r = self.readers.setdefault(t, {})
            if r.get(me[0], 0) < me[1]:
                r[me[0]] = me[1]

    def op(self, e, fn, reads=(), writes=()):
        self._wait(e, self._deps(reads, writes))
        inst = fn(self.eng[e])
        inst.then_inc(self.esem[e], 1)
        self.ecnt[e] += 1
        self.n_inst += 1
        self._commit((e, self.ecnt[e]), reads, writes)

    def dma(self, out, in_, reads=(), writes=(), q='sp'):
        self._wait(q, self._deps(reads, writes))
        s = self.dnext
        self.dnext = (self.dnext + 1) % self.NDS
        if self.know[q].get(s, 0) < self.dval[s]:
            self.eng[q].wait_ge(self.dsem[s], self.dval[s])
            self.know[q][s] = self.dval[s]
        self.eng[q].dma_start(out=out, in_=in_, allow_slow_non_contiguous=True).then_inc(self.dsem[s], 16)
        self.dval[s] += 16
        self.n_inst += 1
        self._commit((s, self.dval[s]), reads, writes)

    def barrier(self):
        for e in self.eng:
            deps = {k: self.ecnt[k] for k in self.eng if self.ecnt[k] > 0}
            for s in range(self.NDS):
                if self.dval[s] > 0:
                    deps[s] = self.dval[s]
            self._wait(e, deps)
        self.last_w = {}
        self.readers = {}

    def finish(self):
        deps = {s: self.dval[s] for s in range(self.NDS) if self.dval[s] > 0}
        for k in self.eng:
            if self.ecnt[k] > 0:
                deps[k] = self.ecnt[k]
        self._wait('sp', deps)

    def ps(self):
        while True:
            i = self.ps_next
            self.ps_next = (i + 1) % 8
            if i not in self.ps_held:
                return self.psum[i], self.ps_tok[i]

    def ps_hold(self, n):
        got = []
        for i in range(8):
            if i not in self.ps_held and len(got) < n:
                self.ps_held.add(i)
                got.append(i)
        return [(self.psum[i], self.ps_tok[i]) for i in got], got

    def ps_release(self, got):
        for i in got:
            self.ps_held.discard(i)

    def mm(self, out, lhsT, rhs, start, stop, reads, writes, r=False):
        if r:
            lhsT = lhsT.bitcast(F32R)
            rhs = rhs.bitcast(F32R)
        self.op('pe', lambda e: e.matmul(out, lhsT, rhs, start=start, stop=stop), reads, writes)

    def tr(self, out, in_, ident, reads, writes):
        self.op('pe', lambda e: e.transpose(out=out, in_=in_, identity=ident), reads, writes)

    def act(self, out, in_, func, reads, writes, **kw):
        self.op('act', lambda e: e.activation(out=out, in_=in_, func=func, **kw), reads, writes)

    def tt(self, out, in0, in1, op, reads, writes, e='dve'):
        self.op(e, lambda g: g.tensor_tensor(out=out, in0=in0, in1=in1, op=op), reads, writes)

    def ts(self, out, in0, s1, s2, op0, op1, reads, writes, e='dve'):
        if op1 is None:
            self.op(e, lambda g: g.tensor_scalar(out=out, in0=in0, scalar1=s1, scalar2=None, op0=op0), reads, writes)
        else:
            self.op(e, lambda g: g.tensor_scalar(out=out, in0=in0, scalar1=s1, scalar2=s2, op0=op0, op1=op1), reads, writes)

    def stt(self, out, in0, scalar, in1, op0, op1, reads, writes):
        self.op('dve', lambda g: g.scalar_tensor_tensor(out=out, in0=in0, scalar=scalar, in1=in1, op0=op0, op1=op1), reads, writes)

    def copy(self, out, in_, reads, writes, e='dve'):
        if e == 'act':
            self.op('act', lambda g: g.copy(out=out, in_=in_), reads, writes)
        else:
            self.op(e, lambda g: g.tensor_copy(out=out, in_=in_), reads, writes)

    def load_r(self, dst, src, stage, t_dst, t_stage, e='dve'):
        if not FAST:
            self.dma(dst, src, (), [t_dst])
            return
        self.dma(stage, src, (), [t_stage])
        self.copy(dst.bitcast(F32R), stage, [t_stage], [t_dst], e=e)

    def rnd(self, ap, tok, e='dve'):
        if FAST:
            self.copy(ap.bitcast(F32R), ap, [tok], [tok], e=e)

    def memset(self, ap, val, writes, e='dve'):
        self.op(e, lambda g: g.memset(ap, val), (), writes)

    def recip(self, out, in_, reads, writes):
        self.op('dve', lambda g: g.reciprocal(out=out, in_=in_), reads, writes)

    def dram_t(self, name, shape, kind="Internal"):
        t = self.nc.dram_tensor(name, list(shape), F32, kind=kind).ap()
        self.dram[name] = t
        return t


def RR(ap):
    return ap.bitcast(F32R) if FAST else ap


class Ctx:
    def dump(self, name, tile_ap, shape, reads):
        if 'dump' not in self.dbg:
            return
        o = self.P.nc.dram_tensor("dmp_" + name, list(shape), F32, kind="ExternalOutput").ap()
        self.P.dma(o, tile_ap, reads, (), q='sp')


_UID = [0]


def SB(nc, name, shape, dt):
    _UID[0] += 1
    return nc.sbuf_tensor("%s_%d" % (name, _UID[0]), shape, dt)


def rope_tables():
    cosT = np.ones((128, T), np.float64)
    sinT = np.zeros((128, T), np.float64)
    n = np.arange(NLAT)
    row = n // 64
    col = n % 64
    for r in range(128):
        d = r % 64
        dd = d if d < 32 else d - 32
        pos = row if d < 32 else col
        j = dd % 16
        first = dd < 16
        inv = 10000.0 ** (-(j / 16.0))
        ang = (pos.astype(np.float32) * np.float32(inv)).astype(np.float64)
        cosT[r, NCTX:] = np.cos(ang)
        sinT[r, NCTX:] = (-np.sin(ang)) if first else np.sin(ang)
    return cosT.astype(np.float32), sinT.astype(np.float32)


def perm_cols(width):
    src = np.zeros(width, np.int64)
    for c in range(width):
        d = c % 64
        dd = d % 32
        src[c] = c + 16 if dd < 16 else c - 16
    return src


def dft_blocks(nhalf, nchunk):
    idx = np.arange(nchunk * 128)
    valid = (idx <= nhalf)
    prod = np.outer(idx, idx).astype(np.float64)
    ang = 2.0 * np.pi * (prod % (2 * nhalf)) / (2 * nhalf)
    m = np.outer(valid, valid)
    cc = (np.cos(ang) * m).astype(np.float32)
    ss = (np.sin(ang) * m).astype(np.float32)

    def blk(mat):
        return np.ascontiguousarray(mat.reshape(nchunk, 128, nchunk, 128).transpose(2, 1, 0, 3))
    return blk(cc), blk(ss)


def hyena_consts(L):
    pos = np.arange(L, dtype=np.float32)
    t = pos / np.float32(max(L - 1, 1))
    bands = np.linspace(1e-4, 15, 16, dtype=np.float32)
    ang = (np.float32(2.0 * math.pi / L) * pos[:, None] * bands[None, :]).astype(np.float32)
    feats = np.concatenate([t[:, None], np.cos(ang), -np.sin(ang)], axis=-1).astype(np.float32)
    dmin = math.log(1e-2) / 1.5
    dmax = math.log(1e-2) / 0.3
    decay = np.abs(np.linspace(dmin, dmax, HY_W, dtype=np.float32))
    dec = np.exp(-t[:, None] * decay[None, :]).astype(np.float32)
    nk = L // 128 + 1
    wk = np.zeros(nk * 128, np.float32)
    wk[:L + 1] = 2.0 / (2 * L)
    wk[0] = 1.0 / (2 * L)
    wk[L] = 1.0 / (2 * L)
    return np.ascontiguousarray(feats.T), dec, wk


def build(n_layers=DEPTH, dbg=()):
    P = Prog()
    nc = P.nc
    K = Ctx()
    K.P = P
    K.dbg = dbg
    K.dbg_out = {}

    def inp(name, shape):
        return nc.dram_tensor(name, list(shape), F32, kind="ExternalInput").ap()

    I = {}
    I['x'] = inp('x', [NLAT, D])
    I['ctx'] = inp('ctx', [NCTX, D])
    I['c'] = inp('c', [1, D])
    I['c_ctx'] = inp('c_ctx', [1, D])
    shapes = dict(
        norm1_g=[4, D], norm2_g=[4, D], ada_w=[4, D, 6 * D], ada_b=[4, 6 * D], w_in=[4, D, IN_W], w_out=[4, D, D],
        mix_norm_g=[4, D], hy_conv_w=[4, 3, 768], hy_conv_b=[4, 768], hy_f_w1=[4, 33, 64], hy_f_b1=[4, 64],
        hy_f_freq1=[4, 64], hy_f_w2=[4, 64, 64], hy_f_b2=[4, 64], hy_f_freq2=[4, 64], hy_f_w3=[4, 64, 1024],
        hy_bias=[4, 2, 256], attn_sink=[4, 8], s5_lam_re=[4, 2, 16, 64], s5_lam_im=[4, 2, 16, 64], s5_log_dt=[4, 2, 16],
        s5_b_re=[4, 2, 16, 64, 16], s5_b_im=[4, 2, 16, 64, 16], s5_c_re=[4, 2, 16, 16, 64], s5_c_im=[4, 2, 16, 16, 64],
        s5_d=[4, 256], s5_glu_w=[4, 256, 256], s5_glu_b=[4, 256], router_w=[D, 16], router_b=[1, 16],
        moe_w_gate=[4, 16, D, D], moe_w_up=[4, 16, D, D], moe_w_down=[4, 16, D, D], final_g=[1, D],
        k_ident=[128, 128], k_ropec=[128, T], k_ropes=[128, T], k_maskl=[128, 128], k_maskr=[128, 128],
        k_ccL=[33, 128, 33, 128], k_ssL=[33, 128, 33, 128], k_ccC=[3, 128, 3, 128], k_ssC=[3, 128, 3, 128],
        k_featL=[33, NLAT], k_decL=[NLAT, 256], k_wkL=[33 * 128, 1], k_featC=[33, NCTX], k_decC=[NCTX, 256], k_wkC=[3 * 128, 1],
        k_iota=[1, 512],
    )
    for k, s in shapes.items():
        I[k] = inp(k, s)
    K.I = I
    out = nc.dram_tensor("out", [NLAT, D], F32, kind="ExternalOutput").ap()
    K.out = out

    K.X = P.dram_t("sX", [T, D])
    K.ZT = P.dram_t("sZT", [T, 768])
    K.QF = P.dram_t("sQF", [512, T])
    K.KF = P.dram_t("sKF", [128, T])
    K.VT = P.dram_t("sVT", [T, 128])
    K.UF = P.dram_t("sUF", [256, T])
    K.MIX = P.dram_t("sMIX", [T, D])
    K.H2T = P.dram_t("sH2T", [D, T])
    K.GTT = P.dram_t("sGTT", [16, T])

    K.ident = nc.alloc_sbuf_tensor("ident", [128, 128], F32)
    K.ones = nc.alloc_sbuf_tensor("ones", [128, 128], F32)
    K.MOD = P.dram_t("sMOD", [12, D])
    K.ZC = P.dram_t("sZC", [T, 768])
    K.KRE = P.dram_t("sKRE", [33 * 128, 512])
    K.KIM = P.dram_t("sKIM", [33 * 128, 512])
    K.t_ident = Tok("ident")
    K.t_ones = Tok("ones")
    K.t_mod = Tok("modbc")
    P.dma(K.ident[:], I['k_ident'][:, :], (), [K.t_ident])
    P.memset(K.ones[:], 1.0, [K.t_ones])

    tX = [("X", i) for i in range(NT)]
    K.tX = tX
    P.dma(K.X[0:NCTX, :], I['ctx'][:, :], (), tX[0:2])
    for i in range(4):
        P.dma(K.X[NCTX + i * 1024: NCTX + (i + 1) * 1024, :], I['x'][i * 1024:(i + 1) * 1024, :], (), tX[2 + 8 * i: 2 + 8 * (i + 1)])
    P.barrier()

    for l in range(n_layers):
        stage_mod(K, l)
        P.barrier()
        stage_inproj(K, l)
        P.barrier()
        if 'inproj' in dbg and l == 0:
            break
        stage_hyena(K, l, NLAT, NCTX, 'L')
        P.barrier()
        stage_hyena(K, l, NCTX, 0, 'C')
        P.barrier()
        if 'hyena' in dbg and l == 0:
            break
        stage_attn(K, l)
        P.barrier()
        if 'attn' in dbg and l == 0:
            break
        stage_s5(K, l)
        P.barrier()
        if 's5' in dbg and l == 0:
            break
        stage_outproj(K, l)
        P.barrier()
        if 'outproj' in dbg and l == 0:
            break
        stage_moe(K, l)
        P.barrier()
    if not dbg:
        stage_final(K)
    for name in dbg:
        if name in P.dram and name not in ('inproj', 'hyena', 'attn', 's5', 'outproj'):
            src = P.dram[name]
            o = nc.dram_tensor("dbg_" + name, list(src.shape), F32, kind="ExternalOutput").ap()
            P.barrier()
            P.dma(o, src, (), ())
    P.finish()
    return P


def stage_mod(K, l):
    P, nc, I = K.P, K.P.nc, K.I
    with SB(nc, "m_cs", [128, 2, 8], F32) as cs, SB(nc, "m_w", [128, 6 * D], F32) as wk, \
            SB(nc, "m_ws", [128, 6 * D], F32) as ws, SB(nc, "m_b", [1, 6 * D], F32) as bt, \
            SB(nc, "m_raw", [128, 6 * D], F32) as raw, SB(nc, "m_g", [128, 2, D], F32) as gn, \
            SB(nc, "m_o", [128, 12, D], F32) as mo:
        t_cs, t_w, t_ws, t_b, t_raw, t_g = Tok("cs"), Tok("w"), Tok("ws"), Tok("b"), Tok("raw"), Tok("g")
        P.dma(cs[:, 0, :], I['c'][0, :].rearrange("(c p) -> p c", p=128), (), [t_cs])
        P.dma(cs[:, 1, :], I['c_ctx'][0, :].rearrange("(c p) -> p c", p=128), (), [t_cs])
        P.act(cs[:], cs[:], AF.Silu, [t_cs], [t_cs])
        P.dma(bt[:], I['ada_b'][l:l + 1, :], (), [t_b])
        P.dma(gn[:, 0, :], I['norm1_g'][l, :].partition_broadcast(128), (), [t_g])
        P.dma(gn[:, 1, :], I['norm2_g'][l, :].partition_broadcast(128), (), [t_g])
        for who in range(2):
            for j in range(12):
                ps, tp = P.ps()
                for k in range(8):
                    P.dma(wk[:, 0:512], I['ada_w'][l, k * 128:(k + 1) * 128, j * 512:(j + 1) * 512], (), [t_w])
                    P.ts(ws[:, 0:512], wk[:, 0:512], cs[:, who, k:k + 1], None, ALU.mult, None, [t_w, t_cs], [t_ws])
                    P.mm(ps[:, :], K.ones[:, :], ws[:, 0:512], k == 0, False, [t_ws, K.t_ones], [tp])
                P.mm(ps[:, :], K.ones[0:1, :], bt[0:1, j * 512:(j + 1) * 512], False, True, [t_b, K.t_ones], [tp])
                P.copy(raw[:, j * 512:(j + 1) * 512], ps[:, :], [tp], [t_raw])
            base = who * 6
            m = mo
            for half in range(2):
                sh = raw[:, (3 * half) * D:(3 * half + 1) * D]
                sc = raw[:, (3 * half + 1) * D:(3 * half + 2) * D]
                gg = raw[:, (3 * half + 2) * D:(3 * half + 3) * D]
                P.stt(m[:, base + 3 * half + 0, :], sc, 1.0, gn[:, half, :], ALU.add, ALU.mult, [t_raw, t_g], [K.t_mod])
                P.copy(m[:, base + 3 * half + 1, :], sh, [t_raw], [K.t_mod])
                P.copy(m[:, base + 3 * half + 2, :], gg, [t_raw], [K.t_mod])
        P.dma(K.MOD.rearrange("(o j) d -> o j d", o=1), mo[0:1, :, :], [K.t_mod], [("MOD", 0)], q='pool')
        P.barrier()


def rms_rstd(K, xt, t_x, width, junk, ss, t_s):
    P = K.P
    P.act(junk, xt, AF.Square, [t_x], [t_s], accum_out=ss[:, 0:1])
    P.ts(ss[:, 0:1], ss[:, 0:1], 1.0 / width, EPS, ALU.mult, ALU.add, [t_s], [t_s])
    P.act(ss[:, 0:1], ss[:, 0:1], AF.Sqrt, [t_s], [t_s])
    P.recip(ss[:, 0:1], ss[:, 0:1], [t_s], [t_s])


def stage_inproj(K, l):
    P, nc, I = K.P, K.P.nc, K.I
    src = perm_cols(640)
    with SB(nc, "i_w", [128, 8, IN_W], F32) as W, SB(nc, "i_wp", [128, 8, 640], F32) as WP, \
            SB(nc, "i_x", [128, 2, D], F32) as xt, SB(nc, "i_h", [128, 2, D], F32) as ht, \
            SB(nc, "i_hT", [128, 8, 512], F32) as hT, SB(nc, "i_ss", [128, 2], F32) as ss, \
            SB(nc, "i_junk", [128, D], F32) as junk, SB(nc, "i_o", [128, 2, 768], F32) as ot, \
            SB(nc, "i_rc", [128, 512], F32) as rc, SB(nc, "i_rs", [128, 512], F32) as rs, \
            SB(nc, "i_t1", [128, 512], F32) as t1, SB(nc, "i_t2", [128, 512], F32) as t2, \
            SB(nc, "i_mod", [128, 4, D], F32) as modbc, SB(nc, "i_wst", [128, 2, IN_W], F32) as wst:
        t_W, t_WP = Tok("W"), Tok("WP")
        t_wst = [Tok("wst0"), Tok("wst1")]
        for jj, r in enumerate((0, 1, 6, 7)):
            P.dma(modbc[:, jj, :], K.MOD[r, :].partition_broadcast(128), (), [K.t_mod])
        t_x = [Tok("x0"), Tok("x1")]
        t_h = [Tok("h0"), Tok("h1")]
        t_s = [Tok("s0"), Tok("s1")]
        t_hT, t_o, t_rc, t_rs, t_t1, t_t2 = Tok("hT"), [Tok("o0"), Tok("o1")], Tok("rc"), Tok("rs"), Tok("t1"), Tok("t2")
        for k in range(8):
            P.load_r(W[:, k, :], I['w_in'][l, k * 128:(k + 1) * 128, :], wst[:, k % 2, :], t_W, t_wst[k % 2], e='act' if k % 2 else 'dve')
        wv = I['w_in'][l, :, HY_END:HY_END + 640].rearrange("(k p) (b two s) -> k p b two s", p=128, two=2, s=16)
        for k in range(8):
            dst = wst[:, k % 2, 0:640].rearrange("p (b two s) -> p b two s", two=2, s=16)
            P.dma(dst[:, :, 0, :], wv[k, :, :, 1, :], (), [t_wst[k % 2]])
            P.dma(dst[:, :, 1, :], wv[k, :, :, 0, :], (), [t_wst[k % 2]])
            P.copy(RR(WP[:, k, :]), wst[:, k % 2, 0:640], [t_wst[k % 2]], [t_WP], e='act' if k % 2 else 'dve')
        ngroups = (T + 511) // 512
        for g in range(ngroups):
            t0 = g * 512
            ntok = min(512, T - t0)
            ntile = ntok // 128
            for i in range(ntile):
                ti = g * 4 + i
                b = i % 2
                isctx = ti < 2
                mb = 2 if isctx else 0
                P.dma(xt[:, b, :], K.X[ti * 128:(ti + 1) * 128, :], [K.tX[ti]], [t_x[b]])
                rms_rstd(K, xt[:, b, :], t_x[b], D, junk[:], ss[:, b:b + 1], t_s[b])
                P.stt(ht[:, b, :], xt[:, b, :], ss[:, b:b + 1], modbc[:, mb + 0, :], ALU.mult, ALU.mult,
                      [t_x[b], t_s[b], K.t_mod], [t_h[b]])
                P.tt(ht[:, b, :], ht[:, b, :], modbc[:, mb + 1, :], ALU.add, [t_h[b], K.t_mod], [t_h[b]])
                for half in range(2):
                    ps, tp = P.ps()
                    for j in range(4):
                        kk = half * 4 + j
                        P.tr(ps[:, j * 128:(j + 1) * 128], ht[:, b, kk * 128:(kk + 1) * 128], K.ident[:], [t_h[b], K.t_ident], [tp])
                    P.copy(RR(hT[:, half * 4:(half + 1) * 4, i * 128:(i + 1) * 128]),
                           ps[:, :].rearrange("p (j t) -> p j t", j=4), [tp], [t_hT], e='act' if half else 'dve')
            for i in range(ntile):
                ti = g * 4 + i
                b = i % 2
                for (c0, cw, dst, dcol) in ((0, 512, K.ZT, 0), (512, 256, K.ZT, 512), (K_END, 128, K.VT, 0)):
                    ps, tp = P.ps()
                    for k in range(8):
                        P.mm(ps[:, 0:cw], hT[:, k, i * 128:(i + 1) * 128], W[:, k, c0:c0 + cw], k == 0, k == 7, [t_hT, t_W], [tp], r=FAST)
                    P.copy(ot[:, b, 0:cw], ps[:, 0:cw], [tp], [t_o[b]], e='act')
                    P.dma(dst[ti * 128:(ti + 1) * 128, dcol:dcol + cw], ot[:, b, 0:cw], [t_o[b]], [(dst.tensor.name, ti)], q='pool')
            P.dma(rc[:, 0:ntok], I['k_ropec'][:, t0:t0 + ntok], (), [t_rc])
            P.dma(rs[:, 0:ntok], I['k_ropes'][:, t0:t0 + ntok], (), [t_rs])
            for cidx in range(5):
                c0 = HY_END + cidx * 128
                ps, tp = P.ps()
                ps2, tp2 = P.ps()
                for k in range(8):
                    P.mm(ps[:, 0:ntok], W[:, k, c0:c0 + 128], hT[:, k, 0:ntok], k == 0, k == 7, [t_hT, t_W], [tp], r=FAST)
                for k in range(8):
                    P.mm(ps2[:, 0:ntok], WP[:, k, cidx * 128:(cidx + 1) * 128], hT[:, k, 0:ntok], k == 0, k == 7, [t_hT, t_WP], [tp2], r=FAST)
                P.tt(t1[:, 0:ntok], ps[:, 0:ntok], rc[:, 0:ntok], ALU.mult, [tp, t_rc], [t_t1])
                P.tt(t2[:, 0:ntok], ps2[:, 0:ntok], rs[:, 0:ntok], ALU.mult, [tp2, t_rs], [t_t2])
                P.tt(t1[:, 0:ntok], t1[:, 0:ntok], t2[:, 0:ntok], ALU.add, [t_t1, t_t2], [t_t1])
                if cidx < 4:
                    P.dma(K.QF[cidx * 128:(cidx + 1) * 128, t0:t0 + ntok], t1[:, 0:ntok], [t_t1], [("QF", g)], q='pool')
                else:
                    P.dma(K.KF[:, t0:t0 + ntok], t1[:, 0:ntok], [t_t1], [("KF", g)], q='pool')
            for cidx in range(2):
                c0 = V_END + cidx * 128
                ps, tp = P.ps()
                for k in range(8):
                    P.mm(ps[:, 0:ntok], W[:, k, c0:c0 + 128], hT[:, k, 0:ntok], k == 0, k == 7, [t_hT, t_W], [tp], r=FAST)
                P.copy(t2[:, 0:ntok], ps[:, 0:ntok], [tp], [t_t2], e='act')
                P.dma(K.UF[cidx * 128:(cidx + 1) * 128, t0:t0 + ntok], t2[:, 0:ntok], [t_t2], [("UF", g)], q='pool')


def sin_chain(K, ps_ap, bcol, fcol, v, r, hid, n, t_ps, t_consts, t_v, t_r, t_hid):
    P = K.P
    P.act(v[:, 0:n], ps_ap, AF.Identity, [t_ps] + t_consts, [t_v], bias=bcol, scale=fcol)
    P.ts(r[:, 0:n], v[:, 0:n], MAGIC, MAGIC, ALU.add, ALU.subtract, [t_v], [t_r])
    P.tt(v[:, 0:n], v[:, 0:n], r[:, 0:n], ALU.subtract, [t_v, t_r], [t_v])
    P.act(hid[:, 0:n], v[:, 0:n], AF.Sin, [t_v], [t_hid], scale=TWO_PI)


def stage_hyena(K, l, L, row0, tag):
    P, nc, I = K.P, K.P.nc, K.I
    NCH = L // 128
    NK = NCH + 1
    CC, SS = I['k_cc' + tag], I['k_ss' + tag]
    featT, dec, wkc = I['k_feat' + tag], I['k_dec' + tag], I['k_wk' + tag]
    with SB(nc, "ha_w", [128, 4, 768], F32) as wb, SB(nc, "ha_z", [128, 3, 768], F32) as z, \
            SB(nc, "ha_t", [128, 2, 768], F32) as tt_:
        t_wb, t_z, t_t = Tok("wb"), [Tok("zm"), Tok("z0"), Tok("zp")], [Tok("ta"), Tok("tb")]
        for j in range(3):
            P.dma(wb[:, j, :], I['hy_conv_w'][l, j, :].partition_broadcast(128), (), [t_wb])
        P.dma(wb[:, 3, :], I['hy_conv_b'][l, :].partition_broadcast(128), (), [t_wb])
        for i in range(NCH):
            r0 = row0 + i * 128
            if i == 0:
                P.memset(z[0:1, 0, :], 0.0, [t_z[0]])
                P.dma(z[1:128, 0, :], K.ZT[r0:r0 + 127, 0:768], (), [t_z[0]])
            else:
                P.dma(z[:, 0, :], K.ZT[r0 - 1:r0 + 127, 0:768], (), [t_z[0]])
            P.dma(z[:, 1, :], K.ZT[r0:r0 + 128, 0:768], (), [t_z[1]])
            if i == NCH - 1:
                P.memset(z[:, 2, :], 0.0, [t_z[2]])
                P.dma(z[0:127, 2, :], K.ZT[r0 + 1:r0 + 128, 0:768], (), [t_z[2]])
            else:
                P.dma(z[:, 2, :], K.ZT[r0 + 1:r0 + 129, 0:768], (), [t_z[2]])
            P.tt(tt_[:, 0, :], z[:, 0, :], wb[:, 0, :], ALU.mult, [t_z[0], t_wb], [t_t[0]])
            P.tt(tt_[:, 1, :], z[:, 1, :], wb[:, 1, :], ALU.mult, [t_z[1], t_wb], [t_t[1]], e='pool')
            P.tt(tt_[:, 0, :], tt_[:, 0, :], tt_[:, 1, :], ALU.add, [t_t[0], t_t[1]], [t_t[0]])
            P.tt(tt_[:, 1, :], z[:, 2, :], wb[:, 2, :], ALU.mult, [t_z[2], t_wb], [t_t[1]], e='pool')
            P.tt(tt_[:, 0, :], tt_[:, 0, :], tt_[:, 1, :], ALU.add, [t_t[0], t_t[1]], [t_t[0]])
            P.tt(tt_[:, 0, :], tt_[:, 0, :], wb[:, 3, :], ALU.add, [t_t[0], t_wb], [t_t[0]])
            P.dma(K.ZC[r0:r0 + 128, :], tt_[:, 0, :], [t_t[0]], [("ZC", i)], q='pool')
    P.barrier()
    HH = (NCH + 1) // 2
    with ExitStack() as es_:
        taps = es_.enter_context(SB(nc, "hb_taps", [128, NCH, 1024], F32))
        tabt = es_.enter_context(SB(nc, "hb_cc", [128, 2, HH, 128], F32))
        tabs_ = es_.enter_context(SB(nc, "hb_ss", [128, 2, HH, 128], F32))
        ft = es_.enter_context(SB(nc, "hb_f", [33, 512], F32))
        w1 = es_.enter_context(SB(nc, "hb_w1", [33, 64], F32))
        w2 = es_.enter_context(SB(nc, "hb_w2", [64, 64], F32))
        w3 = es_.enter_context(SB(nc, "hb_w3", [64, 1024], F32))
        cst = es_.enter_context(SB(nc, "hb_c", [64, 8], F32))
        v = es_.enter_context(SB(nc, "hb_v", [64, 512], F32))
        r = es_.enter_context(SB(nc, "hb_r", [64, 512], F32))
        h1 = es_.enter_context(SB(nc, "hb_h1", [64, 512], F32))
        h2 = es_.enter_context(SB(nc, "hb_h2", [64, 512], F32))
        dct = es_.enter_context(SB(nc, "hb_dec", [128, 256], F32))
        ab = es_.enter_context(SB(nc, "hb_abs", [128, 1024], F32))
        rn = es_.enter_context(SB(nc, "hb_rn", [128, 512], F32))
        tmp = es_.enter_context(SB(nc, "hb_tmp", [128, 512], F32))
        wkt = es_.enter_context(SB(nc, "hb_wk", [128, NK], F32))
        ko = es_.enter_context(SB(nc, "hb_o", [128, 2, 512], F32))
        t_taps = [Tok("taps%d" % c) for c in range(NCH)]
        t_cc, t_ss, t_f, t_w, t_c = Tok("cc"), Tok("ss"), Tok("f"), Tok("w"), Tok("c")
        t_v, t_r, t_h1, t_h2, t_dec, t_ab, t_rn, t_tmp, t_wk = (Tok(n) for n in ("v", "r", "h1", "h2", "dec", "ab", "rn", "tmp", "wk"))
        t_ko = [Tok("ko0"), Tok("ko1")]
        P.dma(w1[:], I['hy_f_w1'][l, :, :], (), [t_w])
        P.dma(w2[:], I['hy_f_w2'][l, :, :], (), [t_w])
        P.dma(w3[:], I['hy_f_w3'][l, :, :], (), [t_w])
        for j, nm in enumerate(('hy_f_b1', 'hy_f_freq1', 'hy_f_b2', 'hy_f_freq2')):
            P.dma(cst[:, j:j + 1], I[nm][l, :].rearrange("(p o) -> p o", o=1), (), [t_c])
        P.ts(cst[:, 4:5], cst[:, 1:2], 1.0 / (2.0 * math.pi), None, ALU.mult, None, [t_c], [t_c])
        P.ts(cst[:, 5:6], cst[:, 3:4], 1.0 / (2.0 * math.pi), None, ALU.mult, None, [t_c], [t_c])
        P.tt(cst[:, 6:7], cst[:, 0:1], cst[:, 4:5], ALU.mult, [t_c], [t_c])
        P.tt(cst[:, 7:8], cst[:, 2:3], cst[:, 5:6], ALU.mult, [t_c], [t_c])
        P.dma(wkt[:], wkc[:, 0].rearrange("(c p) -> p c", p=128), (), [t_wk])
        ng = (L + 511) // 512
        for g in range(ng):
            n0 = g * 512
            n = min(512, L - n0)
            P.dma(ft[:, 0:n], featT[:, n0:n0 + n], (), [t_f])
            ps, tp = P.ps()
            P.mm(ps[0:64, 0:n], w1[0:33, :], ft[0:33, 0:n], True, True, [t_w, t_f], [tp])
            sin_chain(K, ps[0:64, 0:n], cst[:, 6:7], cst[:, 4:5], v, r, h1, n, tp, [t_c], t_v, t_r, t_h1)
            ps, tp = P.ps()
            P.mm(ps[0:64, 0:n], w2[:, :], h1[:, 0:n], True, True, [t_w, t_h1], [tp])
            sin_chain(K, ps[0:64, 0:n], cst[:, 7:8], cst[:, 5:6], v, r, h2, n, tp, [t_c], t_v, t_r, t_h2)
            for sub in range(n // 128):
                c = (n0 // 128) + sub
                P.dma(dct[:], dec[c * 128:(c + 1) * 128, :], (), [t_dec])
                for half in range(2):
                    ps, tp = P.ps()
                    P.mm(ps[:, :], h2[:, sub * 128:(sub + 1) * 128], w3[:, half * 512:(half + 1) * 512], True, True, [t_h2, t_w], [tp])
                    P.tt(RR(taps[:, c, half * 512:(half + 1) * 512].rearrange("p (d c) -> p d c", d=2)),
                         ps[:, :].rearrange("p (d c) -> p d c", d=2),
                         dct[:, :].unsqueeze(1).broadcast_to([128, 2, 256]), ALU.mult, [tp, t_dec], [t_taps[c]])
        tv0 = taps[0:1, 0, :].rearrange("p (o d c) -> p o d c", o=2, d=2)
        P.ts(RR(tv0[:, :, 1, :]), tv0[:, :, 1, :], 0.0, None, ALU.mult, None, [t_taps[0]], [t_taps[0]])
        held, hid_ = P.ps_hold(2)
        for c in range(NCH):
            P.act(ab[:], taps[:, c, :], AF.Abs, [t_taps[c]], [t_ab])
            for half in range(2):
                P.mm(held[half][0][:, :], K.ones[:, :], ab[:, half * 512:(half + 1) * 512], c == 0, c == NCH - 1, [t_ab, K.t_ones], [held[half][1]])
        for o in range(2):
            P.copy(ab[:, o * 512:o * 512 + 256], held[o][0][:, 0:256], [held[o][1]], [t_ab])
            P.tt(rn[:, o * 256:(o + 1) * 256], ab[:, o * 512:o * 512 + 256], held[o][0][:, 256:512], ALU.add, [held[o][1], t_ab], [t_rn])
        P.ps_release(hid_)
        P.recip(rn[:], rn[:], [t_rn], [t_rn])
        for c in range(NCH):
            tv = taps[:, c, :].rearrange("p (o d c) -> p o d c", o=2, d=2)
            tm = tmp[:, :].rearrange("p (o c) -> p o c", o=2)
            P.tt(tm, tv[:, :, 0, :], tv[:, :, 1, :], ALU.add, [t_taps[c]], [t_tmp])
            P.tt(RR(tv[:, :, 1, :]), tv[:, :, 1, :], tv[:, :, 0, :], ALU.subtract, [t_taps[c]], [t_taps[c]])
            P.copy(RR(tv[:, :, 0, :]), tm, [t_tmp], [t_taps[c]])
        rn3 = rn[:, :].rearrange("p (o c) -> p o c", o=2)
        t_st2 = [Tok("fst0"), Tok("fst1")]
        for kc in range(NK):
            for part, TAB, ttab, d in ((0, CC, t_cc, 0), (1, SS, t_ss, 1)):
                ps, tp = P.ps()
                for hf in range(2):
                    cA, cB = hf * HH, min((hf + 1) * HH, NCH)
                    if cA >= cB:
                        continue
                    P.load_r(tabt[:, part, 0:cB - cA, :], TAB[kc, :, cA:cB, :], tabs_[:, part, 0:cB - cA, :], ttab, t_st2[part],
                             e='act' if part == 0 else 'dve')
                    for c in range(cA, cB):
                        tv = taps[:, c, :].rearrange("p (o d c) -> p o d c", o=2, d=2)
                        P.mm(ps[:, :].rearrange("p (o c) -> p o c", o=2), tabt[:, part, c - cA, :], tv[:, :, d, :], c == 0, c == NCH - 1,
                             [ttab, t_taps[c]], [tp], r=FAST)
                P.stt(ko[:, part, :].rearrange("p (o c) -> p o c", o=2), ps[:, :].rearrange("p (o c) -> p o c", o=2),
                      wkt[:, kc:kc + 1], rn3, ALU.mult, ALU.mult, [tp, t_wk, t_rn], [t_ko[part]])
                dst = K.KRE if part == 0 else K.KIM
                P.dma(dst[kc * 128:(kc + 1) * 128, :], ko[:, part, :], [t_ko[part]], [(dst.tensor.name, kc)], q='pool')
    P.barrier()
    with SB(nc, "hc_a", [128, NCH, 256], F32) as a, SB(nc, "hc_p1", [128, NK, 256], F32) as p1, \
            SB(nc, "hc_p2", [128, NK, 256], F32) as p2, SB(nc, "hc_cc", [128, NK, 128], F32) as cct, \
            SB(nc, "hc_ss", [128, NK, 128], F32) as sst, SB(nc, "hc_k", [128, 2, 256], F32) as kk, \
            SB(nc, "hc_t", [128, 2, 256], F32) as tq, SB(nc, "hc_b", [128, 2, 256], F32) as bb, \
            SB(nc, "hc_x", [128, 256], F32) as xg, SB(nc, "hc_y", [128, 256], F32) as yy, \
            SB(nc, "hc_stg", [128, 2, NK, 128], F32) as stg:
        t_stg = [Tok("stg0"), Tok("stg1")]
        t_a = [Tok("a%d" % c) for c in range(NCH)]
        t_p1, t_p2, t_cc, t_ss, t_k, t_b, t_x, t_y = (Tok(n) for n in ("p1", "p2", "cc", "ss", "k", "b", "x", "y"))
        t_q = [Tok("q0"), Tok("q1")]
        for o in range(2):
            P.dma(bb[:, o, :], I['hy_bias'][l, o, :].partition_broadcast(128), (), [t_b])
        for c in range(NCH):
            P.load_r(a[:, c, :], K.ZC[row0 + c * 128: row0 + (c + 1) * 128, 0:256], xg[:], t_a[c], t_x, e='act' if c % 2 else 'dve')
        for o in range(2):
            for kc in range(NK):
                P.load_r(cct[:], CC[kc, :, :, :], stg[:, 0, :, :], t_cc, t_stg[0], e='act')
                P.load_r(sst[:], SS[kc, :, :, :], stg[:, 1, :, :], t_ss, t_stg[1], e='dve')
                P.dma(kk[:, 0, :], K.KRE[kc * 128:(kc + 1) * 128, o * 256:(o + 1) * 256], (), [t_k])
                P.dma(kk[:, 1, :], K.KIM[kc * 128:(kc + 1) * 128, o * 256:(o + 1) * 256], (), [t_k])
                psr, tpr = P.ps()
                psi, tpi = P.ps()
                for c in range(NCH):
                    P.mm(psr[:, 0:256], cct[:, c, :], a[:, c, :], c == 0, c == NCH - 1, [t_cc, t_a[c]], [tpr], r=FAST)
                for c in range(NCH):
                    P.mm(psi[:, 0:256], sst[:, c, :], a[:, c, :], c == 0, c == NCH - 1, [t_ss, t_a[c]], [tpi], r=FAST)
                P.tt(tq[:, 0, :], psr[:, 0:256], kk[:, 0, :], ALU.mult, [tpr, t_k], [t_q[0]])
                P.tt(tq[:, 1, :], psi[:, 0:256], kk[:, 1, :], ALU.mult, [tpi, t_k], [t_q[1]])
                P.tt(RR(p1[:, kc, :]), tq[:, 0, :], tq[:, 1, :], ALU.add, [t_q[0], t_q[1]], [t_p1])
                P.tt(tq[:, 0, :], psi[:, 0:256], kk[:, 0, :], ALU.mult, [tpi, t_k], [t_q[0]])
                P.tt(tq[:, 1, :], psr[:, 0:256], kk[:, 1, :], ALU.mult, [tpr, t_k], [t_q[1]])
                P.tt(RR(p2[:, kc, :]), tq[:, 0, :], tq[:, 1, :], ALU.subtract, [t_q[0], t_q[1]], [t_p2])
            for tc in range(NCH):
                r0 = row0 + tc * 128
                P.load_r(cct[:], CC[tc, :, :, :], stg[:, 0, :, :], t_cc, t_stg[0], e='act')
                P.load_r(sst[:], SS[tc, :, :, :], stg[:, 1, :, :], t_ss, t_stg[1], e='dve')
                P.dma(xg[:], K.ZC[r0:r0 + 128, 256 * (o + 1):256 * (o + 2)], (), [t_x])
                ps, tp = P.ps()
                for kc in range(NK):
                    P.mm(ps[:, 0:256], cct[:, kc, :], p1[:, kc, :], kc == 0, False, [t_cc, t_p1], [tp], r=FAST)
                for kc in range(NK):
                    P.mm(ps[:, 0:256], sst[:, kc, :], p2[:, kc, :], False, kc == NK - 1, [t_ss, t_p2], [tp], r=FAST)
                P.tt(yy[:], a[:, tc, :], bb[:, o, :], ALU.mult, [t_a[tc], t_b], [t_y])
                P.tt(yy[:], yy[:], ps[:, 0:256], ALU.add, [t_y, tp], [t_y])
                if o == 0:
                    P.tt(RR(a[:, tc, :]), yy[:], xg[:], ALU.mult, [t_y, t_x], [t_a[tc]])
                else:
                    P.tt(yy[:], yy[:], xg[:], ALU.mult, [t_y, t_x], [t_y])
                    P.dma(K.MIX[r0:r0 + 128, 0:256], yy[:], [t_y], [("MIX", r0)], q='pool')


def stage_attn(K, l):
    P, nc, I = K.P, K.P.nc, K.I
    with SB(nc, "at_k", [64, 2, T], F32) as kf, SB(nc, "at_v", [128, NT, 2, 65], F32) as v1, \
            SB(nc, "at_es", [128, 8], F32) as es, SB(nc, "at_ml", [128, 128], F32) as ml, \
            SB(nc, "at_mr", [128, 128], F32) as mr, SB(nc, "at_q", [64, 2, 4, 128], F32) as q4, \
            SB(nc, "at_pt", [128, 5, 512], F32) as pt, SB(nc, "at_o", [128, 2, 512], F32) as ot, \
            SB(nc, "at_d", [128, 2, 4], F32) as den:
        t_k, t_v, t_es, t_m = Tok("k"), Tok("v"), Tok("es"), Tok("m")
        t_q = [Tok("q0"), Tok("q1")]
        t_pt = [Tok("pt%d" % i) for i in range(5)]
        t_o = [Tok("o0"), Tok("o1")]
        t_d = Tok("d")
        for h in range(2):
            P.dma(kf[:, h, :], K.KF[h * 64:(h + 1) * 64, :], (), [t_k])
        P.memset(v1[:, :, :, 64:65], 1.0, [t_v])
        vtv = K.VT.rearrange("(t p) (h d) -> p t h d", p=128, h=2)
        for h in range(2):
            P.dma(v1[:, :, h, 0:64], vtv[:, :, h, :], (), [t_v])
        P.dma(es[:], I['attn_sink'][l, :].partition_broadcast(128), (), [t_es])
        P.act(es[:], es[:], AF.Exp, [t_es], [t_es])
        P.dma(ml[:], I['k_maskl'][:, :], (), [t_m])
        P.dma(mr[:], I['k_maskr'][:, :], (), [t_m])
        for qb in range(NT):
            kts = [(0, None), (1, None)]
            if qb >= 2:
                if qb - 1 >= 2:
                    kts.append((qb - 1, ml))
                kts.append((qb, None))
                if qb + 1 < NT:
                    kts.append((qb + 1, mr))
            ob = qb % 2
            for kvh in range(2):
                qi = kvh
                P.dma(q4[:, qi, :, :], K.QF[kvh * 256:(kvh + 1) * 256, qb * 128:(qb + 1) * 128].rearrange("(h d) t -> d h t", d=64),
                      (), [t_q[qi]])
                for idx, (kt, mask) in enumerate(kts):
                    ps, tp = P.ps()
                    P.mm(ps[:, :], kf[:, kvh, kt * 128:(kt + 1) * 128], q4[:, qi, :, :].rearrange("d h t -> d (h t)"), True, True,
                         [t_k, t_q[qi]], [tp])
                    P.act(pt[:, idx, :], ps[:, :], AF.Exp, [tp], [t_pt[idx]], scale=0.125)
                    if mask is not None:
                        pv = pt[:, idx, :].rearrange("p (h t) -> p h t", h=4)
                        P.tt(pv, pv, mask[:, :].unsqueeze(1).broadcast_to([128, 4, 128]), ALU.mult, [t_pt[idx], t_m], [t_pt[idx]])
                pso, tpo = P.ps()
                for hh in range(4):
                    for idx, (kt, mask) in enumerate(kts):
                        P.mm(pso[:, hh * 65:(hh + 1) * 65], pt[:, idx, hh * 128:(hh + 1) * 128], v1[:, kt, kvh, :],
                             idx == 0, idx == len(kts) - 1, [t_pt[idx], t_v], [tpo])
                pv = pso[:, 0:260].rearrange("p (h c) -> p h c", h=4)
                P.tt(den[:, kvh, :], pv[:, :, 64], es[:, kvh * 4:(kvh + 1) * 4], ALU.add, [tpo, t_es], [t_d])
                P.recip(den[:, kvh, :], den[:, kvh, :], [t_d], [t_d])
                P.tt(ot[:, ob, kvh * 256:(kvh + 1) * 256].rearrange("p (h d) -> p h d", h=4), pv[:, :, 0:64],
                     den[:, kvh, :].unsqueeze(2).broadcast_to([128, 4, 64]), ALU.mult, [tpo, t_d], [t_o[ob]])
            P.dma(K.MIX[qb * 128:(qb + 1) * 128, 256:768], ot[:, ob, :], [t_o[ob]], [("MIXa", qb)], q='pool')


def round_frac(K, v, r, t_v, t_r, e='dve'):
    P = K.P
    P.ts(r, v, MAGIC, MAGIC, ALU.add, ALU.subtract, [t_v], [t_r], e=e)
    P.tt(v, v, r, ALU.subtract, [t_v, t_r], [t_v], e=e)


def stage_s5(K, l):
    P, nc, I = K.P, K.P.nc, K.I
    K.GS = K.P.dram.get("sGS")
    if K.GS is None:
        K.GS = P.dram_t("sGS", [256, T])
    with SB(nc, "s_bt", [32, 16, 2, 128], F32) as BT, SB(nc, "s_cb", [128, 16, 2, 32], F32) as CB, \
            SB(nc, "s_par", [128, 12, 16], F32) as par, SB(nc, "s_dsk", [32, 8], F32) as dsk, \
            SB(nc, "s_iota", [128, 512], F32) as iot:
        t_BT, t_CB, t_par, t_dsk, t_iota = Tok("BT"), Tok("CB"), Tok("par"), Tok("dsk"), Tok("iota")
        MAG, PHI = 4, 6
        with SB(nc, "s_b", [128, 2, 16, 16], F32) as bri, SB(nc, "s_bb", [128, 2, 16, 16], F32) as bb, \
                SB(nc, "s_bd", [128, 16, 2, 32], F32) as BD, SB(nc, "s_cnd", [32, 16, 2, 128], F32) as CND, \
                SB(nc, "s_t1", [128, 16, 16], F32) as t1, SB(nc, "s_t2", [128, 16, 16], F32) as t2:
            t_b, t_bb, t_BD, t_CND, t_t1, t_t2 = Tok("b"), Tok("bb"), Tok("BD"), Tok("CND"), Tok("t1"), Tok("t2")
            for d in range(2):
                P.dma(par[:, 0, d * 8:(d + 1) * 8], I['s5_lam_re'][l, d].rearrange("g p -> (g p)").rearrange("(gh q) -> q gh", q=128), (), [t_par])
                P.dma(par[:, 1, d * 8:(d + 1) * 8], I['s5_lam_im'][l, d].rearrange("g p -> (g p)").rearrange("(gh q) -> q gh", q=128), (), [t_par])
                ldv = I['s5_log_dt'][l, d].rearrange("(gh gl) -> gl gh", gl=2)
                for gl in range(2):
                    P.dma(par[gl * 64:(gl + 1) * 64, 2, d * 8:(d + 1) * 8], ldv[gl].partition_broadcast(64), (), [t_par])
                for ri, nm in enumerate(('s5_b_re', 's5_b_im')):
                    P.dma(bri[:, ri, d * 8:(d + 1) * 8, :],
                          I[nm][l, d].rearrange("g p c -> (g p c)").rearrange("(gh q c) -> q gh c", q=128, c=16), (), [t_b])
            P.dma(dsk[:], I['s5_d'][l, :].rearrange("(j r) -> r j", r=32), (), [t_dsk])
            P.dma(iot[:], I['k_iota'][0, :].partition_broadcast(128), (), [t_iota])
            pp = lambda i: par[:, i, :]
            tp_ = [t_par]
            P.act(pp(2), pp(2), AF.Exp, tp_, tp_)
            P.tt(pp(3), pp(0), pp(2), ALU.mult, tp_, tp_)
            P.act(pp(MAG), pp(3), AF.Exp, tp_, tp_)
            P.tt(pp(3), pp(1), pp(2), ALU.mult, tp_, tp_)
            P.ts(pp(PHI), pp(3), 1.0 / (2.0 * math.pi), None, ALU.mult, None, tp_, tp_)
            P.copy(pp(5), pp(PHI), tp_, tp_)
            round_frac(K, pp(5), pp(11), t_par, t_par)
            P.act(pp(8), pp(5), AF.Sin, tp_, tp_, scale=TWO_PI)
            P.ts(pp(5), pp(PHI), 0.25, None, ALU.add, None, tp_, tp_)
            round_frac(K, pp(5), pp(11), t_par, t_par)
            P.act(pp(7), pp(5), AF.Sin, tp_, tp_, scale=TWO_PI)
            P.tt(pp(7), pp(7), pp(MAG), ALU.mult, tp_, tp_)
            P.tt(pp(8), pp(8), pp(MAG), ALU.mult, tp_, tp_)
            P.ts(pp(7), pp(7), -1.0, None, ALU.add, None, tp_, tp_)
            P.tt(pp(3), pp(0), pp(0), ALU.mult, tp_, tp_)
            P.tt(pp(5), pp(1), pp(1), ALU.mult, tp_, tp_)
            P.tt(pp(3), pp(3), pp(5), ALU.add, tp_, tp_)
            P.recip(pp(3), pp(3), tp_, tp_)
            P.tt(pp(9), pp(7), pp(0), ALU.mult, tp_, tp_)
            P.tt(pp(5), pp(8), pp(1), ALU.mult, tp_, tp_)
            P.tt(pp(9), pp(9), pp(5), ALU.add, tp_, tp_)
            P.tt(pp(9), pp(9), pp(3), ALU.mult, tp_, tp_)
            P.tt(pp(10), pp(8), pp(0), ALU.mult, tp_, tp_)
            P.tt(pp(5), pp(7), pp(1), ALU.mult, tp_, tp_)
            P.tt(pp(10), pp(10), pp(5), ALU.subtract, tp_, tp_)
            P.tt(pp(10), pp(10), pp(3), ALU.mult, tp_, tp_)
            cre = par[:, 9, :].unsqueeze(2).broadcast_to([128, 16, 16])
            cim = par[:, 10, :].unsqueeze(2).broadcast_to([128, 16, 16])
            P.tt(t1[:], bri[:, 0, :, :], cre, ALU.mult, [t_b, t_par], [t_t1])
            P.tt(t2[:], bri[:, 1, :, :], cim, ALU.mult, [t_b, t_par], [t_t2])
            P.tt(bb[:, 0, :, :], t1[:], t2[:], ALU.subtract, [t_t1, t_t2], [t_bb])
            P.tt(t1[:], bri[:, 1, :, :], cre, ALU.mult, [t_b, t_par], [t_t1])
            P.tt(t2[:], bri[:, 0, :, :], cim, ALU.mult, [t_b, t_par], [t_t2])
            P.tt(bb[:, 1, :, :], t1[:], t2[:], ALU.add, [t_t1, t_t2], [t_bb])
            P.memset(BD[:], 0.0, [t_BD])
            for ri in range(2):
                P.copy(BD[0:64, :, ri, 0:16], bb[0:64, ri, :, :], [t_bb], [t_BD])
                P.copy(BD[64:128, :, ri, 16:32], bb[64:128, ri, :, :], [t_bb], [t_BD])
            for dg in range(16):
                ps, tp = P.ps()
                for ri in range(2):
                    P.tr(ps[0:32, ri * 128:(ri + 1) * 128], BD[:, dg, ri, :], K.ident[:], [t_BD, K.t_ident], [tp])
                P.copy(BT[:, dg, :, :], ps[0:32, 0:256].rearrange("p (r m) -> p r m", r=2), [tp], [t_BT])
            P.memset(CND[:], 0.0, [t_CND])
            for d in range(2):
                for ri, nm in enumerate(('s5_c_re', 's5_c_im')):
                    cv = I[nm][l, d].rearrange("(gh gl) c p -> gl c gh p", gl=2)
                    for gl in range(2):
                        P.dma(CND[gl * 16:(gl + 1) * 16, d * 8:(d + 1) * 8, ri, gl * 64:(gl + 1) * 64], cv[gl], (), [t_CND])
            for dg in range(16):
                ps, tp = P.ps()
                for ri in range(2):
                    P.tr(ps[:, ri * 32:(ri + 1) * 32], CND[:, dg, ri, :], K.ident[0:32, 0:32], [t_CND, K.t_ident], [tp])
                P.copy(CB[:, dg, 0, :], ps[:, 0:32], [tp], [t_CB])
                P.ts(CB[:, dg, 1, :], ps[:, 32:64], -1.0, None, ALU.mult, None, [tp], [t_CB])
        P.barrier()
        with SB(nc, "s_us", [32, T], F32) as us, SB(nc, "s_y", [32, T], F32) as ysb, \
                SB(nc, "s_tab", [128, 2, 4, 512], F32) as tab2, SB(nc, "s_m", [128, 2, 2, 512], F32) as mm2, \
                SB(nc, "s_w", [128, 2, 2, 512], F32) as ww2, SB(nc, "s_s", [128, 2, 2, 512], F32) as ss2, \
                SB(nc, "s_q", [128, 2, 2, 512], F32) as qq2, SB(nc, "s_c0", [128, 2, 4], F32) as c02, \
                SB(nc, "s_car", [128, 2], F32) as car, SB(nc, "s_mag", [128, 512], F32) as magt, SB(nc, "s_g", [32, 2, T], F32) as gg:
            t_us, t_y, t_car, t_g = (Tok(n) for n in ("us", "y", "car", "g"))
            tk2 = [dict(m=Tok("m%d" % b), w=Tok("w%d" % b), s=Tok("s%d" % b), q=Tok("q%d" % b), c0=Tok("c0%d" % b),
                        tab=[Tok("tabs%d" % b), Tok("tabc%d" % b), Tok("vr%d" % b), Tok("vrr%d" % b)]) for b in range(2)]
            chunk_no = [0]
            t_mag = Tok("mag")
            for j in range(8):
                P.dma(us[:], K.UF[32 * j:32 * (j + 1), :], (), [t_us])
                for d in range(2):
                    dg = d * 8 + j
                    if d == 0:
                        chunks = [(a, min(a + 512, T), False, a) for a in range(0, T, 512)]
                    else:
                        chunks = [(0, NCTX, True, 0)]
                        b = T
                        while b > NCTX:
                            a = max(b - 512, NCTX)
                            chunks.append((a, b, True, NCTX + (T - b)))
                            b = a
                    first = True
                    phi = par[:, PHI, dg:dg + 1]
                    P.copy(magt[:, :], par[:, MAG, dg:dg + 1].broadcast_to([128, 512]), [t_par], [t_mag])
                    for (a, b, rev, i0) in chunks:
                        n = b - a
                        pb = chunk_no[0] % 2
                        chunk_no[0] += 1
                        tab, mm_, ww, ss_, qq, c0 = tab2[:, pb], mm2[:, pb], ww2[:, pb], ss2[:, pb], qq2[:, pb], c02[:, pb]
                        t_m, t_w, t_s, t_q, t_c0, t_tab = tk2[pb]['m'], tk2[pb]['w'], tk2[pb]['s'], tk2[pb]['q'], tk2[pb]['c0'], tk2[pb]['tab']
                        rv = (lambda ap: ap[:, ::-1]) if rev else (lambda ap: ap)
                        ps1, tp1 = P.ps()
                        ps2, tp2 = P.ps()
                        P.mm(ps1[:, 0:n], BT[:, dg, 0, :], us[:, a:b], True, True, [t_BT, t_us], [tp1])
                        P.mm(ps2[:, 0:n], BT[:, dg, 1, :], us[:, a:b], True, True, [t_BT, t_us], [tp2])
                        P.ts(c0[:, 0:1], phi, float(i0), None, ALU.mult, None, [t_par], [t_c0])
                        round_frac(K, c0[:, 0:1], c0[:, 2:3], t_c0, t_c0)
                        P.ts(c0[:, 1:2], c0[:, 0:1], 0.25, None, ALU.add, None, [t_c0], [t_c0])
                        for which in range(2):
                            P.act(tab[:, 2 + which, 0:n], iot[:, 0:n], AF.Identity, [t_iota, t_c0, t_par], [t_tab[2 + which]],
                                  bias=c0[:, which:which + 1], scale=phi)
                            P.ts(qq[:, which, 0:n], tab[:, 2 + which, 0:n], MAGIC, MAGIC, ALU.add, ALU.subtract, [t_tab[2 + which]], [t_q])
                            P.tt(tab[:, 2 + which, 0:n], tab[:, 2 + which, 0:n], qq[:, which, 0:n], ALU.subtract, [t_tab[2 + which], t_q],
                                 [t_tab[2 + which]])
                            P.act(tab[:, which, 0:n], tab[:, 2 + which, 0:n], AF.Sin, [t_tab[2 + which]], [t_tab[which]], scale=TWO_PI)
                        sn = rv(tab[:, 0, 0:n])
                        cs = rv(tab[:, 1, 0:n])
                        tS, tC = t_tab[0], t_tab[1]
                        P.tt(mm_[:, 0, 0:n], ps1[:, 0:n], cs, ALU.mult, [tp1, tC], [t_m])
                        P.tt(qq[:, 0, 0:n], ps2[:, 0:n], sn, ALU.mult, [tp2, tS], [t_q])
                        P.tt(mm_[:, 0, 0:n], mm_[:, 0, 0:n], qq[:, 0, 0:n], ALU.add, [t_m, t_q], [t_m])
                        P.tt(mm_[:, 1, 0:n], ps2[:, 0:n], cs, ALU.mult, [tp2, tC], [t_m])
                        P.tt(qq[:, 1, 0:n], ps1[:, 0:n], sn, ALU.mult, [tp1, tS], [t_q])
                        P.tt(mm_[:, 1, 0:n], mm_[:, 1, 0:n], qq[:, 1, 0:n], ALU.subtract, [t_m, t_q], [t_m])
                        magb = magt[:, 0:n]
                        for ri in range(2):
                            init = 0.0 if first else car[:, ri:ri + 1]
                            P.op('dve', lambda g, ri=ri, init=init: g.tensor_tensor_scan(
                                out=rv(ww[:, ri, 0:n]), data0=magb, data1=rv(mm_[:, ri, 0:n]), initial=init,
                                op0=ALU.mult, op1=ALU.add), [t_m, t_mag, t_car], [t_w])
                        last = a if rev else b - 1
                        P.copy(car[:, :], ww[:, :, last - a], [t_w], [t_car])
                        first = False
                        P.tt(ss_[:, 0, 0:n], ww[:, 0, 0:n], cs, ALU.mult, [t_w, tC], [t_s], e='pool')
                        P.tt(qq[:, 0, 0:n], ww[:, 1, 0:n], sn, ALU.mult, [t_w, tS], [t_q], e='pool')
                        P.tt(ss_[:, 0, 0:n], ss_[:, 0, 0:n], qq[:, 0, 0:n], ALU.subtract, [t_s, t_q], [t_s], e='pool')
                        P.tt(ss_[:, 1, 0:n], ww[:, 0, 0:n], sn, ALU.mult, [t_w, tS], [t_s], e='pool')
                        P.tt(qq[:, 1, 0:n], ww[:, 1, 0:n], cs, ALU.mult, [t_w, tC], [t_q], e='pool')
                        P.tt(ss_[:, 1, 0:n], ss_[:, 1, 0:n], qq[:, 1, 0:n], ALU.add, [t_s, t_q], [t_s], e='pool')
                        psy, tpy = P.ps()
                        P.mm(psy[0:32, 0:n], CB[:, dg, 0, :], ss_[:, 0, 0:n], True, False, [t_CB, t_s], [tpy])
                        P.mm(psy[0:32, 0:n], CB[:, dg, 1, :], ss_[:, 1, 0:n], False, True, [t_CB, t_s], [tpy])
                        if d == 0:
                            P.copy(ysb[:, a:b], psy[0:32, 0:n], [tpy], [t_y], e='act')
                        else:
                            P.tt(ysb[:, a:b], ysb[:, a:b], psy[0:32, 0:n], ALU.add, [t_y, tpy], [t_y])
                P.stt(ysb[:], us[:], dsk[:, j:j + 1], ysb[:], ALU.mult, ALU.add, [t_us, t_dsk, t_y], [t_y])
                P.tt(gg[:, 0, :], ysb[:], ysb[:], ALU.mult, [t_y], [t_g])
                P.ts(gg[:, 0, :], gg[:, 0, :], 0.044715, 1.0, ALU.mult, ALU.add, [t_g], [t_g])
                P.tt(gg[:, 0, :], gg[:, 0, :], ysb[:], ALU.mult, [t_g, t_y], [t_g])
                P.act(gg[:, 1, :], gg[:, 0, :], AF.Sigmoid, [t_g], [t_g], scale=1.5957691216)
                P.tt(gg[:, 1, :], gg[:, 1, :], ysb[:], ALU.mult, [t_g, t_y], [t_g])
                P.dma(K.GS[32 * j:32 * (j + 1), :], gg[:, 1, :], [t_g], [("GS", j)], q='pool')
        P.barrier()
        with SB(nc, "s_gw", [128, 2, 256], F32) as gw, SB(nc, "s_gb", [128, 2], F32) as gb, \
                SB(nc, "s_gt", [128, 2, 512], F32) as gt, SB(nc, "s_sg", [128, 2, 512], F32) as sg, \
                SB(nc, "s_o", [128, 4, 256], F32) as so:
            t_gw, t_gt, t_sg, t_so = Tok("gw"), Tok("gt"), Tok("sg"), Tok("so")
            P.dma(gw[:], I['s5_glu_w'][l].rearrange("(c p) n -> p c n", p=128), (), [t_gw])
            P.dma(gb[:], I['s5_glu_b'][l, :].rearrange("(c p) -> p c", p=128), (), [t_gw])
            for g in range((T + 511) // 512):
                t0 = g * 512
                n = min(512, T - t0)
                P.dma(gt[:, :, 0:n], K.GS[:, t0:t0 + n].rearrange("(c p) t -> p c t", p=128), (), [t_gt])
                for oc in range(2):
                    ps, tp = P.ps()
                    for kc in range(2):
                        P.mm(ps[:, 0:n], gw[:, kc, oc * 128:(oc + 1) * 128], gt[:, kc, 0:n], kc == 0, kc == 1, [t_gw, t_gt], [tp])
                    P.act(sg[:, oc, 0:n], ps[:, 0:n], AF.Sigmoid, [tp, t_gw], [t_sg], bias=gb[:, oc:oc + 1], scale=1.0)
                    P.tt(sg[:, oc, 0:n], sg[:, oc, 0:n], gt[:, oc, 0:n], ALU.mult, [t_sg, t_gt], [t_sg])
                for i in range(n // 128):
                    ps, tp = P.ps()
                    for oc in range(2):
                        P.tr(ps[:, oc * 128:(oc + 1) * 128], sg[:, oc, i * 128:(i + 1) * 128], K.ident[:], [t_sg, K.t_ident], [tp])
                    P.copy(so[:, i, :], ps[:, 0:256], [tp], [t_so], e='act')
                P.dma(K.MIX[t0:t0 + n, 768:1024].rearrange("(i p) c -> p i c", p=128), so[:, 0:n // 128, :], [t_so], [("MIXs", g)], q='pool')


def stage_outproj(K, l):
    P, nc, I = K.P, K.P.nc, K.I
    with SB(nc, "o_w", [128, 8, D], F32) as W, SB(nc, "o_rw", [128, 8, 16], F32) as RW, \
            SB(nc, "o_gain", [128, D], F32) as gain, SB(nc, "o_mod", [128, 6, D], F32) as mod, \
            SB(nc, "o_rb", [128, 16], F32) as rb, SB(nc, "o_mix", [128, D], F32) as mix, \
            SB(nc, "o_x", [128, D], F32) as xt, SB(nc, "o_junk", [128, D], F32) as junk, \
            SB(nc, "o_mT", [128, 8, 128], F32) as mT, SB(nc, "o_h2", [128, D], F32) as h2, \
            SB(nc, "o_hT", [128, 8, 128], F32) as hT, SB(nc, "o_tmp", [128, D], F32) as tmp, \
            SB(nc, "o_st", [128, 8], F32) as st, SB(nc, "o_r", [128, 12, 16], F32) as rr, \
            SB(nc, "o_gT", [16, 128], F32) as gT:
        t_W, t_c, t_mix, t_x, t_mT, t_h2, t_hT, t_tmp, t_st, t_rr, t_gT = (Tok(n) for n in (
            "W", "c", "mix", "x", "mT", "h2", "hT", "tmp", "st", "rr", "gT"))
        for k in range(8):
            P.load_r(W[:, k, :], I['w_out'][l, k * 128:(k + 1) * 128, :], junk[:], t_W, t_tmp, e='act' if k % 2 else 'dve')
        P.dma(RW[:], I['router_w'].rearrange("(k p) n -> p k n", p=128), (), [t_c])
        P.dma(gain[:], I['mix_norm_g'][l, :].partition_broadcast(128), (), [t_c])
        P.dma(rb[:], I['router_b'][0, :].partition_broadcast(128), (), [t_c])
        for jj, r in enumerate((2, 3, 4, 8, 9, 10)):
            P.dma(mod[:, jj, :], K.MOD[r, :].partition_broadcast(128), (), [t_c])
        groups = ((0, 256), (256, 768), (768, 1024))
        for ti in range(NT):
            mb = 3 if ti < 2 else 0
            rows = slice(ti * 128, (ti + 1) * 128)
            P.dma(mix[:], K.MIX[rows, :], (), [t_mix])
            P.dma(xt[:], K.X[rows, :], (), [t_x])
            for gi, (c0, c1) in enumerate(groups):
                rms_rstd(K, mix[:, c0:c1], t_mix, c1 - c0, junk[:, c0:c1], st[:, gi:gi + 1], t_st)
            for gi, (c0, c1) in enumerate(groups):
                P.stt(mix[:, c0:c1], mix[:, c0:c1], st[:, gi:gi + 1], gain[:, c0:c1], ALU.mult, ALU.mult, [t_mix, t_st, t_c], [t_mix])
            for half in range(2):
                ps, tp = P.ps()
                for j in range(4):
                    kk = half * 4 + j
                    P.tr(ps[:, j * 128:(j + 1) * 128], mix[:, kk * 128:(kk + 1) * 128], K.ident[:], [t_mix, K.t_ident], [tp])
                P.copy(RR(mT[:, half * 4:(half + 1) * 4, :]), ps[:, :].rearrange("p (j t) -> p j t", j=4), [tp], [t_mT], e='act' if half else 'dve')
            for half in range(2):
                ps, tp = P.ps()
                for k in range(8):
                    P.mm(ps[:, :], mT[:, k, :], W[:, k, half * 512:(half + 1) * 512], k == 0, k == 7, [t_mT, t_W], [tp], r=FAST)
                hs = slice(half * 512, (half + 1) * 512)
                P.tt(tmp[:, hs], ps[:, :], mod[:, mb + 0, hs], ALU.mult, [tp, t_c], [t_tmp])
                P.tt(xt[:, hs], xt[:, hs], tmp[:, hs], ALU.add, [t_x, t_tmp], [t_x])
            P.dma(K.X[rows, :], xt[:], [t_x], [("X", ti)], q='pool')
            rms_rstd(K, xt[:], t_x, D, junk[:], st[:, 3:4], t_st)
            P.stt(h2[:], xt[:], st[:, 3:4], mod[:, mb + 1, :], ALU.mult, ALU.mult, [t_x, t_st, t_c], [t_h2])
            P.tt(h2[:], h2[:], mod[:, mb + 2, :], ALU.add, [t_h2, t_c], [t_h2])
            for half in range(2):
                ps, tp = P.ps()
                for j in range(4):
                    kk = half * 4 + j
                    P.tr(ps[:, j * 128:(j + 1) * 128], h2[:, kk * 128:(kk + 1) * 128], K.ident[:], [t_h2, K.t_ident], [tp])
                P.copy(hT[:, half * 4:(half + 1) * 4, :], ps[:, :].rearrange("p (j t) -> p j t", j=4), [tp], [t_hT], e='act' if half else 'dve')
            P.dma(K.H2T[:, ti * 128:(ti + 1) * 128].rearrange("(k p) t -> p k t", p=128), hT[:], [t_hT], [("H2T", ti)], q='pool')
            ps, tp = P.ps()
            for k in range(8):
                P.mm(ps[:, 0:16], hT[:, k, :], RW[:, k, :], k == 0, k == 7, [t_hT, t_c], [tp])
            R = lambda i: rr[:, i, :]
            R4 = lambda i: rr[:, i, :].rearrange("p (g e) -> p g e", g=4)
            trr = [t_rr]
            P.op('dve', lambda g: g.reduce_max(out=st[:, 4:5], in_=ps[:, 0:16], axis=AX.X), [tp], [t_st])
            P.ts(st[:, 4:5], st[:, 4:5], -1.0, None, ALU.mult, None, [t_st], [t_st])
            P.act(R(0), ps[:, 0:16], AF.Exp, [tp, t_st], trr, bias=st[:, 4:5], scale=1.0, accum_out=st[:, 5:6])
            P.recip(st[:, 5:6], st[:, 5:6], [t_st, t_rr], [t_st])
            P.ts(R(0), R(0), st[:, 5:6], None, ALU.mult, None, trr + [t_st], trr)
            P.tt(R(1), R(0), rb[:], ALU.add, trr + [t_c], trr)
            P.op('dve', lambda g: g.reduce_max(out=rr[:, 2, 0:4], in_=R4(1), axis=AX.X), trr, trr)
            P.tt(R4(3), R4(1), rr[:, 2, 0:4].unsqueeze(2).broadcast_to([128, 4, 4]), ALU.is_equal, trr, trr)
            P.stt(R(4), R(3), -1e9, R(1), ALU.mult, ALU.add, trr, trr)
            P.op('dve', lambda g: g.reduce_max(out=rr[:, 5, 0:4], in_=R4(4), axis=AX.X), trr, trr)
            P.tt(rr[:, 6, 0:4], rr[:, 2, 0:4], rr[:, 5, 0:4], ALU.add, trr, trr)
            P.op('dve', lambda g: g.reduce_max(out=st[:, 6:7], in_=rr[:, 6, 0:4], axis=AX.X), trr, [t_st])
            P.ts(rr[:, 7, 0:4], rr[:, 6, 0:4], st[:, 6:7], None, ALU.is_equal, None, trr + [t_st], trr)
            P.tt(R4(8), R4(1), rr[:, 5, 0:4].unsqueeze(2).broadcast_to([128, 4, 4]), ALU.is_ge, trr, trr)
            P.tt(R4(8), R4(8), rr[:, 7, 0:4].unsqueeze(2).broadcast_to([128, 4, 4]), ALU.mult, trr, trr)
            P.tt(R(9), R(8), R(0), ALU.mult, trr, trr)
            P.op('dve', lambda g: g.reduce_sum(out=st[:, 7:8], in_=R(9), axis=AX.X), trr, [t_st])
            P.recip(st[:, 7:8], st[:, 7:8], [t_st], [t_st])
            P.ts(R(9), R(9), st[:, 7:8], None, ALU.mult, None, trr + [t_st], trr)
            ps2, tp2 = P.ps()
            P.tr(ps2[0:16, 0:128], R(9), K.ident[:], trr + [K.t_ident], [tp2])
            P.copy(gT[:], ps2[0:16, 0:128], [tp2], [t_gT], e='act')
            P.dma(K.GTT[:, ti * 128:(ti + 1) * 128], gT[:], [t_gT], [("GTT", ti)], q='pool')


def stage_moe(K, l):
    P, nc, I = K.P, K.P.nc, K.I
    GN = 1024
    with SB(nc, "e_h", [128, 8, GN], F32) as hT, SB(nc, "e_g", [128, 8, GN], F32) as GT, \
            SB(nc, "e_y", [128, GN // 128, D], F32) as Y, SB(nc, "e_wd", [128, 8, D], F32) as wd, \
            SB(nc, "e_wg", [128, 2, 8, 128], F32) as wg, SB(nc, "e_wu", [128, 2, 8, 128], F32) as wu, \
            SB(nc, "e_gt", [16, GN], F32) as gt, SB(nc, "e_gb", [128, GN], F32) as gB, \
            SB(nc, "e_sel", [16, 16, 128], F32) as sel, SB(nc, "e_sa", [128, 2, 512], F32) as sA, \
            SB(nc, "e_mod", [128, 2, D], F32) as mod, SB(nc, "e_x", [128, D], F32) as xt, \
            SB(nc, "e_wst", [128, 6, D], F32) as wst:
        t_wst = [Tok("wst%d" % i) for i in range(6)]
        t_h, t_G, t_Y, t_wd, t_gt, t_gB, t_sel, t_mod, t_x = (Tok(n) for n in ("h", "G", "Y", "wd", "gt", "gB", "sel", "mod", "x"))
        t_wg = [Tok("wg0"), Tok("wg1")]
        t_wu = [Tok("wu0"), Tok("wu1")]
        t_sa = [Tok("sa0"), Tok("sa1")]
        for e in range(16):
            P.copy(sel[:, e, :], K.ident[0:16, e:e + 1].broadcast_to([16, 128]), [K.t_ident], [t_sel])
        P.dma(mod[:, 0, :], K.MOD[5, :].partition_broadcast(128), (), [t_mod])
        P.dma(mod[:, 1, :], K.MOD[11, :].partition_broadcast(128), (), [t_mod])
        for g in range((T + GN - 1) // GN):
            t0 = g * GN
            n = min(GN, T - t0)
            nt = n // 128
            for i in range(nt):
                P.load_r(hT[:, :, i * 128:(i + 1) * 128], K.H2T[:, t0 + i * 128:t0 + (i + 1) * 128].rearrange("(k p) t -> p k t", p=128),
                         wst[:, i % 2, :].rearrange("p (k t) -> p k t", k=8), t_h, t_wst[i % 2], e='act' if i % 2 else 'dve')
            P.dma(gt[:, 0:n], K.GTT[:, t0:t0 + n], (), [t_gt])
            P.memset(Y[:, 0:nt, :], 0.0, [t_Y], e='pool')
            for e in range(16):
                for c in range(8):
                    P.load_r(wd[:, c, :], I['moe_w_down'][l, e, c * 128:(c + 1) * 128, :], wst[:, 2 + c % 2, :], t_wd, t_wst[2 + c % 2], e='dve')
                for s0 in range(0, n, 512):
                    sn = min(512, n - s0)
                    ps, tp = P.ps()
                    P.mm(ps[:, 0:sn], sel[:, e, :], gt[:, s0:s0 + sn], True, True, [t_sel, t_gt], [tp])
                    P.copy(gB[:, s0:s0 + sn], ps[:, 0:sn], [tp], [t_gB], e='act')
                wgv = I['moe_w_gate'][l, e].rearrange("(k p) n -> p k n", p=128)
                wuv = I['moe_w_up'][l, e].rearrange("(k p) n -> p k n", p=128)
                for c in range(8):
                    b = c % 2
                    P.load_r(wg[:, b, :, :], wgv[:, :, c * 128:(c + 1) * 128], wst[:, 4, :].rearrange("p (k t) -> p k t", k=8), t_wg[b], t_wst[4], e='act')
                    P.load_r(wu[:, b, :, :], wuv[:, :, c * 128:(c + 1) * 128], wst[:, 5, :].rearrange("p (k t) -> p k t", k=8), t_wu[b], t_wst[5], e='act')
                    for si, s0 in enumerate(range(0, n, 512)):
                        sn = min(512, n - s0)
                        psa, tpa = P.ps()
                        psu, tpu = P.ps()
                        for k in range(8):
                            P.mm(psa[:, 0:sn], wg[:, b, k, :], hT[:, k, s0:s0 + sn], k == 0, k == 7, [t_wg[b], t_h], [tpa], r=FAST)
                        for k in range(8):
                            P.mm(psu[:, 0:sn], wu[:, b, k, :], hT[:, k, s0:s0 + sn], k == 0, k == 7, [t_wu[b], t_h], [tpu], r=FAST)
                        sb_ = si % 2
                        P.act(sA[:, sb_, 0:sn], psa[:, 0:sn], AF.Silu, [tpa], [t_sa[sb_]])
                        P.tt(sA[:, sb_, 0:sn], sA[:, sb_, 0:sn], psu[:, 0:sn], ALU.mult, [t_sa[sb_], tpu], [t_sa[sb_]])
                        P.tt(RR(GT[:, c, s0:s0 + sn]), sA[:, sb_, 0:sn], gB[:, s0:s0 + sn], ALU.mult, [t_sa[sb_], t_gB], [t_G])
                for i in range(nt):
                    for half in range(2):
                        ps, tp = P.ps()
                        for c in range(8):
                            P.mm(ps[:, :], GT[:, c, i * 128:(i + 1) * 128], wd[:, c, half * 512:(half + 1) * 512], c == 0, c == 7, [t_G, t_wd], [tp], r=FAST)
                        hs = slice(half * 512, (half + 1) * 512)
                        P.tt(Y[:, i, hs], Y[:, i, hs], ps[:, :], ALU.add, [t_Y, tp], [t_Y])
            for i in range(nt):
                ti = g * (GN // 128) + i
                mb = 1 if ti < 2 else 0
                rows = slice(ti * 128, (ti + 1) * 128)
                P.dma(xt[:], K.X[rows, :], (), [t_x])
                P.tt(Y[:, i, :], Y[:, i, :], mod[:, mb, :], ALU.mult, [t_Y, t_mod], [t_Y], e='pool')
                P.tt(xt[:], xt[:], Y[:, i, :], ALU.add, [t_x, t_Y], [t_x])
                P.dma(K.X[rows, :], xt[:], [t_x], [("X", ti)], q='pool')


def stage_final(K):
    P, nc, I = K.P, K.P.nc, K.I
    with SB(nc, "f_g", [128, D], F32) as gbc, SB(nc, "f_x", [128, 2, D], F32) as xt, \
            SB(nc, "f_j", [128, D], F32) as junk, SB(nc, "f_s", [128, 2], F32) as st:
        t_g = Tok("g")
        t_x = [Tok("x0"), Tok("x1")]
        t_s = [Tok("s0"), Tok("s1")]
        P.dma(gbc[:], I['final_g'][0, :].partition_broadcast(128), (), [t_g])
        for i in range(NLAT // 128):
            b = i % 2
            P.dma(xt[:, b, :], K.X[NCTX + i * 128:NCTX + (i + 1) * 128, :], (), [t_x[b]])
            rms_rstd(K, xt[:, b, :], t_x[b], D, junk[:], st[:, b:b + 1], t_s[b])
            P.stt(xt[:, b, :], xt[:, b, :], st[:, b:b + 1], gbc[:], ALU.mult, ALU.mult, [t_x[b], t_s[b], t_g], [t_x[b]])
            P.dma(K.out[i * 128:(i + 1) * 128, :], xt[:, b, :], [t_x[b]], [("out", i)], q='pool')


_CONSTS = None


def consts():
    global _CONSTS
    if _CONSTS is not None:
        return _CONSTS
    c = {}
    c['k_ident'] = np.eye(128, dtype=np.float32)
    rc, rs = rope_tables()
    c['k_ropec'], c['k_ropes'] = rc, rs
    j = np.arange(128)[:, None]
    i = np.arange(128)[None, :]
    c['k_maskl'] = (j >= i).astype(np.float32)
    c['k_maskr'] = (j <= i).astype(np.float32)
    c['k_ccL'], c['k_ssL'] = dft_blocks(NLAT, 33)
    c['k_ccC'], c['k_ssC'] = dft_blocks(NCTX, 3)
    f, d, w = hyena_consts(NLAT)
    c['k_featL'], c['k_decL'], c['k_wkL'] = f, d, w.reshape(-1, 1)
    f, d, w = hyena_consts(NCTX)
    c['k_featC'], c['k_decC'], c['k_wkC'] = f, d, w.reshape(-1, 1)
    c['k_iota'] = np.arange(512, dtype=np.float32).reshape(1, 512)
    _CONSTS = c
    return c


def make_in_maps(inputs, cores):
    cs = consts()
    shared = {}
    for k, v in inputs.items():
        if k in ('x', 'c', 'ctx', 'c_ctx'):
            continue
        a = np.ascontiguousarray(np.asarray(v, dtype=np.float32))
        if k in ('router_b', 'final_g'):
            a = a.reshape(1, -1)
        shared[k] = a
    shared.update(cs)
    maps = []
    for b in cores:
        m = dict(shared)
        m['x'] = np.ascontiguousarray(inputs['x'][b], dtype=np.float32)
        m['ctx'] = np.ascontiguousarray(inputs['ctx'][b], dtype=np.float32)
        m['c'] = np.ascontiguousarray(inputs['c'][b], dtype=np.float32).reshape(1, D)
        m['c_ctx'] = np.ascontiguousarray(inputs['c_ctx'], dtype=np.float32).reshape(1, D)
        maps.append(m)
    return maps


def kernel(**inputs):
    P = build()
    maps = make_in_maps(inputs, list(range(8)))
    res = run_bass_kernel_spmd(P.nc, maps, core_ids=list(range(8)))
    return np.stack([r["out"] for r in res.results], axis=0).astype(np.float32)
```

```python
import math
from contextlib import ExitStack
import numpy as np
import concourse.bass as bass
import concourse.mybir as mybir
from concourse.bass_utils import run_bass_kernel_spmd

F32 = mybir.dt.float32
F32R = mybir.dt.float32r
FAST = True
AF = mybir.ActivationFunctionType
ALU = mybir.AluOpType
AX = mybir.AxisListType

D = 1024
NLAT = 4096
NCTX = 256
T = NLAT + NCTX
NT = T // 128
DEPTH = 4
HY_W = 256
ATT_W = 512
S5_W = 256
HY_END = 768
Q_END = HY_END + ATT_W
K_END = Q_END + 128
V_END = K_END + 128
IN_W = V_END + S5_W
EPS = 1e-6
MAGIC = 12582912.0
TWO_PI = 6.283185
NE = 16


class Tok:
    def __init__(self, name):
        self.name = name

    def __repr__(self):
        return self.name


class Prog:
    NDS = 24

    def __init__(self):
        nc = bass.Bass("TRN2", target_bir_lowering=False)
        self.nc = nc
        self.eng = {'pe': nc.tensor, 'dve': nc.vector, 'act': nc.scalar, 'pool': nc.gpsimd, 'sp': nc.sync}
        self.esem = {k: nc.alloc_semaphore("es_" + k) for k in self.eng}
        self.ecnt = {k: 0 for k in self.eng}
        self.dsem = [nc.alloc_semaphore("ds%d" % i) for i in range(self.NDS)]
        self.dval = [0] * self.NDS
        self.dnext = 0
        self.know = {k: {} for k in self.eng}
        self.last_w = {}
        self.readers = {}
        self.n_inst = 0
        self.psum = [nc.alloc_psum_tensor("psb%d" % i, [128, 512], F32) for i in range(8)]
        self.ps_tok = [Tok("ps%d" % i) for i in range(8)]
        self.ps_next = 0
        self.ps_held = set()
        self.dram = {}

    def _sem_of(self, key):
        return self.esem[key] if isinstance(key, str) else self.dsem[key]

    def _deps(self, reads, writes):
        deps = {}

        def add(kv):
            k, v = kv
            if deps.get(k, 0) < v:
                deps[k] = v
        for t in reads:
            if t in self.last_w:
                add(self.last_w[t])
        for t in writes:
            if t in self.last_w:
                add(self.last_w[t])
            for kv in self.readers.get(t, {}).items():
                add(kv)
        return deps

    def _wait(self, e, deps):
        kn = self.know[e]
        for k, v in deps.items():
            if k == e and e == 'pe':
                continue
            if kn.get(k, 0) >= v:
                continue
            self.eng[e].wait_ge(self._sem_of(k), v)
            kn[k] = v
            self.n_inst += 1

    def _commit(self, me, reads, writes):
        for t in writes:
            self.last_w[t] = me
            self.readers[t] = {}
        for t in reads:
            r = self.readers.setdefault(t, {})
            if r.get(me[0], 0) < me[1]:
                r[me[0]] = me[1]

    def op(self, e, fn, reads=(), writes=()):
        self._wait(e, self._deps(reads, writes))
        inst = fn(self.eng[e])
        inst.then_inc(self.esem[e], 1)
        self.ecnt[e] += 1
        self.n_inst += 1
        self._commit((e, self.ecnt[e]), reads, writes)

    def dma(self, out, in_, reads=(), writes=(), q='sp'):
        self._wait(q, self._deps(reads, writes))
        s = self.dnext
        self.dnext = (self.dnext + 1) % self.NDS
        if self.know[q].get(s, 0) < self.dval[s]:
            self.eng[q].wait_ge(self.dsem[s], self.dval[s])
            self.know[q][s] = self.dval[s]
        self.eng[q].dma_start(out=out, in_=in_, allow_slow_non_contiguous=True).then_inc(self.dsem[s], 16)
        self.dval[s] += 16
        self.n_inst += 1
        self._commit((s, self.dval[s]), reads, writes)

    def barrier(self):
        for e in self.eng:
            deps = {k: self.ecnt[k] for k in self.eng if self.ecnt[k] > 0}
            for s in range(self.NDS):
                if self.dval[s] > 0:
                    deps[s] = self.dval[s]
            self._wait(e, deps)
        self.last_w = {}
        self.readers = {}

    def finish(self):
        deps = {s: self.dval[s] for s in range(self.NDS) if self.dval[s] > 0}
        for k in self.eng:
            if self.ecnt[k] > 0:
                deps[k] = self.ecnt[k]
        self._wait('sp', deps)

    def ps(self):
        while True:
            i = self.ps_next
            self.ps_next = (i + 1) % 8
            if i not in self.ps_held:
                return self.psum[i], self.ps_tok[i]

    def ps_hold(self, n):
        got = []
        for i in range(8):
            if i not in self.ps_held and len(got) < n:
                self.ps_held.add(i)
                got.append(i)
        return [(self.psum[i], self.ps_tok[i]) for i in got], got

    def ps_release(self, got):
        for i in got:
            self.ps_held.discard(i)

    def mm(self, out, lhsT, rhs, start, stop, reads, writes, r=False):
        if r:
            lhsT = lhsT.bitcast(F32R)
            rhs = rhs.bitcast(F32R)
        self.op('pe', lambda e: e.matmul(out, lhsT, rhs, start=start, stop=stop), reads, writes)

    def tr(self, out, in_, ident, reads, writes):
        self.op('pe', lambda e: e.transpose(out=out, in_=in_, identity=ident), reads, writes)

    def act(self, out, in_, func, reads, writes, **kw):
        self.op('act', lambda e: e.activation(out=out, in_=in_, func=func, **kw), reads, writes)

    def tt(self, out, in0, in1, op, reads, writes, e='dve'):
        self.op(e, lambda g: g.tensor_tensor(out=out, in0=in0, in1=in1, op=op), reads, writes)

    def ts(self, out, in0, s1, s2, op0, op1, reads, writes, e='dve'):
        if op1 is None:
            self.op(e, lambda g: g.tensor_scalar(out=out, in0=in0, scalar1=s1, scalar2=None, op0=op0), reads, writes)
        else:
            self.op(e, lambda g: g.tensor_scalar(out=out, in0=in0, scalar1=s1, scalar2=s2, op0=op0, op1=op1), reads, writes)

    def stt(self, out, in0, scalar, in1, op0, op1, reads, writes):
        self.op('dve', lambda g: g.scalar_tensor_tensor(out=out, in0=in0, scalar=scalar, in1=in1, op0=op0, op1=op1), reads, writes)

    def copy(self, out, in_, reads, writes, e='dve'):
        if e == 'act':
            self.op('act', lambda g: g.copy(out=out, in_=in_), reads, writes)
        else:
            self.op(e, lambda g: g.tensor_copy(out=out, in_=in_), reads, writes)

    def load_r(self, dst, src, stage, t_dst, t_stage, e='dve', q='sp'):
        if not FAST:
            self.dma(dst, src, (), [t_dst])
            return
        self.dma(stage, src, (), [t_stage], q=q)
        self.copy(dst.bitcast(F32R), stage, [t_stage], [t_dst], e=e)

    def rnd(self, ap, tok, e='dve'):
        if FAST:
            self.copy(ap.bitcast(F32R), ap, [tok], [tok], e=e)

    def memset(self, ap, val, writes, e='dve'):
        self.op(e, lambda g: g.memset(ap, val), (), writes)

    def recip(self, out, in_, reads, writes):
        self.op('dve', lambda g: g.reciprocal(out=out, in_=in_), reads, writes)

    def dram_t(self, name, shape, kind="Internal"):
        t = self.nc.dram_tensor(name, list(shape), F32, kind=kind).ap()
        self.dram[name] = t
        return t


def RR(ap):
    return ap.bitcast(F32R) if FAST else ap


class Ctx:
    def dump(self, name, tile_ap, shape, reads):
        if 'dump' not in self.dbg:
            return
        o = self.P.nc.dram_tensor("dmp_" + name, list(shape), F32, kind="ExternalOutput").ap()
        self.P.dma(o, tile_ap, reads, (), q='sp')


_UID = [0]


def SB(nc, name, shape, dt):
    _UID[0] += 1
    return nc.sbuf_tensor("%s_%d" % (name, _UID[0]), shape, dt)


def rope_tables():
    cosT = np.ones((128, T), np.float64)
    sinT = np.zeros((128, T), np.float64)
    n = np.arange(NLAT)
    row = n // 64
    col = n % 64
    for r in range(128):
        d = r % 64
        dd = d if d < 32 else d - 32
        pos = row if d < 32 else col
        j = dd % 16
        first = dd < 16
        inv = 10000.0 ** (-(j / 16.0))
        ang = (pos.astype(np.float32) * np.float32(inv)).astype(np.float64)
        cosT[r, NCTX:] = np.cos(ang)
        sinT[r, NCTX:] = (-np.sin(ang)) if first else np.sin(ang)
    return cosT.astype(np.float32), sinT.astype(np.float32)


def perm_cols(width):
    src = np.zeros(width, np.int64)
    for c in range(width):
        d = c % 64
        dd = d % 32
        src[c] = c + 16 if dd < 16 else c - 16
    return src


def dft_blocks(nhalf, nchunk):
    idx = np.arange(nchunk * 128)
    valid = (idx <= nhalf)
    prod = np.outer(idx, idx).astype(np.float64)
    ang = 2.0 * np.pi * (prod % (2 * nhalf)) / (2 * nhalf)
    m = np.outer(valid, valid)
    cc = (np.cos(ang) * m).astype(np.float32)
    ss = (np.sin(ang) * m).astype(np.float32)

    def blk(mat):
        return np.ascontiguousarray(mat.reshape(nchunk, 128, nchunk, 128).transpose(2, 1, 0, 3))
    return blk(cc), blk(ss)


def hyena_consts(L):
    pos = np.arange(L, dtype=np.float32)
    t = pos / np.float32(max(L - 1, 1))
    bands = np.linspace(1e-4, 15, 16, dtype=np.float32)
    ang = (np.float32(2.0 * math.pi / L) * pos[:, None] * bands[None, :]).astype(np.float32)
    feats = np.concatenate([t[:, None], np.cos(ang), -np.sin(ang)], axis=-1).astype(np.float32)
    dmin = math.log(1e-2) / 1.5
    dmax = math.log(1e-2) / 0.3
    decay = np.abs(np.linspace(dmin, dmax, HY_W, dtype=np.float32))
    dec = np.exp(-t[:, None] * decay[None, :]).astype(np.float32)
    nk = L // 128 + 1
    wk = np.zeros(nk * 128, np.float32)
    wk[:L + 1] = 2.0 / (2 * L)
    wk[0] = 1.0 / (2 * L)
    wk[L] = 1.0 / (2 * L)
    return np.ascontiguousarray(feats.T), dec, wk


def build(n_layers=DEPTH, dbg=()):
    P = Prog()
    nc = P.nc
    K = Ctx()
    K.P = P
    K.dbg = dbg
    K.dbg_out = {}

    def inp(name, shape):
        return nc.dram_tensor(name, list(shape), F32, kind="ExternalInput").ap()

    I = {}
    I['x'] = inp('x', [NLAT, D])
    I['ctx'] = inp('ctx', [NCTX, D])
    I['c'] = inp('c', [1, D])
    I['c_ctx'] = inp('c_ctx', [1, D])
    shapes = dict(
        norm1_g=[4, D], norm2_g=[4, D], ada_w=[4, D, 6 * D], ada_b=[4, 6 * D], w_in=[4, D, IN_W], w_out=[4, D, D],
        mix_norm_g=[4, D], hy_conv_w=[4, 3, 768], hy_conv_b=[4, 768], hy_f_w1=[4, 33, 64], hy_f_b1=[4, 64],
        hy_f_freq1=[4, 64], hy_f_w2=[4, 64, 64], hy_f_b2=[4, 64], hy_f_freq2=[4, 64], hy_f_w3=[4, 64, 1024],
        hy_bias=[4, 2, 256], attn_sink=[4, 8], s5_lam_re=[4, 2, 16, 64], s5_lam_im=[4, 2, 16, 64], s5_log_dt=[4, 2, 16],
        s5_b_re=[4, 2, 16, 64, 16], s5_b_im=[4, 2, 16, 64, 16], s5_c_re=[4, 2, 16, 16, 64], s5_c_im=[4, 2, 16, 16, 64],
        s5_d=[4, 256], s5_glu_w=[4, 256, 256], s5_glu_b=[4, 256], router_w=[D, 16], router_b=[1, 16],
        moe_w_gate=[4, 16, D, D], moe_w_up=[4, 16, D, D], moe_w_down=[4, 16, D, D], final_g=[1, D],
        k_ident=[128, 128], k_ropec=[128, T], k_ropes=[128, T], k_maskl=[128, 128], k_maskr=[128, 128],
        k_ccL=[33, 128, 33, 128], k_ssL=[33, 128, 33, 128], k_ccC=[3, 128, 3, 128], k_ssC=[3, 128, 3, 128],
        k_featL=[33, NLAT], k_decL=[NLAT, 256], k_wkL=[33 * 128, 1], k_featC=[33, NCTX], k_decC=[NCTX, 256], k_wkC=[3 * 128, 1],
        k_iota=[1, 512],
    )
    for k, s in shapes.items():
        I[k] = inp(k, s)
    K.I = I
    out = nc.dram_tensor("out", [NLAT, D], F32, kind="ExternalOutput").ap()
    K.out = out

    K.X = P.dram_t("sX", [T, D])
    K.ZT = P.dram_t("sZT", [T, 768])
    K.QF = P.dram_t("sQF", [512, T])
    K.KF = P.dram_t("sKF", [128, T])
    K.VT = P.dram_t("sVT", [T, 128])
    K.UF = P.dram_t("sUF", [256, T])
    K.MIX = P.dram_t("sMIX", [T, D])
    K.H2T = P.dram_t("sH2T", [D, T])
    K.GTT = P.dram_t("sGTT", [16, T])

    K.ident = nc.alloc_sbuf_tensor("ident", [128, 128], F32)
    K.ones = nc.alloc_sbuf_tensor("ones", [128, 128], F32)
    K.MOD = P.dram_t("sMOD", [12, D])
    K.ZC = P.dram_t("sZC", [T, 768])
    K.KRE = P.dram_t("sKRE", [33 * 128, 512])
    K.KIM = P.dram_t("sKIM", [33 * 128, 512])
    K.t_ident = Tok("ident")
    K.t_ones = Tok("ones")
    K.t_mod = Tok("modbc")
    P.dma(K.ident[:], I['k_ident'][:, :], (), [K.t_ident])
    P.memset(K.ones[:], 1.0, [K.t_ones])

    tX = [("X", i) for i in range(NT)]
    K.tX = tX
    P.dma(K.X[0:NCTX, :], I['ctx'][:, :], (), tX[0:2])
    for i in range(4):
        P.dma(K.X[NCTX + i * 1024: NCTX + (i + 1) * 1024, :], I['x'][i * 1024:(i + 1) * 1024, :], (), tX[2 + 8 * i: 2 + 8 * (i + 1)])
    P.barrier()

    for l in range(n_layers):
        stage_mod(K, l)
        P.barrier()
        stage_inproj(K, l)
        P.barrier()
        if 'inproj' in dbg and l == 0:
            break
        stage_hyena(K, l, NLAT, NCTX, 'L')
        P.barrier()
        stage_hyena(K, l, NCTX, 0, 'C')
        P.barrier()
        if 'hyena' in dbg and l == 0:
            break
        stage_attn(K, l)
        P.barrier()
        if 'attn' in dbg and l == 0:
            break
        stage_s5(K, l)
        P.barrier()
        if 's5' in dbg and l == 0:
            break
        stage_outproj(K, l)
        P.barrier()
        if 'outproj' in dbg and l == 0:
            break
        stage_moe(K, l)
        P.barrier()
    if not dbg:
        stage_final(K)
    for name in dbg:
        if name in P.dram and name not in ('inproj', 'hyena', 'attn', 's5', 'outproj'):
            src = P.dram[name]
            o = nc.dram_tensor("dbg_" + name, list(src.shape), F32, kind="ExternalOutput").ap()
            P.barrier()
            P.dma(o, src, (), ())
    P.finish()
    return P


def stage_mod(K, l):
    P, nc, I = K.P, K.P.nc, K.I
    with SB(nc, "m_cs", [128, 2, 8], F32) as cs, SB(nc, "m_w", [128, 6 * D], F32) as wk, \
            SB(nc, "m_ws", [128, 6 * D], F32) as ws, SB(nc, "m_b", [1, 6 * D], F32) as bt, \
            SB(nc, "m_raw", [128, 6 * D], F32) as raw, SB(nc, "m_g", [128, 2, D], F32) as gn, \
            SB(nc, "m_o", [128, 12, D], F32) as mo:
        t_cs, t_w, t_ws, t_b, t_raw, t_g = Tok("cs"), Tok("w"), Tok("ws"), Tok("b"), Tok("raw"), Tok("g")
        P.dma(cs[:, 0, :], I['c'][0, :].rearrange("(c p) -> p c", p=128), (), [t_cs])
        P.dma(cs[:, 1, :], I['c_ctx'][0, :].rearrange("(c p) -> p c", p=128), (), [t_cs])
        P.act(cs[:], cs[:], AF.Silu, [t_cs], [t_cs])
        P.dma(bt[:], I['ada_b'][l:l + 1, :], (), [t_b])
        P.dma(gn[:, 0, :], I['norm1_g'][l, :].partition_broadcast(128), (), [t_g])
        P.dma(gn[:, 1, :], I['norm2_g'][l, :].partition_broadcast(128), (), [t_g])
        for who in range(2):
            for j in range(12):
                ps, tp = P.ps()
                for k in range(8):
                    P.dma(wk[:, 0:512], I['ada_w'][l, k * 128:(k + 1) * 128, j * 512:(j + 1) * 512], (), [t_w])
                    P.ts(ws[:, 0:512], wk[:, 0:512], cs[:, who, k:k + 1], None, ALU.mult, None, [t_w, t_cs], [t_ws])
                    P.mm(ps[:, :], K.ones[:, :], ws[:, 0:512], k == 0, False, [t_ws, K.t_ones], [tp])
                P.mm(ps[:, :], K.ones[0:1, :], bt[0:1, j * 512:(j + 1) * 512], False, True, [t_b, K.t_ones], [tp])
                P.copy(raw[:, j * 512:(j + 1) * 512], ps[:, :], [tp], [t_raw])
            base = who * 6
            m = mo
            for half in range(2):
                sh = raw[:, (3 * half) * D:(3 * half + 1) * D]
                sc = raw[:, (3 * half + 1) * D:(3 * half + 2) * D]
                gg = raw[:, (3 * half + 2) * D:(3 * half + 3) * D]
                P.stt(m[:, base + 3 * half + 0, :], sc, 1.0, gn[:, half, :], ALU.add, ALU.mult, [t_raw, t_g], [K.t_mod])
                P.copy(m[:, base + 3 * half + 1, :], sh, [t_raw], [K.t_mod])
                P.copy(m[:, base + 3 * half + 2, :], gg, [t_raw], [K.t_mod])
        P.dma(K.MOD.rearrange("(o j) d -> o j d", o=1), mo[0:1, :, :], [K.t_mod], [("MOD", 0)], q='pool')
        P.barrier()


def rms_rstd(K, xt, t_x, width, junk, ss, t_s):
    P = K.P
    P.act(junk, xt, AF.Square, [t_x], [t_s], accum_out=ss[:, 0:1])
    P.ts(ss[:, 0:1], ss[:, 0:1], 1.0 / width, EPS, ALU.mult, ALU.add, [t_s], [t_s])
    P.act(ss[:, 0:1], ss[:, 0:1], AF.Sqrt, [t_s], [t_s])
    P.recip(ss[:, 0:1], ss[:, 0:1], [t_s], [t_s])


def stage_inproj(K, l):
    P, nc, I = K.P, K.P.nc, K.I
    src = perm_cols(640)
    with SB(nc, "i_w", [128, 8, IN_W], F32) as W, SB(nc, "i_wp", [128, 8, 640], F32) as WP, \
            SB(nc, "i_x", [128, 2, D], F32) as xt, SB(nc, "i_h", [128, 2, D], F32) as ht, \
            SB(nc, "i_hT", [128, 8, 512], F32) as hT, SB(nc, "i_ss", [128, 2], F32) as ss, \
            SB(nc, "i_junk", [128, D], F32) as junk, SB(nc, "i_o", [128, 2, 768], F32) as ot, \
            SB(nc, "i_rc", [128, 512], F32) as rc, SB(nc, "i_rs", [128, 512], F32) as rs, \
            SB(nc, "i_t1", [128, 512], F32) as t1, SB(nc, "i_t2", [128, 512], F32) as t2, \
            SB(nc, "i_mod", [128, 4, D], F32) as modbc, SB(nc, "i_wst", [128, 2, IN_W], F32) as wst:
        t_W, t_WP = Tok("W"), Tok("WP")
        t_wst = [Tok("wst0"), Tok("wst1")]
        for jj, r in enumerate((0, 1, 6, 7)):
            P.dma(modbc[:, jj, :], K.MOD[r, :].partition_broadcast(128), (), [K.t_mod])
        t_x = [Tok("x0"), Tok("x1")]
        t_h = [Tok("h0"), Tok("h1")]
        t_s = [Tok("s0"), Tok("s1")]
        t_hT, t_o, t_rc, t_rs, t_t1, t_t2 = Tok("hT"), [Tok("o0"), Tok("o1")], Tok("rc"), Tok("rs"), Tok("t1"), Tok("t2")
        for k in range(8):
            P.load_r(W[:, k, :], I['w_in'][l, k * 128:(k + 1) * 128, :], wst[:, k % 2, :], t_W, t_wst[k % 2], e='act' if k % 2 else 'dve')
        wv = I['w_in'][l, :, HY_END:HY_END + 640].rearrange("(k p) (b two s) -> k p b two s", p=128, two=2, s=16)
        for k in range(8):
            dst = wst[:, k % 2, 0:640].rearrange("p (b two s) -> p b two s", two=2, s=16)
            P.dma(dst[:, :, 0, :], wv[k, :, :, 1, :], (), [t_wst[k % 2]])
            P.dma(dst[:, :, 1, :], wv[k, :, :, 0, :], (), [t_wst[k % 2]])
            P.copy(RR(WP[:, k, :]), wst[:, k % 2, 0:640], [t_wst[k % 2]], [t_WP], e='act' if k % 2 else 'dve')
        ngroups = (T + 511) // 512
        for g in range(ngroups):
            t0 = g * 512
            ntok = min(512, T - t0)
            ntile = ntok // 128
            for i in range(ntile):
                ti = g * 4 + i
                b = i % 2
                isctx = ti < 2
                mb = 2 if isctx else 0
                P.dma(xt[:, b, :], K.X[ti * 128:(ti + 1) * 128, :], [K.tX[ti]], [t_x[b]])
                rms_rstd(K, xt[:, b, :], t_x[b], D, junk[:], ss[:, b:b + 1], t_s[b])
                P.stt(ht[:, b, :], xt[:, b, :], ss[:, b:b + 1], modbc[:, mb + 0, :], ALU.mult, ALU.mult,
                      [t_x[b], t_s[b], K.t_mod], [t_h[b]])
                P.tt(ht[:, b, :], ht[:, b, :], modbc[:, mb + 1, :], ALU.add, [t_h[b], K.t_mod], [t_h[b]])
                for half in range(2):
                    ps, tp = P.ps()
                    for j in range(4):
                        kk = half * 4 + j
                        P.tr(ps[:, j * 128:(j + 1) * 128], ht[:, b, kk * 128:(kk + 1) * 128], K.ident[:], [t_h[b], K.t_ident], [tp])
                    P.copy(RR(hT[:, half * 4:(half + 1) * 4, i * 128:(i + 1) * 128]),
                           ps[:, :].rearrange("p (j t) -> p j t", j=4), [tp], [t_hT], e='act' if half else 'dve')
            for i in range(ntile):
                ti = g * 4 + i
                b = i % 2
                for (c0, cw, dst, dcol) in ((0, 512, K.ZT, 0), (512, 256, K.ZT, 512), (K_END, 128, K.VT, 0)):
                    ps, tp = P.ps()
                    for k in range(8):
                        P.mm(ps[:, 0:cw], hT[:, k, i * 128:(i + 1) * 128], W[:, k, c0:c0 + cw], k == 0, k == 7, [t_hT, t_W], [tp], r=FAST)
                    P.copy(ot[:, b, 0:cw], ps[:, 0:cw], [tp], [t_o[b]], e='act')
                    P.dma(dst[ti * 128:(ti + 1) * 128, dcol:dcol + cw], ot[:, b, 0:cw], [t_o[b]], [(dst.tensor.name, ti)], q='pool')
            P.dma(rc[:, 0:ntok], I['k_ropec'][:, t0:t0 + ntok], (), [t_rc])
            P.dma(rs[:, 0:ntok], I['k_ropes'][:, t0:t0 + ntok], (), [t_rs])
            for cidx in range(5):
                c0 = HY_END + cidx * 128
                ps, tp = P.ps()
                ps2, tp2 = P.ps()
                for k in range(8):
                    P.mm(ps[:, 0:ntok], W[:, k, c0:c0 + 128], hT[:, k, 0:ntok], k == 0, k == 7, [t_hT, t_W], [tp], r=FAST)
                for k in range(8):
                    P.mm(ps2[:, 0:ntok], WP[:, k, cidx * 128:(cidx + 1) * 128], hT[:, k, 0:ntok], k == 0, k == 7, [t_hT, t_WP], [tp2], r=FAST)
                P.tt(t1[:, 0:ntok], ps[:, 0:ntok], rc[:, 0:ntok], ALU.mult, [tp, t_rc], [t_t1])
                P.tt(t2[:, 0:ntok], ps2[:, 0:ntok], rs[:, 0:ntok], ALU.mult, [tp2, t_rs], [t_t2])
                P.tt(t1[:, 0:ntok], t1[:, 0:ntok], t2[:, 0:ntok], ALU.add, [t_t1, t_t2], [t_t1])
                if cidx < 4:
                    P.dma(K.QF[cidx * 128:(cidx + 1) * 128, t0:t0 + ntok], t1[:, 0:ntok], [t_t1], [("QF", g)], q='pool')
                else:
                    P.dma(K.KF[:, t0:t0 + ntok], t1[:, 0:ntok], [t_t1], [("KF", g)], q='pool')
            for cidx in range(2):
                c0 = V_END + cidx * 128
                ps, tp = P.ps()
                for k in range(8):
                    P.mm(ps[:, 0:ntok], W[:, k, c0:c0 + 128], hT[:, k, 0:ntok], k == 0, k == 7, [t_hT, t_W], [tp], r=FAST)
                P.copy(t2[:, 0:ntok], ps[:, 0:ntok], [tp], [t_t2], e='act')
                P.dma(K.UF[cidx * 128:(cidx + 1) * 128, t0:t0 + ntok], t2[:, 0:ntok], [t_t2], [("UF", g)], q='pool')


def sin_chain(K, ps_ap, bcol, fcol, v, r, hid, n, t_ps, t_consts, t_v, t_r, t_hid):
    P = K.P
    P.act(v[:, 0:n], ps_ap, AF.Identity, [t_ps] + t_consts, [t_v], bias=bcol, scale=fcol)
    P.ts(r[:, 0:n], v[:, 0:n], MAGIC, MAGIC, ALU.add, ALU.subtract, [t_v], [t_r])
    P.tt(v[:, 0:n], v[:, 0:n], r[:, 0:n], ALU.subtract, [t_v, t_r], [t_v])
    P.act(hid[:, 0:n], v[:, 0:n], AF.Sin, [t_v], [t_hid], scale=TWO_PI)


def stage_hyena(K, l, L, row0, tag):
    P, nc, I = K.P, K.P.nc, K.I
    NCH = L // 128
    NK = NCH + 1
    CC, SS = I['k_cc' + tag], I['k_ss' + tag]
    featT, dec, wkc = I['k_feat' + tag], I['k_dec' + tag], I['k_wk' + tag]
    with SB(nc, "ha_w", [128, 4, 768], F32) as wb, SB(nc, "ha_z", [128, 3, 768], F32) as z, \
            SB(nc, "ha_t", [128, 2, 768], F32) as tt_:
        t_wb, t_z, t_t = Tok("wb"), [Tok("zm"), Tok("z0"), Tok("zp")], [Tok("ta"), Tok("tb")]
        for j in range(3):
            P.dma(wb[:, j, :], I['hy_conv_w'][l, j, :].partition_broadcast(128), (), [t_wb])
        P.dma(wb[:, 3, :], I['hy_conv_b'][l, :].partition_broadcast(128), (), [t_wb])
        for i in range(NCH):
            r0 = row0 + i * 128
            if i == 0:
                P.memset(z[0:1, 0, :], 0.0, [t_z[0]])
                P.dma(z[1:128, 0, :], K.ZT[r0:r0 + 127, 0:768], (), [t_z[0]])
            else:
                P.dma(z[:, 0, :], K.ZT[r0 - 1:r0 + 127, 0:768], (), [t_z[0]])
            P.dma(z[:, 1, :], K.ZT[r0:r0 + 128, 0:768], (), [t_z[1]])
            if i == NCH - 1:
                P.memset(z[:, 2, :], 0.0, [t_z[2]])
                P.dma(z[0:127, 2, :], K.ZT[r0 + 1:r0 + 128, 0:768], (), [t_z[2]])
            else:
                P.dma(z[:, 2, :], K.ZT[r0 + 1:r0 + 129, 0:768], (), [t_z[2]])
            P.tt(tt_[:, 0, :], z[:, 0, :], wb[:, 0, :], ALU.mult, [t_z[0], t_wb], [t_t[0]])
            P.tt(tt_[:, 1, :], z[:, 1, :], wb[:, 1, :], ALU.mult, [t_z[1], t_wb], [t_t[1]], e='pool')
            P.tt(tt_[:, 0, :], tt_[:, 0, :], tt_[:, 1, :], ALU.add, [t_t[0], t_t[1]], [t_t[0]])
            P.tt(tt_[:, 1, :], z[:, 2, :], wb[:, 2, :], ALU.mult, [t_z[2], t_wb], [t_t[1]], e='pool')
            P.tt(tt_[:, 0, :], tt_[:, 0, :], tt_[:, 1, :], ALU.add, [t_t[0], t_t[1]], [t_t[0]])
            P.tt(tt_[:, 0, :], tt_[:, 0, :], wb[:, 3, :], ALU.add, [t_t[0], t_wb], [t_t[0]])
            P.dma(K.ZC[r0:r0 + 128, :], tt_[:, 0, :], [t_t[0]], [("ZC", i)], q='pool')
    P.barrier()
    HH = (NCH + 1) // 2
    with ExitStack() as es_:
        taps = es_.enter_context(SB(nc, "hb_taps", [128, NCH, 1024], F32))
        tabt = es_.enter_context(SB(nc, "hb_cc", [128, 2, HH, 128], F32))
        tabs_ = es_.enter_context(SB(nc, "hb_ss", [128, 2, HH, 128], F32))
        ft = es_.enter_context(SB(nc, "hb_f", [33, 512], F32))
        w1 = es_.enter_context(SB(nc, "hb_w1", [33, 64], F32))
        w2 = es_.enter_context(SB(nc, "hb_w2", [64, 64], F32))
        w3 = es_.enter_context(SB(nc, "hb_w3", [64, 1024], F32))
        cst = es_.enter_context(SB(nc, "hb_c", [64, 8], F32))
        v = es_.enter_context(SB(nc, "hb_v", [64, 512], F32))
        r = es_.enter_context(SB(nc, "hb_r", [64, 512], F32))
        h1 = es_.enter_context(SB(nc, "hb_h1", [64, 512], F32))
        h2 = es_.enter_context(SB(nc, "hb_h2", [64, 512], F32))
        dct = es_.enter_context(SB(nc, "hb_dec", [128, 256], F32))
        ab = es_.enter_context(SB(nc, "hb_abs", [128, 1024], F32))
        rn = es_.enter_context(SB(nc, "hb_rn", [128, 512], F32))
        tmp = es_.enter_context(SB(nc, "hb_tmp", [128, 512], F32))
        wkt = es_.enter_context(SB(nc, "hb_wk", [128, NK], F32))
        ko = es_.enter_context(SB(nc, "hb_o", [128, 2, 512], F32))
        t_taps = [Tok("taps%d" % c) for c in range(NCH)]
        t_cc, t_ss, t_f, t_w, t_c = Tok("cc"), Tok("ss"), Tok("f"), Tok("w"), Tok("c")
        t_v, t_r, t_h1, t_h2, t_dec, t_ab, t_rn, t_tmp, t_wk = (Tok(n) for n in ("v", "r", "h1", "h2", "dec", "ab", "rn", "tmp", "wk"))
        t_ko = [Tok("ko0"), Tok("ko1")]
        P.dma(w1[:], I['hy_f_w1'][l, :, :], (), [t_w])
        P.dma(w2[:], I['hy_f_w2'][l, :, :], (), [t_w])
        P.dma(w3[:], I['hy_f_w3'][l, :, :], (), [t_w])
        for j, nm in enumerate(('hy_f_b1', 'hy_f_freq1', 'hy_f_b2', 'hy_f_freq2')):
            P.dma(cst[:, j:j + 1], I[nm][l, :].rearrange("(p o) -> p o", o=1), (), [t_c])
        P.ts(cst[:, 4:5], cst[:, 1:2], 1.0 / (2.0 * math.pi), None, ALU.mult, None, [t_c], [t_c])
        P.ts(cst[:, 5:6], cst[:, 3:4], 1.0 / (2.0 * math.pi), None, ALU.mult, None, [t_c], [t_c])
        P.tt(cst[:, 6:7], cst[:, 0:1], cst[:, 4:5], ALU.mult, [t_c], [t_c])
        P.tt(cst[:, 7:8], cst[:, 2:3], cst[:, 5:6], ALU.mult, [t_c], [t_c])
        P.dma(wkt[:], wkc[:, 0].rearrange("(c p) -> p c", p=128), (), [t_wk])
        ng = (L + 511) // 512
        for g in range(ng):
            n0 = g * 512
            n = min(512, L - n0)
            P.dma(ft[:, 0:n], featT[:, n0:n0 + n], (), [t_f])
            ps, tp = P.ps()
            P.mm(ps[0:64, 0:n], w1[0:33, :], ft[0:33, 0:n], True, True, [t_w, t_f], [tp])
            sin_chain(K, ps[0:64, 0:n], cst[:, 6:7], cst[:, 4:5], v, r, h1, n, tp, [t_c], t_v, t_r, t_h1)
            ps, tp = P.ps()
            P.mm(ps[0:64, 0:n], w2[:, :], h1[:, 0:n], True, True, [t_w, t_h1], [tp])
            sin_chain(K, ps[0:64, 0:n], cst[:, 7:8], cst[:, 5:6], v, r, h2, n, tp, [t_c], t_v, t_r, t_h2)
            for sub in range(n // 128):
                c = (n0 // 128) + sub
                P.dma(dct[:], dec[c * 128:(c + 1) * 128, :], (), [t_dec])
                for half in range(2):
                    ps, tp = P.ps()
                    P.mm(ps[:, :], h2[:, sub * 128:(sub + 1) * 128], w3[:, half * 512:(half + 1) * 512], True, True, [t_h2, t_w], [tp])
                    P.tt(RR(taps[:, c, half * 512:(half + 1) * 512].rearrange("p (d c) -> p d c", d=2)),
                         ps[:, :].rearrange("p (d c) -> p d c", d=2),
                         dct[:, :].unsqueeze(1).broadcast_to([128, 2, 256]), ALU.mult, [tp, t_dec], [t_taps[c]])
        tv0 = taps[0:1, 0, :].rearrange("p (o d c) -> p o d c", o=2, d=2)
        P.ts(RR(tv0[:, :, 1, :]), tv0[:, :, 1, :], 0.0, None, ALU.mult, None, [t_taps[0]], [t_taps[0]])
        held, hid_ = P.ps_hold(2)
        for c in range(NCH):
            P.act(ab[:], taps[:, c, :], AF.Abs, [t_taps[c]], [t_ab])
            for half in range(2):
                P.mm(held[half][0][:, :], K.ones[:, :], ab[:, half * 512:(half + 1) * 512], c == 0, c == NCH - 1, [t_ab, K.t_ones], [held[half][1]])
        for o in range(2):
            P.copy(ab[:, o * 512:o * 512 + 256], held[o][0][:, 0:256], [held[o][1]], [t_ab])
            P.tt(rn[:, o * 256:(o + 1) * 256], ab[:, o * 512:o * 512 + 256], held[o][0][:, 256:512], ALU.add, [held[o][1], t_ab], [t_rn])
        P.ps_release(hid_)
        P.recip(rn[:], rn[:], [t_rn], [t_rn])
        for c in range(NCH):
            tv = taps[:, c, :].rearrange("p (o d c) -> p o d c", o=2, d=2)
            tm = tmp[:, :].rearrange("p (o c) -> p o c", o=2)
            P.tt(tm, tv[:, :, 0, :], tv[:, :, 1, :], ALU.add, [t_taps[c]], [t_tmp])
            P.tt(RR(tv[:, :, 1, :]), tv[:, :, 1, :], tv[:, :, 0, :], ALU.subtract, [t_taps[c]], [t_taps[c]])
            P.copy(RR(tv[:, :, 0, :]), tm, [t_tmp], [t_taps[c]])
        rn3 = rn[:, :].rearrange("p (o c) -> p o c", o=2)
        t_st2 = [Tok("fst0"), Tok("fst1")]
        for kc in range(NK):
            for part, TAB, ttab, d in ((0, CC, t_cc, 0), (1, SS, t_ss, 1)):
                ps, tp = P.ps()
                for hf in range(2):
                    cA, cB = hf * HH, min((hf + 1) * HH, NCH)
                    if cA >= cB:
                        continue
                    P.load_r(tabt[:, part, 0:cB - cA, :], TAB[kc, :, cA:cB, :], tabs_[:, part, 0:cB - cA, :], ttab, t_st2[part],
                             e='act' if part == 0 else 'dve')
                    for c in range(cA, cB):
                        tv = taps[:, c, :].rearrange("p (o d c) -> p o d c", o=2, d=2)
                        P.mm(ps[:, :].rearrange("p (o c) -> p o c", o=2), tabt[:, part, c - cA, :], tv[:, :, d, :], c == 0, c == NCH - 1,
                             [ttab, t_taps[c]], [tp], r=FAST)
                P.stt(ko[:, part, :].rearrange("p (o c) -> p o c", o=2), ps[:, :].rearrange("p (o c) -> p o c", o=2),
                      wkt[:, kc:kc + 1], rn3, ALU.mult, ALU.mult, [tp, t_wk, t_rn], [t_ko[part]])
                dst = K.KRE if part == 0 else K.KIM
                P.dma(dst[kc * 128:(kc + 1) * 128, :], ko[:, part, :], [t_ko[part]], [(dst.tensor.name, kc)], q='pool')
    P.barrier()
    HK = (NK + 1) // 2
    with SB(nc, "hc_a", [128, NCH, 256], F32) as a, SB(nc, "hc_p1", [128, NK, 256], F32) as p1, \
            SB(nc, "hc_p2", [128, NK, 256], F32) as p2, SB(nc, "hc_tb", [128, 4, HK, 128], F32) as tb, \
            SB(nc, "hc_k", [128, 2, 256], F32) as kk, \
            SB(nc, "hc_t", [128, 2, 256], F32) as tq, SB(nc, "hc_b", [128, 2, 256], F32) as bb, \
            SB(nc, "hc_x", [128, 256], F32) as xg, SB(nc, "hc_y", [128, 256], F32) as yy, \
            SB(nc, "hc_stg", [128, 4, HK, 128], F32) as stg:
        t_stg = [Tok("stg%d" % i) for i in range(4)]
        t_tb = [Tok("tb%d" % i) for i in range(4)]
        ldn = [0]

        def load_tabs(blk, cA, cB):
            pb = ldn[0] % 2
            ldn[0] += 1
            P.load_r(tb[:, 2 * pb, 0:cB - cA, :], CC[blk, :, cA:cB, :], stg[:, 2 * pb, 0:cB - cA, :], t_tb[2 * pb], t_stg[2 * pb], e='act')
            P.load_r(tb[:, 2 * pb + 1, 0:cB - cA, :], SS[blk, :, cA:cB, :], stg[:, 2 * pb + 1, 0:cB - cA, :], t_tb[2 * pb + 1], t_stg[2 * pb + 1], e='dve')
            return tb[:, 2 * pb], tb[:, 2 * pb + 1], t_tb[2 * pb], t_tb[2 * pb + 1]
        t_a = [Tok("a%d" % c) for c in range(NCH)]
        t_p1, t_p2, t_k, t_b, t_x, t_y = (Tok(n) for n in ("p1", "p2", "k", "b", "x", "y"))
        t_q = [Tok("q0"), Tok("q1")]
        for o in range(2):
            P.dma(bb[:, o, :], I['hy_bias'][l, o, :].partition_broadcast(128), (), [t_b])
        for c in range(NCH):
            P.load_r(a[:, c, :], K.ZC[row0 + c * 128: row0 + (c + 1) * 128, 0:256], xg[:], t_a[c], t_x, e='act' if c % 2 else 'dve')
        for o in range(2):
            for kc in range(NK):
                P.dma(kk[:, 0, :], K.KRE[kc * 128:(kc + 1) * 128, o * 256:(o + 1) * 256], (), [t_k])
                P.dma(kk[:, 1, :], K.KIM[kc * 128:(kc + 1) * 128, o * 256:(o + 1) * 256], (), [t_k])
                psr, tpr = P.ps()
                psi, tpi = P.ps()
                for cA in range(0, NCH, HK):
                    cB = min(cA + HK, NCH)
                    cct, sst, t_cc, t_ss = load_tabs(kc, cA, cB)
                    for c in range(cA, cB):
                        P.mm(psr[:, 0:256], cct[:, c - cA, :], a[:, c, :], c == 0, c == NCH - 1, [t_cc, t_a[c]], [tpr], r=FAST)
                    for c in range(cA, cB):
                        P.mm(psi[:, 0:256], sst[:, c - cA, :], a[:, c, :], c == 0, c == NCH - 1, [t_ss, t_a[c]], [tpi], r=FAST)
                P.tt(tq[:, 0, :], psr[:, 0:256], kk[:, 0, :], ALU.mult, [tpr, t_k], [t_q[0]])
                P.tt(tq[:, 1, :], psi[:, 0:256], kk[:, 1, :], ALU.mult, [tpi, t_k], [t_q[1]])
                P.tt(RR(p1[:, kc, :]), tq[:, 0, :], tq[:, 1, :], ALU.add, [t_q[0], t_q[1]], [t_p1])
                P.tt(tq[:, 0, :], psi[:, 0:256], kk[:, 0, :], ALU.mult, [tpi, t_k], [t_q[0]])
                P.tt(tq[:, 1, :], psr[:, 0:256], kk[:, 1, :], ALU.mult, [tpr, t_k], [t_q[1]])
                P.tt(RR(p2[:, kc, :]), tq[:, 0, :], tq[:, 1, :], ALU.subtract, [t_q[0], t_q[1]], [t_p2])
            for tc in range(NCH):
                r0 = row0 + tc * 128
                P.dma(xg[:], K.ZC[r0:r0 + 128, 256 * (o + 1):256 * (o + 2)], (), [t_x])
                ps, tp = P.ps()
                for kA in range(0, NK, HK):
                    kB = min(kA + HK, NK)
                    cct, sst, t_cc, t_ss = load_tabs(tc, kA, kB)
                    for kc in range(kA, kB):
                        P.mm(ps[:, 0:256], cct[:, kc - kA, :], p1[:, kc, :], kc == 0, False, [t_cc, t_p1], [tp], r=FAST)
                    for kc in range(kA, kB):
                        P.mm(ps[:, 0:256], sst[:, kc - kA, :], p2[:, kc, :], False, kc == NK - 1, [t_ss, t_p2], [tp], r=FAST)
                P.tt(yy[:], a[:, tc, :], bb[:, o, :], ALU.mult, [t_a[tc], t_b], [t_y])
                P.tt(yy[:], yy[:], ps[:, 0:256], ALU.add, [t_y, tp], [t_y])
                if o == 0:
                    P.tt(RR(a[:, tc, :]), yy[:], xg[:], ALU.mult, [t_y, t_x], [t_a[tc]])
                else:
                    P.tt(yy[:], yy[:], xg[:], ALU.mult, [t_y, t_x], [t_y])
                    P.dma(K.MIX[r0:r0 + 128, 0:256], yy[:], [t_y], [("MIX", r0)], q='pool')


def stage_attn(K, l):
    P, nc, I = K.P, K.P.nc, K.I
    with SB(nc, "at_k", [64, 2, T], F32) as kf, SB(nc, "at_v", [128, NT, 2, 65], F32) as v1, \
            SB(nc, "at_es", [128, 8], F32) as es, SB(nc, "at_ml", [128, 128], F32) as ml, \
            SB(nc, "at_mr", [128, 128], F32) as mr, SB(nc, "at_q", [64, 2, 4, 128], F32) as q4, \
            SB(nc, "at_pt", [128, 5, 512], F32) as pt, SB(nc, "at_o", [128, 2, 512], F32) as ot, \
            SB(nc, "at_d", [128, 2, 4], F32) as den:
        t_k, t_v, t_es, t_m = Tok("k"), Tok("v"), Tok("es"), Tok("m")
        t_q = [Tok("q0"), Tok("q1")]
        t_pt = [Tok("pt%d" % i) for i in range(5)]
        t_o = [Tok("o0"), Tok("o1")]
        t_d = Tok("d")
        for h in range(2):
            P.dma(kf[:, h, :], K.KF[h * 64:(h + 1) * 64, :], (), [t_k])
        P.memset(v1[:, :, :, 64:65], 1.0, [t_v])
        vtv = K.VT.rearrange("(t p) (h d) -> p t h d", p=128, h=2)
        for h in range(2):
            P.dma(v1[:, :, h, 0:64], vtv[:, :, h, :], (), [t_v])
        P.dma(es[:], I['attn_sink'][l, :].partition_broadcast(128), (), [t_es])
        P.act(es[:], es[:], AF.Exp, [t_es], [t_es])
        P.dma(ml[:], I['k_maskl'][:, :], (), [t_m])
        P.dma(mr[:], I['k_maskr'][:, :], (), [t_m])
        for qb in range(NT):
            kts = [(0, None), (1, None)]
            if qb >= 2:
                if qb - 1 >= 2:
                    kts.append((qb - 1, ml))
                kts.append((qb, None))
                if qb + 1 < NT:
                    kts.append((qb + 1, mr))
            ob = qb % 2
            for kvh in range(2):
                qi = kvh
                P.dma(q4[:, qi, :, :], K.QF[kvh * 256:(kvh + 1) * 256, qb * 128:(qb + 1) * 128].rearrange("(h d) t -> d h t", d=64),
                      (), [t_q[qi]])
                for idx, (kt, mask) in enumerate(kts):
                    ps, tp = P.ps()
                    P.mm(ps[:, :], kf[:, kvh, kt * 128:(kt + 1) * 128], q4[:, qi, :, :].rearrange("d h t -> d (h t)"), True, True,
                         [t_k, t_q[qi]], [tp])
                    P.act(pt[:, idx, :], ps[:, :], AF.Exp, [tp], [t_pt[idx]], scale=0.125)
                    if mask is not None:
                        pv = pt[:, idx, :].rearrange("p (h t) -> p h t", h=4)
                        P.tt(pv, pv, mask[:, :].unsqueeze(1).broadcast_to([128, 4, 128]), ALU.mult, [t_pt[idx], t_m], [t_pt[idx]])
                pso, tpo = P.ps()
                for hh in range(4):
                    for idx, (kt, mask) in enumerate(kts):
                        P.mm(pso[:, hh * 65:(hh + 1) * 65], pt[:, idx, hh * 128:(hh + 1) * 128], v1[:, kt, kvh, :],
                             idx == 0, idx == len(kts) - 1, [t_pt[idx], t_v], [tpo])
                pv = pso[:, 0:260].rearrange("p (h c) -> p h c", h=4)
                P.tt(den[:, kvh, :], pv[:, :, 64], es[:, kvh * 4:(kvh + 1) * 4], ALU.add, [tpo, t_es], [t_d])
                P.recip(den[:, kvh, :], den[:, kvh, :], [t_d], [t_d])
                P.tt(ot[:, ob, kvh * 256:(kvh + 1) * 256].rearrange("p (h d) -> p h d", h=4), pv[:, :, 0:64],
                     den[:, kvh, :].unsqueeze(2).broadcast_to([128, 4, 64]), ALU.mult, [tpo, t_d], [t_o[ob]])
            P.dma(K.MIX[qb * 128:(qb + 1) * 128, 256:768], ot[:, ob, :], [t_o[ob]], [("MIXa", qb)], q='pool')


def round_frac(K, v, r, t_v, t_r, e='dve'):
    P = K.P
    P.ts(r, v, MAGIC, MAGIC, ALU.add, ALU.subtract, [t_v], [t_r], e=e)
    P.tt(v, v, r, ALU.subtract, [t_v, t_r], [t_v], e=e)


def stage_s5(K, l):
    P, nc, I = K.P, K.P.nc, K.I
    K.GS = K.P.dram.get("sGS")
    if K.GS is None:
        K.GS = P.dram_t("sGS", [256, T])
    with SB(nc, "s_bt", [32, 16, 2, 128], F32) as BT, SB(nc, "s_cb", [128, 16, 2, 32], F32) as CB, \
            SB(nc, "s_par", [128, 12, 16], F32) as par, SB(nc, "s_dsk", [32, 8], F32) as dsk, \
            SB(nc, "s_iota", [128, 512], F32) as iot:
        t_BT, t_CB, t_par, t_dsk, t_iota = Tok("BT"), Tok("CB"), Tok("par"), Tok("dsk"), Tok("iota")
        MAG, PHI = 4, 6
        with SB(nc, "s_b", [128, 2, 16, 16], F32) as bri, SB(nc, "s_bb", [128, 2, 16, 16], F32) as bb, \
                SB(nc, "s_bd", [128, 16, 2, 32], F32) as BD, SB(nc, "s_cnd", [32, 16, 2, 128], F32) as CND, \
                SB(nc, "s_t1", [128, 16, 16], F32) as t1, SB(nc, "s_t2", [128, 16, 16], F32) as t2:
            t_b, t_bb, t_BD, t_CND, t_t1, t_t2 = Tok("b"), Tok("bb"), Tok("BD"), Tok("CND"), Tok("t1"), Tok("t2")
            for d in range(2):
                P.dma(par[:, 0, d * 8:(d + 1) * 8], I['s5_lam_re'][l, d].rearrange("g p -> (g p)").rearrange("(gh q) -> q gh", q=128), (), [t_par])
                P.dma(par[:, 1, d * 8:(d + 1) * 8], I['s5_lam_im'][l, d].rearrange("g p -> (g p)").rearrange("(gh q) -> q gh", q=128), (), [t_par])
                ldv = I['s5_log_dt'][l, d].rearrange("(gh gl) -> gl gh", gl=2)
                for gl in range(2):
                    P.dma(par[gl * 64:(gl + 1) * 64, 2, d * 8:(d + 1) * 8], ldv[gl].partition_broadcast(64), (), [t_par])
                for ri, nm in enumerate(('s5_b_re', 's5_b_im')):
                    P.dma(bri[:, ri, d * 8:(d + 1) * 8, :],
                          I[nm][l, d].rearrange("g p c -> (g p c)").rearrange("(gh q c) -> q gh c", q=128, c=16), (), [t_b])
            P.dma(dsk[:], I['s5_d'][l, :].rearrange("(j r) -> r j", r=32), (), [t_dsk])
            P.dma(iot[:], I['k_iota'][0, :].partition_broadcast(128), (), [t_iota])
            pp = lambda i: par[:, i, :]
            tp_ = [t_par]
            P.act(pp(2), pp(2), AF.Exp, tp_, tp_)
            P.tt(pp(3), pp(0), pp(2), ALU.mult, tp_, tp_)
            P.act(pp(MAG), pp(3), AF.Exp, tp_, tp_)
            P.tt(pp(3), pp(1), pp(2), ALU.mult, tp_, tp_)
            P.ts(pp(PHI), pp(3), 1.0 / (2.0 * math.pi), None, ALU.mult, None, tp_, tp_)
            P.copy(pp(5), pp(PHI), tp_, tp_)
            round_frac(K, pp(5), pp(11), t_par, t_par)
            P.act(pp(8), pp(5), AF.Sin, tp_, tp_, scale=TWO_PI)
            P.ts(pp(5), pp(PHI), 0.25, None, ALU.add, None, tp_, tp_)
            round_frac(K, pp(5), pp(11), t_par, t_par)
            P.act(pp(7), pp(5), AF.Sin, tp_, tp_, scale=TWO_PI)
            P.tt(pp(7), pp(7), pp(MAG), ALU.mult, tp_, tp_)
            P.tt(pp(8), pp(8), pp(MAG), ALU.mult, tp_, tp_)
            P.ts(pp(7), pp(7), -1.0, None, ALU.add, None, tp_, tp_)
            P.tt(pp(3), pp(0), pp(0), ALU.mult, tp_, tp_)
            P.tt(pp(5), pp(1), pp(1), ALU.mult, tp_, tp_)
            P.tt(pp(3), pp(3), pp(5), ALU.add, tp_, tp_)
            P.recip(pp(3), pp(3), tp_, tp_)
            P.tt(pp(9), pp(7), pp(0), ALU.mult, tp_, tp_)
            P.tt(pp(5), pp(8), pp(1), ALU.mult, tp_, tp_)
            P.tt(pp(9), pp(9), pp(5), ALU.add, tp_, tp_)
            P.tt(pp(9), pp(9), pp(3), ALU.mult, tp_, tp_)
            P.tt(pp(10), pp(8), pp(0), ALU.mult, tp_, tp_)
            P.tt(pp(5), pp(7), pp(1), ALU.mult, tp_, tp_)
            P.tt(pp(10), pp(10), pp(5), ALU.subtract, tp_, tp_)
            P.tt(pp(10), pp(10), pp(3), ALU.mult, tp_, tp_)
            cre = par[:, 9, :].unsqueeze(2).broadcast_to([128, 16, 16])
            cim = par[:, 10, :].unsqueeze(2).broadcast_to([128, 16, 16])
            P.tt(t1[:], bri[:, 0, :, :], cre, ALU.mult, [t_b, t_par], [t_t1])
            P.tt(t2[:], bri[:, 1, :, :], cim, ALU.mult, [t_b, t_par], [t_t2])
            P.tt(bb[:, 0, :, :], t1[:], t2[:], ALU.subtract, [t_t1, t_t2], [t_bb])
            P.tt(t1[:], bri[:, 1, :, :], cre, ALU.mult, [t_b, t_par], [t_t1])
            P.tt(t2[:], bri[:, 0, :, :], cim, ALU.mult, [t_b, t_par], [t_t2])
            P.tt(bb[:, 1, :, :], t1[:], t2[:], ALU.add, [t_t1, t_t2], [t_bb])
            P.memset(BD[:], 0.0, [t_BD])
            for ri in range(2):
                P.copy(BD[0:64, :, ri, 0:16], bb[0:64, ri, :, :], [t_bb], [t_BD])
                P.copy(BD[64:128, :, ri, 16:32], bb[64:128, ri, :, :], [t_bb], [t_BD])
            for dg in range(16):
                ps, tp = P.ps()
                for ri in range(2):
                    P.tr(ps[0:32, ri * 128:(ri + 1) * 128], BD[:, dg, ri, :], K.ident[:], [t_BD, K.t_ident], [tp])
                P.copy(BT[:, dg, :, :], ps[0:32, 0:256].rearrange("p (r m) -> p r m", r=2), [tp], [t_BT])
            P.memset(CND[:], 0.0, [t_CND])
            for d in range(2):
                for ri, nm in enumerate(('s5_c_re', 's5_c_im')):
                    cv = I[nm][l, d].rearrange("(gh gl) c p -> gl c gh p", gl=2)
                    for gl in range(2):
                        P.dma(CND[gl * 16:(gl + 1) * 16, d * 8:(d + 1) * 8, ri, gl * 64:(gl + 1) * 64], cv[gl], (), [t_CND])
            for dg in range(16):
                ps, tp = P.ps()
                for ri in range(2):
                    P.tr(ps[:, ri * 32:(ri + 1) * 32], CND[:, dg, ri, :], K.ident[0:32, 0:32], [t_CND, K.t_ident], [tp])
                P.copy(CB[:, dg, 0, :], ps[:, 0:32], [tp], [t_CB])
                P.ts(CB[:, dg, 1, :], ps[:, 32:64], -1.0, None, ALU.mult, None, [tp], [t_CB])
        P.barrier()
        with SB(nc, "s_us", [32, T], F32) as us, SB(nc, "s_y", [32, T], F32) as ysb, \
                SB(nc, "s_tab", [128, 2, 4, 512], F32) as tab2, SB(nc, "s_m", [128, 2, 2, 512], F32) as mm2, \
                SB(nc, "s_w", [128, 2, 2, 512], F32) as ww2, SB(nc, "s_s", [128, 2, 2, 512], F32) as ss2, \
                SB(nc, "s_q", [128, 2, 2, 512], F32) as qq2, SB(nc, "s_c0", [128, 2, 4], F32) as c02, \
                SB(nc, "s_car", [128, 2], F32) as car, SB(nc, "s_mag", [128, 512], F32) as magt, SB(nc, "s_g", [32, 2, T], F32) as gg:
            t_us, t_y, t_car, t_g = (Tok(n) for n in ("us", "y", "car", "g"))
            tk2 = [dict(m=Tok("m%d" % b), w=Tok("w%d" % b), s=Tok("s%d" % b), q=Tok("q%d" % b), c0=Tok("c0%d" % b),
                        tab=[Tok("tabs%d" % b), Tok("tabc%d" % b), Tok("vr%d" % b), Tok("vrr%d" % b)]) for b in range(2)]
            chunk_no = [0]
            t_mag = Tok("mag")
            for j in range(8):
                P.dma(us[:], K.UF[32 * j:32 * (j + 1), :], (), [t_us])
                for d in range(2):
                    dg = d * 8 + j
                    if d == 0:
                        chunks = [(a, min(a + 512, T), False, a) for a in range(0, T, 512)]
                    else:
                        chunks = [(0, NCTX, True, 0)]
                        b = T
                        while b > NCTX:
                            a = max(b - 512, NCTX)
                            chunks.append((a, b, True, NCTX + (T - b)))
                            b = a
                    first = True
                    phi = par[:, PHI, dg:dg + 1]
                    P.copy(magt[:, :], par[:, MAG, dg:dg + 1].broadcast_to([128, 512]), [t_par], [t_mag])
                    for (a, b, rev, i0) in chunks:
                        n = b - a
                        pb = chunk_no[0] % 2
                        chunk_no[0] += 1
                        tab, mm_, ww, ss_, qq, c0 = tab2[:, pb], mm2[:, pb], ww2[:, pb], ss2[:, pb], qq2[:, pb], c02[:, pb]
                        t_m, t_w, t_s, t_q, t_c0, t_tab = tk2[pb]['m'], tk2[pb]['w'], tk2[pb]['s'], tk2[pb]['q'], tk2[pb]['c0'], tk2[pb]['tab']
                        rv = (lambda ap: ap[:, ::-1]) if rev else (lambda ap: ap)
                        ps1, tp1 = P.ps()
                        ps2, tp2 = P.ps()
                        P.mm(ps1[:, 0:n], BT[:, dg, 0, :], us[:, a:b], True, True, [t_BT, t_us], [tp1])
                        P.mm(ps2[:, 0:n], BT[:, dg, 1, :], us[:, a:b], True, True, [t_BT, t_us], [tp2])
                        P.ts(c0[:, 0:1], phi, float(i0), None, ALU.mult, None, [t_par], [t_c0])
                        round_frac(K, c0[:, 0:1], c0[:, 2:3], t_c0, t_c0)
                        P.ts(c0[:, 1:2], c0[:, 0:1], 0.25, None, ALU.add, None, [t_c0], [t_c0])
                        for which in range(2):
                            P.act(tab[:, 2 + which, 0:n], iot[:, 0:n], AF.Identity, [t_iota, t_c0, t_par], [t_tab[2 + which]],
                                  bias=c0[:, which:which + 1], scale=phi)
                            P.ts(qq[:, which, 0:n], tab[:, 2 + which, 0:n], MAGIC, MAGIC, ALU.add, ALU.subtract, [t_tab[2 + which]], [t_q])
                            P.tt(tab[:, 2 + which, 0:n], tab[:, 2 + which, 0:n], qq[:, which, 0:n], ALU.subtract, [t_tab[2 + which], t_q],
                                 [t_tab[2 + which]])
                            P.act(tab[:, which, 0:n], tab[:, 2 + which, 0:n], AF.Sin, [t_tab[2 + which]], [t_tab[which]], scale=TWO_PI)
                        sn = rv(tab[:, 0, 0:n])
                        cs = rv(tab[:, 1, 0:n])
                        tS, tC = t_tab[0], t_tab[1]
                        P.tt(mm_[:, 0, 0:n], ps1[:, 0:n], cs, ALU.mult, [tp1, tC], [t_m])
                        P.tt(qq[:, 0, 0:n], ps2[:, 0:n], sn, ALU.mult, [tp2, tS], [t_q])
                        P.tt(mm_[:, 0, 0:n], mm_[:, 0, 0:n], qq[:, 0, 0:n], ALU.add, [t_m, t_q], [t_m])
                        P.tt(mm_[:, 1, 0:n], ps2[:, 0:n], cs, ALU.mult, [tp2, tC], [t_m])
                        P.tt(qq[:, 1, 0:n], ps1[:, 0:n], sn, ALU.mult, [tp1, tS], [t_q])
                        P.tt(mm_[:, 1, 0:n], mm_[:, 1, 0:n], qq[:, 1, 0:n], ALU.subtract, [t_m, t_q], [t_m])
                        magb = magt[:, 0:n]
                        for ri in range(2):
                            init = 0.0 if first else car[:, ri:ri + 1]
                            P.op('dve', lambda g, ri=ri, init=init: g.tensor_tensor_scan(
                                out=rv(ww[:, ri, 0:n]), data0=magb, data1=rv(mm_[:, ri, 0:n]), initial=init,
                                op0=ALU.mult, op1=ALU.add), [t_m, t_mag, t_car], [t_w])
                        last = a if rev else b - 1
                        P.copy(car[:, :], ww[:, :, last - a], [t_w], [t_car])
                        first = False
                        P.tt(ss_[:, 0, 0:n], ww[:, 0, 0:n], cs, ALU.mult, [t_w, tC], [t_s], e='pool')
                        P.tt(qq[:, 0, 0:n], ww[:, 1, 0:n], sn, ALU.mult, [t_w, tS], [t_q], e='pool')
                        P.tt(ss_[:, 0, 0:n], ss_[:, 0, 0:n], qq[:, 0, 0:n], ALU.subtract, [t_s, t_q], [t_s], e='pool')
                        P.tt(ss_[:, 1, 0:n], ww[:, 0, 0:n], sn, ALU.mult, [t_w, tS], [t_s], e='pool')
                        P.tt(qq[:, 1, 0:n], ww[:, 1, 0:n], cs, ALU.mult, [t_w, tC], [t_q], e='pool')
                        P.tt(ss_[:, 1, 0:n], ss_[:, 1, 0:n], qq[:, 1, 0:n], ALU.add, [t_s, t_q], [t_s], e='pool')
                        psy, tpy = P.ps()
                        P.mm(psy[0:32, 0:n], CB[:, dg, 0, :], ss_[:, 0, 0:n], True, False, [t_CB, t_s], [tpy])
                        P.mm(psy[0:32, 0:n], CB[:, dg, 1, :], ss_[:, 1, 0:n], False, True, [t_CB, t_s], [tpy])
                        if d == 0:
                            P.copy(ysb[:, a:b], psy[0:32, 0:n], [tpy], [t_y], e='act')
                        else:
                            P.tt(ysb[:, a:b], ysb[:, a:b], psy[0:32, 0:n], ALU.add, [t_y, tpy], [t_y])
                P.stt(ysb[:], us[:], dsk[:, j:j + 1], ysb[:], ALU.mult, ALU.add, [t_us, t_dsk, t_y], [t_y])
                P.tt(gg[:, 0, :], ysb[:], ysb[:], ALU.mult, [t_y], [t_g])
                P.ts(gg[:, 0, :], gg[:, 0, :], 0.044715, 1.0, ALU.mult, ALU.add, [t_g], [t_g])
                P.tt(gg[:, 0, :], gg[:, 0, :], ysb[:], ALU.mult, [t_g, t_y], [t_g])
                P.act(gg[:, 1, :], gg[:, 0, :], AF.Sigmoid, [t_g], [t_g], scale=1.5957691216)
                P.tt(gg[:, 1, :], gg[:, 1, :], ysb[:], ALU.mult, [t_g, t_y], [t_g])
                P.dma(K.GS[32 * j:32 * (j + 1), :], gg[:, 1, :], [t_g], [("GS", j)], q='pool')
        P.barrier()
        with SB(nc, "s_gw", [128, 2, 256], F32) as gw, SB(nc, "s_gb", [128, 2], F32) as gb, \
                SB(nc, "s_gt", [128, 2, 512], F32) as gt, SB(nc, "s_sg", [128, 2, 512], F32) as sg, \
                SB(nc, "s_o", [128, 4, 256], F32) as so:
            t_gw, t_gt, t_sg, t_so = Tok("gw"), Tok("gt"), Tok("sg"), Tok("so")
            P.dma(gw[:], I['s5_glu_w'][l].rearrange("(c p) n -> p c n", p=128), (), [t_gw])
            P.dma(gb[:], I['s5_glu_b'][l, :].rearrange("(c p) -> p c", p=128), (), [t_gw])
            for g in range((T + 511) // 512):
                t0 = g * 512
                n = min(512, T - t0)
                P.dma(gt[:, :, 0:n], K.GS[:, t0:t0 + n].rearrange("(c p) t -> p c t", p=128), (), [t_gt])
                for oc in range(2):
                    ps, tp = P.ps()
                    for kc in range(2):
                        P.mm(ps[:, 0:n], gw[:, kc, oc * 128:(oc + 1) * 128], gt[:, kc, 0:n], kc == 0, kc == 1, [t_gw, t_gt], [tp])
                    P.act(sg[:, oc, 0:n], ps[:, 0:n], AF.Sigmoid, [tp, t_gw], [t_sg], bias=gb[:, oc:oc + 1], scale=1.0)
                    P.tt(sg[:, oc, 0:n], sg[:, oc, 0:n], gt[:, oc, 0:n], ALU.mult, [t_sg, t_gt], [t_sg])
                for i in range(n // 128):
                    ps, tp = P.ps()
                    for oc in range(2):
                        P.tr(ps[:, oc * 128:(oc + 1) * 128], sg[:, oc, i * 128:(i + 1) * 128], K.ident[:], [t_sg, K.t_ident], [tp])
                    P.copy(so[:, i, :], ps[:, 0:256], [tp], [t_so], e='act')
                P.dma(K.MIX[t0:t0 + n, 768:1024].rearrange("(i p) c -> p i c", p=128), so[:, 0:n // 128, :], [t_so], [("MIXs", g)], q='pool')


def stage_outproj(K, l):
    P, nc, I = K.P, K.P.nc, K.I
    with SB(nc, "o_w", [128, 8, D], F32) as W, SB(nc, "o_rw", [128, 8, 16], F32) as RW, \
            SB(nc, "o_gain", [128, D], F32) as gain, SB(nc, "o_mod", [128, 6, D], F32) as mod, \
            SB(nc, "o_rb", [128, 16], F32) as rb, SB(nc, "o_mix", [128, D], F32) as mix, \
            SB(nc, "o_x", [128, D], F32) as xt, SB(nc, "o_junk", [128, D], F32) as junk, \
            SB(nc, "o_mT", [128, 8, 128], F32) as mT, SB(nc, "o_h2", [128, D], F32) as h2, \
            SB(nc, "o_hT", [128, 8, 128], F32) as hT, SB(nc, "o_tmp", [128, D], F32) as tmp, \
            SB(nc, "o_st", [128, 8], F32) as st, SB(nc, "o_r", [128, 12, 16], F32) as rr, \
            SB(nc, "o_gT", [16, 128], F32) as gT:
        t_W, t_c, t_mix, t_x, t_mT, t_h2, t_hT, t_tmp, t_st, t_rr, t_gT = (Tok(n) for n in (
            "W", "c", "mix", "x", "mT", "h2", "hT", "tmp", "st", "rr", "gT"))
        for k in range(8):
            P.load_r(W[:, k, :], I['w_out'][l, k * 128:(k + 1) * 128, :], junk[:], t_W, t_tmp, e='act' if k % 2 else 'dve')
        P.dma(RW[:], I['router_w'].rearrange("(k p) n -> p k n", p=128), (), [t_c])
        P.dma(gain[:], I['mix_norm_g'][l, :].partition_broadcast(128), (), [t_c])
        P.dma(rb[:], I['router_b'][0, :].partition_broadcast(128), (), [t_c])
        for jj, r in enumerate((2, 3, 4, 8, 9, 10)):
            P.dma(mod[:, jj, :], K.MOD[r, :].partition_broadcast(128), (), [t_c])
        groups = ((0, 256), (256, 768), (768, 1024))
        for ti in range(NT):
            mb = 3 if ti < 2 else 0
            rows = slice(ti * 128, (ti + 1) * 128)
            P.dma(mix[:], K.MIX[rows, :], (), [t_mix])
            P.dma(xt[:], K.X[rows, :], (), [t_x])
            for gi, (c0, c1) in enumerate(groups):
                rms_rstd(K, mix[:, c0:c1], t_mix, c1 - c0, junk[:, c0:c1], st[:, gi:gi + 1], t_st)
            for gi, (c0, c1) in enumerate(groups):
                P.stt(mix[:, c0:c1], mix[:, c0:c1], st[:, gi:gi + 1], gain[:, c0:c1], ALU.mult, ALU.mult, [t_mix, t_st, t_c], [t_mix])
            for half in range(2):
                ps, tp = P.ps()
                for j in range(4):
                    kk = half * 4 + j
                    P.tr(ps[:, j * 128:(j + 1) * 128], mix[:, kk * 128:(kk + 1) * 128], K.ident[:], [t_mix, K.t_ident], [tp])
                P.copy(RR(mT[:, half * 4:(half + 1) * 4, :]), ps[:, :].rearrange("p (j t) -> p j t", j=4), [tp], [t_mT], e='act' if half else 'dve')
            for half in range(2):
                ps, tp = P.ps()
                for k in range(8):
                    P.mm(ps[:, :], mT[:, k, :], W[:, k, half * 512:(half + 1) * 512], k == 0, k == 7, [t_mT, t_W], [tp], r=FAST)
                hs = slice(half * 512, (half + 1) * 512)
                P.tt(tmp[:, hs], ps[:, :], mod[:, mb + 0, hs], ALU.mult, [tp, t_c], [t_tmp])
                P.tt(xt[:, hs], xt[:, hs], tmp[:, hs], ALU.add, [t_x, t_tmp], [t_x])
            P.dma(K.X[rows, :], xt[:], [t_x], [("X", ti)], q='pool')
            rms_rstd(K, xt[:], t_x, D, junk[:], st[:, 3:4], t_st)
            P.stt(h2[:], xt[:], st[:, 3:4], mod[:, mb + 1, :], ALU.mult, ALU.mult, [t_x, t_st, t_c], [t_h2])
            P.tt(h2[:], h2[:], mod[:, mb + 2, :], ALU.add, [t_h2, t_c], [t_h2])
            for half in range(2):
                ps, tp = P.ps()
                for j in range(4):
                    kk = half * 4 + j
                    P.tr(ps[:, j * 128:(j + 1) * 128], h2[:, kk * 128:(kk + 1) * 128], K.ident[:], [t_h2, K.t_ident], [tp])
                P.copy(hT[:, half * 4:(half + 1) * 4, :], ps[:, :].rearrange("p (j t) -> p j t", j=4), [tp], [t_hT], e='act' if half else 'dve')
            P.dma(K.H2T[:, ti * 128:(ti + 1) * 128].rearrange("(k p) t -> p k t", p=128), hT[:], [t_hT], [("H2T", ti)], q='pool')
            ps, tp = P.ps()
            for k in range(8):
                P.mm(ps[:, 0:16], hT[:, k, :], RW[:, k, :], k == 0, k == 7, [t_hT, t_c], [tp])
            R = lambda i: rr[:, i, :]
            R4 = lambda i: rr[:, i, :].rearrange("p (g e) -> p g e", g=4)
            trr = [t_rr]
            P.op('dve', lambda g: g.reduce_max(out=st[:, 4:5], in_=ps[:, 0:16], axis=AX.X), [tp], [t_st])
            P.ts(st[:, 4:5], st[:, 4:5], -1.0, None, ALU.mult, None, [t_st], [t_st])
            P.act(R(0), ps[:, 0:16], AF.Exp, [tp, t_st], trr, bias=st[:, 4:5], scale=1.0, accum_out=st[:, 5:6])
            P.recip(st[:, 5:6], st[:, 5:6], [t_st, t_rr], [t_st])
            P.ts(R(0), R(0), st[:, 5:6], None, ALU.mult, None, trr + [t_st], trr)
            P.tt(R(1), R(0), rb[:], ALU.add, trr + [t_c], trr)
            P.op('dve', lambda g: g.reduce_max(out=rr[:, 2, 0:4], in_=R4(1), axis=AX.X), trr, trr)
            P.tt(R4(3), R4(1), rr[:, 2, 0:4].unsqueeze(2).broadcast_to([128, 4, 4]), ALU.is_equal, trr, trr)
            P.stt(R(4), R(3), -1e9, R(1), ALU.mult, ALU.add, trr, trr)
            P.op('dve', lambda g: g.reduce_max(out=rr[:, 5, 0:4], in_=R4(4), axis=AX.X), trr, trr)
            P.tt(rr[:, 6, 0:4], rr[:, 2, 0:4], rr[:, 5, 0:4], ALU.add, trr, trr)
            P.op('dve', lambda g: g.reduce_max(out=st[:, 6:7], in_=rr[:, 6, 0:4], axis=AX.X), trr, [t_st])
            P.ts(rr[:, 7, 0:4], rr[:, 6, 0:4], st[:, 6:7], None, ALU.is_equal, None, trr + [t_st], trr)
            P.tt(R4(8), R4(1), rr[:, 5, 0:4].unsqueeze(2).broadcast_to([128, 4, 4]), ALU.is_ge, trr, trr)
            P.tt(R4(8), R4(8), rr[:, 7, 0:4].unsqueeze(2).broadcast_to([128, 4, 4]), ALU.mult, trr, trr)
            P.tt(R(9), R(8), R(0), ALU.mult, trr, trr)
            P.op('dve', lambda g: g.reduce_sum(out=st[:, 7:8], in_=R(9), axis=AX.X), trr, [t_st])
            P.recip(st[:, 7:8], st[:, 7:8], [t_st], [t_st])
            P.ts(R(9), R(9), st[:, 7:8], None, ALU.mult, None, trr + [t_st], trr)
            ps2, tp2 = P.ps()
            P.tr(ps2[0:16, 0:128], R(9), K.ident[:], trr + [K.t_ident], [tp2])
            P.copy(gT[:], ps2[0:16, 0:128], [tp2], [t_gT], e='act')
            P.dma(K.GTT[:, ti * 128:(ti + 1) * 128], gT[:], [t_gT], [("GTT", ti)], q='pool')


def stage_moe(K, l):
    P, nc, I = K.P, K.P.nc, K.I
    GN = 1024
    with SB(nc, "e_h", [128, 8, GN], F32) as hT, SB(nc, "e_g", [128, 8, GN], F32) as GT, \
            SB(nc, "e_y", [128, GN // 128, D], F32) as Y, SB(nc, "e_wd", [128, 8, D], F32) as wd, \
            SB(nc, "e_wg", [128, 2, 8, 128], F32) as wg, SB(nc, "e_wu", [128, 2, 8, 128], F32) as wu, \
            SB(nc, "e_gt", [16, GN], F32) as gt, SB(nc, "e_gb", [128, GN], F32) as gB, \
            SB(nc, "e_sel", [16, 16, 128], F32) as sel, SB(nc, "e_sa", [128, 2, 512], F32) as sA, \
            SB(nc, "e_mod", [128, 2, D], F32) as mod, SB(nc, "e_x", [128, D], F32) as xt, \
            SB(nc, "e_wst", [128, 6, D], F32) as wst:
        t_wst = [Tok("wst%d" % i) for i in range(6)]
        t_wdc = [Tok("wd%d" % i) for i in range(8)]
        t_h, t_G, t_Y, t_wd, t_gt, t_gB, t_sel, t_mod, t_x = (Tok(n) for n in ("h", "G", "Y", "wd", "gt", "gB", "sel", "mod", "x"))
        t_wg = [Tok("wg0"), Tok("wg1")]
        t_wu = [Tok("wu0"), Tok("wu1")]
        t_sa = [Tok("sa0"), Tok("sa1")]
        for e in range(16):
            P.copy(sel[:, e, :], K.ident[0:16, e:e + 1].broadcast_to([16, 128]), [K.t_ident], [t_sel])
        P.dma(mod[:, 0, :], K.MOD[5, :].partition_broadcast(128), (), [t_mod])
        P.dma(mod[:, 1, :], K.MOD[11, :].partition_broadcast(128), (), [t_mod])
        for g in range((T + GN - 1) // GN):
            t0 = g * GN
            n = min(GN, T - t0)
            nt = n // 128
            for i in range(nt):
                P.load_r(hT[:, :, i * 128:(i + 1) * 128], K.H2T[:, t0 + i * 128:t0 + (i + 1) * 128].rearrange("(k p) t -> p k t", p=128),
                         wst[:, i % 2, :].rearrange("p (k t) -> p k t", k=8), t_h, t_wst[i % 2], e='act' if i % 2 else 'dve')
            P.dma(gt[:, 0:n], K.GTT[:, t0:t0 + n], (), [t_gt])
            P.memset(Y[:, 0:nt, :], 0.0, [t_Y], e='pool')
            for e in range(16):
                for s0 in range(0, n, 512):
                    sn = min(512, n - s0)
                    ps, tp = P.ps()
                    P.mm(ps[:, 0:sn], sel[:, e, :], gt[:, s0:s0 + sn], True, True, [t_sel, t_gt], [tp])
                    P.copy(gB[:, s0:s0 + sn], ps[:, 0:sn], [tp], [t_gB], e='act')
                wgv = I['moe_w_gate'][l, e].rearrange("(k p) n -> p k n", p=128)
                wuv = I['moe_w_up'][l, e].rearrange("(k p) n -> p k n", p=128)
                for c in range(8):
                    b = c % 2
                    P.load_r(wg[:, b, :, :], wgv[:, :, c * 128:(c + 1) * 128], wst[:, 4, :].rearrange("p (k t) -> p k t", k=8), t_wg[b], t_wst[4], e='act')
                    P.load_r(wu[:, b, :, :], wuv[:, :, c * 128:(c + 1) * 128], wst[:, 5, :].rearrange("p (k t) -> p k t", k=8), t_wu[b], t_wst[5], e='act')
                    P.load_r(wd[:, c, :], I['moe_w_down'][l, e, c * 128:(c + 1) * 128, :], wst[:, 2 + c % 2, :], t_wdc[c], t_wst[2 + c % 2], e='dve', q='pool')
                    for si, s0 in enumerate(range(0, n, 512)):
                        sn = min(512, n - s0)
                        psa, tpa = P.ps()
                        psu, tpu = P.ps()
                        for k in range(8):
                            P.mm(psa[:, 0:sn], wg[:, b, k, :], hT[:, k, s0:s0 + sn], k == 0, k == 7, [t_wg[b], t_h], [tpa], r=FAST)
                        for k in range(8):
                            P.mm(psu[:, 0:sn], wu[:, b, k, :], hT[:, k, s0:s0 + sn], k == 0, k == 7, [t_wu[b], t_h], [tpu], r=FAST)
                        sb_ = si % 2
                        P.act(sA[:, sb_, 0:sn], psa[:, 0:sn], AF.Silu, [tpa], [t_sa[sb_]])
                        P.tt(sA[:, sb_, 0:sn], sA[:, sb_, 0:sn], psu[:, 0:sn], ALU.mult, [t_sa[sb_], tpu], [t_sa[sb_]])
                        P.tt(RR(GT[:, c, s0:s0 + sn]), sA[:, sb_, 0:sn], gB[:, s0:s0 + sn], ALU.mult, [t_sa[sb_], t_gB], [t_G])
                for i in range(nt):
                    for half in range(2):
                        ps, tp = P.ps()
                        for c in range(8):
                            P.mm(ps[:, :], GT[:, c, i * 128:(i + 1) * 128], wd[:, c, half * 512:(half + 1) * 512], c == 0, c == 7, [t_G, t_wdc[c]], [tp], r=FAST)
                        hs = slice(half * 512, (half + 1) * 512)
                        P.tt(Y[:, i, hs], Y[:, i, hs], ps[:, :], ALU.add, [t_Y, tp], [t_Y])
            for i in range(nt):
                ti = g * (GN // 128) + i
                mb = 1 if ti < 2 else 0
                rows = slice(ti * 128, (ti + 1) * 128)
                P.dma(xt[:], K.X[rows, :], (), [t_x])
                P.tt(Y[:, i, :], Y[:, i, :], mod[:, mb, :], ALU.mult, [t_Y, t_mod], [t_Y], e='pool')
                P.tt(xt[:], xt[:], Y[:, i, :], ALU.add, [t_x, t_Y], [t_x])
                P.dma(K.X[rows, :], xt[:], [t_x], [("X", ti)], q='pool')


def stage_final(K):
    P, nc, I = K.P, K.P.nc, K.I
    with SB(nc, "f_g", [128, D], F32) as gbc, SB(nc, "f_x", [128, 2, D], F32) as xt, \
            SB(nc, "f_j", [128, D], F32) as junk, SB(nc, "f_s", [128, 2], F32) as st:
        t_g = Tok("g")
        t_x = [Tok("x0"), Tok("x1")]
        t_s = [Tok("s0"), Tok("s1")]
        P.dma(gbc[:], I['final_g'][0, :].partition_broadcast(128), (), [t_g])
        for i in range(NLAT // 128):
            b = i % 2
            P.dma(xt[:, b, :], K.X[NCTX + i * 128:NCTX + (i + 1) * 128, :], (), [t_x[b]])
            rms_rstd(K, xt[:, b, :], t_x[b], D, junk[:], st[:, b:b + 1], t_s[b])
            P.stt(xt[:, b, :], xt[:, b, :], st[:, b:b + 1], gbc[:], ALU.mult, ALU.mult, [t_x[b], t_s[b], t_g], [t_x[b]])
            P.dma(K.out[i * 128:(i + 1) * 128, :], xt[:, b, :], [t_x[b]], [("out", i)], q='pool')


_CONSTS = None


def consts():
    global _CONSTS
    if _CONSTS is not None:
        return _CONSTS
    c = {}
    c['k_ident'] = np.eye(128, dtype=np.float32)
    rc, rs = rope_tables()
    c['k_ropec'], c['k_ropes'] = rc, rs
    j = np.arange(128)[:, None]
    i = np.arange(128)[None, :]
    c['k_maskl'] = (j >= i).astype(np.float32)
    c['k_maskr'] = (j <= i).astype(np.float32)
    c['k_ccL'], c['k_ssL'] = dft_blocks(NLAT, 33)
    c['k_ccC'], c['k_ssC'] = dft_blocks(NCTX, 3)
    f, d, w = hyena_consts(NLAT)
    c['k_featL'], c['k_decL'], c['k_wkL'] = f, d, w.reshape(-1, 1)
    f, d, w = hyena_consts(NCTX)
    c['k_featC'], c['k_decC'], c['k_wkC'] = f, d, w.reshape(-1, 1)
    c['k_iota'] = np.arange(512, dtype=np.float32).reshape(1, 512)
    _CONSTS = c
    return c


def make_in_maps(inputs, cores):
    cs = consts()
    shared = {}
    for k, v in inputs.items():
        if k in ('x', 'c', 'ctx', 'c_ctx'):
            continue
        a = np.ascontiguousarray(np.asarray(v, dtype=np.float32))
        if k in ('router_b', 'final_g'):
            a = a.reshape(1, -1)
        shared[k] = a
    shared.update(cs)
    maps = []
    for b in cores:
        m = dict(shared)
        m['x'] = np.ascontiguousarray(inputs['x'][b], dtype=np.float32)
        m['ctx'] = np.ascontiguousarray(inputs['ctx'][b], dtype=np.float32)
        m['c'] = np.ascontiguousarray(inputs['c'][b], dtype=np.float32).reshape(1, D)
        m['c_ctx'] = np.ascontiguousarray(inputs['c_ctx'], dtype=np.float32).reshape(1, D)
        maps.append(m)
    return maps


def kernel(**inputs):
    P = build()
    maps = make_in_maps(inputs, list(range(8)))
    res = run_bass_kernel_spmd(P.nc, maps, core_ids=list(range(8)))
    return np.stack([r["out"] for r in res.results], axis=0).astype(np.float32)
```

```python
import math
from contextlib import ExitStack
import numpy as np
import concourse.bass as bass
import concourse.mybir as mybir
from concourse.bass_utils import run_bass_kernel_spmd

F32 = mybir.dt.float32
F32R = mybir.dt.float32r
FAST = True
AF = mybir.ActivationFunctionType
ALU = mybir.AluOpType
AX = mybir.AxisListType

D = 1024
NLAT = 4096
NCTX = 256
T = NLAT + NCTX
NT = T // 128
DEPTH = 4
HY_W = 256
ATT_W = 512
S5_W = 256
HY_END = 768
Q_END = HY_END + ATT_W
K_END = Q_END + 128
V_END = K_END + 128
IN_W = V_END + S5_W
EPS = 1e-6
MAGIC = 12582912.0
TWO_PI = 6.283185
NE = 16


class Tok:
    def __init__(self, name):
        self.name = name

    def __repr__(self):
        return self.name


class Prog:
    NDS = 24

    def __init__(self):
        nc = bass.Bass("TRN2", target_bir_lowering=False)
        self.nc = nc
        self.eng = {'pe': nc.tensor, 'dve': nc.vector, 'act': nc.scalar, 'pool': nc.gpsimd, 'sp': nc.sync}
        self.esem = {k: nc.alloc_semaphore("es_" + k) for k in self.eng}
        self.ecnt = {k: 0 for k in self.eng}
        self.dsem = [nc.alloc_semaphore("ds%d" % i) for i in range(self.NDS)]
        self.dval = [0] * self.NDS
        self.dnext = 0
        self.know = {k: {} for k in self.eng}
        self.last_w = {}
        self.readers = {}
        self.n_inst = 0
        self.psum = [nc.alloc_psum_tensor("psb%d" % i, [128, 512], F32) for i in range(8)]
        self.ps_tok = [Tok("ps%d" % i) for i in range(8)]
        self.ps_next = 0
        self.ps_held = set()
        self.dram = {}

    def _sem_of(self, key):
        return self.esem[key] if isinstance(key, str) else self.dsem[key]

    def _deps(self, reads, writes):
        deps = {}

        def add(kv):
            k, v = kv
            if deps.get(k, 0) < v:
                deps[k] = v
        for t in reads:
            if t in self.last_w:
                add(self.last_w[t])
        for t in writes:
            if t in self.last_w:
                add(self.last_w[t])
            for kv in self.readers.get(t, {}).items():
                add(kv)
        return deps

    def _wait(self, e, deps):
        kn = self.know[e]
        for k, v in deps.items():
            if k == e and e == 'pe':
                continue
            if kn.get(k, 0) >= v:
                continue
            self.eng[e].wait_ge(self._sem_of(k), v)
            kn[k] = v
            self.n_inst += 1

    def _commit(self, me, reads, writes):
        for t in writes:
            self.last_w[t] = me
            self.readers[t] = {}
        for t in reads:
            r = self.readers.setdefault(t, {})
            if r.get(me[0], 0) < me[1]:
                r[me[0]] = me[1]

    def op(self, e, fn, reads=(), writes=()):
        self._wait(e, self._deps(reads, writes))
        inst = fn(self.eng[e])
        inst.then_inc(self.esem[e], 1)
        self.ecnt[e] += 1
        self.n_inst += 1
        self._commit((e, self.ecnt[e]), reads, writes)

    def dma(self, out, in_, reads=(), writes=(), q='sp'):
        self._wait(q, self._deps(reads, writes))
        s = self.dnext
        self.dnext = (self.dnext + 1) % self.NDS
        if self.know[q].get(s, 0) < self.dval[s]:
            self.eng[q].wait_ge(self.dsem[s], self.dval[s])
            self.know[q][s] = self.dval[s]
        self.eng[q].dma_start(out=out, in_=in_, allow_slow_non_contiguous=True).then_inc(self.dsem[s], 16)
        self.dval[s] += 16
        self.n_inst += 1
        self._commit((s, self.dval[s]), reads, writes)

    def barrier(self):
        for e in self.eng:
            deps = {k: self.ecnt[k] for k in self.eng if self.ecnt[k] > 0}
            for s in range(self.NDS):
                if self.dval[s] > 0:
                    deps[s] = self.dval[s]
            self._wait(e, deps)
        self.last_w = {}
        self.readers = {}

    def finish(self):
        deps = {s: self.dval[s] for s in range(self.NDS) if self.dval[s] > 0}
        for k in self.eng:
            if self.ecnt[k] > 0:
                deps[k] = self.ecnt[k]
        self._wait('sp', deps)

    def ps(self):
        while True:
            i = self.ps_next
            self.ps_next = (i + 1) % 8
            if i not in self.ps_held:
                return self.psum[i], self.ps_tok[i]

    def ps_hold(self, n):
        got = []
        for i in range(8):
            if i not in self.ps_held and len(got) < n:
                self.ps_held.add(i)
                got.append(i)
        return [(self.psum[i], self.ps_tok[i]) for i in got], got

    def ps_release(self, got):
        for i in got:
            self.ps_held.discard(i)

    def mm(self, out, lhsT, rhs, start, stop, reads, writes, r=False):
        if r:
            lhsT = lhsT.bitcast(F32R)
            rhs = rhs.bitcast(F32R)
        self.op('pe', lambda e: e.matmul(out, lhsT, rhs, start=start, stop=stop), reads, writes)

    def tr(self, out, in_, ident, reads, writes):
        self.op('pe', lambda e: e.transpose(out=out, in_=in_, identity=ident), reads, writes)

    def act(self, out, in_, func, reads, writes, **kw):
        self.op('act', lambda e: e.activation(out=out, in_=in_, func=func, **kw), reads, writes)

    def tt(self, out, in0, in1, op, reads, writes, e='dve'):
        self.op(e, lambda g: g.tensor_tensor(out=out, in0=in0, in1=in1, op=op), reads, writes)

    def ts(self, out, in0, s1, s2, op0, op1, reads, writes, e='dve'):
        if op1 is None:
            self.op(e, lambda g: g.tensor_scalar(out=out, in0=in0, scalar1=s1, scalar2=None, op0=op0), reads, writes)
        else:
            self.op(e, lambda g: g.tensor_scalar(out=out, in0=in0, scalar1=s1, scalar2=s2, op0=op0, op1=op1), reads, writes)

    def stt(self, out, in0, scalar, in1, op0, op1, reads, writes):
        self.op('dve', lambda g: g.scalar_tensor_tensor(out=out, in0=in0, scalar=scalar, in1=in1, op0=op0, op1=op1), reads, writes)

    def copy(self, out, in_, reads, writes, e='dve'):
        if e == 'act':
            self.op('act', lambda g: g.copy(out=out, in_=in_), reads, writes)
        else:
            self.op(e, lambda g: g.tensor_copy(out=out, in_=in_), reads, writes)

    def load_r(self, dst, src, stage, t_dst, t_stage, e='dve', q='sp'):
        if not FAST:
            self.dma(dst, src, (), [t_dst])
            return
        self.dma(stage, src, (), [t_stage], q=q)
        self.copy(dst.bitcast(F32R), stage, [t_stage], [t_dst], e=e)

    def rnd(self, ap, tok, e='dve'):
        if FAST:
            self.copy(ap.bitcast(F32R), ap, [tok], [tok], e=e)

    def memset(self, ap, val, writes, e='dve'):
        self.op(e, lambda g: g.memset(ap, val), (), writes)

    def recip(self, out, in_, reads, writes):
        self.op('dve', lambda g: g.reciprocal(out=out, in_=in_), reads, writes)

    def dram_t(self, name, shape, kind="Internal"):
        t = self.nc.dram_tensor(name, list(shape), F32, kind=kind).ap()
        self.dram[name] = t
        return t


def RR(ap):
    return ap.bitcast(F32R) if FAST else ap


class Ctx:
    def dump(self, name, tile_ap, shape, reads):
        if 'dump' not in self.dbg:
            return
        o = self.P.nc.dram_tensor("dmp_" + name, list(shape), F32, kind="ExternalOutput").ap()
        self.P.dma(o, tile_ap, reads, (), q='sp')


_UID = [0]


def SB(nc, name, shape, dt):
    _UID[0] += 1
    return nc.sbuf_tensor("%s_%d" % (name, _UID[0]), shape, dt)


def rope_tables():
    cosT = np.ones((128, T), np.float64)
    sinT = np.zeros((128, T), np.float64)
    n = np.arange(NLAT)
    row = n // 64
    col = n % 64
    for r in range(128):
        d = r % 64
        dd = d if d < 32 else d - 32
        pos = row if d < 32 else col
        j = dd % 16
        first = dd < 16
        inv = 10000.0 ** (-(j / 16.0))
        ang = (pos.astype(np.float32) * np.float32(inv)).astype(np.float64)
        cosT[r, NCTX:] = np.cos(ang)
        sinT[r, NCTX:] = (-np.sin(ang)) if first else np.sin(ang)
    return cosT.astype(np.float32), sinT.astype(np.float32)


def perm_cols(width):
    src = np.zeros(width, np.int64)
    for c in range(width):
        d = c % 64
        dd = d % 32
        src[c] = c + 16 if dd < 16 else c - 16
    return src


def dft_blocks(nhalf, nchunk):
    idx = np.arange(nchunk * 128)
    valid = (idx <= nhalf)
    prod = np.outer(idx, idx).astype(np.float64)
    ang = 2.0 * np.pi * (prod % (2 * nhalf)) / (2 * nhalf)
    m = np.outer(valid, valid)
    cc = (np.cos(ang) * m).astype(np.float32)
    ss = (np.sin(ang) * m).astype(np.float32)

    def blk(mat):
        return np.ascontiguousarray(mat.reshape(nchunk, 128, nchunk, 128).transpose(2, 1, 0, 3))
    return blk(cc), blk(ss)


def hyena_consts(L):
    pos = np.arange(L, dtype=np.float32)
    t = pos / np.float32(max(L - 1, 1))
    bands = np.linspace(1e-4, 15, 16, dtype=np.float32)
    ang = (np.float32(2.0 * math.pi / L) * pos[:, None] * bands[None, :]).astype(np.float32)
    feats = np.concatenate([t[:, None], np.cos(ang), -np.sin(ang)], axis=-1).astype(np.float32)
    dmin = math.log(1e-2) / 1.5
    dmax = math.log(1e-2) / 0.3
    decay = np.abs(np.linspace(dmin, dmax, HY_W, dtype=np.float32))
    dec = np.exp(-t[:, None] * decay[None, :]).astype(np.float32)
    nk = L // 128 + 1
    wk = np.zeros(nk * 128, np.float32)
    wk[:L + 1] = 2.0 / (2 * L)
    wk[0] = 1.0 / (2 * L)
    wk[L] = 1.0 / (2 * L)
    return np.ascontiguousarray(feats.T), dec, wk


def build(n_layers=DEPTH, dbg=()):
    P = Prog()
    nc = P.nc
    K = Ctx()
    K.P = P
    K.dbg = dbg
    K.dbg_out = {}

    def inp(name, shape):
        return nc.dram_tensor(name, list(shape), F32, kind="ExternalInput").ap()

    I = {}
    I['x'] = inp('x', [NLAT, D])
    I['ctx'] = inp('ctx', [NCTX, D])
    I['c'] = inp('c', [1, D])
    I['c_ctx'] = inp('c_ctx', [1, D])
    shapes = dict(
        norm1_g=[4, D], norm2_g=[4, D], ada_w=[4, D, 6 * D], ada_b=[4, 6 * D], w_in=[4, D, IN_W], w_out=[4, D, D],
        mix_norm_g=[4, D], hy_conv_w=[4, 3, 768], hy_conv_b=[4, 768], hy_f_w1=[4, 33, 64], hy_f_b1=[4, 64],
        hy_f_freq1=[4, 64], hy_f_w2=[4, 64, 64], hy_f_b2=[4, 64], hy_f_freq2=[4, 64], hy_f_w3=[4, 64, 1024],
        hy_bias=[4, 2, 256], attn_sink=[4, 8], s5_lam_re=[4, 2, 16, 64], s5_lam_im=[4, 2, 16, 64], s5_log_dt=[4, 2, 16],
        s5_b_re=[4, 2, 16, 64, 16], s5_b_im=[4, 2, 16, 64, 16], s5_c_re=[4, 2, 16, 16, 64], s5_c_im=[4, 2, 16, 16, 64],
        s5_d=[4, 256], s5_glu_w=[4, 256, 256], s5_glu_b=[4, 256], router_w=[D, 16], router_b=[1, 16],
        moe_w_gate=[4, 16, D, D], moe_w_up=[4, 16, D, D], moe_w_down=[4, 16, D, D], final_g=[1, D],
        k_ident=[128, 128], k_ropec=[128, T], k_ropes=[128, T], k_maskl=[128, 128], k_maskr=[128, 128],
        k_ccL=[33, 128, 33, 128], k_ssL=[33, 128, 33, 128], k_ccC=[3, 128, 3, 128], k_ssC=[3, 128, 3, 128],
        k_featL=[33, NLAT], k_decL=[NLAT, 256], k_wkL=[33 * 128, 1], k_featC=[33, NCTX], k_decC=[NCTX, 256], k_wkC=[3 * 128, 1],
        k_iota=[1, 512],
    )
    for k, s in shapes.items():
        I[k] = inp(k, s)
    K.I = I
    out = nc.dram_tensor("out", [NLAT, D], F32, kind="ExternalOutput").ap()
    K.out = out

    K.X = P.dram_t("sX", [T, D])
    K.ZT = P.dram_t("sZT", [T, 768])
    K.QF = P.dram_t("sQF", [512, T])
    K.KF = P.dram_t("sKF", [128, T])
    K.VT = P.dram_t("sVT", [T, 128])
    K.UF = P.dram_t("sUF", [256, T])
    K.MIX = P.dram_t("sMIX", [T, D])
    K.H2T = P.dram_t("sH2T", [D, T])
    K.GTT = P.dram_t("sGTT", [16, T])

    K.ident = nc.alloc_sbuf_tensor("ident", [128, 128], F32)
    K.ones = nc.alloc_sbuf_tensor("ones", [128, 128], F32)
    K.MOD = P.dram_t("sMOD", [12, D])
    K.ZC = P.dram_t("sZC", [T, 768])
    K.KRE = P.dram_t("sKRE", [33 * 128, 512])
    K.KIM = P.dram_t("sKIM", [33 * 128, 512])
    K.t_ident = Tok("ident")
    K.t_ones = Tok("ones")
    K.t_mod = Tok("modbc")
    P.dma(K.ident[:], I['k_ident'][:, :], (), [K.t_ident])
    P.memset(K.ones[:], 1.0, [K.t_ones])

    tX = [("X", i) for i in range(NT)]
    K.tX = tX
    P.dma(K.X[0:NCTX, :], I['ctx'][:, :], (), tX[0:2])
    for i in range(4):
        P.dma(K.X[NCTX + i * 1024: NCTX + (i + 1) * 1024, :], I['x'][i * 1024:(i + 1) * 1024, :], (), tX[2 + 8 * i: 2 + 8 * (i + 1)])
    P.barrier()

    for l in range(n_layers):
        stage_mod(K, l)
        P.barrier()
        stage_inproj(K, l)
        P.barrier()
        if 'inproj' in dbg and l == 0:
            break
        stage_hyena(K, l, NLAT, NCTX, 'L')
        P.barrier()
        stage_hyena(K, l, NCTX, 0, 'C')
        P.barrier()
        if 'hyena' in dbg and l == 0:
            break
        stage_attn(K, l)
        P.barrier()
        if 'attn' in dbg and l == 0:
            break
        stage_s5(K, l)
        P.barrier()
        if 's5' in dbg and l == 0:
            break
        stage_outproj(K, l)
        P.barrier()
        if 'outproj' in dbg and l == 0:
            break
        stage_moe(K, l)
        P.barrier()
    if not dbg:
        stage_final(K)
    for name in dbg:
        if name in P.dram and name not in ('inproj', 'hyena', 'attn', 's5', 'outproj'):
            src = P.dram[name]
            o = nc.dram_tensor("dbg_" + name, list(src.shape), F32, kind="ExternalOutput").ap()
            P.barrier()
            P.dma(o, src, (), ())
    P.finish()
    return P


def stage_mod(K, l):
    P, nc, I = K.P, K.P.nc, K.I
    with SB(nc, "m_cs", [128, 2, 8], F32) as cs, SB(nc, "m_w", [128, 6 * D], F32) as wk, \
            SB(nc, "m_ws", [128, 6 * D], F32) as ws, SB(nc, "m_b", [1, 6 * D], F32) as bt, \
            SB(nc, "m_raw", [128, 6 * D], F32) as raw, SB(nc, "m_g", [128, 2, D], F32) as gn, \
            SB(nc, "m_o", [128, 12, D], F32) as mo:
        t_cs, t_w, t_ws, t_b, t_raw, t_g = Tok("cs"), Tok("w"), Tok("ws"), Tok("b"), Tok("raw"), Tok("g")
        P.dma(cs[:, 0, :], I['c'][0, :].rearrange("(c p) -> p c", p=128), (), [t_cs])
        P.dma(cs[:, 1, :], I['c_ctx'][0, :].rearrange("(c p) -> p c", p=128), (), [t_cs])
        P.act(cs[:], cs[:], AF.Silu, [t_cs], [t_cs])
        P.dma(bt[:], I['ada_b'][l:l + 1, :], (), [t_b])
        P.dma(gn[:, 0, :], I['norm1_g'][l, :].partition_broadcast(128), (), [t_g])
        P.dma(gn[:, 1, :], I['norm2_g'][l, :].partition_broadcast(128), (), [t_g])
        for who in range(2):
            for j in range(12):
                ps, tp = P.ps()
                for k in range(8):
                    P.dma(wk[:, 0:512], I['ada_w'][l, k * 128:(k + 1) * 128, j * 512:(j + 1) * 512], (), [t_w])
                    P.ts(ws[:, 0:512], wk[:, 0:512], cs[:, who, k:k + 1], None, ALU.mult, None, [t_w, t_cs], [t_ws])
                    P.mm(ps[:, :], K.ones[:, :], ws[:, 0:512], k == 0, False, [t_ws, K.t_ones], [tp])
                P.mm(ps[:, :], K.ones[0:1, :], bt[0:1, j * 512:(j + 1) * 512], False, True, [t_b, K.t_ones], [tp])
                P.copy(raw[:, j * 512:(j + 1) * 512], ps[:, :], [tp], [t_raw])
            base = who * 6
            m = mo
            for half in range(2):
                sh = raw[:, (3 * half) * D:(3 * half + 1) * D]
                sc = raw[:, (3 * half + 1) * D:(3 * half + 2) * D]
                gg = raw[:, (3 * half + 2) * D:(3 * half + 3) * D]
                P.stt(m[:, base + 3 * half + 0, :], sc, 1.0, gn[:, half, :], ALU.add, ALU.mult, [t_raw, t_g], [K.t_mod])
                P.copy(m[:, base + 3 * half + 1, :], sh, [t_raw], [K.t_mod])
                P.copy(m[:, base + 3 * half + 2, :], gg, [t_raw], [K.t_mod])
        P.dma(K.MOD.rearrange("(o j) d -> o j d", o=1), mo[0:1, :, :], [K.t_mod], [("MOD", 0)], q='pool')
        P.barrier()


def rms_rstd(K, xt, t_x, width, junk, ss, t_s):
    P = K.P
    P.act(junk, xt, AF.Square, [t_x], [t_s], accum_out=ss[:, 0:1])
    P.ts(ss[:, 0:1], ss[:, 0:1], 1.0 / width, EPS, ALU.mult, ALU.add, [t_s], [t_s])
    P.act(ss[:, 0:1], ss[:, 0:1], AF.Sqrt, [t_s], [t_s])
    P.recip(ss[:, 0:1], ss[:, 0:1], [t_s], [t_s])


def stage_inproj(K, l):
    P, nc, I = K.P, K.P.nc, K.I
    src = perm_cols(640)
    with SB(nc, "i_w", [128, 8, IN_W], F32) as W, SB(nc, "i_wp", [128, 8, 640], F32) as WP, \
            SB(nc, "i_x", [128, 2, D], F32) as xt, SB(nc, "i_h", [128, 2, D], F32) as ht, \
            SB(nc, "i_hT", [128, 8, 512], F32) as hT, SB(nc, "i_ss", [128, 2], F32) as ss, \
            SB(nc, "i_junk", [128, D], F32) as junk, SB(nc, "i_o", [128, 2, 768], F32) as ot, \
            SB(nc, "i_rc", [128, 512], F32) as rc, SB(nc, "i_rs", [128, 512], F32) as rs, \
            SB(nc, "i_t1", [128, 512], F32) as t1, SB(nc, "i_t2", [128, 512], F32) as t2, \
            SB(nc, "i_mod", [128, 4, D], F32) as modbc, SB(nc, "i_wst", [128, 2, IN_W], F32) as wst:
        t_W, t_WP = Tok("W"), Tok("WP")
        t_wst = [Tok("wst0"), Tok("wst1")]
        for jj, r in enumerate((0, 1, 6, 7)):
            P.dma(modbc[:, jj, :], K.MOD[r, :].partition_broadcast(128), (), [K.t_mod])
        t_x = [Tok("x0"), Tok("x1")]
        t_h = [Tok("h0"), Tok("h1")]
        t_s = [Tok("s0"), Tok("s1")]
        t_hT, t_o, t_rc, t_rs, t_t1, t_t2 = Tok("hT"), [Tok("o0"), Tok("o1")], Tok("rc"), Tok("rs"), Tok("t1"), Tok("t2")
        for k in range(8):
            P.load_r(W[:, k, :], I['w_in'][l, k * 128:(k + 1) * 128, :], wst[:, k % 2, :], t_W, t_wst[k % 2], e='act' if k % 2 else 'dve')
        wv = I['w_in'][l, :, HY_END:HY_END + 640].rearrange("(k p) (b two s) -> k p b two s", p=128, two=2, s=16)
        for k in range(8):
            dst = wst[:, k % 2, 0:640].rearrange("p (b two s) -> p b two s", two=2, s=16)
            P.dma(dst[:, :, 0, :], wv[k, :, :, 1, :], (), [t_wst[k % 2]])
            P.dma(dst[:, :, 1, :], wv[k, :, :, 0, :], (), [t_wst[k % 2]])
            P.copy(RR(WP[:, k, :]), wst[:, k % 2, 0:640], [t_wst[k % 2]], [t_WP], e='act' if k % 2 else 'dve')
        ngroups = (T + 511) // 512
        for g in range(ngroups):
            t0 = g * 512
            ntok = min(512, T - t0)
            ntile = ntok // 128
            for i in range(ntile):
                ti = g * 4 + i
                b = i % 2
                isctx = ti < 2
                mb = 2 if isctx else 0
                P.dma(xt[:, b, :], K.X[ti * 128:(ti + 1) * 128, :], [K.tX[ti]], [t_x[b]])
                rms_rstd(K, xt[:, b, :], t_x[b], D, junk[:], ss[:, b:b + 1], t_s[b])
                P.stt(ht[:, b, :], xt[:, b, :], ss[:, b:b + 1], modbc[:, mb + 0, :], ALU.mult, ALU.mult,
                      [t_x[b], t_s[b], K.t_mod], [t_h[b]])
                P.tt(ht[:, b, :], ht[:, b, :], modbc[:, mb + 1, :], ALU.add, [t_h[b], K.t_mod], [t_h[b]])
                for half in range(2):
                    ps, tp = P.ps()
                    for j in range(4):
                        kk = half * 4 + j
                        P.tr(ps[:, j * 128:(j + 1) * 128], ht[:, b, kk * 128:(kk + 1) * 128], K.ident[:], [t_h[b], K.t_ident], [tp])
                    P.copy(RR(hT[:, half * 4:(half + 1) * 4, i * 128:(i + 1) * 128]),
                           ps[:, :].rearrange("p (j t) -> p j t", j=4), [tp], [t_hT], e='act' if half else 'dve')
            for i in range(ntile):
                ti = g * 4 + i
                b = i % 2
                for (c0, cw, dst, dcol) in ((0, 512, K.ZT, 0), (512, 256, K.ZT, 512), (K_END, 128, K.VT, 0)):
                    ps, tp = P.ps()
                    for k in range(8):
                        P.mm(ps[:, 0:cw], hT[:, k, i * 128:(i + 1) * 128], W[:, k, c0:c0 + cw], k == 0, k == 7, [t_hT, t_W], [tp], r=FAST)
                    P.copy(ot[:, b, 0:cw], ps[:, 0:cw], [tp], [t_o[b]], e='act')
                    P.dma(dst[ti * 128:(ti + 1) * 128, dcol:dcol + cw], ot[:, b, 0:cw], [t_o[b]], [(dst.tensor.name, ti)], q='pool')
            P.dma(rc[:, 0:ntok], I['k_ropec'][:, t0:t0 + ntok], (), [t_rc])
            P.dma(rs[:, 0:ntok], I['k_ropes'][:, t0:t0 + ntok], (), [t_rs])
            for cidx in range(5):
                c0 = HY_END + cidx * 128
                ps, tp = P.ps()
                ps2, tp2 = P.ps()
                for k in range(8):
                    P.mm(ps[:, 0:ntok], W[:, k, c0:c0 + 128], hT[:, k, 0:ntok], k == 0, k == 7, [t_hT, t_W], [tp], r=FAST)
                for k in range(8):
                    P.mm(ps2[:, 0:ntok], WP[:, k, cidx * 128:(cidx + 1) * 128], hT[:, k, 0:ntok], k == 0, k == 7, [t_hT, t_WP], [tp2], r=FAST)
                P.tt(t1[:, 0:ntok], ps[:, 0:ntok], rc[:, 0:ntok], ALU.mult, [tp, t_rc], [t_t1])
                P.tt(t2[:, 0:ntok], ps2[:, 0:ntok], rs[:, 0:ntok], ALU.mult, [tp2, t_rs], [t_t2])
                P.tt(t1[:, 0:ntok], t1[:, 0:ntok], t2[:, 0:ntok], ALU.add, [t_t1, t_t2], [t_t1])
                if cidx < 4:
                    P.dma(K.QF[cidx * 128:(cidx + 1) * 128, t0:t0 + ntok], t1[:, 0:ntok], [t_t1], [("QF", g)], q='pool')
                else:
                    P.dma(K.KF[:, t0:t0 + ntok], t1[:, 0:ntok], [t_t1], [("KF", g)], q='pool')
            for cidx in range(2):
                c0 = V_END + cidx * 128
                ps, tp = P.ps()
                for k in range(8):
                    P.mm(ps[:, 0:ntok], W[:, k, c0:c0 + 128], hT[:, k, 0:ntok], k == 0, k == 7, [t_hT, t_W], [tp], r=FAST)
                P.copy(t2[:, 0:ntok], ps[:, 0:ntok], [tp], [t_t2], e='act')
                P.dma(K.UF[cidx * 128:(cidx + 1) * 128, t0:t0 + ntok], t2[:, 0:ntok], [t_t2], [("UF", g)], q='pool')


def sin_chain(K, ps_ap, bcol, fcol, v, r, hid, n, t_ps, t_consts, t_v, t_r, t_hid):
    P = K.P
    P.act(v[:, 0:n], ps_ap, AF.Identity, [t_ps] + t_consts, [t_v], bias=bcol, scale=fcol)
    P.ts(r[:, 0:n], v[:, 0:n], MAGIC, MAGIC, ALU.add, ALU.subtract, [t_v], [t_r])
    P.tt(v[:, 0:n], v[:, 0:n], r[:, 0:n], ALU.subtract, [t_v, t_r], [t_v])
    P.act(hid[:, 0:n], v[:, 0:n], AF.Sin, [t_v], [t_hid], scale=TWO_PI)


def stage_hyena(K, l, L, row0, tag):
    P, nc, I = K.P, K.P.nc, K.I
    NCH = L // 128
    NK = NCH + 1
    CC, SS = I['k_cc' + tag], I['k_ss' + tag]
    featT, dec, wkc = I['k_feat' + tag], I['k_dec' + tag], I['k_wk' + tag]
    with SB(nc, "ha_w", [128, 4, 768], F32) as wb, SB(nc, "ha_z", [128, 3, 768], F32) as z, \
            SB(nc, "ha_t", [128, 2, 768], F32) as tt_:
        t_wb, t_z, t_t = Tok("wb"), [Tok("zm"), Tok("z0"), Tok("zp")], [Tok("ta"), Tok("tb")]
        for j in range(3):
            P.dma(wb[:, j, :], I['hy_conv_w'][l, j, :].partition_broadcast(128), (), [t_wb])
        P.dma(wb[:, 3, :], I['hy_conv_b'][l, :].partition_broadcast(128), (), [t_wb])
        for i in range(NCH):
            r0 = row0 + i * 128
            if i == 0:
                P.memset(z[0:1, 0, :], 0.0, [t_z[0]])
                P.dma(z[1:128, 0, :], K.ZT[r0:r0 + 127, 0:768], (), [t_z[0]])
            else:
                P.dma(z[:, 0, :], K.ZT[r0 - 1:r0 + 127, 0:768], (), [t_z[0]])
            P.dma(z[:, 1, :], K.ZT[r0:r0 + 128, 0:768], (), [t_z[1]])
            if i == NCH - 1:
                P.memset(z[:, 2, :], 0.0, [t_z[2]])
                P.dma(z[0:127, 2, :], K.ZT[r0 + 1:r0 + 128, 0:768], (), [t_z[2]])
            else:
                P.dma(z[:, 2, :], K.ZT[r0 + 1:r0 + 129, 0:768], (), [t_z[2]])
            P.tt(tt_[:, 0, :], z[:, 0, :], wb[:, 0, :], ALU.mult, [t_z[0], t_wb], [t_t[0]])
            P.tt(tt_[:, 1, :], z[:, 1, :], wb[:, 1, :], ALU.mult, [t_z[1], t_wb], [t_t[1]], e='pool')
            P.tt(tt_[:, 0, :], tt_[:, 0, :], tt_[:, 1, :], ALU.add, [t_t[0], t_t[1]], [t_t[0]])
            P.tt(tt_[:, 1, :], z[:, 2, :], wb[:, 2, :], ALU.mult, [t_z[2], t_wb], [t_t[1]], e='pool')
            P.tt(tt_[:, 0, :], tt_[:, 0, :], tt_[:, 1, :], ALU.add, [t_t[0], t_t[1]], [t_t[0]])
            P.tt(tt_[:, 0, :], tt_[:, 0, :], wb[:, 3, :], ALU.add, [t_t[0], t_wb], [t_t[0]])
            P.dma(K.ZC[r0:r0 + 128, :], tt_[:, 0, :], [t_t[0]], [("ZC", i)], q='pool')
    P.barrier()
    HH = (NCH + 1) // 2
    with ExitStack() as es_:
        taps = es_.enter_context(SB(nc, "hb_taps", [128, NCH, 1024], F32))
        tabt = es_.enter_context(SB(nc, "hb_cc", [128, 2, HH, 128], F32))
        tabs_ = es_.enter_context(SB(nc, "hb_ss", [128, 2, HH, 128], F32))
        ft = es_.enter_context(SB(nc, "hb_f", [33, 512], F32))
        w1 = es_.enter_context(SB(nc, "hb_w1", [33, 64], F32))
        w2 = es_.enter_context(SB(nc, "hb_w2", [64, 64], F32))
        w3 = es_.enter_context(SB(nc, "hb_w3", [64, 1024], F32))
        cst = es_.enter_context(SB(nc, "hb_c", [64, 8], F32))
        v = es_.enter_context(SB(nc, "hb_v", [64, 512], F32))
        r = es_.enter_context(SB(nc, "hb_r", [64, 512], F32))
        h1 = es_.enter_context(SB(nc, "hb_h1", [64, 512], F32))
        h2 = es_.enter_context(SB(nc, "hb_h2", [64, 512], F32))
        dct = es_.enter_context(SB(nc, "hb_dec", [128, 256], F32))
        ab = es_.enter_context(SB(nc, "hb_abs", [128, 1024], F32))
        rn = es_.enter_context(SB(nc, "hb_rn", [128, 512], F32))
        tmp = es_.enter_context(SB(nc, "hb_tmp", [128, 512], F32))
        wkt = es_.enter_context(SB(nc, "hb_wk", [128, NK], F32))
        ko = es_.enter_context(SB(nc, "hb_o", [128, 2, 512], F32))
        t_taps = [Tok("taps%d" % c) for c in range(NCH)]
        t_cc, t_ss, t_f, t_w, t_c = Tok("cc"), Tok("ss"), Tok("f"), Tok("w"), Tok("c")
        t_v, t_r, t_h1, t_h2, t_dec, t_ab, t_rn, t_tmp, t_wk = (Tok(n) for n in ("v", "r", "h1", "h2", "dec", "ab", "rn", "tmp", "wk"))
        t_ko = [Tok("ko0"), Tok("ko1")]
        P.dma(w1[:], I['hy_f_w1'][l, :, :], (), [t_w])
        P.dma(w2[:], I['hy_f_w2'][l, :, :], (), [t_w])
        P.dma(w3[:], I['hy_f_w3'][l, :, :], (), [t_w])
        for j, nm in enumerate(('hy_f_b1', 'hy_f_freq1', 'hy_f_b2', 'hy_f_freq2')):
            P.dma(cst[:, j:j + 1], I[nm][l, :].rearrange("(p o) -> p o", o=1), (), [t_c])
        P.ts(cst[:, 4:5], cst[:, 1:2], 1.0 / (2.0 * math.pi), None, ALU.mult, None, [t_c], [t_c])
        P.ts(cst[:, 5:6], cst[:, 3:4], 1.0 / (2.0 * math.pi), None, ALU.mult, None, [t_c], [t_c])
        P.tt(cst[:, 6:7], cst[:, 0:1], cst[:, 4:5], ALU.mult, [t_c], [t_c])
        P.tt(cst[:, 7:8], cst[:, 2:3], cst[:, 5:6], ALU.mult, [t_c], [t_c])
        P.dma(wkt[:], wkc[:, 0].rearrange("(c p) -> p c", p=128), (), [t_wk])
        ng = (L + 511) // 512
        for g in range(ng):
            n0 = g * 512
            n = min(512, L - n0)
            P.dma(ft[:, 0:n], featT[:, n0:n0 + n], (), [t_f])
            ps, tp = P.ps()
            P.mm(ps[0:64, 0:n], w1[0:33, :], ft[0:33, 0:n], True, True, [t_w, t_f], [tp])
            sin_chain(K, ps[0:64, 0:n], cst[:, 6:7], cst[:, 4:5], v, r, h1, n, tp, [t_c], t_v, t_r, t_h1)
            ps, tp = P.ps()
            P.mm(ps[0:64, 0:n], w2[:, :], h1[:, 0:n], True, True, [t_w, t_h1], [tp])
            sin_chain(K, ps[0:64, 0:n], cst[:, 7:8], cst[:, 5:6], v, r, h2, n, tp, [t_c], t_v, t_r, t_h2)
            for sub in range(n // 128):
                c = (n0 // 128) + sub
                P.dma(dct[:], dec[c * 128:(c + 1) * 128, :], (), [t_dec])
                for half in range(2):
                    ps, tp = P.ps()
                    P.mm(ps[:, :], h2[:, sub * 128:(sub + 1) * 128], w3[:, half * 512:(half + 1) * 512], True, True, [t_h2, t_w], [tp])
                    P.tt(RR(taps[:, c, half * 512:(half + 1) * 512].rearrange("p (d c) -> p d c", d=2)),
                         ps[:, :].rearrange("p (d c) -> p d c", d=2),
                         dct[:, :].unsqueeze(1).broadcast_to([128, 2, 256]), ALU.mult, [tp, t_dec], [t_taps[c]])
        tv0 = taps[0:1, 0, :].rearrange("p (o d c) -> p o d c", o=2, d=2)
        P.ts(RR(tv0[:, :, 1, :]), tv0[:, :, 1, :], 0.0, None, ALU.mult, None, [t_taps[0]], [t_taps[0]])
        held, hid_ = P.ps_hold(2)
        for c in range(NCH):
            P.act(ab[:], taps[:, c, :], AF.Abs, [t_taps[c]], [t_ab])
            for half in range(2):
                P.mm(held[half][0][:, :], K.ones[:, :], ab[:, half * 512:(half + 1) * 512], c == 0, c == NCH - 1, [t_ab, K.t_ones], [held[half][1]])
        for o in range(2):
            P.copy(ab[:, o * 512:o * 512 + 256], held[o][0][:, 0:256], [held[o][1]], [t_ab])
            P.tt(rn[:, o * 256:(o + 1) * 256], ab[:, o * 512:o * 512 + 256], held[o][0][:, 256:512], ALU.add, [held[o][1], t_ab], [t_rn])
        P.ps_release(hid_)
        P.recip(rn[:], rn[:], [t_rn], [t_rn])
        for c in range(NCH):
            tv = taps[:, c, :].rearrange("p (o d c) -> p o d c", o=2, d=2)
            tm = tmp[:, :].rearrange("p (o c) -> p o c", o=2)
            P.tt(tm, tv[:, :, 0, :], tv[:, :, 1, :], ALU.add, [t_taps[c]], [t_tmp])
            P.tt(RR(tv[:, :, 1, :]), tv[:, :, 1, :], tv[:, :, 0, :], ALU.subtract, [t_taps[c]], [t_taps[c]])
            P.copy(RR(tv[:, :, 0, :]), tm, [t_tmp], [t_taps[c]])
        rn3 = rn[:, :].rearrange("p (o c) -> p o c", o=2)
        t_st2 = [Tok("fst0"), Tok("fst1")]
        for kc in range(NK):
            for part, TAB, ttab, d in ((0, CC, t_cc, 0), (1, SS, t_ss, 1)):
                ps, tp = P.ps()
                for hf in range(2):
                    cA, cB = hf * HH, min((hf + 1) * HH, NCH)
                    if cA >= cB:
                        continue
                    P.load_r(tabt[:, part, 0:cB - cA, :], TAB[kc, :, cA:cB, :], tabs_[:, part, 0:cB - cA, :], ttab, t_st2[part],
                             e='act' if part == 0 else 'dve')
                    for c in range(cA, cB):
                        tv = taps[:, c, :].rearrange("p (o d c) -> p o d c", o=2, d=2)
                        P.mm(ps[:, :].rearrange("p (o c) -> p o c", o=2), tabt[:, part, c - cA, :], tv[:, :, d, :], c == 0, c == NCH - 1,
                             [ttab, t_taps[c]], [tp], r=FAST)
                P.stt(ko[:, part, :].rearrange("p (o c) -> p o c", o=2), ps[:, :].rearrange("p (o c) -> p o c", o=2),
                      wkt[:, kc:kc + 1], rn3, ALU.mult, ALU.mult, [tp, t_wk, t_rn], [t_ko[part]])
                dst = K.KRE if part == 0 else K.KIM
                P.dma(dst[kc * 128:(kc + 1) * 128, :], ko[:, part, :], [t_ko[part]], [(dst.tensor.name, kc)], q='pool')
    P.barrier()
    HK = (NK + 1) // 2
    with SB(nc, "hc_a", [128, NCH, 256], F32) as a, SB(nc, "hc_p1", [128, NK, 256], F32) as p1, \
            SB(nc, "hc_p2", [128, NK, 256], F32) as p2, SB(nc, "hc_tb", [128, 4, HK, 128], F32) as tb, \
            SB(nc, "hc_k", [128, 2, 256], F32) as kk, \
            SB(nc, "hc_t", [128, 2, 256], F32) as tq, SB(nc, "hc_b", [128, 2, 256], F32) as bb, \
            SB(nc, "hc_x", [128, 256], F32) as xg, SB(nc, "hc_y", [128, 256], F32) as yy, \
            SB(nc, "hc_stg", [128, 4, HK, 128], F32) as stg:
        t_stg = [Tok("stg%d" % i) for i in range(4)]
        t_tb = [Tok("tb%d" % i) for i in range(4)]
        ldn = [0]

        def load_tabs(blk, cA, cB):
            pb = ldn[0] % 2
            ldn[0] += 1
            P.load_r(tb[:, 2 * pb, 0:cB - cA, :], CC[blk, :, cA:cB, :], stg[:, 2 * pb, 0:cB - cA, :], t_tb[2 * pb], t_stg[2 * pb], e='act')
            P.load_r(tb[:, 2 * pb + 1, 0:cB - cA, :], SS[blk, :, cA:cB, :], stg[:, 2 * pb + 1, 0:cB - cA, :], t_tb[2 * pb + 1], t_stg[2 * pb + 1], e='dve')
            return tb[:, 2 * pb], tb[:, 2 * pb + 1], t_tb[2 * pb], t_tb[2 * pb + 1]
        t_a = [Tok("a%d" % c) for c in range(NCH)]
        t_p1, t_p2, t_k, t_b, t_x, t_y = (Tok(n) for n in ("p1", "p2", "k", "b", "x", "y"))
        t_q = [Tok("q0"), Tok("q1")]
        for o in range(2):
            P.dma(bb[:, o, :], I['hy_bias'][l, o, :].partition_broadcast(128), (), [t_b])
        for c in range(NCH):
            P.load_r(a[:, c, :], K.ZC[row0 + c * 128: row0 + (c + 1) * 128, 0:256], xg[:], t_a[c], t_x, e='act' if c % 2 else 'dve')
        for o in range(2):
            for kc in range(NK):
                P.dma(kk[:, 0, :], K.KRE[kc * 128:(kc + 1) * 128, o * 256:(o + 1) * 256], (), [t_k])
                P.dma(kk[:, 1, :], K.KIM[kc * 128:(kc + 1) * 128, o * 256:(o + 1) * 256], (), [t_k])
                psr, tpr = P.ps()
                psi, tpi = P.ps()
                for cA in range(0, NCH, HK):
                    cB = min(cA + HK, NCH)
                    cct, sst, t_cc, t_ss = load_tabs(kc, cA, cB)
                    for c in range(cA, cB):
                        P.mm(psr[:, 0:256], cct[:, c - cA, :], a[:, c, :], c == 0, c == NCH - 1, [t_cc, t_a[c]], [tpr], r=FAST)
                    for c in range(cA, cB):
                        P.mm(psi[:, 0:256], sst[:, c - cA, :], a[:, c, :], c == 0, c == NCH - 1, [t_ss, t_a[c]], [tpi], r=FAST)
                P.tt(tq[:, 0, :], psr[:, 0:256], kk[:, 0, :], ALU.mult, [tpr, t_k], [t_q[0]])
                P.tt(tq[:, 1, :], psi[:, 0:256], kk[:, 1, :], ALU.mult, [tpi, t_k], [t_q[1]])
                P.tt(RR(p1[:, kc, :]), tq[:, 0, :], tq[:, 1, :], ALU.add, [t_q[0], t_q[1]], [t_p1])
                P.tt(tq[:, 0, :], psi[:, 0:256], kk[:, 0, :], ALU.mult, [tpi, t_k], [t_q[0]])
                P.tt(tq[:, 1, :], psr[:, 0:256], kk[:, 1, :], ALU.mult, [tpr, t_k], [t_q[1]])
                P.tt(RR(p2[:, kc, :]), tq[:, 0, :], tq[:, 1, :], ALU.subtract, [t_q[0], t_q[1]], [t_p2])
            for tc in range(NCH):
                r0 = row0 + tc * 128
                P.dma(xg[:], K.ZC[r0:r0 + 128, 256 * (o + 1):256 * (o + 2)], (), [t_x])
                ps, tp = P.ps()
                for kA in range(0, NK, HK):
                    kB = min(kA + HK, NK)
                    cct, sst, t_cc, t_ss = load_tabs(tc, kA, kB)
                    for kc in range(kA, kB):
                        P.mm(ps[:, 0:256], cct[:, kc - kA, :], p1[:, kc, :], kc == 0, False, [t_cc, t_p1], [tp], r=FAST)
                    for kc in range(kA, kB):
                        P.mm(ps[:, 0:256], sst[:, kc - kA, :], p2[:, kc, :], False, kc == NK - 1, [t_ss, t_p2], [tp], r=FAST)
                P.tt(yy[:], a[:, tc, :], bb[:, o, :], ALU.mult, [t_a[tc], t_b], [t_y])
                P.tt(yy[:], yy[:], ps[:, 0:256], ALU.add, [t_y, tp], [t_y])
                if o == 0:
                    P.tt(RR(a[:, tc, :]), yy[:], xg[:], ALU.mult, [t_y, t_x], [t_a[tc]])
                else:
                    P.tt(yy[:], yy[:], xg[:], ALU.mult, [t_y, t_x], [t_y])
                    P.dma(K.MIX[r0:r0 + 128, 0:256], yy[:], [t_y], [("MIX", r0)], q='pool')


def stage_attn(K, l):
    P, nc, I = K.P, K.P.nc, K.I
    with SB(nc, "at_k", [64, 2, T], F32) as kf, SB(nc, "at_v", [128, NT, 2, 65], F32) as v1, \
            SB(nc, "at_es", [128, 8], F32) as es, SB(nc, "at_ml", [128, 128], F32) as ml, \
            SB(nc, "at_mr", [128, 128], F32) as mr, SB(nc, "at_q", [64, 2, 4, 128], F32) as q4, \
            SB(nc, "at_pt", [128, 5, 512], F32) as pt, SB(nc, "at_o", [128, 2, 512], F32) as ot, \
            SB(nc, "at_d", [128, 2, 4], F32) as den:
        t_k, t_v, t_es, t_m = Tok("k"), Tok("v"), Tok("es"), Tok("m")
        t_q = [Tok("q0"), Tok("q1")]
        t_pt = [Tok("pt%d" % i) for i in range(5)]
        t_o = [Tok("o0"), Tok("o1")]
        t_d = Tok("d")
        for h in range(2):
            P.dma(kf[:, h, :], K.KF[h * 64:(h + 1) * 64, :], (), [t_k])
        P.memset(v1[:, :, :, 64:65], 1.0, [t_v])
        vtv = K.VT.rearrange("(t p) (h d) -> p t h d", p=128, h=2)
        for h in range(2):
            P.dma(v1[:, :, h, 0:64], vtv[:, :, h, :], (), [t_v])
        P.dma(es[:], I['attn_sink'][l, :].partition_broadcast(128), (), [t_es])
        P.act(es[:], es[:], AF.Exp, [t_es], [t_es])
        P.dma(ml[:], I['k_maskl'][:, :], (), [t_m])
        P.dma(mr[:], I['k_maskr'][:, :], (), [t_m])
        for qb in range(NT):
            kts = [(0, None), (1, None)]
            if qb >= 2:
                if qb - 1 >= 2:
                    kts.append((qb - 1, ml))
                kts.append((qb, None))
                if qb + 1 < NT:
                    kts.append((qb + 1, mr))
            ob = qb % 2
            for kvh in range(2):
                qi = kvh
                P.dma(q4[:, qi, :, :], K.QF[kvh * 256:(kvh + 1) * 256, qb * 128:(qb + 1) * 128].rearrange("(h d) t -> d h t", d=64),
                      (), [t_q[qi]])
                for idx, (kt, mask) in enumerate(kts):
                    ps, tp = P.ps()
                    P.mm(ps[:, :], kf[:, kvh, kt * 128:(kt + 1) * 128], q4[:, qi, :, :].rearrange("d h t -> d (h t)"), True, True,
                         [t_k, t_q[qi]], [tp])
                    P.act(pt[:, idx, :], ps[:, :], AF.Exp, [tp], [t_pt[idx]], scale=0.125)
                    if mask is not None:
                        pv = pt[:, idx, :].rearrange("p (h t) -> p h t", h=4)
                        P.tt(pv, pv, mask[:, :].unsqueeze(1).broadcast_to([128, 4, 128]), ALU.mult, [t_pt[idx], t_m], [t_pt[idx]])
                pso, tpo = P.ps()
                for hh in range(4):
                    for idx, (kt, mask) in enumerate(kts):
                        P.mm(pso[:, hh * 65:(hh + 1) * 65], pt[:, idx, hh * 128:(hh + 1) * 128], v1[:, kt, kvh, :],
                             idx == 0, idx == len(kts) - 1, [t_pt[idx], t_v], [tpo])
                pv = pso[:, 0:260].rearrange("p (h c) -> p h c", h=4)
                P.tt(den[:, kvh, :], pv[:, :, 64], es[:, kvh * 4:(kvh + 1) * 4], ALU.add, [tpo, t_es], [t_d])
                P.recip(den[:, kvh, :], den[:, kvh, :], [t_d], [t_d])
                P.tt(ot[:, ob, kvh * 256:(kvh + 1) * 256].rearrange("p (h d) -> p h d", h=4), pv[:, :, 0:64],
                     den[:, kvh, :].unsqueeze(2).broadcast_to([128, 4, 64]), ALU.mult, [tpo, t_d], [t_o[ob]])
            P.dma(K.MIX[qb * 128:(qb + 1) * 128, 256:768], ot[:, ob, :], [t_o[ob]], [("MIXa", qb)], q='pool')


def round_frac(K, v, r, t_v, t_r, e='dve'):
    P = K.P
    P.ts(r, v, MAGIC, MAGIC, ALU.add, ALU.subtract, [t_v], [t_r], e=e)
    P.tt(v, v, r, ALU.subtract, [t_v, t_r], [t_v], e=e)


def stage_s5(K, l):
    P, nc, I = K.P, K.P.nc, K.I
    K.GS = K.P.dram.get("sGS")
    if K.GS is None:
        K.GS = P.dram_t("sGS", [256, T])
    with SB(nc, "s_bt", [32, 16, 2, 128], F32) as BT, SB(nc, "s_cb", [128, 16, 2, 32], F32) as CB, \
            SB(nc, "s_par", [128, 12, 16], F32) as par, SB(nc, "s_dsk", [32, 8], F32) as dsk, \
            SB(nc, "s_iota", [128, 512], F32) as iot:
        t_BT, t_CB, t_par, t_dsk, t_iota = Tok("BT"), Tok("CB"), Tok("par"), Tok("dsk"), Tok("iota")
        MAG, PHI = 4, 6
        with SB(nc, "s_b", [128, 2, 16, 16], F32) as bri, SB(nc, "s_bb", [128, 2, 16, 16], F32) as bb, \
                SB(nc, "s_bd", [128, 16, 2, 32], F32) as BD, SB(nc, "s_cnd", [32, 16, 2, 128], F32) as CND, \
                SB(nc, "s_t1", [128, 16, 16], F32) as t1, SB(nc, "s_t2", [128, 16, 16], F32) as t2:
            t_b, t_bb, t_BD, t_CND, t_t1, t_t2 = Tok("b"), Tok("bb"), Tok("BD"), Tok("CND"), Tok("t1"), Tok("t2")
            for d in range(2):
                P.dma(par[:, 0, d * 8:(d + 1) * 8], I['s5_lam_re'][l, d].rearrange("g p -> (g p)").rearrange("(gh q) -> q gh", q=128), (), [t_par])
                P.dma(par[:, 1, d * 8:(d + 1) * 8], I['s5_lam_im'][l, d].rearrange("g p -> (g p)").rearrange("(gh q) -> q gh", q=128), (), [t_par])
                ldv = I['s5_log_dt'][l, d].rearrange("(gh gl) -> gl gh", gl=2)
                for gl in range(2):
                    P.dma(par[gl * 64:(gl + 1) * 64, 2, d * 8:(d + 1) * 8], ldv[gl].partition_broadcast(64), (), [t_par])
                for ri, nm in enumerate(('s5_b_re', 's5_b_im')):
                    P.dma(bri[:, ri, d * 8:(d + 1) * 8, :],
                          I[nm][l, d].rearrange("g p c -> (g p c)").rearrange("(gh q c) -> q gh c", q=128, c=16), (), [t_b])
            P.dma(dsk[:], I['s5_d'][l, :].rearrange("(j r) -> r j", r=32), (), [t_dsk])
            P.dma(iot[:], I['k_iota'][0, :].partition_broadcast(128), (), [t_iota])
            pp = lambda i: par[:, i, :]
            tp_ = [t_par]
            P.act(pp(2), pp(2), AF.Exp, tp_, tp_)
            P.tt(pp(3), pp(0), pp(2), ALU.mult, tp_, tp_)
            P.act(pp(MAG), pp(3), AF.Exp, tp_, tp_)
            P.tt(pp(3), pp(1), pp(2), ALU.mult, tp_, tp_)
            P.ts(pp(PHI), pp(3), 1.0 / (2.0 * math.pi), None, ALU.mult, None, tp_, tp_)
            P.copy(pp(5), pp(PHI), tp_, tp_)
            round_frac(K, pp(5), pp(11), t_par, t_par)
            P.act(pp(8), pp(5), AF.Sin, tp_, tp_, scale=TWO_PI)
            P.ts(pp(5), pp(PHI), 0.25, None, ALU.add, None, tp_, tp_)
            round_frac(K, pp(5), pp(11), t_par, t_par)
            P.act(pp(7), pp(5), AF.Sin, tp_, tp_, scale=TWO_PI)
            P.tt(pp(7), pp(7), pp(MAG), ALU.mult, tp_, tp_)
            P.tt(pp(8), pp(8), pp(MAG), ALU.mult, tp_, tp_)
            P.ts(pp(7), pp(7), -1.0, None, ALU.add, None, tp_, tp_)
            P.tt(pp(3), pp(0), pp(0), ALU.mult, tp_, tp_)
            P.tt(pp(5), pp(1), pp(1), ALU.mult, tp_, tp_)
            P.tt(pp(3), pp(3), pp(5), ALU.add, tp_, tp_)
            P.recip(pp(3), pp(3), tp_, tp_)
            P.tt(pp(9), pp(7), pp(0), ALU.mult, tp_, tp_)
            P.tt(pp(5), pp(8), pp(1), ALU.mult, tp_, tp_)
            P.tt(pp(9), pp(9), pp(5), ALU.add, tp_, tp_)
            P.tt(pp(9), pp(9), pp(3), ALU.mult, tp_, tp_)
            P.tt(pp(10), pp(8), pp(0), ALU.mult, tp_, tp_)
            P.tt(pp(5), pp(7), pp(1), ALU.mult, tp_, tp_)
            P.tt(pp(10), pp(10), pp(5), ALU.subtract, tp_, tp_)
            P.tt(pp(10), pp(10), pp(3), ALU.mult, tp_, tp_)
            cre = par[:, 9, :].unsqueeze(2).broadcast_to([128, 16, 16])
            cim = par[:, 10, :].unsqueeze(2).broadcast_to([128, 16, 16])
            P.tt(t1[:], bri[:, 0, :, :], cre, ALU.mult, [t_b, t_par], [t_t1])
            P.tt(t2[:], bri[:, 1, :, :], cim, ALU.mult, [t_b, t_par], [t_t2])
            P.tt(bb[:, 0, :, :], t1[:], t2[:], ALU.subtract, [t_t1, t_t2], [t_bb])
            P.tt(t1[:], bri[:, 1, :, :], cre, ALU.mult, [t_b, t_par], [t_t1])
            P.tt(t2[:], bri[:, 0, :, :], cim, ALU.mult, [t_b, t_par], [t_t2])
            P.tt(bb[:, 1, :, :], t1[:], t2[:], ALU.add, [t_t1, t_t2], [t_bb])
            P.memset(BD[:], 0.0, [t_BD])
            for ri in range(2):
                P.copy(BD[0:64, :, ri, 0:16], bb[0:64, ri, :, :], [t_bb], [t_BD])
                P.copy(BD[64:128, :, ri, 16:32], bb[64:128, ri, :, :], [t_bb], [t_BD])
            for dg in range(16):
                ps, tp = P.ps()
                for ri in range(2):
                    P.tr(ps[0:32, ri * 128:(ri + 1) * 128], BD[:, dg, ri, :], K.ident[:], [t_BD, K.t_ident], [tp])
                P.copy(BT[:, dg, :, :], ps[0:32, 0:256].rearrange("p (r m) -> p r m", r=2), [tp], [t_BT])
            P.memset(CND[:], 0.0, [t_CND])
            for d in range(2):
                for ri, nm in enumerate(('s5_c_re', 's5_c_im')):
                    cv = I[nm][l, d].rearrange("(gh gl) c p -> gl c gh p", gl=2)
                    for gl in range(2):
                        P.dma(CND[gl * 16:(gl + 1) * 16, d * 8:(d + 1) * 8, ri, gl * 64:(gl + 1) * 64], cv[gl], (), [t_CND])
            for dg in range(16):
                ps, tp = P.ps()
                for ri in range(2):
                    P.tr(ps[:, ri * 32:(ri + 1) * 32], CND[:, dg, ri, :], K.ident[0:32, 0:32], [t_CND, K.t_ident], [tp])
                P.copy(CB[:, dg, 0, :], ps[:, 0:32], [tp], [t_CB])
                P.ts(CB[:, dg, 1, :], ps[:, 32:64], -1.0, None, ALU.mult, None, [tp], [t_CB])
        P.barrier()
        with SB(nc, "s_us", [32, T], F32) as us, SB(nc, "s_y", [32, T], F32) as ysb, \
                SB(nc, "s_tab", [128, 2, 4, 512], F32) as tab2, SB(nc, "s_m", [128, 2, 2, 512], F32) as mm2, \
                SB(nc, "s_w", [128, 2, 2, 512], F32) as ww2, SB(nc, "s_s", [128, 2, 2, 512], F32) as ss2, \
                SB(nc, "s_q", [128, 2, 2, 512], F32) as qq2, SB(nc, "s_c0", [128, 2, 4], F32) as c02, \
                SB(nc, "s_car", [128, 2], F32) as car, SB(nc, "s_mag", [128, 512], F32) as magt, SB(nc, "s_g", [32, 2, T], F32) as gg:
            t_us, t_y, t_car, t_g = (Tok(n) for n in ("us", "y", "car", "g"))
            tk2 = [dict(m=Tok("m%d" % b), w=Tok("w%d" % b), s=Tok("s%d" % b), q=Tok("q%d" % b), c0=Tok("c0%d" % b),
                        tab=[Tok("tabs%d" % b), Tok("tabc%d" % b), Tok("vr%d" % b), Tok("vrr%d" % b)]) for b in range(2)]
            chunk_no = [0]
            t_mag = Tok("mag")
            items = []
            for j in range(8):
                for d in range(2):
                    if d == 0:
                        chunks = [(a, min(a + 512, T), False, a) for a in range(0, T, 512)]
                    else:
                        chunks = [(0, NCTX, True, 0)]
                        b = T
                        while b > NCTX:
                            a = max(b - 512, NCTX)
                            chunks.append((a, b, True, NCTX + (T - b)))
                            b = a
                    for ci, (a, b, rev, i0) in enumerate(chunks):
                        items.append(dict(j=j, d=d, dg=d * 8 + j, a=a, b=b, rev=rev, i0=i0, first=(ci == 0),
                                          jfirst=(d == 0 and ci == 0), jlast=(d == 1 and ci == len(chunks) - 1), pb=len(items) % 2))

            def emit_tables(it):
                n = it['b'] - it['a']
                pb = it['pb']
                tab, qq, c0 = tab2[:, pb], qq2[:, pb], c02[:, pb]
                t_q, t_c0, t_tab = tk2[pb]['q'], tk2[pb]['c0'], tk2[pb]['tab']
                phi = par[:, PHI, it['dg']:it['dg'] + 1]
                P.ts(c0[:, 0:1], phi, float(it['i0']), None, ALU.mult, None, [t_par], [t_c0])
                round_frac(K, c0[:, 0:1], c0[:, 2:3], t_c0, t_c0)
                P.ts(c0[:, 1:2], c0[:, 0:1], 0.25, None, ALU.add, None, [t_c0], [t_c0])
                for which in range(2):
                    P.act(tab[:, 2 + which, 0:n], iot[:, 0:n], AF.Identity, [t_iota, t_c0, t_par], [t_tab[2 + which]],
                          bias=c0[:, which:which + 1], scale=phi)
                    P.ts(tab[:, which, 0:n], tab[:, 2 + which, 0:n], MAGIC, MAGIC, ALU.add, ALU.subtract, [t_tab[2 + which]], [t_tab[which]])
                    P.tt(tab[:, 2 + which, 0:n], tab[:, 2 + which, 0:n], tab[:, which, 0:n], ALU.subtract, [t_tab[2 + which], t_tab[which]],
                         [t_tab[2 + which]])
                    P.act(tab[:, which, 0:n], tab[:, 2 + which, 0:n], AF.Sin, [t_tab[2 + which]], [t_tab[which]], scale=TWO_PI)

            def emit_main(it):
                j, d, dg, a, b, rev, pb = it['j'], it['d'], it['dg'], it['a'], it['b'], it['rev'], it['pb']
                n = b - a
                tab, mm_, ww, ss_, qq = tab2[:, pb], mm2[:, pb], ww2[:, pb], ss2[:, pb], qq2[:, pb]
                t_m, t_w, t_s, t_q, t_tab = tk2[pb]['m'], tk2[pb]['w'], tk2[pb]['s'], tk2[pb]['q'], tk2[pb]['tab']
                rv = (lambda ap: ap[:, ::-1]) if rev else (lambda ap: ap)
                if it['first']:
                    P.copy(magt[:, :], par[:, MAG, dg:dg + 1].broadcast_to([128, 512]), [t_par], [t_mag])
                ps1, tp1 = P.ps()
                ps2, tp2 = P.ps()
                P.mm(ps1[:, 0:n], BT[:, dg, 0, :], us[:, a:b], True, True, [t_BT, t_us], [tp1])
                P.mm(ps2[:, 0:n], BT[:, dg, 1, :], us[:, a:b], True, True, [t_BT, t_us], [tp2])
                sn = rv(tab[:, 0, 0:n])
                cs = rv(tab[:, 1, 0:n])
                tS, tC = t_tab[0], t_tab[1]
                P.tt(mm_[:, 0, 0:n], ps1[:, 0:n], cs, ALU.mult, [tp1, tC], [t_m])
                P.tt(qq[:, 0, 0:n], ps2[:, 0:n], sn, ALU.mult, [tp2, tS], [t_q])
                P.tt(mm_[:, 0, 0:n], mm_[:, 0, 0:n], qq[:, 0, 0:n], ALU.add, [t_m, t_q], [t_m])
                P.tt(mm_[:, 1, 0:n], ps2[:, 0:n], cs, ALU.mult, [tp2, tC], [t_m])
                P.tt(qq[:, 1, 0:n], ps1[:, 0:n], sn, ALU.mult, [tp1, tS], [t_q])
                P.tt(mm_[:, 1, 0:n], mm_[:, 1, 0:n], qq[:, 1, 0:n], ALU.subtract, [t_m, t_q], [t_m])
                magb = magt[:, 0:n]
                for ri in range(2):
                    init = 0.0 if it['first'] else car[:, ri:ri + 1]
                    P.op('dve', lambda g, ri=ri, init=init: g.tensor_tensor_scan(
                        out=rv(ww[:, ri, 0:n]), data0=magb, data1=rv(mm_[:, ri, 0:n]), initial=init,
                        op0=ALU.mult, op1=ALU.add), [t_m, t_mag, t_car], [t_w])
                last = a if rev else b - 1
                P.copy(car[:, :], ww[:, :, last - a], [t_w], [t_car])
                P.tt(ss_[:, 0, 0:n], ww[:, 0, 0:n], cs, ALU.mult, [t_w, tC], [t_s], e='pool')
                P.tt(qq[:, 0, 0:n], ww[:, 1, 0:n], sn, ALU.mult, [t_w, tS], [t_q], e='pool')
                P.tt(ss_[:, 0, 0:n], ss_[:, 0, 0:n], qq[:, 0, 0:n], ALU.subtract, [t_s, t_q], [t_s], e='pool')
                P.tt(ss_[:, 1, 0:n], ww[:, 0, 0:n], sn, ALU.mult, [t_w, tS], [t_s], e='pool')
                P.tt(qq[:, 1, 0:n], ww[:, 1, 0:n], cs, ALU.mult, [t_w, tC], [t_q], e='pool')
                P.tt(ss_[:, 1, 0:n], ss_[:, 1, 0:n], qq[:, 1, 0:n], ALU.add, [t_s, t_q], [t_s], e='pool')
                psy, tpy = P.ps()
                P.mm(psy[0:32, 0:n], CB[:, dg, 0, :], ss_[:, 0, 0:n], True, False, [t_CB, t_s], [tpy])
                P.mm(psy[0:32, 0:n], CB[:, dg, 1, :], ss_[:, 1, 0:n], False, True, [t_CB, t_s], [tpy])
                if d == 0:
                    P.copy(ysb[:, a:b], psy[0:32, 0:n], [tpy], [t_y], e='act')
                else:
                    P.tt(ysb[:, a:b], ysb[:, a:b], psy[0:32, 0:n], ALU.add, [t_y, tpy], [t_y])

            emit_tables(items[0])
            for idx, it in enumerate(items):
                j = it['j']
                if it['jfirst']:
                    P.dma(us[:], K.UF[32 * j:32 * (j + 1), :], (), [t_us])
                if idx + 1 < len(items):
                    emit_tables(items[idx + 1])
                emit_main(it)
                if not it['jlast']:
                    continue
                P.stt(ysb[:], us[:], dsk[:, j:j + 1], ysb[:], ALU.mult, ALU.add, [t_us, t_dsk, t_y], [t_y])
                P.tt(gg[:, 0, :], ysb[:], ysb[:], ALU.mult, [t_y], [t_g])
                P.ts(gg[:, 0, :], gg[:, 0, :], 0.044715, 1.0, ALU.mult, ALU.add, [t_g], [t_g])
                P.tt(gg[:, 0, :], gg[:, 0, :], ysb[:], ALU.mult, [t_g, t_y], [t_g])
                P.act(gg[:, 1, :], gg[:, 0, :], AF.Sigmoid, [t_g], [t_g], scale=1.5957691216)
                P.tt(gg[:, 1, :], gg[:, 1, :], ysb[:], ALU.mult, [t_g, t_y], [t_g])
                P.dma(K.GS[32 * j:32 * (j + 1), :], gg[:, 1, :], [t_g], [("GS", j)], q='pool')
        P.barrier()
        with SB(nc, "s_gw", [128, 2, 256], F32) as gw, SB(nc, "s_gb", [128, 2], F32) as gb, \
                SB(nc, "s_gt", [128, 2, 512], F32) as gt, SB(nc, "s_sg", [128, 2, 512], F32) as sg, \
                SB(nc, "s_o", [128, 4, 256], F32) as so:
            t_gw, t_gt, t_sg, t_so = Tok("gw"), Tok("gt"), Tok("sg"), Tok("so")
            P.dma(gw[:], I['s5_glu_w'][l].rearrange("(c p) n -> p c n", p=128), (), [t_gw])
            P.dma(gb[:], I['s5_glu_b'][l, :].rearrange("(c p) -> p c", p=128), (), [t_gw])
            for g in range((T + 511) // 512):
                t0 = g * 512
                n = min(512, T - t0)
                P.dma(gt[:, :, 0:n], K.GS[:, t0:t0 + n].rearrange("(c p) t -> p c t", p=128), (), [t_gt])
                for oc in range(2):
                    ps, tp = P.ps()
                    for kc in range(2):
                        P.mm(ps[:, 0:n], gw[:, kc, oc * 128:(oc + 1) * 128], gt[:, kc, 0:n], kc == 0, kc == 1, [t_gw, t_gt], [tp])
                    P.act(sg[:, oc, 0:n], ps[:, 0:n], AF.Sigmoid, [tp, t_gw], [t_sg], bias=gb[:, oc:oc + 1], scale=1.0)
                    P.tt(sg[:, oc, 0:n], sg[:, oc, 0:n], gt[:, oc, 0:n], ALU.mult, [t_sg, t_gt], [t_sg])
                for i in range(n // 128):
                    ps, tp = P.ps()
                    for oc in range(2):
                        P.tr(ps[:, oc * 128:(oc + 1) * 128], sg[:, oc, i * 128:(i + 1) * 128], K.ident[:], [t_sg, K.t_ident], [tp])
                    P.copy(so[:, i, :], ps[:, 0:256], [tp], [t_so], e='act')
                P.dma(K.MIX[t0:t0 + n, 768:1024].rearrange("(i p) c -> p i c", p=128), so[:, 0:n // 128, :], [t_so], [("MIXs", g)], q='pool')


def stage_outproj(K, l):
    P, nc, I = K.P, K.P.nc, K.I
    with SB(nc, "o_w", [128, 8, D], F32) as W, SB(nc, "o_rw", [128, 8, 16], F32) as RW, \
            SB(nc, "o_gain", [128, D], F32) as gain, SB(nc, "o_mod", [128, 6, D], F32) as mod, \
            SB(nc, "o_rb", [128, 16], F32) as rb, SB(nc, "o_mix", [128, D], F32) as mix, \
            SB(nc, "o_x", [128, D], F32) as xt, SB(nc, "o_junk", [128, D], F32) as junk, \
            SB(nc, "o_mT", [128, 8, 128], F32) as mT, SB(nc, "o_h2", [128, D], F32) as h2, \
            SB(nc, "o_hT", [128, 8, 128], F32) as hT, SB(nc, "o_tmp", [128, D], F32) as tmp, \
            SB(nc, "o_st", [128, 8], F32) as st, SB(nc, "o_r", [128, 12, 16], F32) as rr, \
            SB(nc, "o_gT", [16, 128], F32) as gT:
        t_W, t_c, t_mix, t_x, t_mT, t_h2, t_hT, t_tmp, t_st, t_rr, t_gT = (Tok(n) for n in (
            "W", "c", "mix", "x", "mT", "h2", "hT", "tmp", "st", "rr", "gT"))
        for k in range(8):
            P.load_r(W[:, k, :], I['w_out'][l, k * 128:(k + 1) * 128, :], junk[:], t_W, t_tmp, e='act' if k % 2 else 'dve')
        P.dma(RW[:], I['router_w'].rearrange("(k p) n -> p k n", p=128), (), [t_c])
        P.dma(gain[:], I['mix_norm_g'][l, :].partition_broadcast(128), (), [t_c])
        P.dma(rb[:], I['router_b'][0, :].partition_broadcast(128), (), [t_c])
        for jj, r in enumerate((2, 3, 4, 8, 9, 10)):
            P.dma(mod[:, jj, :], K.MOD[r, :].partition_broadcast(128), (), [t_c])
        groups = ((0, 256), (256, 768), (768, 1024))
        for ti in range(NT):
            mb = 3 if ti < 2 else 0
            rows = slice(ti * 128, (ti + 1) * 128)
            P.dma(mix[:], K.MIX[rows, :], (), [t_mix])
            P.dma(xt[:], K.X[rows, :], (), [t_x])
            for gi, (c0, c1) in enumerate(groups):
                rms_rstd(K, mix[:, c0:c1], t_mix, c1 - c0, junk[:, c0:c1], st[:, gi:gi + 1], t_st)
            for gi, (c0, c1) in enumerate(groups):
                P.stt(mix[:, c0:c1], mix[:, c0:c1], st[:, gi:gi + 1], gain[:, c0:c1], ALU.mult, ALU.mult, [t_mix, t_st, t_c], [t_mix])
            for half in range(2):
                ps, tp = P.ps()
                for j in range(4):
                    kk = half * 4 + j
                    P.tr(ps[:, j * 128:(j + 1) * 128], mix[:, kk * 128:(kk + 1) * 128], K.ident[:], [t_mix, K.t_ident], [tp])
                P.copy(RR(mT[:, half * 4:(half + 1) * 4, :]), ps[:, :].rearrange("p (j t) -> p j t", j=4), [tp], [t_mT], e='act' if half else 'dve')
            for half in range(2):
                ps, tp = P.ps()
                for k in range(8):
                    P.mm(ps[:, :], mT[:, k, :], W[:, k, half * 512:(half + 1) * 512], k == 0, k == 7, [t_mT, t_W], [tp], r=FAST)
                hs = slice(half * 512, (half + 1) * 512)
                P.tt(tmp[:, hs], ps[:, :], mod[:, mb + 0, hs], ALU.mult, [tp, t_c], [t_tmp])
                P.tt(xt[:, hs], xt[:, hs], tmp[:, hs], ALU.add, [t_x, t_tmp], [t_x])
            P.dma(K.X[rows, :], xt[:], [t_x], [("X", ti)], q='pool')
            rms_rstd(K, xt[:], t_x, D, junk[:], st[:, 3:4], t_st)
            P.stt(h2[:], xt[:], st[:, 3:4], mod[:, mb + 1, :], ALU.mult, ALU.mult, [t_x, t_st, t_c], [t_h2])
            P.tt(h2[:], h2[:], mod[:, mb + 2, :], ALU.add, [t_h2, t_c], [t_h2])
            for half in range(2):
                ps, tp = P.ps()
                for j in range(4):
                    kk = half * 4 + j
                    P.tr(ps[:, j * 128:(j + 1) * 128], h2[:, kk * 128:(kk + 1) * 128], K.ident[:], [t_h2, K.t_ident], [tp])
                P.copy(hT[:, half * 4:(half + 1) * 4, :], ps[:, :].rearrange("p (j t) -> p j t", j=4), [tp], [t_hT], e='act' if half else 'dve')
            P.dma(K.H2T[:, ti * 128:(ti + 1) * 128].rearrange("(k p) t -> p k t", p=128), hT[:], [t_hT], [("H2T", ti)], q='pool')
            ps, tp = P.ps()
            for k in range(8):
                P.mm(ps[:, 0:16], hT[:, k, :], RW[:, k, :], k == 0, k == 7, [t_hT, t_c], [tp])
            R = lambda i: rr[:, i, :]
            R4 = lambda i: rr[:, i, :].rearrange("p (g e) -> p g e", g=4)
            trr = [t_rr]
            P.op('dve', lambda g: g.reduce_max(out=st[:, 4:5], in_=ps[:, 0:16], axis=AX.X), [tp], [t_st])
            P.ts(st[:, 4:5], st[:, 4:5], -1.0, None, ALU.mult, None, [t_st], [t_st])
            P.act(R(0), ps[:, 0:16], AF.Exp, [tp, t_st], trr, bias=st[:, 4:5], scale=1.0, accum_out=st[:, 5:6])
            P.recip(st[:, 5:6], st[:, 5:6], [t_st, t_rr], [t_st])
            P.ts(R(0), R(0), st[:, 5:6], None, ALU.mult, None, trr + [t_st], trr)
            P.tt(R(1), R(0), rb[:], ALU.add, trr + [t_c], trr)
            P.op('dve', lambda g: g.reduce_max(out=rr[:, 2, 0:4], in_=R4(1), axis=AX.X), trr, trr)
            P.tt(R4(3), R4(1), rr[:, 2, 0:4].unsqueeze(2).broadcast_to([128, 4, 4]), ALU.is_equal, trr, trr)
            P.stt(R(4), R(3), -1e9, R(1), ALU.mult, ALU.add, trr, trr)
            P.op('dve', lambda g: g.reduce_max(out=rr[:, 5, 0:4], in_=R4(4), axis=AX.X), trr, trr)
            P.tt(rr[:, 6, 0:4], rr[:, 2, 0:4], rr[:, 5, 0:4], ALU.add, trr, trr)
            P.op('dve', lambda g: g.reduce_max(out=st[:, 6:7], in_=rr[:, 6, 0:4], axis=AX.X), trr, [t_st])
            P.ts(rr[:, 7, 0:4], rr[:, 6, 0:4], st[:, 6:7], None, ALU.is_equal, None, trr + [t_st], trr)
            P.tt(R4(8), R4(1), rr[:, 5, 0:4].unsqueeze(2).broadcast_to([128, 4, 4]), ALU.is_ge, trr, trr)
            P.tt(R4(8), R4(8), rr[:, 7, 0:4].unsqueeze(2).broadcast_to([128, 4, 4]), ALU.mult, trr, trr)
            P.tt(R(9), R(8), R(0), ALU.mult, trr, trr)
            P.op('dve', lambda g: g.reduce_sum(out=st[:, 7:8], in_=R(9), axis=AX.X), trr, [t_st])
            P.recip(st[:, 7:8], st[:, 7:8], [t_st], [t_st])
            P.ts(R(9), R(9), st[:, 7:8], None, ALU.mult, None, trr + [t_st], trr)
            ps2, tp2 = P.ps()
            P.tr(ps2[0:16, 0:128], R(9), K.ident[:], trr + [K.t_ident], [tp2])
            P.copy(gT[:], ps2[0:16, 0:128], [tp2], [t_gT], e='act')
            P.dma(K.GTT[:, ti * 128:(ti + 1) * 128], gT[:], [t_gT], [("GTT", ti)], q='pool')


def stage_moe(K, l):
    P, nc, I = K.P, K.P.nc, K.I
    GN = 1024
    with SB(nc, "e_h", [128, 8, GN], F32) as hT, SB(nc, "e_g", [128, 8, GN], F32) as GT, \
            SB(nc, "e_y", [128, GN // 128, D], F32) as Y, SB(nc, "e_wd", [128, 8, D], F32) as wd, \
            SB(nc, "e_wg", [128, 2, 8, 128], F32) as wg, SB(nc, "e_wu", [128, 2, 8, 128], F32) as wu, \
            SB(nc, "e_gt", [16, GN], F32) as gt, SB(nc, "e_gb", [128, GN], F32) as gB, \
            SB(nc, "e_sel", [16, 16, 128], F32) as sel, SB(nc, "e_sa", [128, 2, 512], F32) as sA, \
            SB(nc, "e_mod", [128, 2, D], F32) as mod, SB(nc, "e_x", [128, D], F32) as xt, \
            SB(nc, "e_wst", [128, 6, D], F32) as wst:
        t_wst = [Tok("wst%d" % i) for i in range(6)]
        t_wdc = [Tok("wd%d" % i) for i in range(8)]
        t_h, t_G, t_Y, t_wd, t_gt, t_gB, t_sel, t_mod, t_x = (Tok(n) for n in ("h", "G", "Y", "wd", "gt", "gB", "sel", "mod", "x"))
        t_wg = [Tok("wg0"), Tok("wg1")]
        t_wu = [Tok("wu0"), Tok("wu1")]
        t_sa = [Tok("sa0"), Tok("sa1")]
        for e in range(16):
            P.copy(sel[:, e, :], K.ident[0:16, e:e + 1].broadcast_to([16, 128]), [K.t_ident], [t_sel])
        P.dma(mod[:, 0, :], K.MOD[5, :].partition_broadcast(128), (), [t_mod])
        P.dma(mod[:, 1, :], K.MOD[11, :].partition_broadcast(128), (), [t_mod])
        for g in range((T + GN - 1) // GN):
            t0 = g * GN
            n = min(GN, T - t0)
            nt = n // 128
            for i in range(nt):
                P.load_r(hT[:, :, i * 128:(i + 1) * 128], K.H2T[:, t0 + i * 128:t0 + (i + 1) * 128].rearrange("(k p) t -> p k t", p=128),
                         wst[:, i % 2, :].rearrange("p (k t) -> p k t", k=8), t_h, t_wst[i % 2], e='act' if i % 2 else 'dve')
            P.dma(gt[:, 0:n], K.GTT[:, t0:t0 + n], (), [t_gt])
            P.memset(Y[:, 0:nt, :], 0.0, [t_Y], e='pool')
            for e in range(16):
                for s0 in range(0, n, 512):
                    sn = min(512, n - s0)
                    ps, tp = P.ps()
                    P.mm(ps[:, 0:sn], sel[:, e, :], gt[:, s0:s0 + sn], True, True, [t_sel, t_gt], [tp])
                    P.copy(gB[:, s0:s0 + sn], ps[:, 0:sn], [tp], [t_gB], e='act')
                wgv = I['moe_w_gate'][l, e].rearrange("(k p) n -> p k n", p=128)
                wuv = I['moe_w_up'][l, e].rearrange("(k p) n -> p k n", p=128)
                for c in range(8):
                    b = c % 2
                    P.load_r(wg[:, b, :, :], wgv[:, :, c * 128:(c + 1) * 128], wst[:, 4, :].rearrange("p (k t) -> p k t", k=8), t_wg[b], t_wst[4], e='act')
                    P.load_r(wu[:, b, :, :], wuv[:, :, c * 128:(c + 1) * 128], wst[:, 5, :].rearrange("p (k t) -> p k t", k=8), t_wu[b], t_wst[5], e='act')
                    P.load_r(wd[:, c, :], I['moe_w_down'][l, e, c * 128:(c + 1) * 128, :], wst[:, 2 + c % 2, :], t_wdc[c], t_wst[2 + c % 2], e='dve', q='pool')
                    for si, s0 in enumerate(range(0, n, 512)):
                        sn = min(512, n - s0)
                        psa, tpa = P.ps()
                        psu, tpu = P.ps()
                        for k in range(8):
                            P.mm(psa[:, 0:sn], wg[:, b, k, :], hT[:, k, s0:s0 + sn], k == 0, k == 7, [t_wg[b], t_h], [tpa], r=FAST)
                        for k in range(8):
                            P.mm(psu[:, 0:sn], wu[:, b, k, :], hT[:, k, s0:s0 + sn], k == 0, k == 7, [t_wu[b], t_h], [tpu], r=FAST)
                        sb_ = si % 2
                        P.act(sA[:, sb_, 0:sn], psa[:, 0:sn], AF.Silu, [tpa], [t_sa[sb_]])
                        P.tt(sA[:, sb_, 0:sn], sA[:, sb_, 0:sn], psu[:, 0:sn], ALU.mult, [t_sa[sb_], tpu], [t_sa[sb_]])
                        P.tt(RR(GT[:, c, s0:s0 + sn]), sA[:, sb_, 0:sn], gB[:, s0:s0 + sn], ALU.mult, [t_sa[sb_], t_gB], [t_G])
                for i in range(nt):
                    for half in range(2):
                        ps, tp = P.ps()
                        for c in range(8):
                            P.mm(ps[:, :], GT[:, c, i * 128:(i + 1) * 128], wd[:, c, half * 512:(half + 1) * 512], c == 0, c == 7, [t_G, t_wdc[c]], [tp], r=FAST)
                        hs = slice(half * 512, (half + 1) * 512)
                        P.tt(Y[:, i, hs], Y[:, i, hs], ps[:, :], ALU.add, [t_Y, tp], [t_Y])
            for i in range(nt):
                ti = g * (GN // 128) + i
                mb = 1 if ti < 2 else 0
                rows = slice(ti * 128, (ti + 1) * 128)
                P.dma(xt[:], K.X[rows, :], (), [t_x])
                P.tt(Y[:, i, :], Y[:, i, :], mod[:, mb, :], ALU.mult, [t_Y, t_mod], [t_Y], e='pool')
                P.tt(xt[:], xt[:], Y[:, i, :], ALU.add, [t_x, t_Y], [t_x])
                P.dma(K.X[rows, :], xt[:], [t_x], [("X", ti)], q='pool')


def stage_final(K):
    P, nc, I = K.P, K.P.nc, K.I
    with SB(nc, "f_g", [128, D], F32) as gbc, SB(nc, "f_x", [128, 2, D], F32) as xt, \
            SB(nc, "f_j", [128, D], F32) as junk, SB(nc, "f_s", [128, 2], F32) as st:
        t_g = Tok("g")
        t_x = [Tok("x0"), Tok("x1")]
        t_s = [Tok("s0"), Tok("s1")]
        P.dma(gbc[:], I['final_g'][0, :].partition_broadcast(128), (), [t_g])
        for i in range(NLAT // 128):
            b = i % 2
            P.dma(xt[:, b, :], K.X[NCTX + i * 128:NCTX + (i + 1) * 128, :], (), [t_x[b]])
            rms_rstd(K, xt[:, b, :], t_x[b], D, junk[:], st[:, b:b + 1], t_s[b])
            P.stt(xt[:, b, :], xt[:, b, :], st[:, b:b + 1], gbc[:], ALU.mult, ALU.mult, [t_x[b], t_s[b], t_g], [t_x[b]])
            P.dma(K.out[i * 128:(i + 1) * 128, :], xt[:, b, :], [t_x[b]], [("out", i)], q='pool')


_CONSTS = None


def consts():
    global _CONSTS
    if _CONSTS is not None:
        return _CONSTS
    c = {}
    c['k_ident'] = np.eye(128, dtype=np.float32)
    rc, rs = rope_tables()
    c['k_ropec'], c['k_ropes'] = rc, rs
    j = np.arange(128)[:, None]
    i = np.arange(128)[None, :]
    c['k_maskl'] = (j >= i).astype(np.float32)
    c['k_maskr'] = (j <= i).astype(np.float32)
    c['k_ccL'], c['k_ssL'] = dft_blocks(NLAT, 33)
    c['k_ccC'], c['k_ssC'] = dft_blocks(NCTX, 3)
    f, d, w = hyena_consts(NLAT)
    c['k_featL'], c['k_decL'], c['k_wkL'] = f, d, w.reshape(-1, 1)
    f, d, w = hyena_consts(NCTX)
    c['k_featC'], c['k_decC'], c['k_wkC'] = f, d, w.reshape(-1, 1)
    c['k_iota'] = np.arange(512, dtype=np.float32).reshape(1, 512)
    _CONSTS = c
    return c


def make_in_maps(inputs, cores):
    cs = consts()
    shared = {}
    for k, v in inputs.items():
        if k in ('x', 'c', 'ctx', 'c_ctx'):
            continue
        a = np.ascontiguousarray(np.asarray(v, dtype=np.float32))
        if k in ('router_b', 'final_g'):
            a = a.reshape(1, -1)
        shared[k] = a
    shared.update(cs)
    maps = []
    for b in cores:
        m = dict(shared)
        m['x'] = np.ascontiguousarray(inputs['x'][b], dtype=np.float32)
        m['ctx'] = np.ascontiguousarray(inputs['ctx'][b], dtype=np.float32)
        m['c'] = np.ascontiguousarray(inputs['c'][b], dtype=np.float32).reshape(1, D)
        m['c_ctx'] = np.ascontiguousarray(inputs['c_ctx'], dtype=np.float32).reshape(1, D)
        maps.append(m)
    return maps


def kernel(**inputs):
    P = build()
    maps = make_in_maps(inputs, list(range(8)))
    res = run_bass_kernel_spmd(P.nc, maps, core_ids=list(range(8)))
    return np.stack([r["out"] for r in res.results], axis=0).astype(np.float32)
```

```python
import math
from contextlib import ExitStack
import numpy as np
import concourse.bass as bass
import concourse.mybir as mybir
from concourse.bass_utils import run_bass_kernel_spmd

F32 = mybir.dt.float32
F32R = mybir.dt.float32r
FAST = True
AF = mybir.ActivationFunctionType
ALU = mybir.AluOpType
AX = mybir.AxisListType

D = 1024
NLAT = 4096
NCTX = 256
T = NLAT + NCTX
NT = T // 128
DEPTH = 4
HY_W = 256
ATT_W = 512
S5_W = 256
HY_END = 768
Q_END = HY_END + ATT_W
K_END = Q_END + 128
V_END = K_END + 128
IN_W = V_END + S5_W
EPS = 1e-6
MAGIC = 12582912.0
TWO_PI = 6.283185
NE = 16


class Tok:
    def __init__(self, name):
        self.name = name

    def __repr__(self):
        return self.name


class Prog:
    NDS = 24

    def __init__(self):
        nc = bass.Bass("TRN2", target_bir_lowering=False)
        self.nc = nc
        self.eng = {'pe': nc.tensor, 'dve': nc.vector, 'act': nc.scalar, 'pool': nc.gpsimd, 'sp': nc.sync}
        self.esem = {k: nc.alloc_semaphore("es_" + k) for k in self.eng}
        self.ecnt = {k: 0 for k in self.eng}
        self.dsem = [nc.alloc_semaphore("ds%d" % i) for i in range(self.NDS)]
        self.dval = [0] * self.NDS
        self.dnext = 0
        self.know = {k: {} for k in self.eng}
        self.last_w = {}
        self.readers = {}
        self.n_inst = 0
        self.psum = [nc.alloc_psum_tensor("psb%d" % i, [128, 512], F32) for i in range(8)]
        self.ps_tok = [Tok("ps%d" % i) for i in range(8)]
        self.ps_next = 0
        self.ps_held = set()
        self.dram = {}

    def _sem_of(self, key):
        return self.esem[key] if isinstance(key, str) else self.dsem[key]

    def _deps(self, reads, writes):
        deps = {}

        def add(kv):
            k, v = kv
            if deps.get(k, 0) < v:
                deps[k] = v
        for t in reads:
            if t in self.last_w:
                add(self.last_w[t])
        for t in writes:
            if t in self.last_w:
                add(self.last_w[t])
            for kv in self.readers.get(t, {}).items():
                add(kv)
        return deps

    def _wait(self, e, deps):
        kn = self.know[e]
        for k, v in deps.items():
            if k == e and e == 'pe':
                continue
            if kn.get(k, 0) >= v:
                continue
            self.eng[e].wait_ge(self._sem_of(k), v)
            kn[k] = v
            self.n_inst += 1

    def _commit(self, me, reads, writes):
        for t in writes:
            self.last_w[t] = me
            self.readers[t] = {}
        for t in reads:
            r = self.readers.setdefault(t, {})
            if r.get(me[0], 0) < me[1]:
                r[me[0]] = me[1]

    def op(self, e, fn, reads=(), writes=()):
        self._wait(e, self._deps(reads, writes))
        inst = fn(self.eng[e])
        inst.then_inc(self.esem[e], 1)
        self.ecnt[e] += 1
        self.n_inst += 1
        self._commit((e, self.ecnt[e]), reads, writes)

    def dma(self, out, in_, reads=(), writes=(), q='sp'):
        self._wait(q, self._deps(reads, writes))
        s = self.dnext
        self.dnext = (self.dnext + 1) % self.NDS
        if self.know[q].get(s, 0) < self.dval[s]:
            self.eng[q].wait_ge(self.dsem[s], self.dval[s])
            self.know[q][s] = self.dval[s]
        self.eng[q].dma_start(out=out, in_=in_, allow_slow_non_contiguous=True).then_inc(self.dsem[s], 16)
        self.dval[s] += 16
        self.n_inst += 1
        self._commit((s, self.dval[s]), reads, writes)

    def barrier(self):
        for e in self.eng:
            deps = {k: self.ecnt[k] for k in self.eng if self.ecnt[k] > 0}
            for s in range(self.NDS):
                if self.dval[s] > 0:
                    deps[s] = self.dval[s]
            self._wait(e, deps)
        self.last_w = {}
        self.readers = {}

    def finish(self):
        deps = {s: self.dval[s] for s in range(self.NDS) if self.dval[s] > 0}
        for k in self.eng:
            if self.ecnt[k] > 0:
                deps[k] = self.ecnt[k]
        self._wait('sp', deps)

    def ps(self):
        while True:
            i = self.ps_next
            self.ps_next = (i + 1) % 8
            if i not in self.ps_held:
                return self.psum[i], self.ps_tok[i]

    def ps_hold(self, n):
        got = []
        for i in range(8):
            if i not in self.ps_held and len(got) < n:
                self.ps_held.add(i)
                got.append(i)
        return [(self.psum[i], self.ps_tok[i]) for i in got], got

    def ps_release(self, got):
        for i in got:
            self.ps_held.discard(i)

    def mm(self, out, lhsT, rhs, start, stop, reads, writes, r=False):
        if r:
            lhsT = lhsT.bitcast(F32R)
            rhs = rhs.bitcast(F32R)
        self.op('pe', lambda e: e.matmul(out, lhsT, rhs, start=start, stop=stop), reads, writes)

    def tr(self, out, in_, ident, reads, writes):
        self.op('pe', lambda e: e.transpose(out=out, in_=in_, identity=ident), reads, writes)

    def act(self, out, in_, func, reads, writes, **kw):
        self.op('act', lambda e: e.activation(out=out, in_=in_, func=func, **kw), reads, writes)

    def tt(self, out, in0, in1, op, reads, writes, e='dve'):
        self.op(e, lambda g: g.tensor_tensor(out=out, in0=in0, in1=in1, op=op), reads, writes)

    def ts(self, out, in0, s1, s2, op0, op1, reads, writes, e='dve'):
        if op1 is None:
            self.op(e, lambda g: g.tensor_scalar(out=out, in0=in0, scalar1=s1, scalar2=None, op0=op0), reads, writes)
        else:
            self.op(e, lambda g: g.tensor_scalar(out=out, in0=in0, scalar1=s1, scalar2=s2, op0=op0, op1=op1), reads, writes)

    def stt(self, out, in0, scalar, in1, op0, op1, reads, writes):
        self.op('dve', lambda g: g.scalar_tensor_tensor(out=out, in0=in0, scalar=scalar, in1=in1, op0=op0, op1=op1), reads, writes)

    def copy(self, out, in_, reads, writes, e='dve'):
        if e == 'act':
            self.op('act', lambda g: g.copy(out=out, in_=in_), reads, writes)
        else:
            self.op(e, lambda g: g.tensor_copy(out=out, in_=in_), reads, writes)

    def load_r(self, dst, src, stage, t_dst, t_stage, e='dve', q='sp'):
        if not FAST:
            self.dma(dst, src, (), [t_dst])
            return
        self.dma(stage, src, (), [t_stage], q=q)
        self.copy(dst.bitcast(F32R), stage, [t_stage], [t_dst], e=e)

    def rnd(self, ap, tok, e='dve'):
        if FAST:
            self.copy(ap.bitcast(F32R), ap, [tok], [tok], e=e)

    def memset(self, ap, val, writes, e='dve'):
        self.op(e, lambda g: g.memset(ap, val), (), writes)

    def recip(self, out, in_, reads, writes):
        self.op('dve', lambda g: g.reciprocal(out=out, in_=in_), reads, writes)

    def dram_t(self, name, shape, kind="Internal"):
        t = self.nc.dram_tensor(name, list(shape), F32, kind=kind).ap()
        self.dram[name] = t
        return t


def RR(ap):
    return ap.bitcast(F32R) if FAST else ap


class Ctx:
    def dump(self, name, tile_ap, shape, reads):
        if 'dump' not in self.dbg:
            return
        o = self.P.nc.dram_tensor("dmp_" + name, list(shape), F32, kind="ExternalOutput").ap()
        self.P.dma(o, tile_ap, reads, (), q='sp')


_UID = [0]


def SB(nc, name, shape, dt):
    _UID[0] += 1
    return nc.sbuf_tensor("%s_%d" % (name, _UID[0]), shape, dt)


def rope_tables():
    cosT = np.ones((128, T), np.float64)
    sinT = np.zeros((128, T), np.float64)
    n = np.arange(NLAT)
    row = n // 64
    col = n % 64
    for r in range(128):
        d = r % 64
        dd = d if d < 32 else d - 32
        pos = row if d < 32 else col
        j = dd % 16
        first = dd < 16
        inv = 10000.0 ** (-(j / 16.0))
        ang = (pos.astype(np.float32) * np.float32(inv)).astype(np.float64)
        cosT[r, NCTX:] = np.cos(ang)
        sinT[r, NCTX:] = (-np.sin(ang)) if first else np.sin(ang)
    return cosT.astype(np.float32), sinT.astype(np.float32)


def perm_cols(width):
    src = np.zeros(width, np.int64)
    for c in range(width):
        d = c % 64
        dd = d % 32
        src[c] = c + 16 if dd < 16 else c - 16
    return src


def dft_blocks(nhalf, nchunk):
    idx = np.arange(nchunk * 128)
    valid = (idx <= nhalf)
    prod = np.outer(idx, idx).astype(np.float64)
    ang = 2.0 * np.pi * (prod % (2 * nhalf)) / (2 * nhalf)
    m = np.outer(valid, valid)
    cc = (np.cos(ang) * m).astype(np.float32)
    ss = (np.sin(ang) * m).astype(np.float32)

    def blk(mat):
        return np.ascontiguousarray(mat.reshape(nchunk, 128, nchunk, 128).transpose(2, 1, 0, 3))
    return blk(cc), blk(ss)


def hyena_consts(L):
    pos = np.arange(L, dtype=np.float32)
    t = pos / np.float32(max(L - 1, 1))
    bands = np.linspace(1e-4, 15, 16, dtype=np.float32)
    ang = (np.float32(2.0 * math.pi / L) * pos[:, None] * bands[None, :]).astype(np.float32)
    feats = np.concatenate([t[:, None], np.cos(ang), -np.sin(ang)], axis=-1).astype(np.float32)
    dmin = math.log(1e-2) / 1.5
    dmax = math.log(1e-2) / 0.3
    decay = np.abs(np.linspace(dmin, dmax, HY_W, dtype=np.float32))
    dec = np.exp(-t[:, None] * decay[None, :]).astype(np.float32)
    nk = L // 128 + 1
    wk = np.zeros(nk * 128, np.float32)
    wk[:L + 1] = 2.0 / (2 * L)
    wk[0] = 1.0 / (2 * L)
    wk[L] = 1.0 / (2 * L)
    return np.ascontiguousarray(feats.T), dec, wk


def build(n_layers=DEPTH, dbg=()):
    P = Prog()
    nc = P.nc
    K = Ctx()
    K.P = P
    K.dbg = dbg
    K.dbg_out = {}

    def inp(name, shape):
        return nc.dram_tensor(name, list(shape), F32, kind="ExternalInput").ap()

    I = {}
    I['x'] = inp('x', [NLAT, D])
    I['ctx'] = inp('ctx', [NCTX, D])
    I['c'] = inp('c', [1, D])
    I['c_ctx'] = inp('c_ctx', [1, D])
    shapes = dict(
        norm1_g=[4, D], norm2_g=[4, D], ada_w=[4, D, 6 * D], ada_b=[4, 6 * D], w_in=[4, D, IN_W], w_out=[4, D, D],
        mix_norm_g=[4, D], hy_conv_w=[4, 3, 768], hy_conv_b=[4, 768], hy_f_w1=[4, 33, 64], hy_f_b1=[4, 64],
        hy_f_freq1=[4, 64], hy_f_w2=[4, 64, 64], hy_f_b2=[4, 64], hy_f_freq2=[4, 64], hy_f_w3=[4, 64, 1024],
        hy_bias=[4, 2, 256], attn_sink=[4, 8], s5_lam_re=[4, 2, 16, 64], s5_lam_im=[4, 2, 16, 64], s5_log_dt=[4, 2, 16],
        s5_b_re=[4, 2, 16, 64, 16], s5_b_im=[4, 2, 16, 64, 16], s5_c_re=[4, 2, 16, 16, 64], s5_c_im=[4, 2, 16, 16, 64],
        s5_d=[4, 256], s5_glu_w=[4, 256, 256], s5_glu_b=[4, 256], router_w=[D, 16], router_b=[1, 16],
        moe_w_gate=[4, 16, D, D], moe_w_up=[4, 16, D, D], moe_w_down=[4, 16, D, D], final_g=[1, D],
        k_ident=[128, 128], k_ropec=[128, T], k_ropes=[128, T], k_maskl=[128, 128], k_maskr=[128, 128],
        k_ccL=[33, 128, 33, 128], k_ssL=[33, 128, 33, 128], k_ccC=[3, 128, 3, 128], k_ssC=[3, 128, 3, 128],
        k_featL=[33, NLAT], k_decL=[NLAT, 256], k_wkL=[33 * 128, 1], k_featC=[33, NCTX], k_decC=[NCTX, 256], k_wkC=[3 * 128, 1],
        k_iota=[1, 512],
    )
    for k, s in shapes.items():
        I[k] = inp(k, s)
    K.I = I
    out = nc.dram_tensor("out", [NLAT, D], F32, kind="ExternalOutput").ap()
    K.out = out

    K.X = P.dram_t("sX", [T, D])
    K.ZT = P.dram_t("sZT", [T, 768])
    K.QF = P.dram_t("sQF", [512, T])
    K.KF = P.dram_t("sKF", [128, T])
    K.VT = P.dram_t("sVT", [T, 128])
    K.UF = P.dram_t("sUF", [256, T])
    K.MIX = P.dram_t("sMIX", [T, D])
    K.H2T = P.dram_t("sH2T", [D, T])
    K.GTT = P.dram_t("sGTT", [16, T])

    K.ident = nc.alloc_sbuf_tensor("ident", [128, 128], F32)
    K.ones = nc.alloc_sbuf_tensor("ones", [128, 128], F32)
    K.MOD = P.dram_t("sMOD", [12, D])
    K.ZC = P.dram_t("sZC", [T, 768])
    K.KRE = P.dram_t("sKRE", [33 * 128, 512])
    K.KIM = P.dram_t("sKIM", [33 * 128, 512])
    K.t_ident = Tok("ident")
    K.t_ones = Tok("ones")
    K.t_mod = Tok("modbc")
    P.dma(K.ident[:], I['k_ident'][:, :], (), [K.t_ident])
    P.memset(K.ones[:], 1.0, [K.t_ones])

    tX = [("X", i) for i in range(NT)]
    K.tX = tX
    P.dma(K.X[0:NCTX, :], I['ctx'][:, :], (), tX[0:2])
    for i in range(4):
        P.dma(K.X[NCTX + i * 1024: NCTX + (i + 1) * 1024, :], I['x'][i * 1024:(i + 1) * 1024, :], (), tX[2 + 8 * i: 2 + 8 * (i + 1)])
    P.barrier()

    for l in range(n_layers):
        stage_mod(K, l)
        P.barrier()
        stage_inproj(K, l)
        P.barrier()
        if 'inproj' in dbg and l == 0:
            break
        stage_hyena(K, l, NLAT, NCTX, 'L')
        P.barrier()
        if l != DEPTH - 1:
            stage_hyena(K, l, NCTX, 0, 'C')
            P.barrier()
        if 'hyena' in dbg and l == 0:
            break
        stage_attn(K, l)
        P.barrier()
        if 'attn' in dbg and l == 0:
            break
        stage_s5(K, l)
        P.barrier()
        if 's5' in dbg and l == 0:
            break
        stage_outproj(K, l)
        P.barrier()
        if 'outproj' in dbg and l == 0:
            break
        stage_moe(K, l)
        P.barrier()
    if not dbg:
        stage_final(K)
    for name in dbg:
        if name in P.dram and name not in ('inproj', 'hyena', 'attn', 's5', 'outproj'):
            src = P.dram[name]
            o = nc.dram_tensor("dbg_" + name, list(src.shape), F32, kind="ExternalOutput").ap()
            P.barrier()
            P.dma(o, src, (), ())
    P.finish()
    return P


def stage_mod(K, l):
    P, nc, I = K.P, K.P.nc, K.I
    with SB(nc, "m_cs", [128, 2, 8], F32) as cs, SB(nc, "m_w", [128, 6 * D], F32) as wk, \
            SB(nc, "m_ws", [128, 6 * D], F32) as ws, SB(nc, "m_b", [1, 6 * D], F32) as bt, \
            SB(nc, "m_raw", [128, 6 * D], F32) as raw, SB(nc, "m_g", [128, 2, D], F32) as gn, \
            SB(nc, "m_o", [128, 12, D], F32) as mo:
        t_cs, t_w, t_ws, t_b, t_raw, t_g = Tok("cs"), Tok("w"), Tok("ws"), Tok("b"), Tok("raw"), Tok("g")
        P.dma(cs[:, 0, :], I['c'][0, :].rearrange("(c p) -> p c", p=128), (), [t_cs])
        P.dma(cs[:, 1, :], I['c_ctx'][0, :].rearrange("(c p) -> p c", p=128), (), [t_cs])
        P.act(cs[:], cs[:], AF.Silu, [t_cs], [t_cs])
        P.dma(bt[:], I['ada_b'][l:l + 1, :], (), [t_b])
        P.dma(gn[:, 0, :], I['norm1_g'][l, :].partition_broadcast(128), (), [t_g])
        P.dma(gn[:, 1, :], I['norm2_g'][l, :].partition_broadcast(128), (), [t_g])
        for who in range(2):
            for j in range(12):
                ps, tp = P.ps()
                for k in range(8):
                    P.dma(wk[:, 0:512], I['ada_w'][l, k * 128:(k + 1) * 128, j * 512:(j + 1) * 512], (), [t_w])
                    P.ts(ws[:, 0:512], wk[:, 0:512], cs[:, who, k:k + 1], None, ALU.mult, None, [t_w, t_cs], [t_ws])
                    P.mm(ps[:, :], K.ones[:, :], ws[:, 0:512], k == 0, False, [t_ws, K.t_ones], [tp])
                P.mm(ps[:, :], K.ones[0:1, :], bt[0:1, j * 512:(j + 1) * 512], False, True, [t_b, K.t_ones], [tp])
                P.copy(raw[:, j * 512:(j + 1) * 512], ps[:, :], [tp], [t_raw])
            base = who * 6
            m = mo
            for half in range(2):
                sh = raw[:, (3 * half) * D:(3 * half + 1) * D]
                sc = raw[:, (3 * half + 1) * D:(3 * half + 2) * D]
                gg = raw[:, (3 * half + 2) * D:(3 * half + 3) * D]
                P.stt(m[:, base + 3 * half + 0, :], sc, 1.0, gn[:, half, :], ALU.add, ALU.mult, [t_raw, t_g], [K.t_mod])
                P.copy(m[:, base + 3 * half + 1, :], sh, [t_raw], [K.t_mod])
                P.copy(m[:, base + 3 * half + 2, :], gg, [t_raw], [K.t_mod])
        P.dma(K.MOD.rearrange("(o j) d -> o j d", o=1), mo[0:1, :, :], [K.t_mod], [("MOD", 0)], q='pool')
        P.barrier()


def rms_rstd(K, xt, t_x, width, junk, ss, t_s):
    P = K.P
    P.act(junk, xt, AF.Square, [t_x], [t_s], accum_out=ss[:, 0:1])
    P.ts(ss[:, 0:1], ss[:, 0:1], 1.0 / width, EPS, ALU.mult, ALU.add, [t_s], [t_s])
    P.act(ss[:, 0:1], ss[:, 0:1], AF.Sqrt, [t_s], [t_s])
    P.recip(ss[:, 0:1], ss[:, 0:1], [t_s], [t_s])


def stage_inproj(K, l):
    P, nc, I = K.P, K.P.nc, K.I
    src = perm_cols(640)
    with SB(nc, "i_w", [128, 8, IN_W], F32) as W, SB(nc, "i_wp", [128, 8, 640], F32) as WP, \
            SB(nc, "i_x", [128, 2, D], F32) as xt, SB(nc, "i_h", [128, 2, D], F32) as ht, \
            SB(nc, "i_hT", [128, 8, 512], F32) as hT, SB(nc, "i_ss", [128, 2], F32) as ss, \
            SB(nc, "i_junk", [128, D], F32) as junk, SB(nc, "i_o", [128, 2, 768], F32) as ot, \
            SB(nc, "i_rc", [128, 512], F32) as rc, SB(nc, "i_rs", [128, 512], F32) as rs, \
            SB(nc, "i_t1", [128, 512], F32) as t1, SB(nc, "i_t2", [128, 512], F32) as t2, \
            SB(nc, "i_mod", [128, 4, D], F32) as modbc, SB(nc, "i_wst", [128, 2, IN_W], F32) as wst:
        t_W, t_WP = Tok("W"), Tok("WP")
        t_wst = [Tok("wst0"), Tok("wst1")]
        for jj, r in enumerate((0, 1, 6, 7)):
            P.dma(modbc[:, jj, :], K.MOD[r, :].partition_broadcast(128), (), [K.t_mod])
        t_x = [Tok("x0"), Tok("x1")]
        t_h = [Tok("h0"), Tok("h1")]
        t_s = [Tok("s0"), Tok("s1")]
        t_hT, t_o, t_rc, t_rs, t_t1, t_t2 = Tok("hT"), [Tok("o0"), Tok("o1")], Tok("rc"), Tok("rs"), Tok("t1"), Tok("t2")
        for k in range(8):
            P.load_r(W[:, k, :], I['w_in'][l, k * 128:(k + 1) * 128, :], wst[:, k % 2, :], t_W, t_wst[k % 2], e='act' if k % 2 else 'dve')
        wv = I['w_in'][l, :, HY_END:HY_END + 640].rearrange("(k p) (b two s) -> k p b two s", p=128, two=2, s=16)
        for k in range(8):
            dst = wst[:, k % 2, 0:640].rearrange("p (b two s) -> p b two s", two=2, s=16)
            P.dma(dst[:, :, 0, :], wv[k, :, :, 1, :], (), [t_wst[k % 2]])
            P.dma(dst[:, :, 1, :], wv[k, :, :, 0, :], (), [t_wst[k % 2]])
            P.copy(RR(WP[:, k, :]), wst[:, k % 2, 0:640], [t_wst[k % 2]], [t_WP], e='act' if k % 2 else 'dve')
        ngroups = (T + 511) // 512
        for g in range(ngroups):
            t0 = g * 512
            ntok = min(512, T - t0)
            ntile = ntok // 128
            for i in range(ntile):
                ti = g * 4 + i
                b = i % 2
                isctx = ti < 2
                mb = 2 if isctx else 0
                P.dma(xt[:, b, :], K.X[ti * 128:(ti + 1) * 128, :], [K.tX[ti]], [t_x[b]])
                rms_rstd(K, xt[:, b, :], t_x[b], D, junk[:], ss[:, b:b + 1], t_s[b])
                P.stt(ht[:, b, :], xt[:, b, :], ss[:, b:b + 1], modbc[:, mb + 0, :], ALU.mult, ALU.mult,
                      [t_x[b], t_s[b], K.t_mod], [t_h[b]])
                P.tt(ht[:, b, :], ht[:, b, :], modbc[:, mb + 1, :], ALU.add, [t_h[b], K.t_mod], [t_h[b]])
                for half in range(2):
                    ps, tp = P.ps()
                    for j in range(4):
                        kk = half * 4 + j
                        P.tr(ps[:, j * 128:(j + 1) * 128], ht[:, b, kk * 128:(kk + 1) * 128], K.ident[:], [t_h[b], K.t_ident], [tp])
                    P.copy(RR(hT[:, half * 4:(half + 1) * 4, i * 128:(i + 1) * 128]),
                           ps[:, :].rearrange("p (j t) -> p j t", j=4), [tp], [t_hT], e='act' if half else 'dve')
            for i in range(ntile):
                ti = g * 4 + i
                b = i % 2
                for (c0, cw, dst, dcol) in ((0, 512, K.ZT, 0), (512, 256, K.ZT, 512), (K_END, 128, K.VT, 0)):
                    ps, tp = P.ps()
                    for k in range(8):
                        P.mm(ps[:, 0:cw], hT[:, k, i * 128:(i + 1) * 128], W[:, k, c0:c0 + cw], k == 0, k == 7, [t_hT, t_W], [tp], r=FAST)
                    P.copy(ot[:, b, 0:cw], ps[:, 0:cw], [tp], [t_o[b]], e='act')
                    P.dma(dst[ti * 128:(ti + 1) * 128, dcol:dcol + cw], ot[:, b, 0:cw], [t_o[b]], [(dst.tensor.name, ti)], q='pool')
            P.dma(rc[:, 0:ntok], I['k_ropec'][:, t0:t0 + ntok], (), [t_rc])
            P.dma(rs[:, 0:ntok], I['k_ropes'][:, t0:t0 + ntok], (), [t_rs])
            for cidx in range(5):
                c0 = HY_END + cidx * 128
                ps, tp = P.ps()
                ps2, tp2 = P.ps()
                for k in range(8):
                    P.mm(ps[:, 0:ntok], W[:, k, c0:c0 + 128], hT[:, k, 0:ntok], k == 0, k == 7, [t_hT, t_W], [tp], r=FAST)
                for k in range(8):
                    P.mm(ps2[:, 0:ntok], WP[:, k, cidx * 128:(cidx + 1) * 128], hT[:, k, 0:ntok], k == 0, k == 7, [t_hT, t_WP], [tp2], r=FAST)
                P.tt(t1[:, 0:ntok], ps[:, 0:ntok], rc[:, 0:ntok], ALU.mult, [tp, t_rc], [t_t1])
                P.tt(t2[:, 0:ntok], ps2[:, 0:ntok], rs[:, 0:ntok], ALU.mult, [tp2, t_rs], [t_t2])
                P.tt(t1[:, 0:ntok], t1[:, 0:ntok], t2[:, 0:ntok], ALU.add, [t_t1, t_t2], [t_t1])
                if cidx < 4:
                    P.dma(K.QF[cidx * 128:(cidx + 1) * 128, t0:t0 + ntok], t1[:, 0:ntok], [t_t1], [("QF", g)], q='pool')
                else:
                    P.dma(K.KF[:, t0:t0 + ntok], t1[:, 0:ntok], [t_t1], [("KF", g)], q='pool')
            for cidx in range(2):
                c0 = V_END + cidx * 128
                ps, tp = P.ps()
                for k in range(8):
                    P.mm(ps[:, 0:ntok], W[:, k, c0:c0 + 128], hT[:, k, 0:ntok], k == 0, k == 7, [t_hT, t_W], [tp], r=FAST)
                P.copy(t2[:, 0:ntok], ps[:, 0:ntok], [tp], [t_t2], e='act')
                P.dma(K.UF[cidx * 128:(cidx + 1) * 128, t0:t0 + ntok], t2[:, 0:ntok], [t_t2], [("UF", g)], q='pool')


def sin_chain(K, ps_ap, bcol, fcol, v, r, hid, n, t_ps, t_consts, t_v, t_r, t_hid):
    P = K.P
    P.act(v[:, 0:n], ps_ap, AF.Identity, [t_ps] + t_consts, [t_v], bias=bcol, scale=fcol)
    P.ts(r[:, 0:n], v[:, 0:n], MAGIC, MAGIC, ALU.add, ALU.subtract, [t_v], [t_r])
    P.tt(v[:, 0:n], v[:, 0:n], r[:, 0:n], ALU.subtract, [t_v, t_r], [t_v])
    P.act(hid[:, 0:n], v[:, 0:n], AF.Sin, [t_v], [t_hid], scale=TWO_PI)


def stage_hyena(K, l, L, row0, tag):
    P, nc, I = K.P, K.P.nc, K.I
    NCH = L // 128
    NK = NCH + 1
    CC, SS = I['k_cc' + tag], I['k_ss' + tag]
    featT, dec, wkc = I['k_feat' + tag], I['k_dec' + tag], I['k_wk' + tag]
    with SB(nc, "ha_w", [128, 4, 768], F32) as wb, SB(nc, "ha_z", [128, 3, 768], F32) as z, \
            SB(nc, "ha_t", [128, 2, 768], F32) as tt_:
        t_wb, t_z, t_t = Tok("wb"), [Tok("zm"), Tok("z0"), Tok("zp")], [Tok("ta"), Tok("tb")]
        for j in range(3):
            P.dma(wb[:, j, :], I['hy_conv_w'][l, j, :].partition_broadcast(128), (), [t_wb])
        P.dma(wb[:, 3, :], I['hy_conv_b'][l, :].partition_broadcast(128), (), [t_wb])
        for i in range(NCH):
            r0 = row0 + i * 128
            if i == 0:
                P.memset(z[0:1, 0, :], 0.0, [t_z[0]])
                P.dma(z[1:128, 0, :], K.ZT[r0:r0 + 127, 0:768], (), [t_z[0]])
            else:
                P.dma(z[:, 0, :], K.ZT[r0 - 1:r0 + 127, 0:768], (), [t_z[0]])
            P.dma(z[:, 1, :], K.ZT[r0:r0 + 128, 0:768], (), [t_z[1]])
            if i == NCH - 1:
                P.memset(z[:, 2, :], 0.0, [t_z[2]])
                P.dma(z[0:127, 2, :], K.ZT[r0 + 1:r0 + 128, 0:768], (), [t_z[2]])
            else:
                P.dma(z[:, 2, :], K.ZT[r0 + 1:r0 + 129, 0:768], (), [t_z[2]])
            P.tt(tt_[:, 0, :], z[:, 0, :], wb[:, 0, :], ALU.mult, [t_z[0], t_wb], [t_t[0]])
            P.tt(tt_[:, 1, :], z[:, 1, :], wb[:, 1, :], ALU.mult, [t_z[1], t_wb], [t_t[1]], e='pool')
            P.tt(tt_[:, 0, :], tt_[:, 0, :], tt_[:, 1, :], ALU.add, [t_t[0], t_t[1]], [t_t[0]])
            P.tt(tt_[:, 1, :], z[:, 2, :], wb[:, 2, :], ALU.mult, [t_z[2], t_wb], [t_t[1]], e='pool')
            P.tt(tt_[:, 0, :], tt_[:, 0, :], tt_[:, 1, :], ALU.add, [t_t[0], t_t[1]], [t_t[0]])
            P.tt(tt_[:, 0, :], tt_[:, 0, :], wb[:, 3, :], ALU.add, [t_t[0], t_wb], [t_t[0]])
            P.dma(K.ZC[r0:r0 + 128, :], tt_[:, 0, :], [t_t[0]], [("ZC", i)], q='pool')
    P.barrier()
    HH = (NCH + 1) // 2
    with ExitStack() as es_:
        taps = es_.enter_context(SB(nc, "hb_taps", [128, NCH, 1024], F32))
        tabt = es_.enter_context(SB(nc, "hb_cc", [128, 2, HH, 128], F32))
        tabs_ = es_.enter_context(SB(nc, "hb_ss", [128, 2, HH, 128], F32))
        ft = es_.enter_context(SB(nc, "hb_f", [33, 512], F32))
        w1 = es_.enter_context(SB(nc, "hb_w1", [33, 64], F32))
        w2 = es_.enter_context(SB(nc, "hb_w2", [64, 64], F32))
        w3 = es_.enter_context(SB(nc, "hb_w3", [64, 1024], F32))
        cst = es_.enter_context(SB(nc, "hb_c", [64, 8], F32))
        v = es_.enter_context(SB(nc, "hb_v", [64, 512], F32))
        r = es_.enter_context(SB(nc, "hb_r", [64, 512], F32))
        h1 = es_.enter_context(SB(nc, "hb_h1", [64, 512], F32))
        h2 = es_.enter_context(SB(nc, "hb_h2", [64, 512], F32))
        dct = es_.enter_context(SB(nc, "hb_dec", [128, 256], F32))
        ab = es_.enter_context(SB(nc, "hb_abs", [128, 1024], F32))
        rn = es_.enter_context(SB(nc, "hb_rn", [128, 512], F32))
        tmp = es_.enter_context(SB(nc, "hb_tmp", [128, 512], F32))
        wkt = es_.enter_context(SB(nc, "hb_wk", [128, NK], F32))
        ko = es_.enter_context(SB(nc, "hb_o", [128, 2, 512], F32))
        t_taps = [Tok("taps%d" % c) for c in range(NCH)]
        t_cc, t_ss, t_f, t_w, t_c = Tok("cc"), Tok("ss"), Tok("f"), Tok("w"), Tok("c")
        t_v, t_r, t_h1, t_h2, t_dec, t_ab, t_rn, t_tmp, t_wk = (Tok(n) for n in ("v", "r", "h1", "h2", "dec", "ab", "rn", "tmp", "wk"))
        t_ko = [Tok("ko0"), Tok("ko1")]
        P.dma(w1[:], I['hy_f_w1'][l, :, :], (), [t_w])
        P.dma(w2[:], I['hy_f_w2'][l, :, :], (), [t_w])
        P.dma(w3[:], I['hy_f_w3'][l, :, :], (), [t_w])
        for j, nm in enumerate(('hy_f_b1', 'hy_f_freq1', 'hy_f_b2', 'hy_f_freq2')):
            P.dma(cst[:, j:j + 1], I[nm][l, :].rearrange("(p o) -> p o", o=1), (), [t_c])
        P.ts(cst[:, 4:5], cst[:, 1:2], 1.0 / (2.0 * math.pi), None, ALU.mult, None, [t_c], [t_c])
        P.ts(cst[:, 5:6], cst[:, 3:4], 1.0 / (2.0 * math.pi), None, ALU.mult, None, [t_c], [t_c])
        P.tt(cst[:, 6:7], cst[:, 0:1], cst[:, 4:5], ALU.mult, [t_c], [t_c])
        P.tt(cst[:, 7:8], cst[:, 2:3], cst[:, 5:6], ALU.mult, [t_c], [t_c])
        P.dma(wkt[:], wkc[:, 0].rearrange("(c p) -> p c", p=128), (), [t_wk])
        ng = (L + 511) // 512
        for g in range(ng):
            n0 = g * 512
            n = min(512, L - n0)
            P.dma(ft[:, 0:n], featT[:, n0:n0 + n], (), [t_f])
            ps, tp = P.ps()
            P.mm(ps[0:64, 0:n], w1[0:33, :], ft[0:33, 0:n], True, True, [t_w, t_f], [tp])
            sin_chain(K, ps[0:64, 0:n], cst[:, 6:7], cst[:, 4:5], v, r, h1, n, tp, [t_c], t_v, t_r, t_h1)
            ps, tp = P.ps()
            P.mm(ps[0:64, 0:n], w2[:, :], h1[:, 0:n], True, True, [t_w, t_h1], [tp])
            sin_chain(K, ps[0:64, 0:n], cst[:, 7:8], cst[:, 5:6], v, r, h2, n, tp, [t_c], t_v, t_r, t_h2)
            for sub in range(n // 128):
                c = (n0 // 128) + sub
                P.dma(dct[:], dec[c * 128:(c + 1) * 128, :], (), [t_dec])
                for half in range(2):
                    ps, tp = P.ps()
                    P.mm(ps[:, :], h2[:, sub * 128:(sub + 1) * 128], w3[:, half * 512:(half + 1) * 512], True, True, [t_h2, t_w], [tp])
                    P.tt(RR(taps[:, c, half * 512:(half + 1) * 512].rearrange("p (d c) -> p d c", d=2)),
                         ps[:, :].rearrange("p (d c) -> p d c", d=2),
                         dct[:, :].unsqueeze(1).broadcast_to([128, 2, 256]), ALU.mult, [tp, t_dec], [t_taps[c]])
        tv0 = taps[0:1, 0, :].rearrange("p (o d c) -> p o d c", o=2, d=2)
        P.ts(RR(tv0[:, :, 1, :]), tv0[:, :, 1, :], 0.0, None, ALU.mult, None, [t_taps[0]], [t_taps[0]])
        held, hid_ = P.ps_hold(2)
        for c in range(NCH):
            P.act(ab[:], taps[:, c, :], AF.Abs, [t_taps[c]], [t_ab])
            for half in range(2):
                P.mm(held[half][0][:, :], K.ones[:, :], ab[:, half * 512:(half + 1) * 512], c == 0, c == NCH - 1, [t_ab, K.t_ones], [held[half][1]])
        for o in range(2):
            P.copy(ab[:, o * 512:o * 512 + 256], held[o][0][:, 0:256], [held[o][1]], [t_ab])
            P.tt(rn[:, o * 256:(o + 1) * 256], ab[:, o * 512:o * 512 + 256], held[o][0][:, 256:512], ALU.add, [held[o][1], t_ab], [t_rn])
        P.ps_release(hid_)
        P.recip(rn[:], rn[:], [t_rn], [t_rn])
        for c in range(NCH):
            tv = taps[:, c, :].rearrange("p (o d c) -> p o d c", o=2, d=2)
            tm = tmp[:, :].rearrange("p (o c) -> p o c", o=2)
            P.tt(tm, tv[:, :, 0, :], tv[:, :, 1, :], ALU.add, [t_taps[c]], [t_tmp])
            P.tt(RR(tv[:, :, 1, :]), tv[:, :, 1, :], tv[:, :, 0, :], ALU.subtract, [t_taps[c]], [t_taps[c]])
            P.copy(RR(tv[:, :, 0, :]), tm, [t_tmp], [t_taps[c]])
        rn3 = rn[:, :].rearrange("p (o c) -> p o c", o=2)
        t_st2 = [Tok("fst0"), Tok("fst1")]
        for kc in range(NK):
            for part, TAB, ttab, d in ((0, CC, t_cc, 0), (1, SS, t_ss, 1)):
                ps, tp = P.ps()
                for hf in range(2):
                    cA, cB = hf * HH, min((hf + 1) * HH, NCH)
                    if cA >= cB:
                        continue
                    P.load_r(tabt[:, part, 0:cB - cA, :], TAB[kc, :, cA:cB, :], tabs_[:, part, 0:cB - cA, :], ttab, t_st2[part],
                             e='act' if part == 0 else 'dve')
                    for c in range(cA, cB):
                        tv = taps[:, c, :].rearrange("p (o d c) -> p o d c", o=2, d=2)
                        P.mm(ps[:, :].rearrange("p (o c) -> p o c", o=2), tabt[:, part, c - cA, :], tv[:, :, d, :], c == 0, c == NCH - 1,
                             [ttab, t_taps[c]], [tp], r=FAST)
                P.stt(ko[:, part, :].rearrange("p (o c) -> p o c", o=2), ps[:, :].rearrange("p (o c) -> p o c", o=2),
                      wkt[:, kc:kc + 1], rn3, ALU.mult, ALU.mult, [tp, t_wk, t_rn], [t_ko[part]])
                dst = K.KRE if part == 0 else K.KIM
                P.dma(dst[kc * 128:(kc + 1) * 128, :], ko[:, part, :], [t_ko[part]], [(dst.tensor.name, kc)], q='pool')
    P.barrier()
    HK = (NK + 1) // 2
    with SB(nc, "hc_a", [128, NCH, 256], F32) as a, SB(nc, "hc_p1", [128, NK, 256], F32) as p1, \
            SB(nc, "hc_p2", [128, NK, 256], F32) as p2, SB(nc, "hc_tb", [128, 4, HK, 128], F32) as tb, \
            SB(nc, "hc_k", [128, 2, 256], F32) as kk, \
            SB(nc, "hc_t", [128, 2, 256], F32) as tq, SB(nc, "hc_b", [128, 2, 256], F32) as bb, \
            SB(nc, "hc_x", [128, 256], F32) as xg, SB(nc, "hc_y", [128, 256], F32) as yy, \
            SB(nc, "hc_stg", [128, 4, HK, 128], F32) as stg:
        t_stg = [Tok("stg%d" % i) for i in range(4)]
        t_tb = [Tok("tb%d" % i) for i in range(4)]
        ldn = [0]

        def load_tabs(blk, cA, cB):
            pb = ldn[0] % 2
            ldn[0] += 1
            P.load_r(tb[:, 2 * pb, 0:cB - cA, :], CC[blk, :, cA:cB, :], stg[:, 2 * pb, 0:cB - cA, :], t_tb[2 * pb], t_stg[2 * pb], e='act')
            P.load_r(tb[:, 2 * pb + 1, 0:cB - cA, :], SS[blk, :, cA:cB, :], stg[:, 2 * pb + 1, 0:cB - cA, :], t_tb[2 * pb + 1], t_stg[2 * pb + 1], e='dve')
            return tb[:, 2 * pb], tb[:, 2 * pb + 1], t_tb[2 * pb], t_tb[2 * pb + 1]
        t_a = [Tok("a%d" % c) for c in range(NCH)]
        t_p1, t_p2, t_k, t_b, t_x, t_y = (Tok(n) for n in ("p1", "p2", "k", "b", "x", "y"))
        t_q = [Tok("q0"), Tok("q1")]
        for o in range(2):
            P.dma(bb[:, o, :], I['hy_bias'][l, o, :].partition_broadcast(128), (), [t_b])
        for c in range(NCH):
            P.load_r(a[:, c, :], K.ZC[row0 + c * 128: row0 + (c + 1) * 128, 0:256], xg[:], t_a[c], t_x, e='act' if c % 2 else 'dve')
        for o in range(2):
            for kc in range(NK):
                P.dma(kk[:, 0, :], K.KRE[kc * 128:(kc + 1) * 128, o * 256:(o + 1) * 256], (), [t_k])
                P.dma(kk[:, 1, :], K.KIM[kc * 128:(kc + 1) * 128, o * 256:(o + 1) * 256], (), [t_k])
                psr, tpr = P.ps()
                psi, tpi = P.ps()
                for cA in range(0, NCH, HK):
                    cB = min(cA + HK, NCH)
                    cct, sst, t_cc, t_ss = load_tabs(kc, cA, cB)
                    for c in range(cA, cB):
                        P.mm(psr[:, 0:256], cct[:, c - cA, :], a[:, c, :], c == 0, c == NCH - 1, [t_cc, t_a[c]], [tpr], r=FAST)
                    for c in range(cA, cB):
                        P.mm(psi[:, 0:256], sst[:, c - cA, :], a[:, c, :], c == 0, c == NCH - 1, [t_ss, t_a[c]], [tpi], r=FAST)
                P.tt(tq[:, 0, :], psr[:, 0:256], kk[:, 0, :], ALU.mult, [tpr, t_k], [t_q[0]])
                P.tt(tq[:, 1, :], psi[:, 0:256], kk[:, 1, :], ALU.mult, [tpi, t_k], [t_q[1]])
                P.tt(RR(p1[:, kc, :]), tq[:, 0, :], tq[:, 1, :], ALU.add, [t_q[0], t_q[1]], [t_p1])
                P.tt(tq[:, 0, :], psi[:, 0:256], kk[:, 0, :], ALU.mult, [tpi, t_k], [t_q[0]])
                P.tt(tq[:, 1, :], psr[:, 0:256], kk[:, 1, :], ALU.mult, [tpr, t_k], [t_q[1]])
                P.tt(RR(p2[:, kc, :]), tq[:, 0, :], tq[:, 1, :], ALU.subtract, [t_q[0], t_q[1]], [t_p2])
            for tc in range(NCH):
                r0 = row0 + tc * 128
                P.dma(xg[:], K.ZC[r0:r0 + 128, 256 * (o + 1):256 * (o + 2)], (), [t_x])
                ps, tp = P.ps()
                for kA in range(0, NK, HK):
                    kB = min(kA + HK, NK)
                    cct, sst, t_cc, t_ss = load_tabs(tc, kA, kB)
                    for kc in range(kA, kB):
                        P.mm(ps[:, 0:256], cct[:, kc - kA, :], p1[:, kc, :], kc == 0, False, [t_cc, t_p1], [tp], r=FAST)
                    for kc in range(kA, kB):
                        P.mm(ps[:, 0:256], sst[:, kc - kA, :], p2[:, kc, :], False, kc == NK - 1, [t_ss, t_p2], [tp], r=FAST)
                P.tt(yy[:], a[:, tc, :], bb[:, o, :], ALU.mult, [t_a[tc], t_b], [t_y])
                P.tt(yy[:], yy[:], ps[:, 0:256], ALU.add, [t_y, tp], [t_y])
                if o == 0:
                    P.tt(RR(a[:, tc, :]), yy[:], xg[:], ALU.mult, [t_y, t_x], [t_a[tc]])
                else:
                    P.tt(yy[:], yy[:], xg[:], ALU.mult, [t_y, t_x], [t_y])
                    P.dma(K.MIX[r0:r0 + 128, 0:256], yy[:], [t_y], [("MIX", r0)], q='pool')


def stage_attn(K, l):
    P, nc, I = K.P, K.P.nc, K.I
    with SB(nc, "at_k", [64, 2, T], F32) as kf, SB(nc, "at_v", [128, NT, 2, 65], F32) as v1, \
            SB(nc, "at_es", [128, 8], F32) as es, SB(nc, "at_ml", [128, 128], F32) as ml, \
            SB(nc, "at_mr", [128, 128], F32) as mr, SB(nc, "at_q", [64, 2, 4, 128], F32) as q4, \
            SB(nc, "at_pt", [128, 5, 512], F32) as pt, SB(nc, "at_o", [128, 2, 512], F32) as ot, \
            SB(nc, "at_d", [128, 2, 4], F32) as den:
        t_k, t_v, t_es, t_m = Tok("k"), Tok("v"), Tok("es"), Tok("m")
        t_q = [Tok("q0"), Tok("q1")]
        t_pt = [Tok("pt%d" % i) for i in range(5)]
        t_o = [Tok("o0"), Tok("o1")]
        t_d = Tok("d")
        for h in range(2):
            P.dma(kf[:, h, :], K.KF[h * 64:(h + 1) * 64, :], (), [t_k])
        P.memset(v1[:, :, :, 64:65], 1.0, [t_v])
        vtv = K.VT.rearrange("(t p) (h d) -> p t h d", p=128, h=2)
        for h in range(2):
            P.dma(v1[:, :, h, 0:64], vtv[:, :, h, :], (), [t_v])
        P.dma(es[:], I['attn_sink'][l, :].partition_broadcast(128), (), [t_es])
        P.act(es[:], es[:], AF.Exp, [t_es], [t_es])
        P.dma(ml[:], I['k_maskl'][:, :], (), [t_m])
        P.dma(mr[:], I['k_maskr'][:, :], (), [t_m])
        for qb in range(2 if l == DEPTH - 1 else 0, NT):
            kts = [(0, None), (1, None)]
            if qb >= 2:
                if qb - 1 >= 2:
                    kts.append((qb - 1, ml))
                kts.append((qb, None))
                if qb + 1 < NT:
                    kts.append((qb + 1, mr))
            ob = qb % 2
            for kvh in range(2):
                qi = kvh
                P.dma(q4[:, qi, :, :], K.QF[kvh * 256:(kvh + 1) * 256, qb * 128:(qb + 1) * 128].rearrange("(h d) t -> d h t", d=64),
                      (), [t_q[qi]])
                for idx, (kt, mask) in enumerate(kts):
                    ps, tp = P.ps()
                    P.mm(ps[:, :], kf[:, kvh, kt * 128:(kt + 1) * 128], q4[:, qi, :, :].rearrange("d h t -> d (h t)"), True, True,
                         [t_k, t_q[qi]], [tp])
                    P.act(pt[:, idx, :], ps[:, :], AF.Exp, [tp], [t_pt[idx]], scale=0.125)
                    if mask is not None:
                        pv = pt[:, idx, :].rearrange("p (h t) -> p h t", h=4)
                        P.tt(pv, pv, mask[:, :].unsqueeze(1).broadcast_to([128, 4, 128]), ALU.mult, [t_pt[idx], t_m], [t_pt[idx]])
                pso, tpo = P.ps()
                for hh in range(4):
                    for idx, (kt, mask) in enumerate(kts):
                        P.mm(pso[:, hh * 65:(hh + 1) * 65], pt[:, idx, hh * 128:(hh + 1) * 128], v1[:, kt, kvh, :],
                             idx == 0, idx == len(kts) - 1, [t_pt[idx], t_v], [tpo])
                pv = pso[:, 0:260].rearrange("p (h c) -> p h c", h=4)
                P.tt(den[:, kvh, :], pv[:, :, 64], es[:, kvh * 4:(kvh + 1) * 4], ALU.add, [tpo, t_es], [t_d])
                P.recip(den[:, kvh, :], den[:, kvh, :], [t_d], [t_d])
                P.tt(ot[:, ob, kvh * 256:(kvh + 1) * 256].rearrange("p (h d) -> p h d", h=4), pv[:, :, 0:64],
                     den[:, kvh, :].unsqueeze(2).broadcast_to([128, 4, 64]), ALU.mult, [tpo, t_d], [t_o[ob]])
            P.dma(K.MIX[qb * 128:(qb + 1) * 128, 256:768], ot[:, ob, :], [t_o[ob]], [("MIXa", qb)], q='pool')


def round_frac(K, v, r, t_v, t_r, e='dve'):
    P = K.P
    P.ts(r, v, MAGIC, MAGIC, ALU.add, ALU.subtract, [t_v], [t_r], e=e)
    P.tt(v, v, r, ALU.subtract, [t_v, t_r], [t_v], e=e)


def stage_s5(K, l):
    P, nc, I = K.P, K.P.nc, K.I
    K.GS = K.P.dram.get("sGS")
    if K.GS is None:
        K.GS = P.dram_t("sGS", [256, T])
    with SB(nc, "s_bt", [32, 16, 2, 128], F32) as BT, SB(nc, "s_cb", [128, 16, 2, 32], F32) as CB, \
            SB(nc, "s_par", [128, 12, 16], F32) as par, SB(nc, "s_dsk", [32, 8], F32) as dsk, \
            SB(nc, "s_iota", [128, 512], F32) as iot:
        t_BT, t_CB, t_par, t_dsk, t_iota = Tok("BT"), Tok("CB"), Tok("par"), Tok("dsk"), Tok("iota")
        MAG, PHI = 4, 6
        with SB(nc, "s_b", [128, 2, 16, 16], F32) as bri, SB(nc, "s_bb", [128, 2, 16, 16], F32) as bb, \
                SB(nc, "s_bd", [128, 16, 2, 32], F32) as BD, SB(nc, "s_cnd", [32, 16, 2, 128], F32) as CND, \
                SB(nc, "s_t1", [128, 16, 16], F32) as t1, SB(nc, "s_t2", [128, 16, 16], F32) as t2:
            t_b, t_bb, t_BD, t_CND, t_t1, t_t2 = Tok("b"), Tok("bb"), Tok("BD"), Tok("CND"), Tok("t1"), Tok("t2")
            for d in range(2):
                P.dma(par[:, 0, d * 8:(d + 1) * 8], I['s5_lam_re'][l, d].rearrange("g p -> (g p)").rearrange("(gh q) -> q gh", q=128), (), [t_par])
                P.dma(par[:, 1, d * 8:(d + 1) * 8], I['s5_lam_im'][l, d].rearrange("g p -> (g p)").rearrange("(gh q) -> q gh", q=128), (), [t_par])
                ldv = I['s5_log_dt'][l, d].rearrange("(gh gl) -> gl gh", gl=2)
                for gl in range(2):
                    P.dma(par[gl * 64:(gl + 1) * 64, 2, d * 8:(d + 1) * 8], ldv[gl].partition_broadcast(64), (), [t_par])
                for ri, nm in enumerate(('s5_b_re', 's5_b_im')):
                    P.dma(bri[:, ri, d * 8:(d + 1) * 8, :],
                          I[nm][l, d].rearrange("g p c -> (g p c)").rearrange("(gh q c) -> q gh c", q=128, c=16), (), [t_b])
            P.dma(dsk[:], I['s5_d'][l, :].rearrange("(j r) -> r j", r=32), (), [t_dsk])
            P.dma(iot[:], I['k_iota'][0, :].partition_broadcast(128), (), [t_iota])
            pp = lambda i: par[:, i, :]
            tp_ = [t_par]
            P.act(pp(2), pp(2), AF.Exp, tp_, tp_)
            P.tt(pp(3), pp(0), pp(2), ALU.mult, tp_, tp_)
            P.act(pp(MAG), pp(3), AF.Exp, tp_, tp_)
            P.tt(pp(3), pp(1), pp(2), ALU.mult, tp_, tp_)
            P.ts(pp(PHI), pp(3), 1.0 / (2.0 * math.pi), None, ALU.mult, None, tp_, tp_)
            P.copy(pp(5), pp(PHI), tp_, tp_)
            round_frac(K, pp(5), pp(11), t_par, t_par)
            P.act(pp(8), pp(5), AF.Sin, tp_, tp_, scale=TWO_PI)
            P.ts(pp(5), pp(PHI), 0.25, None, ALU.add, None, tp_, tp_)
            round_frac(K, pp(5), pp(11), t_par, t_par)
            P.act(pp(7), pp(5), AF.Sin, tp_, tp_, scale=TWO_PI)
            P.tt(pp(7), pp(7), pp(MAG), ALU.mult, tp_, tp_)
            P.tt(pp(8), pp(8), pp(MAG), ALU.mult, tp_, tp_)
            P.ts(pp(7), pp(7), -1.0, None, ALU.add, None, tp_, tp_)
            P.tt(pp(3), pp(0), pp(0), ALU.mult, tp_, tp_)
            P.tt(pp(5), pp(1), pp(1), ALU.mult, tp_, tp_)
            P.tt(pp(3), pp(3), pp(5), ALU.add, tp_, tp_)
            P.recip(pp(3), pp(3), tp_, tp_)
            P.tt(pp(9), pp(7), pp(0), ALU.mult, tp_, tp_)
            P.tt(pp(5), pp(8), pp(1), ALU.mult, tp_, tp_)
            P.tt(pp(9), pp(9), pp(5), ALU.add, tp_, tp_)
            P.tt(pp(9), pp(9), pp(3), ALU.mult, tp_, tp_)
            P.tt(pp(10), pp(8), pp(0), ALU.mult, tp_, tp_)
            P.tt(pp(5), pp(7), pp(1), ALU.mult, tp_, tp_)
            P.tt(pp(10), pp(10), pp(5), ALU.subtract, tp_, tp_)
            P.tt(pp(10), pp(10), pp(3), ALU.mult, tp_, tp_)
            cre = par[:, 9, :].unsqueeze(2).broadcast_to([128, 16, 16])
            cim = par[:, 10, :].unsqueeze(2).broadcast_to([128, 16, 16])
            P.tt(t1[:], bri[:, 0, :, :], cre, ALU.mult, [t_b, t_par], [t_t1])
            P.tt(t2[:], bri[:, 1, :, :], cim, ALU.mult, [t_b, t_par], [t_t2])
            P.tt(bb[:, 0, :, :], t1[:], t2[:], ALU.subtract, [t_t1, t_t2], [t_bb])
            P.tt(t1[:], bri[:, 1, :, :], cre, ALU.mult, [t_b, t_par], [t_t1])
            P.tt(t2[:], bri[:, 0, :, :], cim, ALU.mult, [t_b, t_par], [t_t2])
            P.tt(bb[:, 1, :, :], t1[:], t2[:], ALU.add, [t_t1, t_t2], [t_bb])
            P.memset(BD[:], 0.0, [t_BD])
            for ri in range(2):
                P.copy(BD[0:64, :, ri, 0:16], bb[0:64, ri, :, :], [t_bb], [t_BD])
                P.copy(BD[64:128, :, ri, 16:32], bb[64:128, ri, :, :], [t_bb], [t_BD])
            for dg in range(16):
                ps, tp = P.ps()
                for ri in range(2):
                    P.tr(ps[0:32, ri * 128:(ri + 1) * 128], BD[:, dg, ri, :], K.ident[:], [t_BD, K.t_ident], [tp])
                P.copy(BT[:, dg, :, :], ps[0:32, 0:256].rearrange("p (r m) -> p r m", r=2), [tp], [t_BT])
            P.memset(CND[:], 0.0, [t_CND])
            for d in range(2):
                for ri, nm in enumerate(('s5_c_re', 's5_c_im')):
                    cv = I[nm][l, d].rearrange("(gh gl) c p -> gl c gh p", gl=2)
                    for gl in range(2):
                        P.dma(CND[gl * 16:(gl + 1) * 16, d * 8:(d + 1) * 8, ri, gl * 64:(gl + 1) * 64], cv[gl], (), [t_CND])
            for dg in range(16):
                ps, tp = P.ps()
                for ri in range(2):
                    P.tr(ps[:, ri * 32:(ri + 1) * 32], CND[:, dg, ri, :], K.ident[0:32, 0:32], [t_CND, K.t_ident], [tp])
                P.copy(CB[:, dg, 0, :], ps[:, 0:32], [tp], [t_CB])
                P.ts(CB[:, dg, 1, :], ps[:, 32:64], -1.0, None, ALU.mult, None, [tp], [t_CB])
        P.barrier()
        with SB(nc, "s_us", [32, T], F32) as us, SB(nc, "s_y", [32, T], F32) as ysb, \
                SB(nc, "s_tab", [128, 2, 4, 512], F32) as tab2, SB(nc, "s_m", [128, 2, 2, 512], F32) as mm2, \
                SB(nc, "s_w", [128, 2, 2, 512], F32) as ww2, SB(nc, "s_s", [128, 2, 2, 512], F32) as ss2, \
                SB(nc, "s_q", [128, 2, 2, 512], F32) as qq2, SB(nc, "s_c0", [128, 2, 4], F32) as c02, \
                SB(nc, "s_car", [128, 2], F32) as car, SB(nc, "s_mag", [128, 512], F32) as magt, SB(nc, "s_g", [32, 2, T], F32) as gg:
            t_us, t_y, t_car, t_g = (Tok(n) for n in ("us", "y", "car", "g"))
            tk2 = [dict(m=Tok("m%d" % b), w=Tok("w%d" % b), s=Tok("s%d" % b), q=Tok("q%d" % b), c0=Tok("c0%d" % b),
                        tab=[Tok("tabs%d" % b), Tok("tabc%d" % b), Tok("vr%d" % b), Tok("vrr%d" % b)]) for b in range(2)]
            chunk_no = [0]
            t_mag = Tok("mag")
            items = []
            for j in range(8):
                for d in range(2):
                    if d == 0:
                        chunks = [(a, min(a + 512, T), False, a) for a in range(0, T, 512)]
                    else:
                        chunks = [(0, NCTX, True, 0)]
                        b = T
                        while b > NCTX:
                            a = max(b - 512, NCTX)
                            chunks.append((a, b, True, NCTX + (T - b)))
                            b = a
                    for ci, (a, b, rev, i0) in enumerate(chunks):
                        items.append(dict(j=j, d=d, dg=d * 8 + j, a=a, b=b, rev=rev, i0=i0, first=(ci == 0),
                                          jfirst=(d == 0 and ci == 0), jlast=(d == 1 and ci == len(chunks) - 1), pb=len(items) % 2))

            def emit_tables(it):
                n = it['b'] - it['a']
                pb = it['pb']
                tab, qq, c0 = tab2[:, pb], qq2[:, pb], c02[:, pb]
                t_q, t_c0, t_tab = tk2[pb]['q'], tk2[pb]['c0'], tk2[pb]['tab']
                phi = par[:, PHI, it['dg']:it['dg'] + 1]
                P.ts(c0[:, 0:1], phi, float(it['i0']), None, ALU.mult, None, [t_par], [t_c0])
                round_frac(K, c0[:, 0:1], c0[:, 2:3], t_c0, t_c0)
                P.ts(c0[:, 1:2], c0[:, 0:1], 0.25, None, ALU.add, None, [t_c0], [t_c0])
                for which in range(2):
                    P.act(tab[:, 2 + which, 0:n], iot[:, 0:n], AF.Identity, [t_iota, t_c0, t_par], [t_tab[2 + which]],
                          bias=c0[:, which:which + 1], scale=phi)
                    P.ts(tab[:, which, 0:n], tab[:, 2 + which, 0:n], MAGIC, MAGIC, ALU.add, ALU.subtract, [t_tab[2 + which]], [t_tab[which]])
                    P.tt(tab[:, 2 + which, 0:n], tab[:, 2 + which, 0:n], tab[:, which, 0:n], ALU.subtract, [t_tab[2 + which], t_tab[which]],
                         [t_tab[2 + which]])
                    P.act(tab[:, which, 0:n], tab[:, 2 + which, 0:n], AF.Sin, [t_tab[2 + which]], [t_tab[which]], scale=TWO_PI)

            def emit_main(it):
                j, d, dg, a, b, rev, pb = it['j'], it['d'], it['dg'], it['a'], it['b'], it['rev'], it['pb']
                n = b - a
                tab, mm_, ww, ss_, qq = tab2[:, pb], mm2[:, pb], ww2[:, pb], ss2[:, pb], qq2[:, pb]
                t_m, t_w, t_s, t_q, t_tab = tk2[pb]['m'], tk2[pb]['w'], tk2[pb]['s'], tk2[pb]['q'], tk2[pb]['tab']
                rv = (lambda ap: ap[:, ::-1]) if rev else (lambda ap: ap)
                if it['first']:
                    P.copy(magt[:, :], par[:, MAG, dg:dg + 1].broadcast_to([128, 512]), [t_par], [t_mag])
                ps1, tp1 = P.ps()
                ps2, tp2 = P.ps()
                P.mm(ps1[:, 0:n], BT[:, dg, 0, :], us[:, a:b], True, True, [t_BT, t_us], [tp1])
                P.mm(ps2[:, 0:n], BT[:, dg, 1, :], us[:, a:b], True, True, [t_BT, t_us], [tp2])
                sn = rv(tab[:, 0, 0:n])
                cs = rv(tab[:, 1, 0:n])
                tS, tC = t_tab[0], t_tab[1]
                P.tt(mm_[:, 0, 0:n], ps1[:, 0:n], cs, ALU.mult, [tp1, tC], [t_m])
                P.tt(qq[:, 0, 0:n], ps2[:, 0:n], sn, ALU.mult, [tp2, tS], [t_q])
                P.tt(mm_[:, 0, 0:n], mm_[:, 0, 0:n], qq[:, 0, 0:n], ALU.add, [t_m, t_q], [t_m])
                P.tt(mm_[:, 1, 0:n], ps2[:, 0:n], cs, ALU.mult, [tp2, tC], [t_m])
                P.tt(qq[:, 1, 0:n], ps1[:, 0:n], sn, ALU.mult, [tp1, tS], [t_q])
                P.tt(mm_[:, 1, 0:n], mm_[:, 1, 0:n], qq[:, 1, 0:n], ALU.subtract, [t_m, t_q], [t_m])
                magb = magt[:, 0:n]
                for ri in range(2):
                    init = 0.0 if it['first'] else car[:, ri:ri + 1]
                    P.op('dve', lambda g, ri=ri, init=init: g.tensor_tensor_scan(
                        out=rv(ww[:, ri, 0:n]), data0=magb, data1=rv(mm_[:, ri, 0:n]), initial=init,
                        op0=ALU.mult, op1=ALU.add), [t_m, t_mag, t_car], [t_w])
                last = a if rev else b - 1
                P.copy(car[:, :], ww[:, :, last - a], [t_w], [t_car])
                P.tt(ss_[:, 0, 0:n], ww[:, 0, 0:n], cs, ALU.mult, [t_w, tC], [t_s], e='pool')
                P.tt(qq[:, 0, 0:n], ww[:, 1, 0:n], sn, ALU.mult, [t_w, tS], [t_q], e='pool')
                P.tt(ss_[:, 0, 0:n], ss_[:, 0, 0:n], qq[:, 0, 0:n], ALU.subtract, [t_s, t_q], [t_s], e='pool')
                P.tt(ss_[:, 1, 0:n], ww[:, 0, 0:n], sn, ALU.mult, [t_w, tS], [t_s], e='pool')
                P.tt(qq[:, 1, 0:n], ww[:, 1, 0:n], cs, ALU.mult, [t_w, tC], [t_q], e='pool')
                P.tt(ss_[:, 1, 0:n], ss_[:, 1, 0:n], qq[:, 1, 0:n], ALU.add, [t_s, t_q], [t_s], e='pool')
                psy, tpy = P.ps()
                P.mm(psy[0:32, 0:n], CB[:, dg, 0, :], ss_[:, 0, 0:n], True, False, [t_CB, t_s], [tpy])
                P.mm(psy[0:32, 0:n], CB[:, dg, 1, :], ss_[:, 1, 0:n], False, True, [t_CB, t_s], [tpy])
                if d == 0:
                    P.copy(ysb[:, a:b], psy[0:32, 0:n], [tpy], [t_y], e='act')
                else:
                    P.tt(ysb[:, a:b], ysb[:, a:b], psy[0:32, 0:n], ALU.add, [t_y, tpy], [t_y])

            emit_tables(items[0])
            for idx, it in enumerate(items):
                j = it['j']
                if it['jfirst']:
                    P.dma(us[:], K.UF[32 * j:32 * (j + 1), :], (), [t_us])
                if idx + 1 < len(items):
                    emit_tables(items[idx + 1])
                emit_main(it)
                if not it['jlast']:
                    continue
                P.stt(ysb[:], us[:], dsk[:, j:j + 1], ysb[:], ALU.mult, ALU.add, [t_us, t_dsk, t_y], [t_y])
                P.tt(gg[:, 0, :], ysb[:], ysb[:], ALU.mult, [t_y], [t_g])
                P.ts(gg[:, 0, :], gg[:, 0, :], 0.044715, 1.0, ALU.mult, ALU.add, [t_g], [t_g])
                P.tt(gg[:, 0, :], gg[:, 0, :], ysb[:], ALU.mult, [t_g, t_y], [t_g])
                P.act(gg[:, 1, :], gg[:, 0, :], AF.Sigmoid, [t_g], [t_g], scale=1.5957691216)
                P.tt(gg[:, 1, :], gg[:, 1, :], ysb[:], ALU.mult, [t_g, t_y], [t_g])
                P.dma(K.GS[32 * j:32 * (j + 1), :], gg[:, 1, :], [t_g], [("GS", j)], q='pool')
        P.barrier()
        with SB(nc, "s_gw", [128, 2, 256], F32) as gw, SB(nc, "s_gb", [128, 2], F32) as gb, \
                SB(nc, "s_gt", [128, 2, 512], F32) as gt, SB(nc, "s_sg", [128, 2, 512], F32) as sg, \
                SB(nc, "s_o", [128, 4, 256], F32) as so:
            t_gw, t_gt, t_sg, t_so = Tok("gw"), Tok("gt"), Tok("sg"), Tok("so")
            P.dma(gw[:], I['s5_glu_w'][l].rearrange("(c p) n -> p c n", p=128), (), [t_gw])
            P.dma(gb[:], I['s5_glu_b'][l, :].rearrange("(c p) -> p c", p=128), (), [t_gw])
            for g in range((T + 511) // 512):
                t0 = g * 512
                n = min(512, T - t0)
                P.dma(gt[:, :, 0:n], K.GS[:, t0:t0 + n].rearrange("(c p) t -> p c t", p=128), (), [t_gt])
                for oc in range(2):
                    ps, tp = P.ps()
                    for kc in range(2):
                        P.mm(ps[:, 0:n], gw[:, kc, oc * 128:(oc + 1) * 128], gt[:, kc, 0:n], kc == 0, kc == 1, [t_gw, t_gt], [tp])
                    P.act(sg[:, oc, 0:n], ps[:, 0:n], AF.Sigmoid, [tp, t_gw], [t_sg], bias=gb[:, oc:oc + 1], scale=1.0)
                    P.tt(sg[:, oc, 0:n], sg[:, oc, 0:n], gt[:, oc, 0:n], ALU.mult, [t_sg, t_gt], [t_sg])
                for i in range(n // 128):
                    ps, tp = P.ps()
                    for oc in range(2):
                        P.tr(ps[:, oc * 128:(oc + 1) * 128], sg[:, oc, i * 128:(i + 1) * 128], K.ident[:], [t_sg, K.t_ident], [tp])
                    P.copy(so[:, i, :], ps[:, 0:256], [tp], [t_so], e='act')
                P.dma(K.MIX[t0:t0 + n, 768:1024].rearrange("(i p) c -> p i c", p=128), so[:, 0:n // 128, :], [t_so], [("MIXs", g)], q='pool')


def stage_outproj(K, l):
    P, nc, I = K.P, K.P.nc, K.I
    with SB(nc, "o_w", [128, 8, D], F32) as W, SB(nc, "o_rw", [128, 8, 16], F32) as RW, \
            SB(nc, "o_gain", [128, D], F32) as gain, SB(nc, "o_mod", [128, 6, D], F32) as mod, \
            SB(nc, "o_rb", [128, 16], F32) as rb, SB(nc, "o_mix", [128, D], F32) as mix, \
            SB(nc, "o_x", [128, D], F32) as xt, SB(nc, "o_junk", [128, D], F32) as junk, \
            SB(nc, "o_mT", [128, 8, 128], F32) as mT, SB(nc, "o_h2", [128, D], F32) as h2, \
            SB(nc, "o_hT", [128, 8, 128], F32) as hT, SB(nc, "o_tmp", [128, D], F32) as tmp, \
            SB(nc, "o_st", [128, 8], F32) as st, SB(nc, "o_r", [128, 12, 16], F32) as rr, \
            SB(nc, "o_gT", [16, 128], F32) as gT:
        t_W, t_c, t_mix, t_x, t_mT, t_h2, t_hT, t_tmp, t_st, t_rr, t_gT = (Tok(n) for n in (
            "W", "c", "mix", "x", "mT", "h2", "hT", "tmp", "st", "rr", "gT"))
        for k in range(8):
            P.load_r(W[:, k, :], I['w_out'][l, k * 128:(k + 1) * 128, :], junk[:], t_W, t_tmp, e='act' if k % 2 else 'dve')
        P.dma(RW[:], I['router_w'].rearrange("(k p) n -> p k n", p=128), (), [t_c])
        P.dma(gain[:], I['mix_norm_g'][l, :].partition_broadcast(128), (), [t_c])
        P.dma(rb[:], I['router_b'][0, :].partition_broadcast(128), (), [t_c])
        for jj, r in enumerate((2, 3, 4, 8, 9, 10)):
            P.dma(mod[:, jj, :], K.MOD[r, :].partition_broadcast(128), (), [t_c])
        groups = ((0, 256), (256, 768), (768, 1024))
        for ti in range(2 if l == DEPTH - 1 else 0, NT):
            mb = 3 if ti < 2 else 0
            rows = slice(ti * 128, (ti + 1) * 128)
            P.dma(mix[:], K.MIX[rows, :], (), [t_mix])
            P.dma(xt[:], K.X[rows, :], (), [t_x])
            for gi, (c0, c1) in enumerate(groups):
                rms_rstd(K, mix[:, c0:c1], t_mix, c1 - c0, junk[:, c0:c1], st[:, gi:gi + 1], t_st)
            for gi, (c0, c1) in enumerate(groups):
                P.stt(mix[:, c0:c1], mix[:, c0:c1], st[:, gi:gi + 1], gain[:, c0:c1], ALU.mult, ALU.mult, [t_mix, t_st, t_c], [t_mix])
            for half in range(2):
                ps, tp = P.ps()
                for j in range(4):
                    kk = half * 4 + j
                    P.tr(ps[:, j * 128:(j + 1) * 128], mix[:, kk * 128:(kk + 1) * 128], K.ident[:], [t_mix, K.t_ident], [tp])
                P.copy(RR(mT[:, half * 4:(half + 1) * 4, :]), ps[:, :].rearrange("p (j t) -> p j t", j=4), [tp], [t_mT], e='act' if half else 'dve')
            for half in range(2):
                ps, tp = P.ps()
                for k in range(8):
                    P.mm(ps[:, :], mT[:, k, :], W[:, k, half * 512:(half + 1) * 512], k == 0, k == 7, [t_mT, t_W], [tp], r=FAST)
                hs = slice(half * 512, (half + 1) * 512)
                P.tt(tmp[:, hs], ps[:, :], mod[:, mb + 0, hs], ALU.mult, [tp, t_c], [t_tmp])
                P.tt(xt[:, hs], xt[:, hs], tmp[:, hs], ALU.add, [t_x, t_tmp], [t_x])
            P.dma(K.X[rows, :], xt[:], [t_x], [("X", ti)], q='pool')
            rms_rstd(K, xt[:], t_x, D, junk[:], st[:, 3:4], t_st)
            P.stt(h2[:], xt[:], st[:, 3:4], mod[:, mb + 1, :], ALU.mult, ALU.mult, [t_x, t_st, t_c], [t_h2])
            P.tt(h2[:], h2[:], mod[:, mb + 2, :], ALU.add, [t_h2, t_c], [t_h2])
            for half in range(2):
                ps, tp = P.ps()
                for j in range(4):
                    kk = half * 4 + j
                    P.tr(ps[:, j * 128:(j + 1) * 128], h2[:, kk * 128:(kk + 1) * 128], K.ident[:], [t_h2, K.t_ident], [tp])
                P.copy(hT[:, half * 4:(half + 1) * 4, :], ps[:, :].rearrange("p (j t) -> p j t", j=4), [tp], [t_hT], e='act' if half else 'dve')
            P.dma(K.H2T[:, ti * 128:(ti + 1) * 128].rearrange("(k p) t -> p k t", p=128), hT[:], [t_hT], [("H2T", ti)], q='pool')
            ps, tp = P.ps()
            for k in range(8):
                P.mm(ps[:, 0:16], hT[:, k, :], RW[:, k, :], k == 0, k == 7, [t_hT, t_c], [tp])
            R = lambda i: rr[:, i, :]
            R4 = lambda i: rr[:, i, :].rearrange("p (g e) -> p g e", g=4)
            trr = [t_rr]
            P.op('dve', lambda g: g.reduce_max(out=st[:, 4:5], in_=ps[:, 0:16], axis=AX.X), [tp], [t_st])
            P.ts(st[:, 4:5], st[:, 4:5], -1.0, None, ALU.mult, None, [t_st], [t_st])
            P.act(R(0), ps[:, 0:16], AF.Exp, [tp, t_st], trr, bias=st[:, 4:5], scale=1.0, accum_out=st[:, 5:6])
            P.recip(st[:, 5:6], st[:, 5:6], [t_st, t_rr], [t_st])
            P.ts(R(0), R(0), st[:, 5:6], None, ALU.mult, None, trr + [t_st], trr)
            P.tt(R(1), R(0), rb[:], ALU.add, trr + [t_c], trr)
            P.op('dve', lambda g: g.reduce_max(out=rr[:, 2, 0:4], in_=R4(1), axis=AX.X), trr, trr)
            P.tt(R4(3), R4(1), rr[:, 2, 0:4].unsqueeze(2).broadcast_to([128, 4, 4]), ALU.is_equal, trr, trr)
            P.stt(R(4), R(3), -1e9, R(1), ALU.mult, ALU.add, trr, trr)
            P.op('dve', lambda g: g.reduce_max(out=rr[:, 5, 0:4], in_=R4(4), axis=AX.X), trr, trr)
            P.tt(rr[:, 6, 0:4], rr[:, 2, 0:4], rr[:, 5, 0:4], ALU.add, trr, trr)
            P.op('dve', lambda g: g.reduce_max(out=st[:, 6:7], in_=rr[:, 6, 0:4], axis=AX.X), trr, [t_st])
            P.ts(rr[:, 7, 0:4], rr[:, 6, 0:4], st[:, 6:7], None, ALU.is_equal, None, trr + [t_st], trr)
            P.tt(R4(8), R4(1), rr[:, 5, 0:4].unsqueeze(2).broadcast_to([128, 4, 4]), ALU.is_ge, trr, trr)
            P.tt(R4(8), R4(8), rr[:, 7, 0:4].unsqueeze(2).broadcast_to([128, 4, 4]), ALU.mult, trr, trr)
            P.tt(R(9), R(8), R(0), ALU.mult, trr, trr)
            P.op('dve', lambda g: g.reduce_sum(out=st[:, 7:8], in_=R(9), axis=AX.X), trr, [t_st])
            P.recip(st[:, 7:8], st[:, 7:8], [t_st], [t_st])
            P.ts(R(9), R(9), st[:, 7:8], None, ALU.mult, None, trr + [t_st], trr)
            ps2, tp2 = P.ps()
            P.tr(ps2[0:16, 0:128], R(9), K.ident[:], trr + [K.t_ident], [tp2])
            P.copy(gT[:], ps2[0:16, 0:128], [tp2], [t_gT], e='act')
            P.dma(K.GTT[:, ti * 128:(ti + 1) * 128], gT[:], [t_gT], [("GTT", ti)], q='pool')


def stage_moe(K, l):
    P, nc, I = K.P, K.P.nc, K.I
    GN = 1024
    with SB(nc, "e_h", [128, 8, GN], F32) as hT, SB(nc, "e_g", [128, 8, GN], F32) as GT, \
            SB(nc, "e_y", [128, GN // 128, D], F32) as Y, SB(nc, "e_wd", [128, 8, D], F32) as wd, \
            SB(nc, "e_wg", [128, 2, 8, 128], F32) as wg, SB(nc, "e_wu", [128, 2, 8, 128], F32) as wu, \
            SB(nc, "e_gt", [16, GN], F32) as gt, SB(nc, "e_gb", [128, GN], F32) as gB, \
            SB(nc, "e_sel", [16, 16, 128], F32) as sel, SB(nc, "e_sa", [128, 2, 512], F32) as sA, \
            SB(nc, "e_mod", [128, 2, D], F32) as mod, SB(nc, "e_x", [128, D], F32) as xt, \
            SB(nc, "e_wst", [128, 6, D], F32) as wst:
        t_wst = [Tok("wst%d" % i) for i in range(6)]
        t_wdc = [Tok("wd%d" % i) for i in range(8)]
        t_h, t_G, t_Y, t_wd, t_gt, t_gB, t_sel, t_mod, t_x = (Tok(n) for n in ("h", "G", "Y", "wd", "gt", "gB", "sel", "mod", "x"))
        t_wg = [Tok("wg0"), Tok("wg1")]
        t_wu = [Tok("wu0"), Tok("wu1")]
        t_sa = [Tok("sa0"), Tok("sa1")]
        for e in range(16):
            P.copy(sel[:, e, :], K.ident[0:16, e:e + 1].broadcast_to([16, 128]), [K.t_ident], [t_sel])
        P.dma(mod[:, 0, :], K.MOD[5, :].partition_broadcast(128), (), [t_mod])
        P.dma(mod[:, 1, :], K.MOD[11, :].partition_broadcast(128), (), [t_mod])
        tstart = NCTX if l == DEPTH - 1 else 0
        for g in range((T - tstart + GN - 1) // GN):
            t0 = tstart + g * GN
            n = min(GN, T - t0)
            nt = n // 128
            for i in range(nt):
                P.load_r(hT[:, :, i * 128:(i + 1) * 128], K.H2T[:, t0 + i * 128:t0 + (i + 1) * 128].rearrange("(k p) t -> p k t", p=128),
                         wst[:, i % 2, :].rearrange("p (k t) -> p k t", k=8), t_h, t_wst[i % 2], e='act' if i % 2 else 'dve')
            P.dma(gt[:, 0:n], K.GTT[:, t0:t0 + n], (), [t_gt])
            P.memset(Y[:, 0:nt, :], 0.0, [t_Y], e='pool')
            for e in range(16):
                for s0 in range(0, n, 512):
                    sn = min(512, n - s0)
                    ps, tp = P.ps()
                    P.mm(ps[:, 0:sn], sel[:, e, :], gt[:, s0:s0 + sn], True, True, [t_sel, t_gt], [tp])
                    P.copy(gB[:, s0:s0 + sn], ps[:, 0:sn], [tp], [t_gB], e='act')
                wgv = I['moe_w_gate'][l, e].rearrange("(k p) n -> p k n", p=128)
                wuv = I['moe_w_up'][l, e].rearrange("(k p) n -> p k n", p=128)
                for c in range(8):
                    b = c % 2
                    P.load_r(wg[:, b, :, :], wgv[:, :, c * 128:(c + 1) * 128], wst[:, 4, :].rearrange("p (k t) -> p k t", k=8), t_wg[b], t_wst[4], e='act')
                    P.load_r(wu[:, b, :, :], wuv[:, :, c * 128:(c + 1) * 128], wst[:, 5, :].rearrange("p (k t) -> p k t", k=8), t_wu[b], t_wst[5], e='act')
                    P.load_r(wd[:, c, :], I['moe_w_down'][l, e, c * 128:(c + 1) * 128, :], wst[:, 2 + c % 2, :], t_wdc[c], t_wst[2 + c % 2], e='dve', q='pool')
                    for si, s0 in enumerate(range(0, n, 512)):
                        sn = min(512, n - s0)
                        psa, tpa = P.ps()
                        psu, tpu = P.ps()
                        for k in range(8):
                            P.mm(psa[:, 0:sn], wg[:, b, k, :], hT[:, k, s0:s0 + sn], k == 0, k == 7, [t_wg[b], t_h], [tpa], r=FAST)
                        for k in range(8):
                            P.mm(psu[:, 0:sn], wu[:, b, k, :], hT[:, k, s0:s0 + sn], k == 0, k == 7, [t_wu[b], t_h], [tpu], r=FAST)
                        sb_ = si % 2
                        P.act(sA[:, sb_, 0:sn], psa[:, 0:sn], AF.Silu, [tpa], [t_sa[sb_]])
                        P.tt(sA[:, sb_, 0:sn], sA[:, sb_, 0:sn], psu[:, 0:sn], ALU.mult, [t_sa[sb_], tpu], [t_sa[sb_]])
                        P.tt(RR(GT[:, c, s0:s0 + sn]), sA[:, sb_, 0:sn], gB[:, s0:s0 + sn], ALU.mult, [t_sa[sb_], t_gB], [t_G])
                for i in range(nt):
                    for half in range(2):
                        ps, tp = P.ps()
                        for c in range(8):
                            P.mm(ps[:, :], GT[:, c, i * 128:(i + 1) * 128], wd[:, c, half * 512:(half + 1) * 512], c == 0, c == 7, [t_G, t_wdc[c]], [tp], r=FAST)
                        hs = slice(half * 512, (half + 1) * 512)
                        P.tt(Y[:, i, hs], Y[:, i, hs], ps[:, :], ALU.add, [t_Y, tp], [t_Y])
            for i in range(nt):
                ti = t0 // 128 + i
                mb = 1 if ti < 2 else 0
                rows = slice(ti * 128, (ti + 1) * 128)
                P.dma(xt[:], K.X[rows, :], (), [t_x])
                P.tt(Y[:, i, :], Y[:, i, :], mod[:, mb, :], ALU.mult, [t_Y, t_mod], [t_Y], e='pool')
                P.tt(xt[:], xt[:], Y[:, i, :], ALU.add, [t_x, t_Y], [t_x])
                P.dma(K.X[rows, :], xt[:], [t_x], [("X", ti)], q='pool')


def stage_final(K):
    P, nc, I = K.P, K.P.nc, K.I
    with SB(nc, "f_g", [128, D], F32) as gbc, SB(nc, "f_x", [128, 2, D], F32) as xt, \
            SB(nc, "f_j", [128, D], F32) as junk, SB(nc, "f_s", [128, 2], F32) as st:
        t_g = Tok("g")
        t_x = [Tok("x0"), Tok("x1")]
        t_s = [Tok("s0"), Tok("s1")]
        P.dma(gbc[:], I['final_g'][0, :].partition_broadcast(128), (), [t_g])
        for i in range(NLAT // 128):
            b = i % 2
            P.dma(xt[:, b, :], K.X[NCTX + i * 128:NCTX + (i + 1) * 128, :], (), [t_x[b]])
            rms_rstd(K, xt[:, b, :], t_x[b], D, junk[:], st[:, b:b + 1], t_s[b])
            P.stt(xt[:, b, :], xt[:, b, :], st[:, b:b + 1], gbc[:], ALU.mult, ALU.mult, [t_x[b], t_s[b], t_g], [t_x[b]])
            P.dma(K.out[i * 128:(i + 1) * 128, :], xt[:, b, :], [t_x[b]], [("out", i)], q='pool')


_CONSTS = None


def consts():
    global _CONSTS
    if _CONSTS is not None:
        return _CONSTS
    c = {}
    c['k_ident'] = np.eye(128, dtype=np.float32)
    rc, rs = rope_tables()
    c['k_ropec'], c['k_ropes'] = rc, rs
    j = np.arange(128)[:, None]
    i = np.arange(128)[None, :]
    c['k_maskl'] = (j >= i).astype(np.float32)
    c['k_maskr'] = (j <= i).astype(np.float32)
    c['k_ccL'], c['k_ssL'] = dft_blocks(NLAT, 33)
    c['k_ccC'], c['k_ssC'] = dft_blocks(NCTX, 3)
    f, d, w = hyena_consts(NLAT)
    c['k_featL'], c['k_decL'], c['k_wkL'] = f, d, w.reshape(-1, 1)
    f, d, w = hyena_consts(NCTX)
    c['k_featC'], c['k_decC'], c['k_wkC'] = f, d, w.reshape(-1, 1)
    c['k_iota'] = np.arange(512, dtype=np.float32).reshape(1, 512)
    _CONSTS = c
    return c


def make_in_maps(inputs, cores):
    cs = consts()
    shared = {}
    for k, v in inputs.items():
        if k in ('x', 'c', 'ctx', 'c_ctx'):
            continue
        a = np.ascontiguousarray(np.asarray(v, dtype=np.float32))
        if k in ('router_b', 'final_g'):
            a = a.reshape(1, -1)
        shared[k] = a
    shared.update(cs)
    maps = []
    for b in cores:
        m = dict(shared)
        m['x'] = np.ascontiguousarray(inputs['x'][b], dtype=np.float32)
        m['ctx'] = np.ascontiguousarray(inputs['ctx'][b], dtype=np.float32)
        m['c'] = np.ascontiguousarray(inputs['c'][b], dtype=np.float32).reshape(1, D)
        m['c_ctx'] = np.ascontiguousarray(inputs['c_ctx'], dtype=np.float32).reshape(1, D)
        maps.append(m)
    return maps


def kernel(**inputs):
    P = build()
    maps = make_in_maps(inputs, list(range(8)))
    res = run_bass_kernel_spmd(P.nc, maps, core_ids=list(range(8)))
    return np.stack([r["out"] for r in res.results], axis=0).astype(np.float32)
```

```python
import math
from contextlib import ExitStack
import numpy as np
import concourse.bass as bass
import concourse.mybir as mybir
from concourse.bass_utils import run_bass_kernel_spmd

F32 = mybir.dt.float32
F32R = mybir.dt.float32r
FAST = True
AF = mybir.ActivationFunctionType
ALU = mybir.AluOpType
AX = mybir.AxisListType

D = 1024
NLAT = 4096
NCTX = 256
T = NLAT + NCTX
NT = T // 128
DEPTH = 4
HY_W = 256
ATT_W = 512
S5_W = 256
HY_END = 768
Q_END = HY_END + ATT_W
K_END = Q_END + 128
V_END = K_END + 128
IN_W = V_END + S5_W
EPS = 1e-6
MAGIC = 12582912.0
TWO_PI = 6.283185
NE = 16


class Tok:
    def __init__(self, name):
        self.name = name

    def __repr__(self):
        return self.name


class Prog:
    NDS = 24

    def __init__(self):
        nc = bass.Bass("TRN2", target_bir_lowering=False)
        self.nc = nc
        self.eng = {'pe': nc.tensor, 'dve': nc.vector, 'act': nc.scalar, 'pool': nc.gpsimd, 'sp': nc.sync}
        self.esem = {k: nc.alloc_semaphore("es_" + k) for k in self.eng}
        self.ecnt = {k: 0 for k in self.eng}
        self.dsem = [nc.alloc_semaphore("ds%d" % i) for i in range(self.NDS)]
        self.dval = [0] * self.NDS
        self.dnext = 0
        self.know = {k: {} for k in self.eng}
        self.last_w = {}
        self.readers = {}
        self.n_inst = 0
        self.psum = [nc.alloc_psum_tensor("psb%d" % i, [128, 512], F32) for i in range(8)]
        self.ps_tok = [Tok("ps%d" % i) for i in range(8)]
        self.ps_next = 0
        self.ps_held = set()
        self.dram = {}

    def _sem_of(self, key):
        return self.esem[key] if isinstance(key, str) else self.dsem[key]

    def _deps(self, reads, writes):
        deps = {}

        def add(kv):
            k, v = kv
            if deps.get(k, 0) < v:
                deps[k] = v
        for t in reads:
            if t in self.last_w:
                add(self.last_w[t])
        for t in writes:
            if t in self.last_w:
                add(self.last_w[t])
            for kv in self.readers.get(t, {}).items():
                add(kv)
        return deps

    def _wait(self, e, deps):
        kn = self.know[e]
        for k, v in deps.items():
            if k == e and e == 'pe':
                continue
            if kn.get(k, 0) >= v:
                continue
            self.eng[e].wait_ge(self._sem_of(k), v)
            kn[k] = v
            self.n_inst += 1

    def _commit(self, me, reads, writes):
        for t in writes:
            self.last_w[t] = me
            self.readers[t] = {}
        for t in reads:
            r = self.readers.setdefault(t, {})
            if r.get(me[0], 0) < me[1]:
                r[me[0]] = me[1]

    def op(self, e, fn, reads=(), writes=()):
        self._wait(e, self._deps(reads, writes))
        inst = fn(self.eng[e])
        inst.then_inc(self.esem[e], 1)
        self.ecnt[e] += 1
        self.n_inst += 1
        self._commit((e, self.ecnt[e]), reads, writes)

    def dma(self, out, in_, reads=(), writes=(), q='sp'):
        self._wait(q, self._deps(reads, writes))
        s = self.dnext
        self.dnext = (self.dnext + 1) % self.NDS
        if self.know[q].get(s, 0) < self.dval[s]:
            self.eng[q].wait_ge(self.dsem[s], self.dval[s])
            self.know[q][s] = self.dval[s]
        self.eng[q].dma_start(out=out, in_=in_, allow_slow_non_contiguous=True).then_inc(self.dsem[s], 16)
        self.dval[s] += 16
        self.n_inst += 1
        self._commit((s, self.dval[s]), reads, writes)

    def barrier(self):
        for e in self.eng:
            deps = {k: self.ecnt[k] for k in self.eng if self.ecnt[k] > 0}
            for s in range(self.NDS):
                if self.dval[s] > 0:
                    deps[s] = self.dval[s]
            self._wait(e, deps)
        self.last_w = {}
        self.readers = {}

    def finish(self):
        deps = {s: self.dval[s] for s in range(self.NDS) if self.dval[s] > 0}
        for k in self.eng:
            if self.ecnt[k] > 0:
                deps[k] = self.ecnt[k]
        self._wait('sp', deps)

    def ps(self):
        while True:
            i = self.ps_next
            self.ps_next = (i + 1) % 8
            if i not in self.ps_held:
                return self.psum[i], self.ps_tok[i]

    def ps_hold(self, n):
        got = []
        for i in range(8):
            if i not in self.ps_held and len(got) < n:
                self.ps_held.add(i)
                got.append(i)
        return [(self.psum[i], self.ps_tok[i]) for i in got], got

    def ps_release(self, got):
        for i in got:
            self.ps_held.discard(i)

    def mm(self, out, lhsT, rhs, start, stop, reads, writes, r=False):
        if r:
            lhsT = lhsT.bitcast(F32R)
            rhs = rhs.bitcast(F32R)
        self.op('pe', lambda e: e.matmul(out, lhsT, rhs, start=start, stop=stop), reads, writes)

    def tr(self, out, in_, ident, reads, writes):
        self.op('pe', lambda e: e.transpose(out=out, in_=in_, identity=ident), reads, writes)

    def act(self, out, in_, func, reads, writes, **kw):
        self.op('act', lambda e: e.activation(out=out, in_=in_, func=func, **kw), reads, writes)

    def tt(self, out, in0, in1, op, reads, writes, e='dve'):
        self.op(e, lambda g: g.tensor_tensor(out=out, in0=in0, in1=in1, op=op), reads, writes)

    def ts(self, out, in0, s1, s2, op0, op1, reads, writes, e='dve'):
        if op1 is None:
            self.op(e, lambda g: g.tensor_scalar(out=out, in0=in0, scalar1=s1, scalar2=None, op0=op0), reads, writes)
        else:
            self.op(e, lambda g: g.tensor_scalar(out=out, in0=in0, scalar1=s1, scalar2=s2, op0=op0, op1=op1), reads, writes)

    def stt(self, out, in0, scalar, in1, op0, op1, reads, writes):
        self.op('dve', lambda g: g.scalar_tensor_tensor(out=out, in0=in0, scalar=scalar, in1=in1, op0=op0, op1=op1), reads, writes)

    def copy(self, out, in_, reads, writes, e='dve'):
        if e == 'act':
            self.op('act', lambda g: g.copy(out=out, in_=in_), reads, writes)
        else:
            self.op(e, lambda g: g.tensor_copy(out=out, in_=in_), reads, writes)

    def load_r(self, dst, src, stage, t_dst, t_stage, e='dve', q='sp'):
        if not FAST:
            self.dma(dst, src, (), [t_dst])
            return
        self.dma(stage, src, (), [t_stage], q=q)
        self.copy(dst.bitcast(F32R), stage, [t_stage], [t_dst], e=e)

    def rnd(self, ap, tok, e='dve'):
        if FAST:
            self.copy(ap.bitcast(F32R), ap, [tok], [tok], e=e)

    def memset(self, ap, val, writes, e='dve'):
        self.op(e, lambda g: g.memset(ap, val), (), writes)

    def recip(self, out, in_, reads, writes):
        self.op('dve', lambda g: g.reciprocal(out=out, in_=in_), reads, writes)

    def dram_t(self, name, shape, kind="Internal"):
        t = self.nc.dram_tensor(name, list(shape), F32, kind=kind).ap()
        self.dram[name] = t
        return t


def RR(ap):
    return ap.bitcast(F32R) if FAST else ap


class Ctx:
    def dump(self, name, tile_ap, shape, reads):
        if 'dump' not in self.dbg:
            return
        o = self.P.nc.dram_tensor("dmp_" + name, list(shape), F32, kind="ExternalOutput").ap()
        self.P.dma(o, tile_ap, reads, (), q='sp')


_UID = [0]


def SB(nc, name, shape, dt):
    _UID[0] += 1
    return nc.sbuf_tensor("%s_%d" % (name, _UID[0]), shape, dt)


def rope_tables():
    cosT = np.ones((128, T), np.float64)
    sinT = np.zeros((128, T), np.float64)
    n = np.arange(NLAT)
    row = n // 64
    col = n % 64
    for r in range(128):
        d = r % 64
        dd = d if d < 32 else d - 32
        pos = row if d < 32 else col
        j = dd % 16
        first = dd < 16
        inv = 10000.0 ** (-(j / 16.0))
        ang = (pos.astype(np.float32) * np.float32(inv)).astype(np.float64)
        cosT[r, NCTX:] = np.cos(ang)
        sinT[r, NCTX:] = (-np.sin(ang)) if first else np.sin(ang)
    return cosT.astype(np.float32), sinT.astype(np.float32)


def perm_cols(width):
    src = np.zeros(width, np.int64)
    for c in range(width):
        d = c % 64
        dd = d % 32
        src[c] = c + 16 if dd < 16 else c - 16
    return src


def dft_blocks(nhalf, nchunk):
    idx = np.arange(nchunk * 128)
    valid = (idx <= nhalf)
    prod = np.outer(idx, idx).astype(np.float64)
    ang = 2.0 * np.pi * (prod % (2 * nhalf)) / (2 * nhalf)
    m = np.outer(valid, valid)
    cc = (np.cos(ang) * m).astype(np.float32)
    ss = (np.sin(ang) * m).astype(np.float32)

    def blk(mat):
        return np.ascontiguousarray(mat.reshape(nchunk, 128, nchunk, 128).transpose(2, 1, 0, 3))
    return blk(cc), blk(ss)


def hyena_consts(L):
    pos = np.arange(L, dtype=np.float32)
    t = pos / np.float32(max(L - 1, 1))
    bands = np.linspace(1e-4, 15, 16, dtype=np.float32)
    ang = (np.float32(2.0 * math.pi / L) * pos[:, None] * bands[None, :]).astype(np.float32)
    feats = np.concatenate([t[:, None], np.cos(ang), -np.sin(ang)], axis=-1).astype(np.float32)
    dmin = math.log(1e-2) / 1.5
    dmax = math.log(1e-2) / 0.3
    decay = np.abs(np.linspace(dmin, dmax, HY_W, dtype=np.float32))
    dec = np.exp(-t[:, None] * decay[None, :]).astype(np.float32)
    nk = L // 128 + 1
    wk = np.zeros(nk * 128, np.float32)
    wk[:L + 1] = 2.0 / (2 * L)
    wk[0] = 1.0 / (2 * L)
    wk[L] = 1.0 / (2 * L)
    return np.ascontiguousarray(feats.T), dec, wk


def build(n_layers=DEPTH, dbg=()):
    P = Prog()
    nc = P.nc
    K = Ctx()
    K.P = P
    K.dbg = dbg
    K.dbg_out = {}

    def inp(name, shape):
        return nc.dram_tensor(name, list(shape), F32, kind="ExternalInput").ap()

    I = {}
    I['x'] = inp('x', [NLAT, D])
    I['ctx'] = inp('ctx', [NCTX, D])
    I['c'] = inp('c', [1, D])
    I['c_ctx'] = inp('c_ctx', [1, D])
    shapes = dict(
        norm1_g=[4, D], norm2_g=[4, D], ada_w=[4, D, 6 * D], ada_b=[4, 6 * D], w_in=[4, D, IN_W], w_out=[4, D, D],
        mix_norm_g=[4, D], hy_conv_w=[4, 3, 768], hy_conv_b=[4, 768], hy_f_w1=[4, 33, 64], hy_f_b1=[4, 64],
        hy_f_freq1=[4, 64], hy_f_w2=[4, 64, 64], hy_f_b2=[4, 64], hy_f_freq2=[4, 64], hy_f_w3=[4, 64, 1024],
        hy_bias=[4, 2, 256], attn_sink=[4, 8], s5_lam_re=[4, 2, 16, 64], s5_lam_im=[4, 2, 16, 64], s5_log_dt=[4, 2, 16],
        s5_b_re=[4, 2, 16, 64, 16], s5_b_im=[4, 2, 16, 64, 16], s5_c_re=[4, 2, 16, 16, 64], s5_c_im=[4, 2, 16, 16, 64],
        s5_d=[4, 256], s5_glu_w=[4, 256, 256], s5_glu_b=[4, 256], router_w=[D, 16], router_b=[1, 16],
        moe_w_gate=[4, 16, D, D], moe_w_up=[4, 16, D, D], moe_w_down=[4, 16, D, D], final_g=[1, D],
        k_ident=[128, 128], k_ropec=[128, T], k_ropes=[128, T], k_maskl=[128, 128], k_maskr=[128, 128],
        k_ccL=[33, 128, 33, 128], k_ssL=[33, 128, 33, 128], k_ccC=[3, 128, 3, 128], k_ssC=[3, 128, 3, 128],
        k_featL=[33, NLAT], k_decL=[NLAT, 256], k_wkL=[33 * 128, 1], k_featC=[33, NCTX], k_decC=[NCTX, 256], k_wkC=[3 * 128, 1],
        k_iota=[1, 512],
    )
    for k, s in shapes.items():
        I[k] = inp(k, s)
    K.I = I
    out = nc.dram_tensor("out", [NLAT, D], F32, kind="ExternalOutput").ap()
    K.out = out

    K.X = P.dram_t("sX", [T, D])
    K.ZT = P.dram_t("sZT", [T, 768])
    K.QF = P.dram_t("sQF", [512, T])
    K.KF = P.dram_t("sKF", [128, T])
    K.VT = P.dram_t("sVT", [T, 128])
    K.UF = P.dram_t("sUF", [256, T])
    K.MIX = P.dram_t("sMIX", [T, D])
    K.H2T = P.dram_t("sH2T", [D, T])
    K.GTT = P.dram_t("sGTT", [16, T])

    K.ident = nc.alloc_sbuf_tensor("ident", [128, 128], F32)
    K.ones = nc.alloc_sbuf_tensor("ones", [128, 128], F32)
    K.MOD = P.dram_t("sMOD", [12, D])
    K.ZC = P.dram_t("sZC", [T, 768])
    K.KRE = P.dram_t("sKRE", [33 * 128, 512])
    K.KIM = P.dram_t("sKIM", [33 * 128, 512])
    K.t_ident = Tok("ident")
    K.t_ones = Tok("ones")
    K.t_mod = Tok("modbc")
    P.dma(K.ident[:], I['k_ident'][:, :], (), [K.t_ident])
    P.memset(K.ones[:], 1.0, [K.t_ones])

    tX = [("X", i) for i in range(NT)]
    K.tX = tX
    P.dma(K.X[0:NCTX, :], I['ctx'][:, :], (), tX[0:2])
    for i in range(4):
        P.dma(K.X[NCTX + i * 1024: NCTX + (i + 1) * 1024, :], I['x'][i * 1024:(i + 1) * 1024, :], (), tX[2 + 8 * i: 2 + 8 * (i + 1)])
    P.barrier()

    for l in range(n_layers):
        stage_mod(K, l)
        P.barrier()
        stage_inproj(K, l)
        P.barrier()
        if 'inproj' in dbg and l == 0:
            break
        stage_hyena(K, l, NLAT, NCTX, 'L')
        P.barrier()
        if l != DEPTH - 1:
            stage_hyena(K, l, NCTX, 0, 'C')
            P.barrier()
        if 'hyena' in dbg and l == 0:
            break
        stage_attn(K, l)
        P.barrier()
        if 'attn' in dbg and l == 0:
            break
        stage_s5(K, l)
        P.barrier()
        if 's5' in dbg and l == 0:
            break
        stage_outproj(K, l)
        P.barrier()
        if 'outproj' in dbg and l == 0:
            break
        stage_moe(K, l)
        P.barrier()
    if not dbg:
        stage_final(K)
    for name in dbg:
        if name in P.dram and name not in ('inproj', 'hyena', 'attn', 's5', 'outproj'):
            src = P.dram[name]
            o = nc.dram_tensor("dbg_" + name, list(src.shape), F32, kind="ExternalOutput").ap()
            P.barrier()
            P.dma(o, src, (), ())
    P.finish()
    return P


def stage_mod(K, l):
    P, nc, I = K.P, K.P.nc, K.I
    with SB(nc, "m_cs", [128, 2, 8], F32) as cs, SB(nc, "m_w", [128, 6 * D], F32) as wk, \
            SB(nc, "m_ws", [128, 6 * D], F32) as ws, SB(nc, "m_b", [1, 6 * D], F32) as bt, \
            SB(nc, "m_raw", [128, 2, 6 * D], F32) as raw2, SB(nc, "m_g", [128, 2, D], F32) as gn, \
            SB(nc, "m_o", [128, 12, D], F32) as mo:
        t_cs, t_w, t_ws, t_b, t_raw, t_g = Tok("cs"), Tok("w"), Tok("ws"), Tok("b"), Tok("raw"), Tok("g")
        P.dma(cs[:, 0, :], I['c'][0, :].rearrange("(c p) -> p c", p=128), (), [t_cs])
        P.dma(cs[:, 1, :], I['c_ctx'][0, :].rearrange("(c p) -> p c", p=128), (), [t_cs])
        P.act(cs[:], cs[:], AF.Silu, [t_cs], [t_cs])
        P.dma(bt[:], I['ada_b'][l:l + 1, :], (), [t_b])
        P.dma(gn[:, 0, :], I['norm1_g'][l, :].partition_broadcast(128), (), [t_g])
        P.dma(gn[:, 1, :], I['norm2_g'][l, :].partition_broadcast(128), (), [t_g])
        t_wk2 = [Tok("wk0"), Tok("wk1")]
        t_ws2 = [Tok("ws0"), Tok("ws1"), Tok("ws2"), Tok("ws3")]
        t_raw2 = [Tok("raw0"), Tok("raw1")]
        for j in range(12):
            pss = [P.ps(), P.ps()]
            for k in range(8):
                kb = k % 2
                P.dma(wk[:, kb * 512:(kb + 1) * 512], I['ada_w'][l, k * 128:(k + 1) * 128, j * 512:(j + 1) * 512], (), [t_wk2[kb]])
                for who in range(2):
                    sl = slice((2 * kb + who) * 512, (2 * kb + who + 1) * 512)
                    P.ts(ws[:, sl], wk[:, kb * 512:(kb + 1) * 512], cs[:, who, k:k + 1], None, ALU.mult, None,
                         [t_wk2[kb], t_cs], [t_ws2[2 * kb + who]])
                    P.mm(pss[who][0][:, :], K.ones[:, :], ws[:, sl], k == 0, False, [t_ws2[2 * kb + who], K.t_ones], [pss[who][1]])
            for who in range(2):
                P.mm(pss[who][0][:, :], K.ones[0:1, :], bt[0:1, j * 512:(j + 1) * 512], False, True, [t_b, K.t_ones], [pss[who][1]])
                P.copy(raw2[:, who, j * 512:(j + 1) * 512], pss[who][0][:, :], [pss[who][1]], [t_raw2[who]], e='act' if who else 'dve')
        for who in range(2):
            raw = raw2[:, who, :]
            t_raw = t_raw2[who]
            base = who * 6
            m = mo
            for half in range(2):
                sh = raw[:, (3 * half) * D:(3 * half + 1) * D]
                sc = raw[:, (3 * half + 1) * D:(3 * half + 2) * D]
                gg = raw[:, (3 * half + 2) * D:(3 * half + 3) * D]
                P.stt(m[:, base + 3 * half + 0, :], sc, 1.0, gn[:, half, :], ALU.add, ALU.mult, [t_raw, t_g], [K.t_mod])
                P.copy(m[:, base + 3 * half + 1, :], sh, [t_raw], [K.t_mod])
                P.copy(m[:, base + 3 * half + 2, :], gg, [t_raw], [K.t_mod])
        P.dma(K.MOD.rearrange("(o j) d -> o j d", o=1), mo[0:1, :, :], [K.t_mod], [("MOD", 0)], q='pool')
        P.barrier()


def rms_rstd(K, xt, t_x, width, junk, ss, t_s):
    P = K.P
    P.act(junk, xt, AF.Square, [t_x], [t_s], accum_out=ss[:, 0:1])
    P.ts(ss[:, 0:1], ss[:, 0:1], 1.0 / width, EPS, ALU.mult, ALU.add, [t_s], [t_s])
    P.act(ss[:, 0:1], ss[:, 0:1], AF.Sqrt, [t_s], [t_s])
    P.recip(ss[:, 0:1], ss[:, 0:1], [t_s], [t_s])


def stage_inproj(K, l):
    P, nc, I = K.P, K.P.nc, K.I
    src = perm_cols(640)
    with SB(nc, "i_w", [128, 8, IN_W], F32) as W, SB(nc, "i_wp", [128, 8, 640], F32) as WP, \
            SB(nc, "i_x", [128, 2, D], F32) as xt, SB(nc, "i_h", [128, 2, D], F32) as ht, \
            SB(nc, "i_hT", [128, 8, 512], F32) as hT, SB(nc, "i_ss", [128, 2], F32) as ss, \
            SB(nc, "i_junk", [128, D], F32) as junk, SB(nc, "i_o", [128, 2, 768], F32) as ot, \
            SB(nc, "i_rc", [128, 512], F32) as rc, SB(nc, "i_rs", [128, 512], F32) as rs, \
            SB(nc, "i_t1", [128, 512], F32) as t1, SB(nc, "i_t2", [128, 512], F32) as t2, \
            SB(nc, "i_mod", [128, 4, D], F32) as modbc, SB(nc, "i_wst", [128, 2, IN_W], F32) as wst:
        t_W, t_WP = Tok("W"), Tok("WP")
        t_wst = [Tok("wst0"), Tok("wst1")]
        for jj, r in enumerate((0, 1, 6, 7)):
            P.dma(modbc[:, jj, :], K.MOD[r, :].partition_broadcast(128), (), [K.t_mod])
        t_x = [Tok("x0"), Tok("x1")]
        t_h = [Tok("h0"), Tok("h1")]
        t_s = [Tok("s0"), Tok("s1")]
        t_hT, t_o, t_rc, t_rs, t_t1, t_t2 = Tok("hT"), [Tok("o0"), Tok("o1")], Tok("rc"), Tok("rs"), Tok("t1"), Tok("t2")
        for k in range(8):
            P.load_r(W[:, k, :], I['w_in'][l, k * 128:(k + 1) * 128, :], wst[:, k % 2, :], t_W, t_wst[k % 2], e='act' if k % 2 else 'dve')
        wv = I['w_in'][l, :, HY_END:HY_END + 640].rearrange("(k p) (b two s) -> k p b two s", p=128, two=2, s=16)
        for k in range(8):
            dst = wst[:, k % 2, 0:640].rearrange("p (b two s) -> p b two s", two=2, s=16)
            P.dma(dst[:, :, 0, :], wv[k, :, :, 1, :], (), [t_wst[k % 2]])
            P.dma(dst[:, :, 1, :], wv[k, :, :, 0, :], (), [t_wst[k % 2]])
            P.copy(RR(WP[:, k, :]), wst[:, k % 2, 0:640], [t_wst[k % 2]], [t_WP], e='act' if k % 2 else 'dve')
        ngroups = (T + 511) // 512
        for g in range(ngroups):
            t0 = g * 512
            ntok = min(512, T - t0)
            ntile = ntok // 128
            for i in range(ntile):
                ti = g * 4 + i
                b = i % 2
                isctx = ti < 2
                mb = 2 if isctx else 0
                P.dma(xt[:, b, :], K.X[ti * 128:(ti + 1) * 128, :], [K.tX[ti]], [t_x[b]])
                rms_rstd(K, xt[:, b, :], t_x[b], D, junk[:], ss[:, b:b + 1], t_s[b])
                P.stt(ht[:, b, :], xt[:, b, :], ss[:, b:b + 1], modbc[:, mb + 0, :], ALU.mult, ALU.mult,
                      [t_x[b], t_s[b], K.t_mod], [t_h[b]])
                P.tt(ht[:, b, :], ht[:, b, :], modbc[:, mb + 1, :], ALU.add, [t_h[b], K.t_mod], [t_h[b]])
                for half in range(2):
                    ps, tp = P.ps()
                    for j in range(4):
                        kk = half * 4 + j
                        P.tr(ps[:, j * 128:(j + 1) * 128], ht[:, b, kk * 128:(kk + 1) * 128], K.ident[:], [t_h[b], K.t_ident], [tp])
                    P.copy(RR(hT[:, half * 4:(half + 1) * 4, i * 128:(i + 1) * 128]),
                           ps[:, :].rearrange("p (j t) -> p j t", j=4), [tp], [t_hT], e='act' if half else 'dve')
            for i in range(ntile):
                ti = g * 4 + i
                b = i % 2
                for (c0, cw, dst, dcol) in ((0, 512, K.ZT, 0), (512, 256, K.ZT, 512), (K_END, 128, K.VT, 0)):
                    ps, tp = P.ps()
                    for k in range(8):
                        P.mm(ps[:, 0:cw], hT[:, k, i * 128:(i + 1) * 128], W[:, k, c0:c0 + cw], k == 0, k == 7, [t_hT, t_W], [tp], r=FAST)
                    P.copy(ot[:, b, 0:cw], ps[:, 0:cw], [tp], [t_o[b]], e='act')
                    P.dma(dst[ti * 128:(ti + 1) * 128, dcol:dcol + cw], ot[:, b, 0:cw], [t_o[b]], [(dst.tensor.name, ti)], q='pool')
            P.dma(rc[:, 0:ntok], I['k_ropec'][:, t0:t0 + ntok], (), [t_rc])
            P.dma(rs[:, 0:ntok], I['k_ropes'][:, t0:t0 + ntok], (), [t_rs])
            for cidx in range(5):
                c0 = HY_END + cidx * 128
                ps, tp = P.ps()
                ps2, tp2 = P.ps()
                for k in range(8):
                    P.mm(ps[:, 0:ntok], W[:, k, c0:c0 + 128], hT[:, k, 0:ntok], k == 0, k == 7, [t_hT, t_W], [tp], r=FAST)
                for k in range(8):
                    P.mm(ps2[:, 0:ntok], WP[:, k, cidx * 128:(cidx + 1) * 128], hT[:, k, 0:ntok], k == 0, k == 7, [t_hT, t_WP], [tp2], r=FAST)
                P.tt(t1[:, 0:ntok], ps[:, 0:ntok], rc[:, 0:ntok], ALU.mult, [tp, t_rc], [t_t1])
                P.tt(t2[:, 0:ntok], ps2[:, 0:ntok], rs[:, 0:ntok], ALU.mult, [tp2, t_rs], [t_t2])
                P.tt(t1[:, 0:ntok], t1[:, 0:ntok], t2[:, 0:ntok], ALU.add, [t_t1, t_t2], [t_t1])
                if cidx < 4:
                    P.dma(K.QF[cidx * 128:(cidx + 1) * 128, t0:t0 + ntok], t1[:, 0:ntok], [t_t1], [("QF", g)], q='pool')
                else:
                    P.dma(K.KF[:, t0:t0 + ntok], t1[:, 0:ntok], [t_t1], [("KF", g)], q='pool')
            for cidx in range(2):
                c0 = V_END + cidx * 128
                ps, tp = P.ps()
                for k in range(8):
                    P.mm(ps[:, 0:ntok], W[:, k, c0:c0 + 128], hT[:, k, 0:ntok], k == 0, k == 7, [t_hT, t_W], [tp], r=FAST)
                P.copy(t2[:, 0:ntok], ps[:, 0:ntok], [tp], [t_t2], e='act')
                P.dma(K.UF[cidx * 128:(cidx + 1) * 128, t0:t0 + ntok], t2[:, 0:ntok], [t_t2], [("UF", g)], q='pool')


def sin_chain(K, ps_ap, bcol, fcol, v, r, hid, n, t_ps, t_consts, t_v, t_r, t_hid):
    P = K.P
    P.act(v[:, 0:n], ps_ap, AF.Identity, [t_ps] + t_consts, [t_v], bias=bcol, scale=fcol)
    P.ts(r[:, 0:n], v[:, 0:n], MAGIC, MAGIC, ALU.add, ALU.subtract, [t_v], [t_r])
    P.tt(v[:, 0:n], v[:, 0:n], r[:, 0:n], ALU.subtract, [t_v, t_r], [t_v])
    P.act(hid[:, 0:n], v[:, 0:n], AF.Sin, [t_v], [t_hid], scale=TWO_PI)


def stage_hyena(K, l, L, row0, tag):
    P, nc, I = K.P, K.P.nc, K.I
    NCH = L // 128
    NK = NCH + 1
    CC, SS = I['k_cc' + tag], I['k_ss' + tag]
    featT, dec, wkc = I['k_feat' + tag], I['k_dec' + tag], I['k_wk' + tag]
    with SB(nc, "ha_w", [128, 4, 768], F32) as wb, SB(nc, "ha_z", [128, 3, 768], F32) as z, \
            SB(nc, "ha_t", [128, 2, 768], F32) as tt_:
        t_wb, t_z, t_t = Tok("wb"), [Tok("zm"), Tok("z0"), Tok("zp")], [Tok("ta"), Tok("tb")]
        for j in range(3):
            P.dma(wb[:, j, :], I['hy_conv_w'][l, j, :].partition_broadcast(128), (), [t_wb])
        P.dma(wb[:, 3, :], I['hy_conv_b'][l, :].partition_broadcast(128), (), [t_wb])
        for i in range(NCH):
            r0 = row0 + i * 128
            if i == 0:
                P.memset(z[0:1, 0, :], 0.0, [t_z[0]])
                P.dma(z[1:128, 0, :], K.ZT[r0:r0 + 127, 0:768], (), [t_z[0]])
            else:
                P.dma(z[:, 0, :], K.ZT[r0 - 1:r0 + 127, 0:768], (), [t_z[0]])
            P.dma(z[:, 1, :], K.ZT[r0:r0 + 128, 0:768], (), [t_z[1]])
            if i == NCH - 1:
                P.memset(z[:, 2, :], 0.0, [t_z[2]])
                P.dma(z[0:127, 2, :], K.ZT[r0 + 1:r0 + 128, 0:768], (), [t_z[2]])
            else:
                P.dma(z[:, 2, :], K.ZT[r0 + 1:r0 + 129, 0:768], (), [t_z[2]])
            P.tt(tt_[:, 0, :], z[:, 0, :], wb[:, 0, :], ALU.mult, [t_z[0], t_wb], [t_t[0]])
            P.tt(tt_[:, 1, :], z[:, 1, :], wb[:, 1, :], ALU.mult, [t_z[1], t_wb], [t_t[1]], e='pool')
            P.tt(tt_[:, 0, :], tt_[:, 0, :], tt_[:, 1, :], ALU.add, [t_t[0], t_t[1]], [t_t[0]])
            P.tt(tt_[:, 1, :], z[:, 2, :], wb[:, 2, :], ALU.mult, [t_z[2], t_wb], [t_t[1]], e='pool')
            P.tt(tt_[:, 0, :], tt_[:, 0, :], tt_[:, 1, :], ALU.add, [t_t[0], t_t[1]], [t_t[0]])
            P.tt(tt_[:, 0, :], tt_[:, 0, :], wb[:, 3, :], ALU.add, [t_t[0], t_wb], [t_t[0]])
            P.dma(K.ZC[r0:r0 + 128, :], tt_[:, 0, :], [t_t[0]], [("ZC", i)], q='pool')
    P.barrier()
    HH = (NCH + 1) // 2
    with ExitStack() as es_:
        taps = es_.enter_context(SB(nc, "hb_taps", [128, NCH, 1024], F32))
        tabt = es_.enter_context(SB(nc, "hb_cc", [128, 2, HH, 128], F32))
        tabs_ = es_.enter_context(SB(nc, "hb_ss", [128, 2, HH, 128], F32))
        ft = es_.enter_context(SB(nc, "hb_f", [33, 512], F32))
        w1 = es_.enter_context(SB(nc, "hb_w1", [33, 64], F32))
        w2 = es_.enter_context(SB(nc, "hb_w2", [64, 64], F32))
        w3 = es_.enter_context(SB(nc, "hb_w3", [64, 1024], F32))
        cst = es_.enter_context(SB(nc, "hb_c", [64, 8], F32))
        v = es_.enter_context(SB(nc, "hb_v", [64, 512], F32))
        r = es_.enter_context(SB(nc, "hb_r", [64, 512], F32))
        h1 = es_.enter_context(SB(nc, "hb_h1", [64, 512], F32))
        h2 = es_.enter_context(SB(nc, "hb_h2", [64, 512], F32))
        dct = es_.enter_context(SB(nc, "hb_dec", [128, 256], F32))
        ab = es_.enter_context(SB(nc, "hb_abs", [128, 1024], F32))
        rn = es_.enter_context(SB(nc, "hb_rn", [128, 512], F32))
        tmp = es_.enter_context(SB(nc, "hb_tmp", [128, 512], F32))
        wkt = es_.enter_context(SB(nc, "hb_wk", [128, NK], F32))
        ko = es_.enter_context(SB(nc, "hb_o", [128, 2, 512], F32))
        t_taps = [Tok("taps%d" % c) for c in range(NCH)]
        t_cc, t_ss, t_f, t_w, t_c = Tok("cc"), Tok("ss"), Tok("f"), Tok("w"), Tok("c")
        t_v, t_r, t_h1, t_h2, t_dec, t_ab, t_rn, t_tmp, t_wk = (Tok(n) for n in ("v", "r", "h1", "h2", "dec", "ab", "rn", "tmp", "wk"))
        t_ko = [Tok("ko0"), Tok("ko1")]
        P.dma(w1[:], I['hy_f_w1'][l, :, :], (), [t_w])
        P.dma(w2[:], I['hy_f_w2'][l, :, :], (), [t_w])
        P.dma(w3[:], I['hy_f_w3'][l, :, :], (), [t_w])
        for j, nm in enumerate(('hy_f_b1', 'hy_f_freq1', 'hy_f_b2', 'hy_f_freq2')):
            P.dma(cst[:, j:j + 1], I[nm][l, :].rearrange("(p o) -> p o", o=1), (), [t_c])
        P.ts(cst[:, 4:5], cst[:, 1:2], 1.0 / (2.0 * math.pi), None, ALU.mult, None, [t_c], [t_c])
        P.ts(cst[:, 5:6], cst[:, 3:4], 1.0 / (2.0 * math.pi), None, ALU.mult, None, [t_c], [t_c])
        P.tt(cst[:, 6:7], cst[:, 0:1], cst[:, 4:5], ALU.mult, [t_c], [t_c])
        P.tt(cst[:, 7:8], cst[:, 2:3], cst[:, 5:6], ALU.mult, [t_c], [t_c])
        P.dma(wkt[:], wkc[:, 0].rearrange("(c p) -> p c", p=128), (), [t_wk])
        ng = (L + 511) // 512
        for g in range(ng):
            n0 = g * 512
            n = min(512, L - n0)
            P.dma(ft[:, 0:n], featT[:, n0:n0 + n], (), [t_f])
            ps, tp = P.ps()
            P.mm(ps[0:64, 0:n], w1[0:33, :], ft[0:33, 0:n], True, True, [t_w, t_f], [tp])
            sin_chain(K, ps[0:64, 0:n], cst[:, 6:7], cst[:, 4:5], v, r, h1, n, tp, [t_c], t_v, t_r, t_h1)
            ps, tp = P.ps()
            P.mm(ps[0:64, 0:n], w2[:, :], h1[:, 0:n], True, True, [t_w, t_h1], [tp])
            sin_chain(K, ps[0:64, 0:n], cst[:, 7:8], cst[:, 5:6], v, r, h2, n, tp, [t_c], t_v, t_r, t_h2)
            for sub in range(n // 128):
                c = (n0 // 128) + sub
                P.dma(dct[:], dec[c * 128:(c + 1) * 128, :], (), [t_dec])
                for half in range(2):
                    ps, tp = P.ps()
                    P.mm(ps[:, :], h2[:, sub * 128:(sub + 1) * 128], w3[:, half * 512:(half + 1) * 512], True, True, [t_h2, t_w], [tp])
                    P.tt(RR(taps[:, c, half * 512:(half + 1) * 512].rearrange("p (d c) -> p d c", d=2)),
                         ps[:, :].rearrange("p (d c) -> p d c", d=2),
                         dct[:, :].unsqueeze(1).broadcast_to([128, 2, 256]), ALU.mult, [tp, t_dec], [t_taps[c]])
        tv0 = taps[0:1, 0, :].rearrange("p (o d c) -> p o d c", o=2, d=2)
        P.ts(RR(tv0[:, :, 1, :]), tv0[:, :, 1, :], 0.0, None, ALU.mult, None, [t_taps[0]], [t_taps[0]])
        held, hid_ = P.ps_hold(2)
        for c in range(NCH):
            P.act(ab[:], taps[:, c, :], AF.Abs, [t_taps[c]], [t_ab])
            for half in range(2):
                P.mm(held[half][0][:, :], K.ones[:, :], ab[:, half * 512:(half + 1) * 512], c == 0, c == NCH - 1, [t_ab, K.t_ones], [held[half][1]])
        for o in range(2):
            P.copy(ab[:, o * 512:o * 512 + 256], held[o][0][:, 0:256], [held[o][1]], [t_ab])
            P.tt(rn[:, o * 256:(o + 1) * 256], ab[:, o * 512:o * 512 + 256], held[o][0][:, 256:512], ALU.add, [held[o][1], t_ab], [t_rn])
        P.ps_release(hid_)
        P.recip(rn[:], rn[:], [t_rn], [t_rn])
        for c in range(NCH):
            tv = taps[:, c, :].rearrange("p (o d c) -> p o d c", o=2, d=2)
            tm = tmp[:, :].rearrange("p (o c) -> p o c", o=2)
            P.tt(tm, tv[:, :, 0, :], tv[:, :, 1, :], ALU.add, [t_taps[c]], [t_tmp])
            P.tt(RR(tv[:, :, 1, :]), tv[:, :, 1, :], tv[:, :, 0, :], ALU.subtract, [t_taps[c]], [t_taps[c]])
            P.copy(RR(tv[:, :, 0, :]), tm, [t_tmp], [t_taps[c]])
        rn3 = rn[:, :].rearrange("p (o c) -> p o c", o=2)
        t_st2 = [Tok("fst0"), Tok("fst1")]
        for kc in range(NK):
            for part, TAB, ttab, d in ((0, CC, t_cc, 0), (1, SS, t_ss, 1)):
                ps, tp = P.ps()
                for hf in range(2):
                    cA, cB = hf * HH, min((hf + 1) * HH, NCH)
                    if cA >= cB:
                        continue
                    P.load_r(tabt[:, part, 0:cB - cA, :], TAB[kc, :, cA:cB, :], tabs_[:, part, 0:cB - cA, :], ttab, t_st2[part],
                             e='act' if part == 0 else 'dve')
                    for c in range(cA, cB):
                        tv = taps[:, c, :].rearrange("p (o d c) -> p o d c", o=2, d=2)
                        P.mm(ps[:, :].rearrange("p (o c) -> p o c", o=2), tabt[:, part, c - cA, :], tv[:, :, d, :], c == 0, c == NCH - 1,
                             [ttab, t_taps[c]], [tp], r=FAST)
                P.stt(ko[:, part, :].rearrange("p (o c) -> p o c", o=2), ps[:, :].rearrange("p (o c) -> p o c", o=2),
                      wkt[:, kc:kc + 1], rn3, ALU.mult, ALU.mult, [tp, t_wk, t_rn], [t_ko[part]])
                dst = K.KRE if part == 0 else K.KIM
                P.dma(dst[kc * 128:(kc + 1) * 128, :], ko[:, part, :], [t_ko[part]], [(dst.tensor.name, kc)], q='pool')
    P.barrier()
    HK = (NK + 1) // 2
    with SB(nc, "hc_a", [128, NCH, 256], F32) as a, SB(nc, "hc_p1", [128, NK, 256], F32) as p1, \
            SB(nc, "hc_p2", [128, NK, 256], F32) as p2, SB(nc, "hc_tb", [128, 4, HK, 128], F32) as tb, \
            SB(nc, "hc_k", [128, 2, 256], F32) as kk, \
            SB(nc, "hc_t", [128, 2, 256], F32) as tq, SB(nc, "hc_b", [128, 2, 256], F32) as bb, \
            SB(nc, "hc_x", [128, 256], F32) as xg, SB(nc, "hc_y", [128, 256], F32) as yy, \
            SB(nc, "hc_stg", [128, 4, HK, 128], F32) as stg:
        t_stg = [Tok("stg%d" % i) for i in range(4)]
        t_tb = [Tok("tb%d" % i) for i in range(4)]
        ldn = [0]

        def load_tabs(blk, cA, cB):
            pb = ldn[0] % 2
            ldn[0] += 1
            P.load_r(tb[:, 2 * pb, 0:cB - cA, :], CC[blk, :, cA:cB, :], stg[:, 2 * pb, 0:cB - cA, :], t_tb[2 * pb], t_stg[2 * pb], e='act')
            P.load_r(tb[:, 2 * pb + 1, 0:cB - cA, :], SS[blk, :, cA:cB, :], stg[:, 2 * pb + 1, 0:cB - cA, :], t_tb[2 * pb + 1], t_stg[2 * pb + 1], e='dve')
            return tb[:, 2 * pb], tb[:, 2 * pb + 1], t_tb[2 * pb], t_tb[2 * pb + 1]
        t_a = [Tok("a%d" % c) for c in range(NCH)]
        t_p1, t_p2, t_k, t_b, t_x, t_y = (Tok(n) for n in ("p1", "p2", "k", "b", "x", "y"))
        t_q = [Tok("q0"), Tok("q1")]
        for o in range(2):
            P.dma(bb[:, o, :], I['hy_bias'][l, o, :].partition_broadcast(128), (), [t_b])
        for c in range(NCH):
            P.load_r(a[:, c, :], K.ZC[row0 + c * 128: row0 + (c + 1) * 128, 0:256], xg[:], t_a[c], t_x, e='act' if c % 2 else 'dve')
        for o in range(2):
            for kc in range(NK):
                P.dma(kk[:, 0, :], K.KRE[kc * 128:(kc + 1) * 128, o * 256:(o + 1) * 256], (), [t_k])
                P.dma(kk[:, 1, :], K.KIM[kc * 128:(kc + 1) * 128, o * 256:(o + 1) * 256], (), [t_k])
                psr, tpr = P.ps()
                psi, tpi = P.ps()
                for cA in range(0, NCH, HK):
                    cB = min(cA + HK, NCH)
                    cct, sst, t_cc, t_ss = load_tabs(kc, cA, cB)
                    for c in range(cA, cB):
                        P.mm(psr[:, 0:256], cct[:, c - cA, :], a[:, c, :], c == 0, c == NCH - 1, [t_cc, t_a[c]], [tpr], r=FAST)
                    for c in range(cA, cB):
                        P.mm(psi[:, 0:256], sst[:, c - cA, :], a[:, c, :], c == 0, c == NCH - 1, [t_ss, t_a[c]], [tpi], r=FAST)
                P.tt(tq[:, 0, :], psr[:, 0:256], kk[:, 0, :], ALU.mult, [tpr, t_k], [t_q[0]])
                P.tt(tq[:, 1, :], psi[:, 0:256], kk[:, 1, :], ALU.mult, [tpi, t_k], [t_q[1]])
                P.tt(RR(p1[:, kc, :]), tq[:, 0, :], tq[:, 1, :], ALU.add, [t_q[0], t_q[1]], [t_p1])
                P.tt(tq[:, 0, :], psi[:, 0:256], kk[:, 0, :], ALU.mult, [tpi, t_k], [t_q[0]])
                P.tt(tq[:, 1, :], psr[:, 0:256], kk[:, 1, :], ALU.mult, [tpr, t_k], [t_q[1]])
                P.tt(RR(p2[:, kc, :]), tq[:, 0, :], tq[:, 1, :], ALU.subtract, [t_q[0], t_q[1]], [t_p2])
            for tc in range(NCH):
                r0 = row0 + tc * 128
                P.dma(xg[:], K.ZC[r0:r0 + 128, 256 * (o + 1):256 * (o + 2)], (), [t_x])
                ps, tp = P.ps()
                for kA in range(0, NK, HK):
                    kB = min(kA + HK, NK)
                    cct, sst, t_cc, t_ss = load_tabs(tc, kA, kB)
                    for kc in range(kA, kB):
                        P.mm(ps[:, 0:256], cct[:, kc - kA, :], p1[:, kc, :], kc == 0, False, [t_cc, t_p1], [tp], r=FAST)
                    for kc in range(kA, kB):
                        P.mm(ps[:, 0:256], sst[:, kc - kA, :], p2[:, kc, :], False, kc == NK - 1, [t_ss, t_p2], [tp], r=FAST)
                P.tt(yy[:], a[:, tc, :], bb[:, o, :], ALU.mult, [t_a[tc], t_b], [t_y])
                P.tt(yy[:], yy[:], ps[:, 0:256], ALU.add, [t_y, tp], [t_y])
                if o == 0:
                    P.tt(RR(a[:, tc, :]), yy[:], xg[:], ALU.mult, [t_y, t_x], [t_a[tc]])
                else:
                    P.tt(yy[:], yy[:], xg[:], ALU.mult, [t_y, t_x], [t_y])
                    P.dma(K.MIX[r0:r0 + 128, 0:256], yy[:], [t_y], [("MIX", r0)], q='pool')


def stage_attn(K, l):
    P, nc, I = K.P, K.P.nc, K.I
    with SB(nc, "at_k", [64, 2, T], F32) as kf, SB(nc, "at_v", [128, NT, 2, 65], F32) as v1, \
            SB(nc, "at_es", [128, 8], F32) as es, SB(nc, "at_ml", [128, 128], F32) as ml, \
            SB(nc, "at_mr", [128, 128], F32) as mr, SB(nc, "at_q", [64, 2, 4, 128], F32) as q4, \
            SB(nc, "at_pt", [128, 5, 512], F32) as pt, SB(nc, "at_o", [128, 2, 512], F32) as ot, \
            SB(nc, "at_d", [128, 2, 4], F32) as den:
        t_k, t_v, t_es, t_m = Tok("k"), Tok("v"), Tok("es"), Tok("m")
        t_q = [Tok("q0"), Tok("q1")]
        t_pt = [Tok("pt%d" % i) for i in range(5)]
        t_o = [Tok("o0"), Tok("o1")]
        t_d = Tok("d")
        for h in range(2):
            P.dma(kf[:, h, :], K.KF[h * 64:(h + 1) * 64, :], (), [t_k])
        P.memset(v1[:, :, :, 64:65], 1.0, [t_v])
        vtv = K.VT.rearrange("(t p) (h d) -> p t h d", p=128, h=2)
        for h in range(2):
            P.dma(v1[:, :, h, 0:64], vtv[:, :, h, :], (), [t_v])
        P.dma(es[:], I['attn_sink'][l, :].partition_broadcast(128), (), [t_es])
        P.act(es[:], es[:], AF.Exp, [t_es], [t_es])
        P.dma(ml[:], I['k_maskl'][:, :], (), [t_m])
        P.dma(mr[:], I['k_maskr'][:, :], (), [t_m])
        for qb in range(2 if l == DEPTH - 1 else 0, NT):
            kts = [(0, None), (1, None)]
            if qb >= 2:
                if qb - 1 >= 2:
                    kts.append((qb - 1, ml))
                kts.append((qb, None))
                if qb + 1 < NT:
                    kts.append((qb + 1, mr))
            ob = qb % 2
            for kvh in range(2):
                qi = kvh
                P.dma(q4[:, qi, :, :], K.QF[kvh * 256:(kvh + 1) * 256, qb * 128:(qb + 1) * 128].rearrange("(h d) t -> d h t", d=64),
                      (), [t_q[qi]])
                for idx, (kt, mask) in enumerate(kts):
                    ps, tp = P.ps()
                    P.mm(ps[:, :], kf[:, kvh, kt * 128:(kt + 1) * 128], q4[:, qi, :, :].rearrange("d h t -> d (h t)"), True, True,
                         [t_k, t_q[qi]], [tp])
                    P.act(pt[:, idx, :], ps[:, :], AF.Exp, [tp], [t_pt[idx]], scale=0.125)
                    if mask is not None:
                        pv = pt[:, idx, :].rearrange("p (h t) -> p h t", h=4)
                        P.tt(pv, pv, mask[:, :].unsqueeze(1).broadcast_to([128, 4, 128]), ALU.mult, [t_pt[idx], t_m], [t_pt[idx]])
                pso, tpo = P.ps()
                for hh in range(4):
                    for idx, (kt, mask) in enumerate(kts):
                        P.mm(pso[:, hh * 65:(hh + 1) * 65], pt[:, idx, hh * 128:(hh + 1) * 128], v1[:, kt, kvh, :],
                             idx == 0, idx == len(kts) - 1, [t_pt[idx], t_v], [tpo])
                pv = pso[:, 0:260].rearrange("p (h c) -> p h c", h=4)
                P.tt(den[:, kvh, :], pv[:, :, 64], es[:, kvh * 4:(kvh + 1) * 4], ALU.add, [tpo, t_es], [t_d])
                P.recip(den[:, kvh, :], den[:, kvh, :], [t_d], [t_d])
                P.tt(ot[:, ob, kvh * 256:(kvh + 1) * 256].rearrange("p (h d) -> p h d", h=4), pv[:, :, 0:64],
                     den[:, kvh, :].unsqueeze(2).broadcast_to([128, 4, 64]), ALU.mult, [tpo, t_d], [t_o[ob]])
            P.dma(K.MIX[qb * 128:(qb + 1) * 128, 256:768], ot[:, ob, :], [t_o[ob]], [("MIXa", qb)], q='pool')


def round_frac(K, v, r, t_v, t_r, e='dve'):
    P = K.P
    P.ts(r, v, MAGIC, MAGIC, ALU.add, ALU.subtract, [t_v], [t_r], e=e)
    P.tt(v, v, r, ALU.subtract, [t_v, t_r], [t_v], e=e)


def stage_s5(K, l):
    P, nc, I = K.P, K.P.nc, K.I
    K.GS = K.P.dram.get("sGS")
    if K.GS is None:
        K.GS = P.dram_t("sGS", [256, T])
    with SB(nc, "s_bt", [32, 16, 2, 128], F32) as BT, SB(nc, "s_cb", [128, 16, 2, 32], F32) as CB, \
            SB(nc, "s_par", [128, 12, 16], F32) as par, SB(nc, "s_dsk", [32, 8], F32) as dsk, \
            SB(nc, "s_iota", [128, 512], F32) as iot:
        t_BT, t_CB, t_par, t_dsk, t_iota = Tok("BT"), Tok("CB"), Tok("par"), Tok("dsk"), Tok("iota")
        MAG, PHI = 4, 6
        with SB(nc, "s_b", [128, 2, 16, 16], F32) as bri, SB(nc, "s_bb", [128, 2, 16, 16], F32) as bb, \
                SB(nc, "s_bd", [128, 16, 2, 32], F32) as BD, SB(nc, "s_cnd", [32, 16, 2, 128], F32) as CND, \
                SB(nc, "s_t1", [128, 16, 16], F32) as t1, SB(nc, "s_t2", [128, 16, 16], F32) as t2:
            t_b, t_bb, t_BD, t_CND, t_t1, t_t2 = Tok("b"), Tok("bb"), Tok("BD"), Tok("CND"), Tok("t1"), Tok("t2")
            for d in range(2):
                P.dma(par[:, 0, d * 8:(d + 1) * 8], I['s5_lam_re'][l, d].rearrange("g p -> (g p)").rearrange("(gh q) -> q gh", q=128), (), [t_par])
                P.dma(par[:, 1, d * 8:(d + 1) * 8], I['s5_lam_im'][l, d].rearrange("g p -> (g p)").rearrange("(gh q) -> q gh", q=128), (), [t_par])
                ldv = I['s5_log_dt'][l, d].rearrange("(gh gl) -> gl gh", gl=2)
                for gl in range(2):
                    P.dma(par[gl * 64:(gl + 1) * 64, 2, d * 8:(d + 1) * 8], ldv[gl].partition_broadcast(64), (), [t_par])
                for ri, nm in enumerate(('s5_b_re', 's5_b_im')):
                    P.dma(bri[:, ri, d * 8:(d + 1) * 8, :],
                          I[nm][l, d].rearrange("g p c -> (g p c)").rearrange("(gh q c) -> q gh c", q=128, c=16), (), [t_b])
            P.dma(dsk[:], I['s5_d'][l, :].rearrange("(j r) -> r j", r=32), (), [t_dsk])
            P.dma(iot[:], I['k_iota'][0, :].partition_broadcast(128), (), [t_iota])
            pp = lambda i: par[:, i, :]
            tp_ = [t_par]
            P.act(pp(2), pp(2), AF.Exp, tp_, tp_)
            P.tt(pp(3), pp(0), pp(2), ALU.mult, tp_, tp_)
            P.act(pp(MAG), pp(3), AF.Exp, tp_, tp_)
            P.tt(pp(3), pp(1), pp(2), ALU.mult, tp_, tp_)
            P.ts(pp(PHI), pp(3), 1.0 / (2.0 * math.pi), None, ALU.mult, None, tp_, tp_)
            P.copy(pp(5), pp(PHI), tp_, tp_)
            round_frac(K, pp(5), pp(11), t_par, t_par)
            P.act(pp(8), pp(5), AF.Sin, tp_, tp_, scale=TWO_PI)
            P.ts(pp(5), pp(PHI), 0.25, None, ALU.add, None, tp_, tp_)
            round_frac(K, pp(5), pp(11), t_par, t_par)
            P.act(pp(7), pp(5), AF.Sin, tp_, tp_, scale=TWO_PI)
            P.tt(pp(7), pp(7), pp(MAG), ALU.mult, tp_, tp_)
            P.tt(pp(8), pp(8), pp(MAG), ALU.mult, tp_, tp_)
            P.ts(pp(7), pp(7), -1.0, None, ALU.add, None, tp_, tp_)
            P.tt(pp(3), pp(0), pp(0), ALU.mult, tp_, tp_)
            P.tt(pp(5), pp(1), pp(1), ALU.mult, tp_, tp_)
            P.tt(pp(3), pp(3), pp(5), ALU.add, tp_, tp_)
            P.recip(pp(3), pp(3), tp_, tp_)
            P.tt(pp(9), pp(7), pp(0), ALU.mult, tp_, tp_)
            P.tt(pp(5), pp(8), pp(1), ALU.mult, tp_, tp_)
            P.tt(pp(9), pp(9), pp(5), ALU.add, tp_, tp_)
            P.tt(pp(9), pp(9), pp(3), ALU.mult, tp_, tp_)
            P.tt(pp(10), pp(8), pp(0), ALU.mult, tp_, tp_)
            P.tt(pp(5), pp(7), pp(1), ALU.mult, tp_, tp_)
            P.tt(pp(10), pp(10), pp(5), ALU.subtract, tp_, tp_)
            P.tt(pp(10), pp(10), pp(3), ALU.mult, tp_, tp_)
            cre = par[:, 9, :].unsqueeze(2).broadcast_to([128, 16, 16])
            cim = par[:, 10, :].unsqueeze(2).broadcast_to([128, 16, 16])
            P.tt(t1[:], bri[:, 0, :, :], cre, ALU.mult, [t_b, t_par], [t_t1])
            P.tt(t2[:], bri[:, 1, :, :], cim, ALU.mult, [t_b, t_par], [t_t2])
            P.tt(bb[:, 0, :, :], t1[:], t2[:], ALU.subtract, [t_t1, t_t2], [t_bb])
            P.tt(t1[:], bri[:, 1, :, :], cre, ALU.mult, [t_b, t_par], [t_t1])
            P.tt(t2[:], bri[:, 0, :, :], cim, ALU.mult, [t_b, t_par], [t_t2])
            P.tt(bb[:, 1, :, :], t1[:], t2[:], ALU.add, [t_t1, t_t2], [t_bb])
            P.memset(BD[:], 0.0, [t_BD])
            for ri in range(2):
                P.copy(BD[0:64, :, ri, 0:16], bb[0:64, ri, :, :], [t_bb], [t_BD])
                P.copy(BD[64:128, :, ri, 16:32], bb[64:128, ri, :, :], [t_bb], [t_BD])
            for dg in range(16):
                ps, tp = P.ps()
                for ri in range(2):
                    P.tr(ps[0:32, ri * 128:(ri + 1) * 128], BD[:, dg, ri, :], K.ident[:], [t_BD, K.t_ident], [tp])
                P.copy(BT[:, dg, :, :], ps[0:32, 0:256].rearrange("p (r m) -> p r m", r=2), [tp], [t_BT])
            P.memset(CND[:], 0.0, [t_CND])
            for d in range(2):
                for ri, nm in enumerate(('s5_c_re', 's5_c_im')):
                    cv = I[nm][l, d].rearrange("(gh gl) c p -> gl c gh p", gl=2)
                    for gl in range(2):
                        P.dma(CND[gl * 16:(gl + 1) * 16, d * 8:(d + 1) * 8, ri, gl * 64:(gl + 1) * 64], cv[gl], (), [t_CND])
            for dg in range(16):
                ps, tp = P.ps()
                for ri in range(2):
                    P.tr(ps[:, ri * 32:(ri + 1) * 32], CND[:, dg, ri, :], K.ident[0:32, 0:32], [t_CND, K.t_ident], [tp])
                P.copy(CB[:, dg, 0, :], ps[:, 0:32], [tp], [t_CB])
                P.ts(CB[:, dg, 1, :], ps[:, 32:64], -1.0, None, ALU.mult, None, [tp], [t_CB])
        P.barrier()
        with SB(nc, "s_us", [32, T], F32) as us, SB(nc, "s_y", [32, T], F32) as ysb, \
                SB(nc, "s_tab", [128, 2, 4, 512], F32) as tab2, SB(nc, "s_m", [128, 2, 2, 512], F32) as mm2, \
                SB(nc, "s_w", [128, 2, 2, 512], F32) as ww2, SB(nc, "s_s", [128, 2, 2, 512], F32) as ss2, \
                SB(nc, "s_q", [128, 2, 2, 512], F32) as qq2, SB(nc, "s_c0", [128, 2, 4], F32) as c02, \
                SB(nc, "s_car", [128, 2], F32) as car, SB(nc, "s_mag", [128, 512], F32) as magt, SB(nc, "s_g", [32, 2, T], F32) as gg:
            t_us, t_y, t_car, t_g = (Tok(n) for n in ("us", "y", "car", "g"))
            tk2 = [dict(m=Tok("m%d" % b), w=Tok("w%d" % b), s=Tok("s%d" % b), q=Tok("q%d" % b), c0=Tok("c0%d" % b),
                        tab=[Tok("tabs%d" % b), Tok("tabc%d" % b), Tok("vr%d" % b), Tok("vrr%d" % b)]) for b in range(2)]
            chunk_no = [0]
            t_mag = Tok("mag")
            items = []
            for j in range(8):
                for d in range(2):
                    if d == 0:
                        chunks = [(a, min(a + 512, T), False, a) for a in range(0, T, 512)]
                    else:
                        chunks = [(0, NCTX, True, 0)]
                        b = T
                        while b > NCTX:
                            a = max(b - 512, NCTX)
                            chunks.append((a, b, True, NCTX + (T - b)))
                            b = a
                    for ci, (a, b, rev, i0) in enumerate(chunks):
                        items.append(dict(j=j, d=d, dg=d * 8 + j, a=a, b=b, rev=rev, i0=i0, first=(ci == 0),
                                          jfirst=(d == 0 and ci == 0), jlast=(d == 1 and ci == len(chunks) - 1), pb=len(items) % 2))

            def emit_tables(it):
                n = it['b'] - it['a']
                pb = it['pb']
                tab, qq, c0 = tab2[:, pb], qq2[:, pb], c02[:, pb]
                t_q, t_c0, t_tab = tk2[pb]['q'], tk2[pb]['c0'], tk2[pb]['tab']
                phi = par[:, PHI, it['dg']:it['dg'] + 1]
                P.ts(c0[:, 0:1], phi, float(it['i0']), None, ALU.mult, None, [t_par], [t_c0])
                round_frac(K, c0[:, 0:1], c0[:, 2:3], t_c0, t_c0)
                P.ts(c0[:, 1:2], c0[:, 0:1], 0.25, None, ALU.add, None, [t_c0], [t_c0])
                for which in range(2):
                    P.act(tab[:, 2 + which, 0:n], iot[:, 0:n], AF.Identity, [t_iota, t_c0, t_par], [t_tab[2 + which]],
                          bias=c0[:, which:which + 1], scale=phi)
                    P.ts(tab[:, which, 0:n], tab[:, 2 + which, 0:n], MAGIC, MAGIC, ALU.add, ALU.subtract, [t_tab[2 + which]], [t_tab[which]])
                    P.tt(tab[:, 2 + which, 0:n], tab[:, 2 + which, 0:n], tab[:, which, 0:n], ALU.subtract, [t_tab[2 + which], t_tab[which]],
                         [t_tab[2 + which]])
                    P.act(tab[:, which, 0:n], tab[:, 2 + which, 0:n], AF.Sin, [t_tab[2 + which]], [t_tab[which]], scale=TWO_PI)

            def emit_main(it):
                j, d, dg, a, b, rev, pb = it['j'], it['d'], it['dg'], it['a'], it['b'], it['rev'], it['pb']
                n = b - a
                tab, mm_, ww, ss_, qq = tab2[:, pb], mm2[:, pb], ww2[:, pb], ss2[:, pb], qq2[:, pb]
                t_m, t_w, t_s, t_q, t_tab = tk2[pb]['m'], tk2[pb]['w'], tk2[pb]['s'], tk2[pb]['q'], tk2[pb]['tab']
                rv = (lambda ap: ap[:, ::-1]) if rev else (lambda ap: ap)
                if it['first']:
                    P.copy(magt[:, :], par[:, MAG, dg:dg + 1].broadcast_to([128, 512]), [t_par], [t_mag])
                ps1, tp1 = P.ps()
                ps2, tp2 = P.ps()
                P.mm(ps1[:, 0:n], BT[:, dg, 0, :], us[:, a:b], True, True, [t_BT, t_us], [tp1])
                P.mm(ps2[:, 0:n], BT[:, dg, 1, :], us[:, a:b], True, True, [t_BT, t_us], [tp2])
                sn = rv(tab[:, 0, 0:n])
                cs = rv(tab[:, 1, 0:n])
                tS, tC = t_tab[0], t_tab[1]
                P.tt(mm_[:, 0, 0:n], ps1[:, 0:n], cs, ALU.mult, [tp1, tC], [t_m])
                P.tt(qq[:, 0, 0:n], ps2[:, 0:n], sn, ALU.mult, [tp2, tS], [t_q])
                P.tt(mm_[:, 0, 0:n], mm_[:, 0, 0:n], qq[:, 0, 0:n], ALU.add, [t_m, t_q], [t_m])
                P.tt(mm_[:, 1, 0:n], ps2[:, 0:n], cs, ALU.mult, [tp2, tC], [t_m])
                P.tt(qq[:, 1, 0:n], ps1[:, 0:n], sn, ALU.mult, [tp1, tS], [t_q])
                P.tt(mm_[:, 1, 0:n], mm_[:, 1, 0:n], qq[:, 1, 0:n], ALU.subtract, [t_m, t_q], [t_m])
                magb = magt[:, 0:n]
                for ri in range(2):
                    init = 0.0 if it['first'] else car[:, ri:ri + 1]
                    P.op('dve', lambda g, ri=ri, init=init: g.tensor_tensor_scan(
                        out=rv(ww[:, ri, 0:n]), data0=magb, data1=rv(mm_[:, ri, 0:n]), initial=init,
                        op0=ALU.mult, op1=ALU.add), [t_m, t_mag, t_car], [t_w])
                last = a if rev else b - 1
                P.copy(car[:, :], ww[:, :, last - a], [t_w], [t_car])
                P.tt(ss_[:, 0, 0:n], ww[:, 0, 0:n], cs, ALU.mult, [t_w, tC], [t_s], e='pool')
                P.tt(qq[:, 0, 0:n], ww[:, 1, 0:n], sn, ALU.mult, [t_w, tS], [t_q], e='pool')
                P.tt(ss_[:, 0, 0:n], ss_[:, 0, 0:n], qq[:, 0, 0:n], ALU.subtract, [t_s, t_q], [t_s], e='pool')
                P.tt(ss_[:, 1, 0:n], ww[:, 0, 0:n], sn, ALU.mult, [t_w, tS], [t_s], e='pool')
                P.tt(qq[:, 1, 0:n], ww[:, 1, 0:n], cs, ALU.mult, [t_w, tC], [t_q], e='pool')
                P.tt(ss_[:, 1, 0:n], ss_[:, 1, 0:n], qq[:, 1, 0:n], ALU.add, [t_s, t_q], [t_s], e='pool')
                psy, tpy = P.ps()
                P.mm(psy[0:32, 0:n], CB[:, dg, 0, :], ss_[:, 0, 0:n], True, False, [t_CB, t_s], [tpy])
                P.mm(psy[0:32, 0:n], CB[:, dg, 1, :], ss_[:, 1, 0:n], False, True, [t_CB, t_s], [tpy])
                if d == 0:
                    P.copy(ysb[:, a:b], psy[0:32, 0:n], [tpy], [t_y], e='act')
                else:
                    P.tt(ysb[:, a:b], ysb[:, a:b], psy[0:32, 0:n], ALU.add, [t_y, tpy], [t_y])

            emit_tables(items[0])
            for idx, it in enumerate(items):
                j = it['j']
                if it['jfirst']:
                    P.dma(us[:], K.UF[32 * j:32 * (j + 1), :], (), [t_us])
                if idx + 1 < len(items):
                    emit_tables(items[idx + 1])
                emit_main(it)
                if not it['jlast']:
                    continue
                P.stt(ysb[:], us[:], dsk[:, j:j + 1], ysb[:], ALU.mult, ALU.add, [t_us, t_dsk, t_y], [t_y])
                P.tt(gg[:, 0, :], ysb[:], ysb[:], ALU.mult, [t_y], [t_g])
                P.ts(gg[:, 0, :], gg[:, 0, :], 0.044715, 1.0, ALU.mult, ALU.add, [t_g], [t_g])
                P.tt(gg[:, 0, :], gg[:, 0, :], ysb[:], ALU.mult, [t_g, t_y], [t_g])
                P.act(gg[:, 1, :], gg[:, 0, :], AF.Sigmoid, [t_g], [t_g], scale=1.5957691216)
                P.tt(gg[:, 1, :], gg[:, 1, :], ysb[:], ALU.mult, [t_g, t_y], [t_g])
                P.dma(K.GS[32 * j:32 * (j + 1), :], gg[:, 1, :], [t_g], [("GS", j)], q='pool')
        P.barrier()
        with SB(nc, "s_gw", [128, 2, 256], F32) as gw, SB(nc, "s_gb", [128, 2], F32) as gb, \
                SB(nc, "s_gt", [128, 2, 512], F32) as gt, SB(nc, "s_sg", [128, 2, 512], F32) as sg, \
                SB(nc, "s_o", [128, 4, 256], F32) as so:
            t_gw, t_gt, t_sg, t_so = Tok("gw"), Tok("gt"), Tok("sg"), Tok("so")
            P.dma(gw[:], I['s5_glu_w'][l].rearrange("(c p) n -> p c n", p=128), (), [t_gw])
            P.dma(gb[:], I['s5_glu_b'][l, :].rearrange("(c p) -> p c", p=128), (), [t_gw])
            for g in range((T + 511) // 512):
                t0 = g * 512
                n = min(512, T - t0)
                P.dma(gt[:, :, 0:n], K.GS[:, t0:t0 + n].rearrange("(c p) t -> p c t", p=128), (), [t_gt])
                for oc in range(2):
                    ps, tp = P.ps()
                    for kc in range(2):
                        P.mm(ps[:, 0:n], gw[:, kc, oc * 128:(oc + 1) * 128], gt[:, kc, 0:n], kc == 0, kc == 1, [t_gw, t_gt], [tp])
                    P.act(sg[:, oc, 0:n], ps[:, 0:n], AF.Sigmoid, [tp, t_gw], [t_sg], bias=gb[:, oc:oc + 1], scale=1.0)
                    P.tt(sg[:, oc, 0:n], sg[:, oc, 0:n], gt[:, oc, 0:n], ALU.mult, [t_sg, t_gt], [t_sg])
                for i in range(n // 128):
                    ps, tp = P.ps()
                    for oc in range(2):
                        P.tr(ps[:, oc * 128:(oc + 1) * 128], sg[:, oc, i * 128:(i + 1) * 128], K.ident[:], [t_sg, K.t_ident], [tp])
                    P.copy(so[:, i, :], ps[:, 0:256], [tp], [t_so], e='act')
                P.dma(K.MIX[t0:t0 + n, 768:1024].rearrange("(i p) c -> p i c", p=128), so[:, 0:n // 128, :], [t_so], [("MIXs", g)], q='pool')


def stage_outproj(K, l):
    P, nc, I = K.P, K.P.nc, K.I
    with SB(nc, "o_w", [128, 8, D], F32) as W, SB(nc, "o_rw", [128, 8, 16], F32) as RW, \
            SB(nc, "o_gain", [128, D], F32) as gain, SB(nc, "o_mod", [128, 6, D], F32) as mod, \
            SB(nc, "o_rb", [128, 16], F32) as rb, SB(nc, "o_mix", [128, 2, D], F32) as mix2, \
            SB(nc, "o_x", [128, 2, D], F32) as xt2, SB(nc, "o_junk", [128, D], F32) as junk, \
            SB(nc, "o_mT", [128, 2, 8, 128], F32) as mT2, SB(nc, "o_h2", [128, 2, D], F32) as h22, \
            SB(nc, "o_hT", [128, 2, 8, 128], F32) as hT2, SB(nc, "o_tmp", [128, 2, D], F32) as tmp2, \
            SB(nc, "o_st", [128, 2, 8], F32) as st2, SB(nc, "o_r", [128, 2, 12, 16], F32) as rr2, \
            SB(nc, "o_gT", [16, 2, 128], F32) as gT2:
        t_W, t_c, t_jk = Tok("W"), Tok("c"), Tok("jk")
        tkp = [tuple(Tok(n + str(pb_)) for n in ("mix", "x", "mT", "h2", "hT", "tmp", "st", "rr", "gT")) for pb_ in range(2)]
        for k in range(8):
            P.load_r(W[:, k, :], I['w_out'][l, k * 128:(k + 1) * 128, :], junk[:], t_W, t_jk, e='act' if k % 2 else 'dve')
        P.dma(RW[:], I['router_w'].rearrange("(k p) n -> p k n", p=128), (), [t_c])
        P.dma(gain[:], I['mix_norm_g'][l, :].partition_broadcast(128), (), [t_c])
        P.dma(rb[:], I['router_b'][0, :].partition_broadcast(128), (), [t_c])
        for jj, r in enumerate((2, 3, 4, 8, 9, 10)):
            P.dma(mod[:, jj, :], K.MOD[r, :].partition_broadcast(128), (), [t_c])
        groups = ((0, 256), (256, 768), (768, 1024))
        for ti in range(2 if l == DEPTH - 1 else 0, NT):
            mb = 3 if ti < 2 else 0
            rows = slice(ti * 128, (ti + 1) * 128)
            pb = ti % 2
            mix, xt, mT, h2, hT, tmp, st, rr, gT = (mix2[:, pb], xt2[:, pb], mT2[:, pb], h22[:, pb], hT2[:, pb], tmp2[:, pb],
                                                    st2[:, pb], rr2[:, pb], gT2[:, pb])
            t_mix, t_x, t_mT, t_h2, t_hT, t_tmp, t_st, t_rr, t_gT = tkp[pb]
            P.dma(mix[:], K.MIX[rows, :], (), [t_mix])
            P.dma(xt[:], K.X[rows, :], (), [t_x])
            for gi, (c0, c1) in enumerate(groups):
                rms_rstd(K, mix[:, c0:c1], t_mix, c1 - c0, junk[:, c0:c1], st[:, gi:gi + 1], t_st)
            for gi, (c0, c1) in enumerate(groups):
                P.stt(mix[:, c0:c1], mix[:, c0:c1], st[:, gi:gi + 1], gain[:, c0:c1], ALU.mult, ALU.mult, [t_mix, t_st, t_c], [t_mix])
            for half in range(2):
                ps, tp = P.ps()
                for j in range(4):
                    kk = half * 4 + j
                    P.tr(ps[:, j * 128:(j + 1) * 128], mix[:, kk * 128:(kk + 1) * 128], K.ident[:], [t_mix, K.t_ident], [tp])
                P.copy(RR(mT[:, half * 4:(half + 1) * 4, :]), ps[:, :].rearrange("p (j t) -> p j t", j=4), [tp], [t_mT], e='act' if half else 'dve')
            for half in range(2):
                ps, tp = P.ps()
                for k in range(8):
                    P.mm(ps[:, :], mT[:, k, :], W[:, k, half * 512:(half + 1) * 512], k == 0, k == 7, [t_mT, t_W], [tp], r=FAST)
                hs = slice(half * 512, (half + 1) * 512)
                P.tt(tmp[:, hs], ps[:, :], mod[:, mb + 0, hs], ALU.mult, [tp, t_c], [t_tmp])
                P.tt(xt[:, hs], xt[:, hs], tmp[:, hs], ALU.add, [t_x, t_tmp], [t_x])
            P.dma(K.X[rows, :], xt[:], [t_x], [("X", ti)], q='pool')
            rms_rstd(K, xt[:], t_x, D, junk[:], st[:, 3:4], t_st)
            P.stt(h2[:], xt[:], st[:, 3:4], mod[:, mb + 1, :], ALU.mult, ALU.mult, [t_x, t_st, t_c], [t_h2])
            P.tt(h2[:], h2[:], mod[:, mb + 2, :], ALU.add, [t_h2, t_c], [t_h2])
            for half in range(2):
                ps, tp = P.ps()
                for j in range(4):
                    kk = half * 4 + j
                    P.tr(ps[:, j * 128:(j + 1) * 128], h2[:, kk * 128:(kk + 1) * 128], K.ident[:], [t_h2, K.t_ident], [tp])
                P.copy(hT[:, half * 4:(half + 1) * 4, :], ps[:, :].rearrange("p (j t) -> p j t", j=4), [tp], [t_hT], e='act' if half else 'dve')
            P.dma(K.H2T[:, ti * 128:(ti + 1) * 128].rearrange("(k p) t -> p k t", p=128), hT[:], [t_hT], [("H2T", ti)], q='pool')
            ps, tp = P.ps()
            for k in range(8):
                P.mm(ps[:, 0:16], hT[:, k, :], RW[:, k, :], k == 0, k == 7, [t_hT, t_c], [tp])
            R = lambda i: rr[:, i, :]
            R4 = lambda i: rr[:, i, :].rearrange("p (g e) -> p g e", g=4)
            trr = [t_rr]
            P.op('dve', lambda g: g.reduce_max(out=st[:, 4:5], in_=ps[:, 0:16], axis=AX.X), [tp], [t_st])
            P.ts(st[:, 4:5], st[:, 4:5], -1.0, None, ALU.mult, None, [t_st], [t_st])
            P.act(R(0), ps[:, 0:16], AF.Exp, [tp, t_st], trr, bias=st[:, 4:5], scale=1.0, accum_out=st[:, 5:6])
            P.recip(st[:, 5:6], st[:, 5:6], [t_st, t_rr], [t_st])
            P.ts(R(0), R(0), st[:, 5:6], None, ALU.mult, None, trr + [t_st], trr)
            P.tt(R(1), R(0), rb[:], ALU.add, trr + [t_c], trr)
            P.op('dve', lambda g: g.reduce_max(out=rr[:, 2, 0:4], in_=R4(1), axis=AX.X), trr, trr)
            P.tt(R4(3), R4(1), rr[:, 2, 0:4].unsqueeze(2).broadcast_to([128, 4, 4]), ALU.is_equal, trr, trr)
            P.stt(R(4), R(3), -1e9, R(1), ALU.mult, ALU.add, trr, trr)
            P.op('dve', lambda g: g.reduce_max(out=rr[:, 5, 0:4], in_=R4(4), axis=AX.X), trr, trr)
            P.tt(rr[:, 6, 0:4], rr[:, 2, 0:4], rr[:, 5, 0:4], ALU.add, trr, trr)
            P.op('dve', lambda g: g.reduce_max(out=st[:, 6:7], in_=rr[:, 6, 0:4], axis=AX.X), trr, [t_st])
            P.ts(rr[:, 7, 0:4], rr[:, 6, 0:4], st[:, 6:7], None, ALU.is_equal, None, trr + [t_st], trr)
            P.tt(R4(8), R4(1), rr[:, 5, 0:4].unsqueeze(2).broadcast_to([128, 4, 4]), ALU.is_ge, trr, trr)
            P.tt(R4(8), R4(8), rr[:, 7, 0:4].unsqueeze(2).broadcast_to([128, 4, 4]), ALU.mult, trr, trr)
            P.tt(R(9), R(8), R(0), ALU.mult, trr, trr)
            P.op('dve', lambda g: g.reduce_sum(out=st[:, 7:8], in_=R(9), axis=AX.X), trr, [t_st])
            P.recip(st[:, 7:8], st[:, 7:8], [t_st], [t_st])
            P.ts(R(9), R(9), st[:, 7:8], None, ALU.mult, None, trr + [t_st], trr)
            ps2, tp2 = P.ps()
            P.tr(ps2[0:16, 0:128], R(9), K.ident[:], trr + [K.t_ident], [tp2])
            P.copy(gT[:], ps2[0:16, 0:128], [tp2], [t_gT], e='act')
            P.dma(K.GTT[:, ti * 128:(ti + 1) * 128], gT[:], [t_gT], [("GTT", ti)], q='pool')


def stage_moe(K, l):
    P, nc, I = K.P, K.P.nc, K.I
    GN = 1024
    with SB(nc, "e_h", [128, 8, GN], F32) as hT, SB(nc, "e_g", [128, 8, GN], F32) as GT, \
            SB(nc, "e_y", [128, GN // 128, D], F32) as Y, SB(nc, "e_wd", [128, 8, D], F32) as wd, \
            SB(nc, "e_wg", [128, 2, 8, 128], F32) as wg, SB(nc, "e_wu", [128, 2, 8, 128], F32) as wu, \
            SB(nc, "e_gt", [16, GN], F32) as gt, SB(nc, "e_gb", [128, GN], F32) as gB, \
            SB(nc, "e_sel", [16, 16, 128], F32) as sel, SB(nc, "e_sa", [128, 2, 512], F32) as sA, \
            SB(nc, "e_mod", [128, 2, D], F32) as mod, SB(nc, "e_x", [128, D], F32) as xt, \
            SB(nc, "e_wst", [128, 6, D], F32) as wst:
        t_wst = [Tok("wst%d" % i) for i in range(6)]
        t_wdc = [Tok("wd%d" % i) for i in range(8)]
        t_h, t_G, t_Y, t_wd, t_gt, t_gB, t_sel, t_mod, t_x = (Tok(n) for n in ("h", "G", "Y", "wd", "gt", "gB", "sel", "mod", "x"))
        t_wg = [Tok("wg0"), Tok("wg1")]
        t_wu = [Tok("wu0"), Tok("wu1")]
        t_sa = [Tok("sa0"), Tok("sa1")]
        for e in range(16):
            P.copy(sel[:, e, :], K.ident[0:16, e:e + 1].broadcast_to([16, 128]), [K.t_ident], [t_sel])
        P.dma(mod[:, 0, :], K.MOD[5, :].partition_broadcast(128), (), [t_mod])
        P.dma(mod[:, 1, :], K.MOD[11, :].partition_broadcast(128), (), [t_mod])
        tstart = NCTX if l == DEPTH - 1 else 0
        for g in range((T - tstart + GN - 1) // GN):
            t0 = tstart + g * GN
            n = min(GN, T - t0)
            nt = n // 128
            for i in range(nt):
                P.load_r(hT[:, :, i * 128:(i + 1) * 128], K.H2T[:, t0 + i * 128:t0 + (i + 1) * 128].rearrange("(k p) t -> p k t", p=128),
                         wst[:, i % 2, :].rearrange("p (k t) -> p k t", k=8), t_h, t_wst[i % 2], e='act' if i % 2 else 'dve')
            P.dma(gt[:, 0:n], K.GTT[:, t0:t0 + n], (), [t_gt])
            P.memset(Y[:, 0:nt, :], 0.0, [t_Y], e='pool')
            for e in range(16):
                for s0 in range(0, n, 512):
                    sn = min(512, n - s0)
                    ps, tp = P.ps()
                    P.mm(ps[:, 0:sn], sel[:, e, :], gt[:, s0:s0 + sn], True, True, [t_sel, t_gt], [tp])
                    P.copy(gB[:, s0:s0 + sn], ps[:, 0:sn], [tp], [t_gB], e='act')
                wgv = I['moe_w_gate'][l, e].rearrange("(k p) n -> p k n", p=128)
                wuv = I['moe_w_up'][l, e].rearrange("(k p) n -> p k n", p=128)
                for c in range(8):
                    b = c % 2
                    P.load_r(wg[:, b, :, :], wgv[:, :, c * 128:(c + 1) * 128], wst[:, 4, :].rearrange("p (k t) -> p k t", k=8), t_wg[b], t_wst[4], e='act')
                    P.load_r(wu[:, b, :, :], wuv[:, :, c * 128:(c + 1) * 128], wst[:, 5, :].rearrange("p (k t) -> p k t", k=8), t_wu[b], t_wst[5], e='act')
                    P.load_r(wd[:, c, :], I['moe_w_down'][l, e, c * 128:(c + 1) * 128, :], wst[:, 2 + c % 2, :], t_wdc[c], t_wst[2 + c % 2], e='dve', q='pool')
                    for si, s0 in enumerate(range(0, n, 512)):
                        sn = min(512, n - s0)
                        psa, tpa = P.ps()
                        psu, tpu = P.ps()
                        for k in range(8):
                            P.mm(psa[:, 0:sn], wg[:, b, k, :], hT[:, k, s0:s0 + sn], k == 0, k == 7, [t_wg[b], t_h], [tpa], r=FAST)
                        for k in range(8):
                            P.mm(psu[:, 0:sn], wu[:, b, k, :], hT[:, k, s0:s0 + sn], k == 0, k == 7, [t_wu[b], t_h], [tpu], r=FAST)
                        sb_ = si % 2
                        P.act(sA[:, sb_, 0:sn], psa[:, 0:sn], AF.Silu, [tpa], [t_sa[sb_]])
                        P.tt(sA[:, sb_, 0:sn], sA[:, sb_, 0:sn], psu[:, 0:sn], ALU.mult, [t_sa[sb_], tpu], [t_sa[sb_]])
                        P.tt(RR(GT[:, c, s0:s0 + sn]), sA[:, sb_, 0:sn], gB[:, s0:s0 + sn], ALU.mult, [t_sa[sb_], t_gB], [t_G])
                for i in range(nt):
                    for half in range(2):
                        ps, tp = P.ps()
                        for c in range(8):
                            P.mm(ps[:, :], GT[:, c, i * 128:(i + 1) * 128], wd[:, c, half * 512:(half + 1) * 512], c == 0, c == 7, [t_G, t_wdc[c]], [tp], r=FAST)
                        hs = slice(half * 512, (half + 1) * 512)
                        P.tt(Y[:, i, hs], Y[:, i, hs], ps[:, :], ALU.add, [t_Y, tp], [t_Y])
            for i in range(nt):
                ti = t0 // 128 + i
                mb = 1 if ti < 2 else 0
                rows = slice(ti * 128, (ti + 1) * 128)
                P.dma(xt[:], K.X[rows, :], (), [t_x])
                P.tt(Y[:, i, :], Y[:, i, :], mod[:, mb, :], ALU.mult, [t_Y, t_mod], [t_Y], e='pool')
                P.tt(xt[:], xt[:], Y[:, i, :], ALU.add, [t_x, t_Y], [t_x])
                P.dma(K.X[rows, :], xt[:], [t_x], [("X", ti)], q='pool')


def stage_final(K):
    P, nc, I = K.P, K.P.nc, K.I
    with SB(nc, "f_g", [128, D], F32) as gbc, SB(nc, "f_x", [128, 2, D], F32) as xt, \
            SB(nc, "f_j", [128, D], F32) as junk, SB(nc, "f_s", [128, 2], F32) as st:
        t_g = Tok("g")
        t_x = [Tok("x0"), Tok("x1")]
        t_s = [Tok("s0"), Tok("s1")]
        P.dma(gbc[:], I['final_g'][0, :].partition_broadcast(128), (), [t_g])
        for i in range(NLAT // 128):
            b = i % 2
            P.dma(xt[:, b, :], K.X[NCTX + i * 128:NCTX + (i + 1) * 128, :], (), [t_x[b]])
            rms_rstd(K, xt[:, b, :], t_x[b], D, junk[:], st[:, b:b + 1], t_s[b])
            P.stt(xt[:, b, :], xt[:, b, :], st[:, b:b + 1], gbc[:], ALU.mult, ALU.mult, [t_x[b], t_s[b], t_g], [t_x[b]])
            P.dma(K.out[i * 128:(i + 1) * 128, :], xt[:, b, :], [t_x[b]], [("out", i)], q='pool')


_CONSTS = None


def consts():
    global _CONSTS
    if _CONSTS is not None:
        return _CONSTS
    c = {}
    c['k_ident'] = np.eye(128, dtype=np.float32)
    rc, rs = rope_tables()
    c['k_ropec'], c['k_ropes'] = rc, rs
    j = np.arange(128)[:, None]
    i = np.arange(128)[None, :]
    c['k_maskl'] = (j >= i).astype(np.float32)
    c['k_maskr'] = (j <= i).astype(np.float32)
    c['k_ccL'], c['k_ssL'] = dft_blocks(NLAT, 33)
    c['k_ccC'], c['k_ssC'] = dft_blocks(NCTX, 3)
    f, d, w = hyena_consts(NLAT)
    c['k_featL'], c['k_decL'], c['k_wkL'] = f, d, w.reshape(-1, 1)
    f, d, w = hyena_consts(NCTX)
    c['k_featC'], c['k_decC'], c['k_wkC'] = f, d, w.reshape(-1, 1)
    c['k_iota'] = np.arange(512, dtype=np.float32).reshape(1, 512)
    _CONSTS = c
    return c


def make_in_maps(inputs, cores):
    cs = consts()
    shared = {}
    for k, v in inputs.items():
        if k in ('x', 'c', 'ctx', 'c_ctx'):
            continue
        a = np.ascontiguousarray(np.asarray(v, dtype=np.float32))
        if k in ('router_b', 'final_g'):
            a = a.reshape(1, -1)
        shared[k] = a
    shared.update(cs)
    maps = []
    for b in cores:
        m = dict(shared)
        m['x'] = np.ascontiguousarray(inputs['x'][b], dtype=np.float32)
        m['ctx'] = np.ascontiguousarray(inputs['ctx'][b], dtype=np.float32)
        m['c'] = np.ascontiguousarray(inputs['c'][b], dtype=np.float32).reshape(1, D)
        m['c_ctx'] = np.ascontiguousarray(inputs['c_ctx'], dtype=np.float32).reshape(1, D)
        maps.append(m)
    return maps


def kernel(**inputs):
    P = build()
    maps = make_in_maps(inputs, list(range(8)))
    res = run_bass_kernel_spmd(P.nc, maps, core_ids=list(range(8)))
    return np.stack([r["out"] for r in res.results], axis=0).astype(np.float32)
```
